# Optimizing a Trainium2 kernel written in Bass

```python
import jax, jax.numpy as jnp
from jax import lax
import numpy as np

D_MODEL = 1024
BATCH = 8
SEQ = 8192
DEPTH = 1

D_MIX = D_MODEL
D_CONV = D_MIX // 2
D_MLSTM = D_MIX - D_CONV
N_HEADS = 4
HEAD_DIM = D_MLSTM // N_HEADS
CONV_WIDTH = 31
QK_CONV_WIDTH = 4
CHUNK = 128
N_GROUPS = 4
EXPERTS_PER_GROUP = 8
N_EXPERTS = N_GROUPS * EXPERTS_PER_GROUP
TOP_K = 2
D_EXPERT = D_MODEL // 2
MOE_BLOCK = 256
EPS = 1e-6
F_BIAS_LO = 3.0
F_BIAS_HI = 6.0
D_IN = 2 * D_CONV + 4 * D_MLSTM + 2 * N_HEADS

kernel_name = "hymba_conv_mlstm_hier_moe"


def rmsnorm(x, g):
    xf = x.astype(jnp.float32)
    y = xf * lax.rsqrt(jnp.mean(xf * xf, axis=-1, keepdims=True) + EPS)
    return (y * g.astype(jnp.float32)).astype(x.dtype)


def layernorm(x, g, b):
    xf = x.astype(jnp.float32)
    mu = jnp.mean(xf, axis=-1, keepdims=True)
    var = jnp.mean(jnp.square(xf - mu), axis=-1, keepdims=True)
    y = (xf - mu) * lax.rsqrt(var + EPS)
    return (y * g.astype(jnp.float32) + b.astype(jnp.float32)).astype(x.dtype)


def causal_depthwise_conv(x, w, b):
    width, ch = w.shape
    y = lax.conv_general_dilated(
        x, w[:, None, :].astype(x.dtype), window_strides=(1,), padding=[(width - 1, 0)],
        dimension_numbers=("NWC", "WIO", "NWC"), feature_group_count=ch)
    return y + b.astype(x.dtype)


def mlstm_chunkwise(q, k, v, i_pre, f_pre):
    B, H, S, Dh = q.shape
    nc = S // CHUNK
    k = k * (Dh ** -0.5)
    log_f = jax.nn.log_sigmoid(f_pre)

    def to_chunks(a):
        a = a.reshape(a.shape[:2] + (nc, CHUNK) + a.shape[3:])
        return jnp.moveaxis(a, 2, 0)

    qc, kc, vc = to_chunks(q), to_chunks(k), to_chunks(v)
    ic, fc = to_chunks(i_pre), to_chunks(log_f)
    causal = jnp.tril(jnp.ones((CHUNK, CHUNK), dtype=bool))

    def step(carry, inp):
        C, n, m = carry
        qb, kb, vb, ib, fb = inp
        b = jnp.cumsum(fb, axis=-1)
        d = b[..., :, None] - b[..., None, :] + ib[..., None, :]
        d = jnp.where(causal, d, -jnp.inf)
        g = b + m[..., None]
        m_t = jnp.maximum(g, jnp.max(d, axis=-1))
        w_intra = jnp.exp(d - m_t[..., None])
        w_inter = jnp.exp(g - m_t)
        s = jnp.einsum("bhtd,bhsd->bhts", qb, kb) * w_intra
        num = (jnp.einsum("bhts,bhsd->bhtd", s, vb)
               + w_inter[..., None] * jnp.einsum("bhvk,bhtk->bhtv", C, qb))
        den = jnp.sum(s, axis=-1) + w_inter * jnp.einsum("bhk,bhtk->bht", n, qb)
        h = num / jnp.maximum(jnp.abs(den), jnp.exp(-m_t))[..., None]
        b_last = b[..., -1]
        log_w = b_last[..., None] - b + ib
        m_new = jnp.maximum(b_last + m, jnp.max(log_w, axis=-1))
        decay = jnp.exp(b_last + m - m_new)
        w_state = jnp.exp(log_w - m_new[..., None])
        kw = kb * w_state[..., None]
        C_new = decay[..., None, None] * C + jnp.einsum("bhsv,bhsk->bhvk", vb, kw)
        n_new = decay[..., None] * n + jnp.sum(kw, axis=2)
        return (C_new, n_new, m_new), h

    init = (jnp.zeros((B, H, Dh, Dh), jnp.float32),
            jnp.zeros((B, H, Dh), jnp.float32),
            jnp.zeros((B, H), jnp.float32))
    _, hc = lax.scan(step, init, (qc, kc, vc, ic, fc))
    return jnp.moveaxis(hc, 0, 2).reshape(B, H, S, Dh)


def hybrid_mixer(h, w_in, b_in, conv_w, conv_b, conv_ln_g, conv_ln_b,
                 qk_conv_w, qk_conv_b, mlstm_norm_g, w_out):
    B, S, _ = h.shape
    z = jnp.einsum("bsd,de->bse", h, w_in) + b_in
    splits = np.cumsum([D_CONV, D_CONV, D_MLSTM, D_MLSTM, D_MLSTM, D_MLSTM, N_HEADS])
    conv_a, conv_gate, q, k, v, o, i_pre, f_pre = jnp.split(z, [int(i) for i in splits], axis=-1)

    u = conv_a * jax.nn.sigmoid(conv_gate)
    u = causal_depthwise_conv(u, conv_w, conv_b)
    u = jax.nn.silu(layernorm(u, conv_ln_g, conv_ln_b))

    qk = jax.nn.silu(causal_depthwise_conv(jnp.concatenate([q, k], axis=-1), qk_conv_w, qk_conv_b))
    q, k = qk[..., :D_MLSTM], qk[..., D_MLSTM:]

    def heads(a):
        return jnp.transpose(a.reshape(B, S, N_HEADS, HEAD_DIM), (0, 2, 1, 3)).astype(jnp.float32)

    ht = mlstm_chunkwise(heads(q), heads(k), heads(v),
                         jnp.transpose(i_pre, (0, 2, 1)).astype(jnp.float32),
                         jnp.transpose(f_pre, (0, 2, 1)).astype(jnp.float32))
    ht = jnp.transpose(ht, (0, 2, 1, 3))
    hm = jax.nn.sigmoid(o.astype(jnp.float32)).reshape(B, S, N_HEADS, HEAD_DIM) * ht
    mu = jnp.mean(hm, axis=-1, keepdims=True)
    var = jnp.mean(jnp.square(hm - mu), axis=-1, keepdims=True)
    hm = (hm - mu) * lax.rsqrt(var + EPS) * mlstm_norm_g.astype(jnp.float32).reshape(N_HEADS, HEAD_DIM)
    hm = hm.reshape(B, S, D_MLSTM).astype(h.dtype)

    y = jnp.concatenate([u, hm], axis=-1)
    return jnp.einsum("bse,ed->bsd", y, w_out)


def hierarchical_moe(h, rg_w, rg_b, re_w, re_b, w_gate, w_up, w_down):
    B, S, D = h.shape
    T = B * S
    xt = h.reshape(T, D)
    group_logits = (xt @ rg_w + rg_b).astype(jnp.float32)
    group_prob = jax.nn.softmax(group_logits, axis=-1)
    grp = jnp.argmax(group_logits, axis=-1).astype(jnp.int32)
    p_grp = jnp.take_along_axis(group_prob, grp[:, None], axis=-1)
    expert_logits = (xt @ re_w + re_b).astype(jnp.float32).reshape(T, N_GROUPS, EXPERTS_PER_GROUP)
    in_group = expert_logits[jnp.arange(T), grp]
    top_val, top_idx = lax.top_k(in_group, TOP_K)
    gate = p_grp * jax.nn.softmax(top_val, axis=-1)
    expert_id = grp[:, None] * EXPERTS_PER_GROUP + top_idx.astype(jnp.int32)

    n_assign = T * TOP_K
    e_flat = expert_id.reshape(-1)
    tok_flat = jnp.repeat(jnp.arange(T, dtype=jnp.int32), TOP_K)
    g_flat = gate.reshape(-1)
    order = jnp.argsort(e_flat)
    e_sorted, tok_sorted, g_sorted = e_flat[order], tok_flat[order], g_flat[order]
    counts = jnp.zeros((N_EXPERTS,), jnp.int32).at[e_flat].add(1)
    starts = jnp.cumsum(counts) - counts
    padded = (counts + MOE_BLOCK - 1) // MOE_BLOCK * MOE_BLOCK
    padded_ends = jnp.cumsum(padded)
    padded_starts = padded_ends - padded
    dest = padded_starts[e_sorted] + (jnp.arange(n_assign, dtype=jnp.int32) - starts[e_sorted])
    n_blocks = -(-n_assign // MOE_BLOCK) + N_EXPERTS
    n_slots = n_blocks * MOE_BLOCK
    slot_tok = jnp.zeros((n_slots,), jnp.int32).at[dest].set(tok_sorted)
    slot_gate = jnp.zeros((n_slots,), jnp.float32).at[dest].set(g_sorted)
    block_start = jnp.arange(n_blocks, dtype=jnp.int32) * MOE_BLOCK
    block_expert = jnp.minimum(jnp.sum(block_start[:, None] >= padded_ends[None, :], axis=1),
                               N_EXPERTS - 1).astype(jnp.int32)

    xs = xt[slot_tok].reshape(n_blocks, MOE_BLOCK, D)

    def expert_block(args):
        xb, e = args
        a = xb @ w_gate[e]
        u = xb @ w_up[e]
        return (jax.nn.silu(a) * u) @ w_down[e]

    ys = lax.map(expert_block, (xs, block_expert)).reshape(n_slots, D)
    ys = ys * slot_gate[:, None].astype(ys.dtype)
    out = jnp.zeros((T, D), ys.dtype).at[slot_tok].add(ys)
    return out.reshape(B, S, D).astype(h.dtype)


def setup_inputs(seed: int = 0) -> dict:
    key = jax.random.key(seed)
    ks = jax.random.split(key, 21)
    f32 = jnp.float32
    L = DEPTH

    def nrm(k, shape, scale):
        return jax.random.normal(k, shape, f32) * scale

    b_in = nrm(ks[3], (L, D_IN), 0.02)
    b_in = b_in.at[:, D_IN - N_HEADS:].add(jnp.linspace(F_BIAS_LO, F_BIAS_HI, N_HEADS, dtype=f32))
    return {
        "x": nrm(ks[0], (BATCH, SEQ, D_MODEL), 1.0),
        "norm_mix_g": 1.0 + nrm(ks[1], (L, D_MODEL), 0.02),
        "w_in": nrm(ks[2], (L, D_MODEL, D_IN), D_MODEL ** -0.5),
        "b_in": b_in,
        "conv_w": nrm(ks[4], (L, CONV_WIDTH, D_CONV), CONV_WIDTH ** -0.5),
        "conv_b": nrm(ks[5], (L, D_CONV), 0.02),
        "conv_ln_g": 1.0 + nrm(ks[6], (L, D_CONV), 0.02),
        "conv_ln_b": nrm(ks[7], (L, D_CONV), 0.02),
        "qk_conv_w": nrm(ks[8], (L, QK_CONV_WIDTH, 2 * D_MLSTM), QK_CONV_WIDTH ** -0.5),
        "qk_conv_b": nrm(ks[9], (L, 2 * D_MLSTM), 0.02),
        "mlstm_norm_g": 1.0 + nrm(ks[10], (L, D_MLSTM), 0.02),
        "w_out": nrm(ks[11], (L, D_MIX, D_MODEL), D_MIX ** -0.5),
        "norm_ffn_g": 1.0 + nrm(ks[12], (L, D_MODEL), 0.02),
        "router_group_w": nrm(ks[13], (L, D_MODEL, N_GROUPS), D_MODEL ** -0.5),
        "router_group_b": nrm(ks[14], (L, N_GROUPS), 0.01),
        "router_expert_w": nrm(ks[15], (L, D_MODEL, N_EXPERTS), D_MODEL ** -0.5),
        "router_expert_b": nrm(ks[16], (L, N_EXPERTS), 0.01),
        "expert_w_gate": nrm(ks[17], (L, N_EXPERTS, D_MODEL, D_EXPERT), D_MODEL ** -0.5),
        "expert_w_up": nrm(ks[18], (L, N_EXPERTS, D_MODEL, D_EXPERT), D_MODEL ** -0.5),
        "expert_w_down": nrm(ks[19], (L, N_EXPERTS, D_EXPERT, D_MODEL), D_EXPERT ** -0.5),
        "final_norm_g": 1.0 + nrm(ks[20], (D_MODEL,), 0.02),
    }


def reference(x, norm_mix_g, w_in, b_in, conv_w, conv_b, conv_ln_g, conv_ln_b,
              qk_conv_w, qk_conv_b, mlstm_norm_g, w_out, norm_ffn_g,
              router_group_w, router_group_b, router_expert_w, router_expert_b,
              expert_w_gate, expert_w_up, expert_w_down, final_norm_g):
    for l in range(DEPTH):
        h = rmsnorm(x, norm_mix_g[l])
        x = x + hybrid_mixer(h, w_in[l], b_in[l], conv_w[l], conv_b[l], conv_ln_g[l], conv_ln_b[l],
                             qk_conv_w[l], qk_conv_b[l], mlstm_norm_g[l], w_out[l])
        h = rmsnorm(x, norm_ffn_g[l])
        x = x + hierarchical_moe(h, router_group_w[l], router_group_b[l],
                                 router_expert_w[l], router_expert_b[l],
                                 expert_w_gate[l], expert_w_up[l], expert_w_down[l])
    return rmsnorm(x, final_norm_g)
```

```python
import contextlib
import numpy as np
import concourse.bass as bass
import concourse.mybir as mybir
from concourse.bass_utils import run_bass_kernel_spmd
from concourse.alu_op_type import AluOpType as ALU

F32 = mybir.dt.float32
BF16 = mybir.dt.bfloat16
I32 = mybir.dt.int32
AF = mybir.ActivationFunctionType
AX = mybir.AxisListType

D = 1024
DIN = 3080
NE = 32
DE = 512
EPS = 1e-6
KAPPA = 0.25 * (128.0 ** -0.5)


class Res:
    __slots__ = ("name", "w", "rd", "acc")

    def __init__(self, name):
        self.name = name
        self.w = None
        self.rd = []
        self.acc = []


class DmaSem:
    def __init__(self, sem):
        self.sem = sem
        self.count = 0


class Prog:
    ENGS = ("pe", "act", "dve", "pool", "sp")

    def __init__(self, nc, stack):
        self.nc = nc
        self.stack = stack
        self.items = {e: [] for e in self.ENGS}
        self.count = {e: 0 for e in self.ENGS}
        self.sem = {e: stack.enter_context(nc.semaphore("c_" + e)) for e in self.ENGS}
        self.known = {e: {} for e in self.ENGS}
        self.ndma = 0
        self.sync_same_engine = True

    def dsem(self, name=None):
        self.ndma += 1
        return DmaSem(self.stack.enter_context(self.nc.semaphore(name or ("d%d" % self.ndma))))

    def _deps(self, eng, reads, writes, accum):
        toks = []
        for r in reads:
            if r.w is not None:
                toks.append(r.w)
            toks.extend(r.acc)
        for w in writes:
            if w.w is not None:
                toks.append(w.w)
            toks.extend(w.rd)
            toks.extend(w.acc)
        for a in accum:
            if a.w is not None:
                toks.append(a.w)
            toks.extend(a.rd)
        waits = []
        kn = self.known[eng]
        own = self.sem[eng]
        best = {}
        for (s, v) in toks:
            if s is own and (eng == "pe" or not self.sync_same_engine):
                continue
            if kn.get(id(s), 0) >= v:
                continue
            if id(s) not in best or best[id(s)][1] < v:
                best[id(s)] = (s, v)
        for (s, v) in best.values():
            kn[id(s)] = v
            waits.append((s, v))
        return waits

    def op(self, eng, fn, reads=(), writes=(), accum=(), dma=None):
        waits = self._deps(eng, reads, writes, accum)
        if dma is None:
            self.count[eng] += 1
            tok = (self.sem[eng], self.count[eng])
            inc = 1
        else:
            dma.count += 16
            tok = (dma.sem, dma.count)
            inc = 16
        self.items[eng].append((waits, fn, tok[0], inc))
        for r in reads:
            r.rd.append(tok)
        for w in writes:
            w.w = tok
            w.rd = []
            w.acc = []
        for a in accum:
            a.acc.append(tok)
        return tok

    def wait_all(self, eng, ress):
        waits = self._deps(eng, ress, ress, ())
        self.items[eng].append((waits, None, None, 0))

    def emit(self):
        nc = self.nc
        with nc.Block() as block:
            def run(name, handle):
                for (waits, fn, sem, inc) in self.items[name]:
                    for (s, v) in waits:
                        handle.wait_ge(s, v)
                    if fn is not None:
                        fn(handle).then_inc(sem, inc)

            @block.tensor
            def _(e):
                run("pe", e)

            @block.scalar
            def _(e):
                run("act", e)

            @block.vector
            def _(e):
                run("dve", e)

            @block.gpsimd
            def _(e):
                run("pool", e)

            @block.sync
            def _(e):
                run("sp", e)


def build_program(S=8192, CAP=640, NQ=2, debug=False):
    TT = NQ * 128
    NT = S // TT
    NCH = S // 128
    NB = CAP // 128
    NSLOT = NE * CAP
    NG = NQ * 4
    assert S % TT == 0 and CAP % 128 == 0

    nc = bass.Bass("TRN2", target_bir_lowering=False)
    dt = nc.dram_tensor

    def din(name, shape, dtype=F32):
        return dt(name, list(shape), dtype, kind="ExternalInput").ap()

    x_d = din("x", [S, D])
    win_d = din("w_in", [D, DIN])
    wout_d = din("w_out", [D, D])
    g1_d = din("g1", [128, 8])
    bfm_d = din("bfm", [128, 16])
    btm_d = din("btm", [1, 1032])
    cw_d = din("cw", [128, 4 * 31])
    cb_d = din("cb", [128, 4])
    lng_d = din("lng", [128, 4])
    lnb_d = din("lnb", [128, 4])
    qkw_d = din("qkw", [128, 8 * 4])
    qkb_d = din("qkb", [128, 8])
    mg_d = din("mg", [128, 4])
    g2_d = din("g2", [1, D])
    gf_d = din("gf", [1, D])
    rw_d = din("rw", [D, 36])
    rb_d = din("rb", [1, 36])
    ecap_d = din("ecap", [1, 32])
    wg_d = din("wg", [NE, D, DE])
    wu_d = din("wu", [NE, D, DE])
    wd_d = din("wd", [NE, DE, D])
    ident_d = din("ident", [128, 128])
    triI_d = din("tri_incl", [128, 128])
    triS_d = din("tri_strict", [128, 128])
    out_d = dt("out", [S, D], F32, kind="ExternalOutput").ap()
    x1_d = dt("x1s", [S, D], F32, kind="Internal").ap()
    xs_d = dt("xs", [NSLOT, D], BF16, kind="ExternalOutput" if debug else "Internal").ap()
    ys_d = dt("ys", [NSLOT, D], F32, kind="ExternalOutput" if debug else "Internal").ap()
    dbg = {}
    if debug:
        dbg["x1"] = dt("dbg_x1", [S, D], F32, kind="ExternalOutput").ap()
        dbg["idx"] = dt("dbg_idx", [128, NCH * 2], I32, kind="ExternalOutput").ap()
        dbg["gw"] = dt("dbg_gw", [128, NCH * 2], F32, kind="ExternalOutput").ap()
        dbg["yT"] = dt("dbg_yT", [128, 8 * S], F32, kind="ExternalOutput").ap()

    with contextlib.ExitStack() as st:
        P = Prog(nc, st)

        def sb(name, shape, dtype=F32):
            return st.enter_context(nc.sbuf_tensor("s_" + name, list(shape), dtype))

        def mk(eng, fn, reads=(), writes=(), accum=(), dma=None):
            return P.op(eng, fn, reads, writes, accum, dma)

        dumps = {}

        def dump(name, ap, reads):
            if not debug or name in dumps:
                return
            dumps[name] = dt("dmp_" + name, list(ap.shape), ap.dtype, kind="ExternalOutput").ap()
            mk("sp", lambda e: e.dma_start(out=dumps[name], in_=ap), reads=reads, accum=[R_dbg], dma=P.dsem())

        def emit_rsqrt(y, v, t, R):
            mk("dve", lambda e: e.tensor_scalar(out=y.bitcast(I32), in0=v.bitcast(I32), scalar1=-0.5, scalar2=1597463007.0, op0=ALU.mult, op1=ALU.add),
               reads=R, writes=R)
            for _ in range(3):
                mk("dve", lambda e: e.scalar_tensor_tensor(out=t, in0=y, scalar=-0.5, in1=y, op0=ALU.mult, op1=ALU.mult), reads=R, writes=R)
                mk("dve", lambda e: e.tensor_tensor(out=t, in0=t, in1=v, op=ALU.mult), reads=R, writes=R)
                mk("dve", lambda e: e.scalar_tensor_tensor(out=y, in0=t, scalar=1.5, in1=y, op0=ALU.add, op1=ALU.mult), reads=R, writes=R)

        banks = [st.enter_context(nc.psum_tensor("bank%d" % i, [128, 512], F32)) for i in range(8)]
        bank_res = [Res("bank%d" % i) for i in range(8)]
        bank_ctr = [0]

        def getbank():
            i = bank_ctr[0] % 8
            bank_ctr[0] += 1
            return banks[i], bank_res[i]

        def b16(bank):
            return bank[:, :].bitcast(BF16)

        didx = sb("didx", [128, NCH * 2], I32)
        gwt = sb("gwt", [128, NCH * 2], F32)
        R_didx = Res("didx")
        R_gwt = Res("gwt")
        R_xs = Res("xs")
        R_ys = Res("ys")
        R_x1 = Res("x1d")
        R_out = Res("out")
        R_dbg = Res("dbg")
        identb = sb("identb", [128, 128], BF16)
        R_const = Res("const")

        ld = P.dsem("ld_const")

        with contextlib.ExitStack() as st2:
            def sb2(name, shape, dtype=F32):
                return st2.enter_context(nc.sbuf_tensor("s_" + name, list(shape), dtype))

            winb = sb2("winb", [128, 8, DIN], BF16)
            woutb = sb2("woutb", [128, 8, D], BF16)
            dconv = sb2("dconv", [128, 4 * 31, 128], BF16)
            dqk = sb2("dqk", [128, 8 * 4, 128], BF16)
            identf = sb2("identf", [128, 128], F32)
            triI = sb2("triI", [128, 128], F32)
            onesf = sb2("onesf", [128, 128], F32)
            maskk = sb2("maskk", [128, 128], F32)
            triSb = sb2("triSb", [128, 128], BF16)
            onesb = sb2("onesb", [128, 128], BF16)
            o512b = sb2("o512b", [128, 128], BF16)
            g1 = sb2("g1", [128, 8])
            bfm = sb2("bfm", [128, 16])
            hbfm = sb2("hbfm", [128, 16])
            btm = sb2("btm", [128, 1032])
            cw = sb2("cw", [128, 124])
            cb = sb2("cb", [128, 4])
            lng = sb2("lng", [128, 4])
            lnb = sb2("lnb", [128, 4])
            hlng = sb2("hlng", [128, 4])
            hlnb = sb2("hlnb", [128, 4])
            qkw = sb2("qkw", [128, 32])
            qkb = sb2("qkb", [128, 8])
            hqkb = sb2("hqkb", [128, 8])
            mg = sb2("mg", [128, 4])
            g2bc = sb2("g2bc", [128, D])
            rwf = sb2("rwf", [128, 8, 36])
            rwb = sb2("rwb", [128, 8, 36], BF16)
            rbbc = sb2("rbbc", [128, 36])
            basep = sb2("basep", [128, 32])
            halfc = sb2("halfc", [128, 1])
            epsc = sb2("epsc", [128, 1])

            xt = [sb2("xt%d" % i, [128, NQ, D]) for i in range(2)]
            R_xt = [Res("xt0"), Res("xt1")]
            xsem = [P.dsem("x0"), P.dsem("x1")]
            hb = sb2("hb", [128, NQ, D], BF16); R_hb = Res("hb")
            h2 = sb2("h2", [128, NQ, D], BF16); R_h2 = Res("h2")
            hT = sb2("hT", [128, 8, TT], BF16); R_hT = Res("hT")
            h2T = sb2("h2T", [128, 8, TT], BF16); R_h2T = Res("h2T")
            ubuf = sb2("ubuf", [128, 4, TT + 32], BF16); R_ubuf = Res("ubuf")
            qkbuf = sb2("qkbuf", [128, 8, TT + 4], BF16); R_qkbuf = Res("qkbuf")
            NTMP = 6
            tmp = [sb2("tmp%d" % i, [128, TT]) for i in range(NTMP)]
            R_tmp = [Res("tmp%d" % i) for i in range(NTMP)]
            tmp_ctr = [0]

            def gettmp():
                i = tmp_ctr[0] % NTMP
                tmp_ctr[0] += 1
                return tmp[i], R_tmp[i]

            vsb = sb2("vsb", [128, NQ, 512]); R_vsb = Res("vsb")
            osb = sb2("osb", [128, NQ, 512]); R_osb = Res("osb")
            gi = sb2("gi", [128, NG]); gfp = sb2("gfp", [128, NG]); R_g = Res("gates")
            gsm = sb2("gsm", [128, 8, NG]); R_gsm = Res("gsm")
            ev = sb2("ev", [128, NG]); fl = sb2("fl", [128, NG]); dec = sb2("dec", [128, NG])
            R_ev = Res("ev")
            cv = sb2("cv", [128, 4, TT]); R_cv = [Res("cv%d" % i) for i in range(4)]
            cvb = sb2("cvb", [128, 4, TT], BF16); R_cvb = Res("cvb")
            sqb = sb2("sqb", [128, 4, TT], BF16); R_sqb = Res("sqb")
            mean_sb = sb2("mean_sb", [128, TT]); rstdt = sb2("rstdt", [128, TT]); msq = sb2("msq", [128, TT])
            R_stats = Res("stats")
            qkT = sb2("qkT", [128, 8, TT], BF16); R_qkT = Res("qkT")
            ktm = sb2("ktm", [128, NQ, 512], BF16); R_ktm = Res("ktm")
            vt = [sb2("vt%d" % i, [128, 4, 129], BF16) for i in range(2)]; R_vt = [Res("vt0"), Res("vt1")]
            stT = [sb2("stT%d" % i, [128, 4, 128], BF16) for i in range(2)]; R_stT = [Res("stT0"), Res("stT1")]
            Chat = sb2("Chat", [128, 4, 129]); Chb = sb2("Chb", [128, 4, 129], BF16)
            R_Chat = Res("Chat"); R_Chb = Res("Chb")
            hm = sb2("hm", [128, 4, 128]); R_hm = Res("hm")
            hmn = sb2("hmn", [128, 4, 128], BF16); R_hmn = Res("hmn")
            sm = sb2("sm", [128, 12, 4]); R_sm = Res("sm")
            junk = sb2("junk", [128, 128]); R_junk = Res("junk")
            yT = sb2("yT", [128, 8, TT], BF16); R_yT = Res("yT")
            ss = sb2("ss", [128, 8, NQ]); R_ss = Res("ss")
            rt = sb2("rt", [128, 16, 36]); R_rt = Res("rt")
            cmbb = sb2("cmbb", [128, 32], BF16)
            x1sem = [P.dsem("x1st0"), P.dsem("x1st1")]
            dbsem = [P.dsem("db0"), P.dsem("db1")]
            scsem = P.dsem("scat")

            def cld(eng, dst, src):
                mk(eng, lambda e: e.dma_start(out=dst, in_=src), accum=[R_const], dma=ld)

            cld("sp", identf[:, :], ident_d)
            cld("sp", triI[:, :], triI_d)
            cld("sp", g1[:, :], g1_d)
            cld("sp", bfm[:, :], bfm_d)
            cld("sp", btm[:, :], btm_d.partition_broadcast(128))
            cld("sp", cw[:, :], cw_d)
            cld("sp", cb[:, :], cb_d)
            cld("sp", lng[:, :], lng_d)
            cld("sp", lnb[:, :], lnb_d)
            cld("sp", qkw[:, :], qkw_d)
            cld("sp", qkb[:, :], qkb_d)
            cld("sp", mg[:, :], mg_d)
            cld("sp", g2bc[:, :], g2_d.partition_broadcast(128))
            cld("sp", rbbc[:, :], rb_d.partition_broadcast(128))
            R_base = Res("basep")
            mk("sp", lambda e: e.dma_start(out=basep[:, :], in_=ecap_d.partition_broadcast(128)), writes=[R_base], dma=P.dsem("base"))
            cld("sp", rwf[:, :, :], rw_d.rearrange("(k p) n -> p k n", p=128))
            cld("pool", triSb[:, :], triS_d)
            cld("pool", identb[:, :], ident_d)
            R_c2 = Res("const2")

            def cop(eng, fn):
                mk(eng, fn, reads=[R_const], accum=[R_c2])

            cop("dve", lambda e: e.memset(onesf[:, :], 1.0))
            cop("dve", lambda e: e.memset(onesb[:, :], 1.0))
            cop("dve", lambda e: e.memset(o512b[:, :], 1.0 / 512.0))
            cop("dve", lambda e: e.memset(halfc[:, :], 0.5))
            cop("dve", lambda e: e.memset(epsc[:, :], EPS))
            cop("dve", lambda e: e.tensor_scalar(out=maskk[:, :], in0=triI[:, :], scalar1=KAPPA, scalar2=None, op0=ALU.mult))
            cop("dve", lambda e: e.tensor_scalar(out=hbfm[:, :], in0=bfm[:, :], scalar1=0.5, scalar2=None, op0=ALU.mult))
            cop("dve", lambda e: e.tensor_scalar(out=hlng[:, :], in0=lng[:, :], scalar1=0.5, scalar2=None, op0=ALU.mult))
            cop("dve", lambda e: e.tensor_scalar(out=hlnb[:, :], in0=lnb[:, :], scalar1=0.5, scalar2=None, op0=ALU.mult))
            cop("dve", lambda e: e.tensor_scalar(out=hqkb[:, :], in0=qkb[:, :], scalar1=0.5, scalar2=None, op0=ALU.mult))
            cop("dve", lambda e: e.tensor_copy(out=rwb[:, :, :], in_=rwf[:, :, :]))
            mk("dve", lambda e: e.memset(Chat[:, :, :], 0.0), writes=[R_Chat])
            mk("dve", lambda e: e.memset(Chb[:, :, :], 0.0), writes=[R_Chb])
            mk("dve", lambda e: e.memset(ubuf[:, :, :], 0.0), writes=[R_ubuf])
            mk("dve", lambda e: e.memset(qkbuf[:, :, :], 0.0), writes=[R_qkbuf])
            for i in range(124):
                eng = "dve" if i % 2 == 0 else "pool"
                cop(eng, lambda e, i=i: e.tensor_scalar(out=dconv[:, i, :], in0=identf[:, :], scalar1=cw[:, i:i + 1],
                                                        scalar2=0.5, op0=ALU.mult, op1=ALU.mult))
            for i in range(32):
                eng = "dve" if i % 2 == 0 else "pool"
                cop(eng, lambda e, i=i: e.tensor_scalar(out=dqk[:, i, :], in0=identf[:, :], scalar1=qkw[:, i:i + 1],
                                                        scalar2=None, op0=ALU.mult))
            R_stage = R_xt
            HW = DIN // 2
            for k in range(8):
                for hf in range(2):
                    s = (2 * k + hf) % 2
                    flat = xt[s][:, :, :].rearrange("p q d -> p (q d)")
                    mk("sp", lambda e, k=k, hf=hf, flat=flat: e.dma_start(out=flat[:, 0:HW], in_=win_d[k * 128:(k + 1) * 128, hf * HW:(hf + 1) * HW]),
                       writes=[R_stage[s]], dma=xsem[s])
                    eng = "dve" if s == 0 else "pool"
                    mk(eng, lambda e, k=k, hf=hf, flat=flat: e.tensor_scalar(out=winb[:, k, hf * HW:(hf + 1) * HW], in0=flat[:, 0:HW],
                                                                            scalar1=g1[:, k:k + 1], scalar2=None, op0=ALU.mult),
                       reads=[R_stage[s], R_const], accum=[R_c2])
            for k in range(8):
                s = k % 2
                mk("sp", lambda e, k=k, s=s: e.dma_start(out=xt[s][:, 0, :], in_=wout_d[k * 128:(k + 1) * 128, :]),
                   writes=[R_stage[s]], dma=xsem[s])
                sc = 0.5 if k < 4 else mg[:, k - 4:k - 3]
                eng = "dve" if k % 2 == 0 else "pool"
                mk(eng, lambda e, k=k, s=s, sc=sc: e.tensor_scalar(out=woutb[:, k, :], in0=xt[s][:, 0, :], scalar1=sc,
                                                                   scalar2=None, op0=ALU.mult),
                   reads=[R_stage[s], R_const], accum=[R_c2])
            RC = [R_const, R_c2]

            def tile_front(T):
                s = T % 2
                X = xt[s]
                mk("sp", lambda e: e.dma_start(out=X[:, :, :], in_=x_d[T * TT:(T + 1) * TT, :].rearrange("(q p) d -> p q d", p=128)),
                   writes=[R_xt[s]], dma=xsem[s])
                for q in range(NQ):
                    mk("act", lambda e, q=q: e.activation(out=hb[:, q, :], in_=X[:, q, :], func=AF.Square, accum_out=ss[:, 0, q:q + 1]),
                       reads=[R_xt[s]], writes=[R_hb, R_ss])
                mk("dve", lambda e: e.tensor_scalar(out=ss[:, 1, :], in0=ss[:, 0, :], scalar1=1.0 / D, scalar2=EPS, op0=ALU.mult, op1=ALU.add),
                   reads=[R_ss], writes=[R_ss])
                emit_rsqrt(ss[:, 2, :], ss[:, 1, :], ss[:, 3, :], [R_ss])
                for q in range(NQ):
                    mk("act", lambda e, q=q: e.activation(out=hb[:, q, :], in_=X[:, q, :], func=AF.Identity, scale=ss[:, 2, q:q + 1]),
                       reads=[R_xt[s], R_ss], writes=[R_hb])
                for q in range(NQ):
                    bk, rb_ = getbank()
                    for k in range(8):
                        mk("pe", lambda e, q=q, k=k, bk=bk: e.transpose(out=b16(bk)[:, k * 128:(k + 1) * 128], in_=hb[:, q, k * 128:(k + 1) * 128], identity=identb[:, :]),
                           reads=[R_hb] + RC, writes=[rb_])
                    mk("dve", lambda e, q=q, bk=bk: e.tensor_copy(out=hT[:, :, q * 128:(q + 1) * 128], in_=b16(bk)[:, 0:1024].rearrange("p (k t) -> p k t", k=8)),
                       reads=[rb_], writes=[R_hT])
                order = [4, 0, 5, 1, 6, 2, 7, 3] + list(range(8, 16))
                thg = {}
                for j in order:
                    bk, rb_ = getbank()
                    for k in range(8):
                        mk("pe", lambda e, j=j, k=k, bk=bk: e.matmul(bk[:, 0:TT], lhsT=winb[:, k, j * 128:(j + 1) * 128], rhs=hT[:, k, :],
                                                                     start=(k == 0), stop=(k == 7)),
                           reads=[R_hT] + RC, writes=[rb_])
                    if 4 <= j < 8:
                        t, rt_ = gettmp()
                        thg[j - 4] = (t, rt_)
                        mk("act", lambda e, j=j, bk=bk, t=t: e.activation(out=t[:, :], in_=bk[:, 0:TT], func=AF.Tanh, scale=0.5, bias=hbfm[:, j:j + 1]),
                           reads=[rb_] + RC, writes=[rt_])
                    elif j < 4:
                        t, rt_ = thg[j]
                        a, ra_ = gettmp()
                        mk("act", lambda e, j=j, bk=bk, a=a: e.activation(out=a[:, :], in_=bk[:, 0:TT], func=AF.Identity, bias=bfm[:, j:j + 1]),
                           reads=[rb_] + RC, writes=[ra_])
                        mk("dve", lambda e, j=j, t=t, a=a: e.scalar_tensor_tensor(out=ubuf[:, j, 30:30 + TT], in0=t[:, :], scalar=1.0, in1=a[:, :],
                                                                                  op0=ALU.add, op1=ALU.mult),
                           reads=[rt_, ra_], writes=[R_ubuf])
                    else:
                        jp = j - 8
                        if jp % 2 == 0:
                            mk("act", lambda e, j=j, jp=jp, bk=bk: e.activation(out=qkbuf[:, jp, 3:3 + TT], in_=bk[:, 0:TT], func=AF.Identity, bias=bfm[:, j:j + 1]),
                               reads=[rb_] + RC, writes=[R_qkbuf])
                        else:
                            mk("dve", lambda e, j=j, jp=jp, bk=bk: e.tensor_scalar(out=qkbuf[:, jp, 3:3 + TT], in0=bk[:, 0:TT], scalar1=bfm[:, j:j + 1], scalar2=None, op0=ALU.add),
                               reads=[rb_] + RC, writes=[R_qkbuf])
                bg, rbg = getbank()
                for q in range(NQ):
                    bv, rbv = getbank()
                    bo, rbo = getbank()
                    for k in range(8):
                        lt = hT[:, k, q * 128:(q + 1) * 128]
                        mk("pe", lambda e, k=k, bv=bv, lt=lt: e.matmul(bv[:, 0:512], lhsT=lt, rhs=winb[:, k, 2048:2560], start=(k == 0), stop=(k == 7)),
                           reads=[R_hT] + RC, writes=[rbv])
                        mk("pe", lambda e, k=k, bo=bo, lt=lt: e.matmul(bo[:, 0:512], lhsT=lt, rhs=winb[:, k, 2560:3072], start=(k == 0), stop=(k == 7)),
                           reads=[R_hT] + RC, writes=[rbo])
                    for k in range(8):
                        lt = hT[:, k, q * 128:(q + 1) * 128]
                        mk("pe", lambda e, k=k, q=q, lt=lt: e.matmul(bg[:, q * 8:(q + 1) * 8], lhsT=lt, rhs=winb[:, k, 3072:3080], start=(k == 0), stop=(k == 7)),
                           reads=[R_hT] + RC, writes=[rbg])
                    mk("dve", lambda e, q=q, bv=bv: e.tensor_tensor(out=vsb[:, q, :], in0=bv[:, 0:512], in1=btm[:, 0:512], op=ALU.add),
                       reads=[rbv] + RC, writes=[R_vsb])
                    mk("dve", lambda e, q=q, bo=bo: e.tensor_tensor(out=osb[:, q, :], in0=bo[:, 0:512], in1=btm[:, 512:1024], op=ALU.add),
                       reads=[rbo] + RC, writes=[R_osb])
                    mk("act", lambda e, q=q: e.activation(out=osb[:, q, :], in_=osb[:, q, :], func=AF.Tanh, scale=0.5),
                       reads=[R_osb], writes=[R_osb])
                    mk("pool", lambda e, q=q: e.tensor_scalar(out=osb[:, q, :], in0=osb[:, q, :], scalar1=0.5, scalar2=0.5, op0=ALU.mult, op1=ALU.add),
                       reads=[R_osb], writes=[R_osb])
                for q in range(NQ):
                    mk("dve", lambda e, q=q: e.tensor_tensor(out=gi[:, q * 4:(q + 1) * 4], in0=bg[:, q * 8:q * 8 + 4], in1=btm[:, 1024:1028], op=ALU.add),
                       reads=[rbg] + RC, writes=[R_g])
                    mk("dve", lambda e, q=q: e.tensor_tensor(out=gfp[:, q * 4:(q + 1) * 4], in0=bg[:, q * 8 + 4:q * 8 + 8], in1=btm[:, 1028:1032], op=ALU.add),
                       reads=[rbg] + RC, writes=[R_g])

            def tile_gates(T):
                G = gsm
                mk("act", lambda e: e.activation(out=G[:, 0, :], in_=gfp[:, :], func=AF.Abs),
                   reads=[R_g], writes=[R_gsm])
                mk("act", lambda e: e.activation(out=G[:, 1, :], in_=G[:, 0, :], func=AF.Exp, scale=-1.0),
                   reads=[R_gsm], writes=[R_gsm])
                mk("act", lambda e: e.activation(out=G[:, 2, :], in_=G[:, 1, :], func=AF.Ln, bias=1.0),
                   reads=[R_gsm], writes=[R_gsm])
                mk("dve", lambda e: e.tensor_scalar(out=G[:, 3, :], in0=gfp[:, :], scalar1=0.0, scalar2=None, op0=ALU.min),
                   reads=[R_g], writes=[R_gsm])
                mk("dve", lambda e: e.tensor_tensor(out=G[:, 4, :], in0=G[:, 2, :], in1=G[:, 3, :], op=ALU.subtract),
                   reads=[R_gsm], writes=[R_gsm])
                bk, rb_ = getbank()
                mk("pe", lambda e: e.matmul(bk[:, 0:NG], lhsT=triI[:, :], rhs=G[:, 4, :], start=True, stop=True),
                   reads=[R_gsm] + RC, writes=[rb_])
                mk("pe", lambda e: e.matmul(bk[:, 16:16 + NG], lhsT=onesf[:, :], rhs=G[:, 4, :], start=True, stop=True),
                   reads=[R_gsm] + RC, writes=[rb_])
                mk("dve", lambda e: e.tensor_tensor(out=G[:, 5, :], in0=bk[:, 0:NG], in1=gi[:, :], op=ALU.add),
                   reads=[rb_, R_g], writes=[R_gsm])
                mk("act", lambda e: e.activation(out=ev[:, :], in_=G[:, 5, :], func=AF.Exp),
                   reads=[R_gsm], writes=[R_ev])
                mk("act", lambda e: e.activation(out=fl[:, :], in_=bk[:, 0:NG], func=AF.Exp),
                   reads=[rb_], writes=[R_ev])
                mk("act", lambda e: e.activation(out=dec[:, :], in_=bk[:, 16:16 + NG], func=AF.Exp, scale=-1.0),
                   reads=[rb_], writes=[R_ev])

            def tile_conv(T):
                for jc in range(4):
                    bk, rb_ = getbank()
                    for j in range(31):
                        mk("pe", lambda e, jc=jc, j=j, bk=bk: e.matmul(bk[:, 0:TT], lhsT=dconv[:, jc * 31 + j, :], rhs=ubuf[:, jc, j:j + TT],
                                                                       start=(j == 0), stop=(j == 30)),
                           reads=[R_ubuf] + RC, writes=[rb_])
                    mk("act", lambda e, jc=jc, bk=bk: e.activation(out=cv[:, jc, :], in_=bk[:, 0:TT], func=AF.Identity, bias=cb[:, jc:jc + 1]),
                       reads=[rb_] + RC, writes=[R_cv[jc]])
                    mk("pool", lambda e, jc=jc: e.tensor_copy(out=cvb[:, jc, :], in_=cv[:, jc, :]),
                       reads=[R_cv[jc]], writes=[R_cvb])
                    mk("act", lambda e, jc=jc: e.activation(out=sqb[:, jc, :], in_=cv[:, jc, :], func=AF.Square),
                       reads=[R_cv[jc]], writes=[R_sqb])
                mk("pool", lambda e: e.tensor_copy(out=ubuf[:, :, 0:30], in_=ubuf[:, :, TT:TT + 30]),
                   reads=[], writes=[R_ubuf])
                bm, rbm = getbank()
                be, rbe = getbank()
                for jc in range(4):
                    mk("pe", lambda e, jc=jc: e.matmul(bm[:, 0:TT], lhsT=o512b[:, :], rhs=cvb[:, jc, :], start=(jc == 0), stop=(jc == 3)),
                       reads=[R_cvb] + RC, writes=[rbm])
                for jc in range(4):
                    mk("pe", lambda e, jc=jc: e.matmul(be[:, 0:TT], lhsT=o512b[:, :], rhs=sqb[:, jc, :], start=(jc == 0), stop=(jc == 3)),
                       reads=[R_sqb] + RC, writes=[rbe])
                mk("act", lambda e: e.activation(out=mean_sb[:, :], in_=bm[:, 0:TT], func=AF.Identity),
                   reads=[rbm], writes=[R_stats])
                mk("dve", lambda e: e.tensor_tensor(out=msq[:, :], in0=mean_sb[:, :], in1=mean_sb[:, :], op=ALU.mult),
                   reads=[R_stats], writes=[R_stats])
                mk("dve", lambda e: e.tensor_tensor(out=msq[:, :], in0=be[:, 0:TT], in1=msq[:, :], op=ALU.subtract),
                   reads=[rbe, R_stats], writes=[R_stats])
                mk("act", lambda e: e.activation(out=rstdt[:, :], in_=msq[:, :], func=AF.Ln, bias=epsc[:, 0:1]),
                   reads=[R_stats] + RC, writes=[R_stats])
                mk("act", lambda e: e.activation(out=rstdt[:, :], in_=rstdt[:, :], func=AF.Exp, scale=-0.5),
                   reads=[R_stats], writes=[R_stats])
                for jc in range(4):
                    mk("pool", lambda e, jc=jc: e.tensor_tensor(out=cv[:, jc, :], in0=cv[:, jc, :], in1=mean_sb[:, :], op=ALU.subtract),
                       reads=[R_stats, R_cv[jc]], writes=[R_cv[jc]])
                    mk("dve", lambda e, jc=jc: e.tensor_tensor(out=cv[:, jc, :], in0=cv[:, jc, :], in1=rstdt[:, :], op=ALU.mult),
                       reads=[R_stats, R_cv[jc]], writes=[R_cv[jc]])
                    t, rt_ = gettmp()
                    mk("act", lambda e, jc=jc, t=t: e.activation(out=t[:, :], in_=cv[:, jc, :], func=AF.Tanh, scale=hlng[:, jc:jc + 1], bias=hlnb[:, jc:jc + 1]),
                       reads=[R_cv[jc]] + RC, writes=[rt_])
                    mk("act", lambda e, jc=jc: e.activation(out=cv[:, jc, :], in_=cv[:, jc, :], func=AF.Identity, scale=lng[:, jc:jc + 1], bias=lnb[:, jc:jc + 1]),
                       reads=[R_cv[jc]] + RC, writes=[R_cv[jc]])
                    mk("dve", lambda e, jc=jc, t=t: e.scalar_tensor_tensor(out=yT[:, jc, :], in0=t[:, :], scalar=1.0, in1=cv[:, jc, :], op0=ALU.add, op1=ALU.mult),
                       reads=[rt_, R_cv[jc]], writes=[R_yT])

            def tile_qkconv(T):
                for jp in range(8):
                    bk, rb_ = getbank()
                    for j in range(4):
                        mk("pe", lambda e, jp=jp, j=j, bk=bk: e.matmul(bk[:, 0:TT], lhsT=dqk[:, jp * 4 + j, :], rhs=qkbuf[:, jp, j:j + TT],
                                                                       start=(j == 0), stop=(j == 3)),
                           reads=[R_qkbuf] + RC, writes=[rb_])
                    t, rt_ = gettmp()
                    z, rz_ = gettmp()
                    mk("act", lambda e, jp=jp, bk=bk, t=t: e.activation(out=t[:, :], in_=bk[:, 0:TT], func=AF.Tanh, scale=0.5, bias=hqkb[:, jp:jp + 1]),
                       reads=[rb_] + RC, writes=[rt_])
                    mk("act", lambda e, jp=jp, bk=bk, z=z: e.activation(out=z[:, :], in_=bk[:, 0:TT], func=AF.Identity, bias=qkb[:, jp:jp + 1]),
                       reads=[rb_] + RC, writes=[rz_])
                    mk("dve", lambda e, jp=jp, t=t, z=z: e.scalar_tensor_tensor(out=qkT[:, jp, :], in0=t[:, :], scalar=1.0, in1=z[:, :], op0=ALU.add, op1=ALU.mult),
                       reads=[rt_, rz_], writes=[R_qkT])
                mk("pool", lambda e: e.tensor_copy(out=qkbuf[:, :, 0:3], in_=qkbuf[:, :, TT:TT + 3]),
                   reads=[], writes=[R_qkbuf])
                for q in range(NQ):
                    bk, rb_ = getbank()
                    for h in range(4):
                        mk("pe", lambda e, q=q, h=h, bk=bk: e.transpose(out=b16(bk)[:, h * 128:(h + 1) * 128], in_=qkT[:, 4 + h, q * 128:(q + 1) * 128], identity=identb[:, :]),
                           reads=[R_qkT] + RC, writes=[rb_])
                    mk("act", lambda e, q=q, bk=bk: e.activation(out=ktm[:, q, :], in_=b16(bk)[:, 0:512], func=AF.Identity),
                       reads=[rb_], writes=[R_ktm])

            def chunk_mlstm(T, q):
                c = T * NQ + q
                s = c % 2
                V = vt[s]; ST_ = stT[s]
                evq = ev[:, q * 4:(q + 1) * 4]
                cs = slice(q * 128, (q + 1) * 128)
                mk("dve", lambda e: e.tensor_tensor(out=V[:, :, 0:128], in0=vsb[:, q, :].rearrange("p (h d) -> p h d", h=4),
                                                    in1=evq.unsqueeze(2).broadcast_to([128, 4, 128]), op=ALU.mult),
                   reads=[R_vsb, R_ev], writes=[R_vt[s]])
                mk("pool", lambda e: e.tensor_copy(out=V[:, :, 128:129], in_=evq.unsqueeze(2)),
                   reads=[R_ev], writes=[R_vt[s]])
                bs, rbs = getbank()
                for h in range(4):
                    mk("pe", lambda e, h=h: e.matmul(bs[:, h * 128:(h + 1) * 128], lhsT=qkT[:, 4 + h, cs], rhs=qkT[:, h, cs], start=True, stop=True),
                       reads=[R_qkT], writes=[rbs])
                mk("dve", lambda e: e.tensor_tensor(out=ST_[:, :, :], in0=bs[:, 0:512].rearrange("p (h t) -> p h t", h=4),
                                                    in1=maskk[:, :].unsqueeze(1).broadcast_to([128, 4, 128]), op=ALU.mult),
                   reads=[rbs] + RC, writes=[R_stT[s]])
                bo = [getbank(), getbank()]
                for h in range(4):
                    bk, rb_ = bo[h // 2]
                    off = (h % 2) * 129
                    mk("pe", lambda e, h=h, bk=bk, off=off: e.matmul(bk[:, off:off + 129], lhsT=ST_[:, h, :], rhs=V[:, h, :], start=True, stop=False),
                       reads=[R_stT[s], R_vt[s]], writes=[rb_])
                    mk("pe", lambda e, h=h, bk=bk, off=off: e.matmul(bk[:, off:off + 129], lhsT=qkT[:, h, cs], rhs=Chb[:, h, :], start=False, stop=True),
                       reads=[R_qkT, R_Chb] + RC, writes=[rb_])
                bd = [getbank(), getbank()]
                for h in range(4):
                    bk, rb_ = bd[h // 2]
                    off = (h % 2) * 129
                    mk("pe", lambda e, h=h, bk=bk, off=off: e.matmul(bk[:, off:off + 129], lhsT=ktm[:, q, h * 128:(h + 1) * 128], rhs=V[:, h, :], start=True, stop=True),
                       reads=[R_ktm, R_vt[s]], writes=[rb_])
                for p in range(2):
                    bk, rb_ = bd[p]
                    cview = Chat[:, 2 * p:2 * p + 2, :].rearrange("p h c -> p (h c)")
                    mk("dve", lambda e, bk=bk, cview=cview: e.scalar_tensor_tensor(out=cview, in0=bk[:, 0:258], scalar=KAPPA, in1=cview, op0=ALU.mult, op1=ALU.add),
                       reads=[rb_, R_Chat], writes=[R_Chat])
                mk("pool", lambda e: e.tensor_tensor(out=Chat[:, :, :], in0=Chat[:, :, :], in1=dec[:, q * 4:(q + 1) * 4].unsqueeze(2).broadcast_to([128, 4, 129]), op=ALU.mult),
                   reads=[R_Chat, R_ev], writes=[R_Chat])
                mk("pool", lambda e: e.tensor_copy(out=Chb[:, :, :], in_=Chat[:, :, :]),
                   reads=[R_Chat], writes=[R_Chb])
                for p in range(2):
                    bk, rb_ = bo[p]
                    mk("dve", lambda e, p=p, bk=bk: e.tensor_scalar(out=sm[:, 10, 2 * p:2 * p + 2].unsqueeze(2),
                                                                    in0=bk[:, 0:258].rearrange("p (h c) -> p h c", h=2)[:, :, 128:129],
                                                                    scalar1=-1.0, scalar2=None, op0=ALU.mult),
                       reads=[rb_], writes=[R_sm])
                mk("dve", lambda e: e.scalar_tensor_tensor(out=sm[:, 0, :], in0=sm[:, 10, :], scalar=-1.0, in1=sm[:, 10, :], op0=ALU.mult, op1=ALU.max),
                   reads=[R_sm], writes=[R_sm])
                mk("dve", lambda e: e.tensor_tensor(out=sm[:, 0, :], in0=sm[:, 0, :], in1=fl[:, q * 4:(q + 1) * 4], op=ALU.max),
                   reads=[R_sm, R_ev], writes=[R_sm])
                mk("dve", lambda e: e.reciprocal(out=sm[:, 1, :], in_=sm[:, 0, :]), reads=[R_sm], writes=[R_sm])
                for h in range(4):
                    bk, rb_ = bo[h // 2]
                    off = (h % 2) * 129
                    mk("dve", lambda e, h=h, bk=bk, off=off: e.scalar_tensor_tensor(out=hm[:, h, :], in0=bk[:, off:off + 128], scalar=sm[:, 1, h:h + 1],
                                                                                    in1=osb[:, q, h * 128:(h + 1) * 128], op0=ALU.mult, op1=ALU.mult,
                                                                                    accum_out=sm[:, 2, h:h + 1]),
                       reads=[rb_, R_sm, R_osb], writes=[R_hm, R_sm])
                for h in range(4):
                    mk("act", lambda e, h=h: e.activation(out=junk[:, :], in_=hm[:, h, :], func=AF.Square, accum_out=sm[:, 3, h:h + 1]),
                       reads=[R_hm], writes=[R_junk, R_sm])
                mk("dve", lambda e: e.tensor_scalar(out=sm[:, 4, :], in0=sm[:, 2, :], scalar1=1.0 / 128, scalar2=None, op0=ALU.mult),
                   reads=[R_sm], writes=[R_sm])
                mk("dve", lambda e: e.tensor_tensor(out=sm[:, 5, :], in0=sm[:, 4, :], in1=sm[:, 4, :], op=ALU.mult),
                   reads=[R_sm], writes=[R_sm])
                mk("dve", lambda e: e.scalar_tensor_tensor(out=sm[:, 6, :], in0=sm[:, 3, :], scalar=1.0 / 128, in1=sm[:, 5, :], op0=ALU.mult, op1=ALU.subtract),
                   reads=[R_sm], writes=[R_sm])
                mk("dve", lambda e: e.tensor_scalar(out=sm[:, 8, :], in0=sm[:, 6, :], scalar1=EPS, scalar2=None, op0=ALU.add),
                   reads=[R_sm], writes=[R_sm])
                emit_rsqrt(sm[:, 7, :], sm[:, 8, :], sm[:, 9, :], [R_sm])
                for h in range(4):
                    eng = "dve" if h % 2 == 0 else "pool"
                    mk(eng, lambda e, h=h: e.tensor_scalar(out=hmn[:, h, :], in0=hm[:, h, :], scalar1=sm[:, 4, h:h + 1], scalar2=sm[:, 7, h:h + 1],
                                                           op0=ALU.subtract, op1=ALU.mult),
                       reads=[R_hm, R_sm], writes=[R_hmn])
                bk, rb_ = getbank()
                for h in range(4):
                    mk("pe", lambda e, h=h: e.transpose(out=b16(bk)[:, h * 128:(h + 1) * 128], in_=hmn[:, h, :], identity=identb[:, :]),
                       reads=[R_hmn] + RC, writes=[rb_])
                mk("act", lambda e: e.activation(out=yT[:, 4:8, cs], in_=b16(bk)[:, 0:512].rearrange("p (h t) -> p h t", h=4), func=AF.Identity),
                   reads=[rb_], writes=[R_yT])

            def tile_back(T):
                s = T % 2
                X = xt[s]
                for q in range(NQ):
                    cs = slice(q * 128, (q + 1) * 128)
                    c = T * NQ + q
                    ba, rba = getbank()
                    bb, rbb = getbank()
                    for k in range(8):
                        mk("pe", lambda e, k=k, ba=ba, cs=cs: e.matmul(ba[:, 0:512], lhsT=yT[:, k, cs], rhs=woutb[:, k, 0:512], start=(k == 0), stop=(k == 7)),
                           reads=[R_yT] + RC, writes=[rba])
                        mk("pe", lambda e, k=k, bb=bb, cs=cs: e.matmul(bb[:, 0:512], lhsT=yT[:, k, cs], rhs=woutb[:, k, 512:1024], start=(k == 0), stop=(k == 7)),
                           reads=[R_yT] + RC, writes=[rbb])
                    mk("dve", lambda e, q=q, ba=ba: e.tensor_tensor(out=X[:, q, 0:512], in0=ba[:, 0:512], in1=X[:, q, 0:512], op=ALU.add),
                       reads=[rba, R_xt[s]], writes=[R_xt[s]])
                    mk("dve", lambda e, q=q, bb=bb: e.tensor_tensor(out=X[:, q, 512:1024], in0=bb[:, 0:512], in1=X[:, q, 512:1024], op=ALU.add),
                       reads=[rbb, R_xt[s]], writes=[R_xt[s]])
                    mk("act", lambda e, q=q: e.activation(out=h2[:, q, :], in_=X[:, q, :], func=AF.Square, accum_out=ss[:, 4, q:q + 1]),
                       reads=[R_xt[s]], writes=[R_h2, R_ss])
                mk("sp", lambda e: e.dma_start(out=x1_d[T * TT:(T + 1) * TT, :].rearrange("(q p) d -> p q d", p=128), in_=X[:, :, :]),
                   reads=[R_xt[s]], accum=[R_x1], dma=x1sem[s])
                if debug:
                    mk("sp", lambda e: e.dma_start(out=dbg["x1"][T * TT:(T + 1) * TT, :].rearrange("(q p) d -> p q d", p=128), in_=X[:, :, :]),
                       reads=[R_xt[s]], accum=[R_dbg], dma=dbsem[s])
                mk("dve", lambda e: e.tensor_scalar(out=ss[:, 5, :], in0=ss[:, 4, :], scalar1=1.0 / D, scalar2=EPS, op0=ALU.mult, op1=ALU.add),
                   reads=[R_ss], writes=[R_ss])
                emit_rsqrt(ss[:, 6, :], ss[:, 5, :], ss[:, 7, :], [R_ss])
                for q in range(NQ):
                    mk("dve", lambda e, q=q: e.scalar_tensor_tensor(out=h2[:, q, :], in0=X[:, q, :], scalar=ss[:, 6, q:q + 1], in1=g2bc[:, :], op0=ALU.mult, op1=ALU.mult),
                       reads=[R_xt[s], R_ss] + RC, writes=[R_h2])
                br, rbr = getbank()
                for q in range(NQ):
                    cs = slice(q * 128, (q + 1) * 128)
                    bk, rb_ = getbank()
                    for k in range(8):
                        mk("pe", lambda e, q=q, k=k, bk=bk: e.transpose(out=b16(bk)[:, k * 128:(k + 1) * 128], in_=h2[:, q, k * 128:(k + 1) * 128], identity=identb[:, :]),
                           reads=[R_h2] + RC, writes=[rb_])
                    mk("act", lambda e, q=q, bk=bk, cs=cs: e.activation(out=h2T[:, :, cs], in_=b16(bk)[:, 0:1024].rearrange("p (k t) -> p k t", k=8), func=AF.Identity),
                       reads=[rb_], writes=[R_h2T])
                    for k in range(8):
                        mk("pe", lambda e, q=q, k=k, cs=cs: e.matmul(br[:, q * 36:(q + 1) * 36], lhsT=h2T[:, k, cs], rhs=rwb[:, k, :], start=(k == 0), stop=(k == 7)),
                           reads=[R_h2T] + RC, writes=[rbr])
                for q in range(NQ):
                    c = T * NQ + q
                    lg = rt[:, 0, :]
                    mk("dve", lambda e, q=q, lg=lg: e.tensor_tensor(out=lg, in0=br[:, q * 36:(q + 1) * 36], in1=rbbc[:, :], op=ALU.add),
                       reads=[rbr] + RC, writes=[R_rt])
                    r1 = [R_rt]
                    mk("dve", lambda e: e.tensor_reduce(out=rt[:, 1, 0:1], in_=rt[:, 0, 0:4], axis=AX.X, op=ALU.max), reads=r1, writes=r1)
                    mk("dve", lambda e: e.tensor_scalar(out=rt[:, 2, 0:4], in0=rt[:, 0, 0:4], scalar1=rt[:, 1, 0:1], scalar2=None, op0=ALU.is_equal), reads=r1, writes=r1)
                    mk("dve", lambda e: e.tensor_scalar(out=rt[:, 1, 1:2], in0=rt[:, 1, 0:1], scalar1=-1.0, scalar2=None, op0=ALU.mult), reads=r1, writes=r1)
                    mk("act", lambda e: e.activation(out=rt[:, 3, 0:4], in_=rt[:, 0, 0:4], func=AF.Exp, bias=rt[:, 1, 1:2], accum_out=rt[:, 1, 2:3]), reads=r1, writes=r1)
                    mk("dve", lambda e: e.reciprocal(out=rt[:, 1, 3:4], in_=rt[:, 1, 2:3]), reads=r1, writes=r1)
                    mk("dve", lambda e: e.tensor_scalar(out=rt[:, 4, 0:4], in0=rt[:, 2, 0:4], scalar1=-1.0, scalar2=1e30, op0=ALU.add, op1=ALU.mult), reads=r1, writes=r1)
                    mk("dve", lambda e: e.tensor_tensor(out=rt[:, 5, 0:32].rearrange("p (g k) -> p g k", g=4), in0=rt[:, 0, 4:36].rearrange("p (g k) -> p g k", g=4),
                                                        in1=rt[:, 4, 0:4].unsqueeze(2).broadcast_to([128, 4, 8]), op=ALU.add), reads=r1, writes=r1)
                    mk("dve", lambda e: e.max(out=rt[:, 6, 0:8], in_=rt[:, 5, 0:32]), reads=r1, writes=r1)
                    mk("dve", lambda e: e.tensor_scalar(out=rt[:, 7, 0:32], in0=rt[:, 5, 0:32], scalar1=rt[:, 6, 0:1], scalar2=None, op0=ALU.is_equal), reads=r1, writes=r1)
                    mk("dve", lambda e: e.tensor_scalar(out=rt[:, 8, 0:32], in0=rt[:, 5, 0:32], scalar1=rt[:, 6, 1:2], scalar2=None, op0=ALU.is_equal), reads=r1, writes=r1)
                    mk("dve", lambda e: e.tensor_tensor(out=rt[:, 1, 4:5], in0=rt[:, 6, 1:2], in1=rt[:, 6, 0:1], op=ALU.subtract), reads=r1, writes=r1)
                    mk("act", lambda e: e.activation(out=rt[:, 1, 5:6], in_=rt[:, 1, 4:5], func=AF.Exp), reads=r1, writes=r1)
                    mk("dve", lambda e: e.tensor_scalar(out=rt[:, 1, 6:7], in0=rt[:, 1, 5:6], scalar1=1.0, scalar2=None, op0=ALU.add), reads=r1, writes=r1)
                    mk("dve", lambda e: e.reciprocal(out=rt[:, 1, 7:8], in_=rt[:, 1, 6:7]), reads=r1, writes=r1)
                    mk("dve", lambda e, c=c: e.tensor_tensor(out=gwt[:, 2 * c:2 * c + 1], in0=rt[:, 1, 7:8], in1=rt[:, 1, 3:4], op=ALU.mult), reads=r1, writes=[R_gwt])
                    mk("dve", lambda e, c=c: e.tensor_tensor(out=gwt[:, 2 * c + 1:2 * c + 2], in0=rt[:, 1, 3:4], in1=gwt[:, 2 * c:2 * c + 1], op=ALU.subtract), reads=r1 + [R_gwt], writes=[R_gwt])
                    mk("dve", lambda e: e.tensor_tensor(out=cmbb[:, :], in0=rt[:, 7, 0:32], in1=rt[:, 8, 0:32], op=ALU.add), reads=r1, writes=r1)
                    bk, rb_ = getbank()
                    mk("pe", lambda e, bk=bk: e.matmul(bk[:, 0:32], lhsT=triSb[:, :], rhs=cmbb[:, :], start=True, stop=True), reads=r1 + RC, writes=[rb_])
                    mk("pe", lambda e, bk=bk: e.matmul(bk[:, 32:64], lhsT=onesb[:, :], rhs=cmbb[:, :], start=True, stop=True), reads=r1 + RC, writes=[rb_])
                    mk("dve", lambda e, bk=bk: e.tensor_tensor(out=rt[:, 9, 0:32], in0=bk[:, 0:32], in1=basep[:, :], op=ALU.add), reads=[rb_, R_base] + r1, writes=r1)
                    mk("dve", lambda e: e.scalar_tensor_tensor(out=rt[:, 10, 0:32], in0=rt[:, 7, 0:32], scalar=1.0, in1=rt[:, 9, 0:32], op0=ALU.mult, op1=ALU.mult, accum_out=rt[:, 1, 8:9]), reads=r1, writes=r1)
                    mk("dve", lambda e: e.scalar_tensor_tensor(out=rt[:, 10, 0:32], in0=rt[:, 8, 0:32], scalar=1.0, in1=rt[:, 9, 0:32], op0=ALU.mult, op1=ALU.mult, accum_out=rt[:, 1, 9:10]), reads=r1, writes=r1)
                    mk("dve", lambda e, c=c: e.tensor_scalar(out=didx[:, 2 * c:2 * c + 2], in0=rt[:, 1, 8:10], scalar1=float(NSLOT - 1), scalar2=None, op0=ALU.min), reads=r1, writes=[R_didx])
                    mk("dve", lambda e, bk=bk: e.tensor_tensor(out=basep[:, :], in0=basep[:, :], in1=bk[:, 32:64], op=ALU.add), reads=[rb_], writes=[R_base])
                    for j in range(2):
                        mk("pool", lambda e, q=q, c=c, j=j: e.indirect_dma_start(out=xs_d, out_offset=bass.IndirectOffsetOnAxis(ap=didx[:, 2 * c + j:2 * c + j + 1], axis=0),
                                                                                 in_=h2[:, q, :], in_offset=None),
                           reads=[R_h2, R_didx], accum=[R_xs], dma=scsem)

            for T in range(NT):
                tile_front(T)
                if T == 0:
                    dump("winb", winb[:, 0, :], RC)
                    dump("woutb", woutb[:, 4, :], RC)
                    dump("dconv", dconv[:, 0:4, :], RC)
                    dump("ss", ss[:, :, :], [R_ss])
                    dump("hb", hb[:, :, :], [R_hb])
                    dump("hT", hT[:, :, :], [R_hT])
                    dump("ubuf", ubuf[:, :, :], [R_ubuf])
                    dump("qkbuf", qkbuf[:, :, :], [R_qkbuf])
                    dump("vsb", vsb[:, :, :], [R_vsb])
                    dump("osb", osb[:, :, :], [R_osb])
                    dump("gi", gi[:, :], [R_g])
                    dump("gfp", gfp[:, :], [R_g])
                tile_gates(T)
                if T == 0:
                    dump("gsm", gsm[:, :, :], [R_gsm])
                    dump("ev", ev[:, :], [R_ev])
                    dump("fl", fl[:, :], [R_ev])
                    dump("dec", dec[:, :], [R_ev])
                tile_conv(T)
                if T == 0:
                    dump("cv", cv[:, :, :], R_cv)
                    dump("rstdt", rstdt[:, :], [R_stats])
                    dump("mean", mean_sb[:, :], [R_stats])
                tile_qkconv(T)
                if T == 0:
                    dump("qkT", qkT[:, :, :], [R_qkT])
                    dump("ktm", ktm[:, :, :], [R_ktm])
                for q in range(NQ):
                    chunk_mlstm(T, q)
                    if T == 0 and q == 0:
                        dump("vt", vt[0][:, :, :], [R_vt[0]])
                        dump("stT", stT[0][:, :, :], [R_stT[0]])
                        dump("Chat", Chat[:, :, :], [R_Chat])
                        dump("hm", hm[:, :, :], [R_hm])
                        dump("hmn", hmn[:, :, :], [R_hmn])
                        dump("sm", sm[:, :, :], [R_sm])
                if debug and S <= 1024:
                    for k in range(8):
                        t, rt_ = gettmp()
                        mk("dve", lambda e, k=k, t=t: e.tensor_copy(out=t[:, :], in_=yT[:, k, :]), reads=[R_yT], writes=[rt_])
                        mk("sp", lambda e, k=k, t=t, T=T: e.dma_start(out=dbg["yT"][:, k * S + T * TT:k * S + (T + 1) * TT], in_=t[:, :]), reads=[rt_], accum=[R_dbg], dma=P.dsem())
                tile_back(T)

            allres = [R_xt[0], R_xt[1], R_hb, R_h2, R_hT, R_h2T, R_ubuf, R_qkbuf, R_vsb, R_osb, R_g, R_gsm, R_ev, R_cvb, R_sqb, R_stats,
                      R_qkT, R_ktm, R_vt[0], R_vt[1], R_stT[0], R_stT[1], R_Chat, R_Chb, R_hm, R_hmn, R_sm, R_junk, R_yT, R_ss, R_rt,
                      R_const, R_c2, R_base] + R_cv + R_tmp + bank_res
            R_phase = Res("phase")
            for eng in ("pe", "act", "dve", "pool", "sp"):
                P.wait_all(eng, allres)

        with contextlib.ExitStack() as st3:
            def sb3(name, shape, dtype=F32):
                return st3.enter_context(nc.sbuf_tensor("s_" + name, list(shape), dtype))

            wgb = [sb3("wgb%d" % i, [128, 8, DE], BF16) for i in range(2)]
            wub = [sb3("wub%d" % i, [128, 8, DE], BF16) for i in range(2)]
            wdb = [sb3("wdb%d" % i, [128, 4, D], BF16) for i in range(2)]
            R_w = [Res("w0"), Res("w1")]
            wsem = [P.dsem("w0"), P.dsem("w1")]
            xg = [sb3("xg%d" % i, [128, D], BF16) for i in range(3)]
            R_xg = [Res("xg%d" % i) for i in range(3)]
            xgsem = [P.dsem("xg%d" % i) for i in range(3)]
            xTe = [sb3("xTe%d" % i, [128, 8, CAP], BF16) for i in range(2)]
            R_xTe = [Res("xTe0"), Res("xTe1")]
            actT = [sb3("actT%d" % i, [128, 4, CAP], BF16) for i in range(2)]
            R_actT = [Res("actT0"), Res("actT1")]
            sil = [sb3("sil%d" % i, [128, 512]) for i in range(2)]
            R_sil = [Res("sil0"), Res("sil1")]
            yo = [sb3("yo%d" % i, [128, D]) for i in range(3)]
            R_yo = [Res("yo%d" % i) for i in range(3)]
            yosem = [P.dsem("yo%d" % i) for i in range(3)]
            gfbc = sb3("gfbc", [128, D])
            R_gf = Res("gfbc")
            gfsem = P.dsem("gf")
            mk("sp", lambda e: e.dma_start(out=gfbc[:, :], in_=gf_d.partition_broadcast(128)), writes=[R_gf], dma=gfsem)

            def load_w(e_):
                s = e_ % 2
                for (dst, src) in ((wgb[s], wg_d), (wub[s], wu_d)):
                    for k in range(8):
                        mk("pool", lambda e, dst=dst, src=src, k=k: e.dma_start(out=dst[:, k, :], in_=src[e_, k * 128:(k + 1) * 128, :]),
                           accum=[R_w[s]], dma=wsem[s])
                for k in range(4):
                    mk("pool", lambda e, k=k: e.dma_start(out=wdb[s][:, k, :], in_=wd_d[e_, k * 128:(k + 1) * 128, :]),
                       accum=[R_w[s]], dma=wsem[s])

            sil_ctr = [0]
            xg_ctr = [0]
            yo_ctr = [0]
            load_w(0)
            segs = []
            o = 0
            while o < CAP:
                n = min(512, CAP - o)
                segs.append((o, n))
                o += n
            def expert(e_):
                s = e_ % 2
                if e_ + 1 < NE:
                    load_w(e_ + 1)
                XT = xTe[s]
                for b in range(NB):
                    gi_ = xg_ctr[0] % 3
                    xg_ctr[0] += 1
                    row0 = e_ * CAP + b * 128
                    mk("sp", lambda e, gi_=gi_, row0=row0: e.dma_start(out=xg[gi_][:, :], in_=xs_d[row0:row0 + 128, :]),
                       reads=[R_xs], writes=[R_xg[gi_]], dma=xgsem[gi_])
                    bk, rb_ = getbank()
                    for k in range(8):
                        mk("pe", lambda e, gi_=gi_, k=k, bk=bk: e.transpose(out=b16(bk)[:, k * 128:(k + 1) * 128], in_=xg[gi_][:, k * 128:(k + 1) * 128], identity=identb[:, :]),
                           reads=[R_xg[gi_], R_const], writes=[rb_])
                    eng = "dve" if b % 2 == 0 else "act"
                    if eng == "dve":
                        mk("dve", lambda e, b=b, bk=bk: e.tensor_copy(out=XT[:, :, b * 128:(b + 1) * 128], in_=b16(bk)[:, 0:1024].rearrange("p (k t) -> p k t", k=8)),
                           reads=[rb_], writes=[R_xTe[s]])
                    else:
                        mk("act", lambda e, b=b, bk=bk: e.activation(out=XT[:, :, b * 128:(b + 1) * 128], in_=b16(bk)[:, 0:1024].rearrange("p (k t) -> p k t", k=8), func=AF.Identity),
                           reads=[rb_], writes=[R_xTe[s]])
                for j in range(4):
                    for (o, n) in segs:
                        bg_, rbg_ = getbank()
                        bu_, rbu_ = getbank()
                        for k in range(8):
                            mk("pe", lambda e, j=j, k=k, o=o, n=n, bg_=bg_: e.matmul(bg_[:, 0:n], lhsT=wgb[s][:, k, j * 128:(j + 1) * 128], rhs=XT[:, k, o:o + n], start=(k == 0), stop=(k == 7)),
                               reads=[R_w[s], R_xTe[s]], writes=[rbg_])
                        for k in range(8):
                            mk("pe", lambda e, j=j, k=k, o=o, n=n, bu_=bu_: e.matmul(bu_[:, 0:n], lhsT=wub[s][:, k, j * 128:(j + 1) * 128], rhs=XT[:, k, o:o + n], start=(k == 0), stop=(k == 7)),
                               reads=[R_w[s], R_xTe[s]], writes=[rbu_])
                        si = sil_ctr[0] % 2
                        sil_ctr[0] += 1
                        mk("act", lambda e, si=si, n=n, bg_=bg_: e.activation(out=sil[si][:, 0:n], in_=bg_[:, 0:n], func=AF.Silu),
                           reads=[rbg_], writes=[R_sil[si]])
                        mk("dve", lambda e, si=si, j=j, o=o, n=n, bu_=bu_: e.tensor_tensor(out=actT[s][:, j, o:o + n], in0=sil[si][:, 0:n], in1=bu_[:, 0:n], op=ALU.mult),
                           reads=[R_sil[si], rbu_], writes=[R_actT[s]])
                for b in range(NB):
                    ba, rba = getbank()
                    bb, rbb = getbank()
                    for j in range(4):
                        mk("pe", lambda e, j=j, b=b, ba=ba: e.matmul(ba[:, 0:512], lhsT=actT[s][:, j, b * 128:(b + 1) * 128], rhs=wdb[s][:, j, 0:512], start=(j == 0), stop=(j == 3)),
                           reads=[R_w[s], R_actT[s]], writes=[rba])
                        mk("pe", lambda e, j=j, b=b, bb=bb: e.matmul(bb[:, 0:512], lhsT=actT[s][:, j, b * 128:(b + 1) * 128], rhs=wdb[s][:, j, 512:1024], start=(j == 0), stop=(j == 3)),
                           reads=[R_w[s], R_actT[s]], writes=[rbb])
                    yi = yo_ctr[0] % 3
                    yo_ctr[0] += 1
                    mk("act", lambda e, yi=yi, ba=ba: e.activation(out=yo[yi][:, 0:512], in_=ba[:, 0:512], func=AF.Identity),
                       reads=[rba], writes=[R_yo[yi]])
                    mk("dve", lambda e, yi=yi, bb=bb: e.tensor_copy(out=yo[yi][:, 512:1024], in_=bb[:, 0:512]),
                       reads=[rbb], writes=[R_yo[yi]])
                    row0 = e_ * CAP + b * 128
                    mk("sp", lambda e, yi=yi, row0=row0: e.dma_start(out=ys_d[row0:row0 + 128, :], in_=yo[yi][:, :]),
                       reads=[R_yo[yi]], accum=[R_ys], dma=yosem[yi])

            for e_ in range(NE):
                expert(e_)

            xa = [sb3("xa%d" % i, [128, D]) for i in range(2)]
            ya = [sb3("ya%d" % i, [128, D]) for i in range(2)]
            yb = [sb3("yb%d" % i, [128, D]) for i in range(2)]
            ob = [sb3("ob%d" % i, [128, D]) for i in range(2)]
            R_xa = [Res("xa0"), Res("xa1")]; R_ya = [Res("ya0"), Res("ya1")]; R_yb = [Res("yb0"), Res("yb1")]; R_ob = [Res("ob0"), Res("ob1")]
            xasem = [P.dsem(), P.dsem()]; yasem = [P.dsem(), P.dsem()]; ybsem = [P.dsem(), P.dsem()]; obsem = [P.dsem(), P.dsem()]
            fs = sb3("fs", [128, 4, NCH]); R_fs = Res("fs")
            jk = sb3("jk", [128, D], BF16); R_jk = Res("jk")
            for c in range(NCH):
                s = c % 2
                mk("sp", lambda e, c=c, s=s: e.dma_start(out=xa[s][:, :], in_=x1_d[c * 128:(c + 1) * 128, :]), reads=[R_x1], writes=[R_xa[s]], dma=xasem[s])
                mk("pool", lambda e, c=c, s=s: e.indirect_dma_start(out=ya[s][:, :], out_offset=None, in_=ys_d,
                                                                    in_offset=bass.IndirectOffsetOnAxis(ap=didx[:, 2 * c:2 * c + 1], axis=0)),
                   reads=[R_ys, R_didx], writes=[R_ya[s]], dma=yasem[s])
                mk("pool", lambda e, c=c, s=s: e.indirect_dma_start(out=yb[s][:, :], out_offset=None, in_=ys_d,
                                                                    in_offset=bass.IndirectOffsetOnAxis(ap=didx[:, 2 * c + 1:2 * c + 2], axis=0)),
                   reads=[R_ys, R_didx], writes=[R_yb[s]], dma=ybsem[s])
                mk("dve", lambda e, c=c, s=s: e.scalar_tensor_tensor(out=xa[s][:, :], in0=ya[s][:, :], scalar=gwt[:, 2 * c:2 * c + 1], in1=xa[s][:, :], op0=ALU.mult, op1=ALU.add),
                   reads=[R_ya[s], R_xa[s], R_gwt], writes=[R_xa[s]])
                mk("dve", lambda e, c=c, s=s: e.scalar_tensor_tensor(out=xa[s][:, :], in0=yb[s][:, :], scalar=gwt[:, 2 * c + 1:2 * c + 2], in1=xa[s][:, :], op0=ALU.mult, op1=ALU.add),
                   reads=[R_yb[s], R_xa[s], R_gwt], writes=[R_xa[s]])
                mk("act", lambda e, c=c, s=s: e.activation(out=jk[:, :], in_=xa[s][:, :], func=AF.Square, accum_out=fs[:, 0, c:c + 1]),
                   reads=[R_xa[s]], writes=[R_jk, R_fs])
                mk("dve", lambda e, c=c: e.tensor_scalar(out=fs[:, 1, c:c + 1], in0=fs[:, 0, c:c + 1], scalar1=1.0 / D, scalar2=EPS, op0=ALU.mult, op1=ALU.add),
                   reads=[R_fs], writes=[R_fs])
                emit_rsqrt(fs[:, 2, c:c + 1], fs[:, 1, c:c + 1], fs[:, 3, c:c + 1], [R_fs])
                mk("act", lambda e, c=c, s=s: e.activation(out=xa[s][:, :], in_=xa[s][:, :], func=AF.Identity, scale=fs[:, 2, c:c + 1]),
                   reads=[R_xa[s], R_fs], writes=[R_xa[s]])
                mk("pool", lambda e, s=s: e.tensor_tensor(out=ob[s][:, :], in0=xa[s][:, :], in1=gfbc[:, :], op=ALU.mult),
                   reads=[R_xa[s], R_gf], writes=[R_ob[s]])
                mk("sp", lambda e, c=c, s=s: e.dma_start(out=out_d[c * 128:(c + 1) * 128, :], in_=ob[s][:, :]),
                   reads=[R_ob[s]], accum=[R_out], dma=obsem[s])
            if debug:
                mk("sp", lambda e: e.dma_start(out=dbg["idx"], in_=didx[:, :]), reads=[R_didx], accum=[R_dbg], dma=obsem[0])
                mk("sp", lambda e: e.dma_start(out=dbg["gw"], in_=gwt[:, :]), reads=[R_gwt], accum=[R_dbg], dma=obsem[1])
            fin = [R_out, R_dbg, R_x1, R_xs, R_ys, R_didx, R_gwt, R_fs, R_jk, R_gf] + R_xa + R_ya + R_yb + R_ob + R_w + R_xg + R_xTe + R_actT + R_sil + R_yo + bank_res
            for eng in ("sp", "pe", "act", "dve", "pool"):
                P.wait_all(eng, fin)

        P.emit()
    return nc


def make_shared(inp, CAP):
    f = np.float32

    def a(v):
        return np.ascontiguousarray(v, dtype=f)

    def pk(vec, n):
        return a(np.asarray(vec).reshape(n, 128).T)

    b_in = np.asarray(inp["b_in"][0])
    sh = {
        "w_in": a(inp["w_in"][0]),
        "w_out": a(inp["w_out"][0]),
        "g1": pk(inp["norm_mix_g"][0], 8),
        "bfm": pk(b_in[0:2048], 16),
        "btm": a(b_in[2048:3080].reshape(1, 1032)),
        "cw": a(np.asarray(inp["conv_w"][0]).reshape(31, 4, 128).transpose(2, 1, 0).reshape(128, 124)),
        "cb": pk(inp["conv_b"][0], 4),
        "lng": pk(inp["conv_ln_g"][0], 4),
        "lnb": pk(inp["conv_ln_b"][0], 4),
        "qkw": a(np.asarray(inp["qk_conv_w"][0]).reshape(4, 8, 128).transpose(2, 1, 0).reshape(128, 32)),
        "qkb": pk(inp["qk_conv_b"][0], 8),
        "mg": pk(inp["mlstm_norm_g"][0], 4),
        "g2": a(np.asarray(inp["norm_ffn_g"][0]).reshape(1, D)),
        "gf": a(np.asarray(inp["final_norm_g"]).reshape(1, D)),
        "rw": a(np.concatenate([inp["router_group_w"][0], inp["router_expert_w"][0]], axis=1)),
        "rb": a(np.concatenate([inp["router_group_b"][0], inp["router_expert_b"][0]]).reshape(1, 36)),
        "ecap": a((np.arange(32) * CAP).reshape(1, 32)),
        "wg": a(inp["expert_w_gate"][0]),
        "wu": a(inp["expert_w_up"][0]),
        "wd": a(inp["expert_w_down"][0]),
        "ident": a(np.eye(128)),
        "tri_incl": a(np.triu(np.ones((128, 128)))),
        "tri_strict": a(np.triu(np.ones((128, 128)), 1)),
    }
    return sh


_CACHE = {}


def kernel(**inputs):
    x = np.asarray(inputs["x"], dtype=np.float32)
    B, S, _ = x.shape
    CAP = 1024
    key = (S, CAP)
    if key not in _CACHE:
        _CACHE[key] = build_program(S=S, CAP=CAP, NQ=2)
    nc = _CACHE[key]
    sh = make_shared(inputs, CAP)
    in_maps = []
    for b in range(B):
        m = dict(sh)
        m["x"] = np.ascontiguousarray(x[b])
        in_maps.append(m)
    res = run_bass_kernel_spmd(nc, in_maps, core_ids=list(range(B)))
    return np.stack([np.asarray(r["out"]) for r in res.results], axis=0).astype(np.float32)
```

```python
import contextlib
import numpy as np
import concourse.bass as bass
import concourse.mybir as mybir
from concourse.bass_utils import run_bass_kernel_spmd
from concourse.alu_op_type import AluOpType as ALU

F32 = mybir.dt.float32
BF16 = mybir.dt.bfloat16
I32 = mybir.dt.int32
AF = mybir.ActivationFunctionType
AX = mybir.AxisListType

D = 1024
DIN = 3080
NE = 32
DE = 512
EPS = 1e-6
KAPPA = 0.25 * (128.0 ** -0.5)


class Res:
    __slots__ = ("name", "w", "rd", "acc")

    def __init__(self, name):
        self.name = name
        self.w = None
        self.rd = []
        self.acc = []


class DmaSem:
    def __init__(self, sem):
        self.sem = sem
        self.count = 0


class Prog:
    ENGS = ("pe", "act", "dve", "pool", "sp")

    def __init__(self, nc, stack):
        self.nc = nc
        self.stack = stack
        self.items = {e: [] for e in self.ENGS}
        self.count = {e: 0 for e in self.ENGS}
        self.sem = {e: stack.enter_context(nc.semaphore("c_" + e)) for e in self.ENGS}
        self.known = {e: {} for e in self.ENGS}
        self.ndma = 0
        self.sync_same_engine = True

    def dsem(self, name=None):
        self.ndma += 1
        return DmaSem(self.stack.enter_context(self.nc.semaphore(name or ("d%d" % self.ndma))))

    def _deps(self, eng, reads, writes, accum):
        toks = []
        for r in reads:
            if r.w is not None:
                toks.append(r.w)
            toks.extend(r.acc)
        for w in writes:
            if w.w is not None:
                toks.append(w.w)
            toks.extend(w.rd)
            toks.extend(w.acc)
        for a in accum:
            if a.w is not None:
                toks.append(a.w)
            toks.extend(a.rd)
        waits = []
        kn = self.known[eng]
        own = self.sem[eng]
        best = {}
        for (s, v) in toks:
            if s is own and (eng == "pe" or not self.sync_same_engine):
                continue
            if kn.get(id(s), 0) >= v:
                continue
            if id(s) not in best or best[id(s)][1] < v:
                best[id(s)] = (s, v)
        for (s, v) in best.values():
            kn[id(s)] = v
            waits.append((s, v))
        return waits

    def op(self, eng, fn, reads=(), writes=(), accum=(), dma=None):
        waits = self._deps(eng, reads, writes, accum)
        if dma is None:
            self.count[eng] += 1
            tok = (self.sem[eng], self.count[eng])
            inc = 1
        else:
            dma.count += 16
            tok = (dma.sem, dma.count)
            inc = 16
        self.items[eng].append((waits, fn, tok[0], inc))
        for r in reads:
            r.rd.append(tok)
        for w in writes:
            w.w = tok
            w.rd = []
            w.acc = []
        for a in accum:
            a.acc.append(tok)
        return tok

    def wait_all(self, eng, ress):
        waits = self._deps(eng, ress, ress, ())
        self.items[eng].append((waits, None, None, 0))

    def emit(self):
        nc = self.nc
        with nc.Block() as block:
            def run(name, handle):
                for (waits, fn, sem, inc) in self.items[name]:
                    for (s, v) in waits:
                        handle.wait_ge(s, v)
                    if fn is not None:
                        fn(handle).then_inc(sem, inc)

            @block.tensor
            def _(e):
                run("pe", e)

            @block.scalar
            def _(e):
                run("act", e)

            @block.vector
            def _(e):
                run("dve", e)

            @block.gpsimd
            def _(e):
                run("pool", e)

            @block.sync
            def _(e):
                run("sp", e)


def build_program(S=8192, CAP=640, NQ=2, debug=False):
    TT = NQ * 128
    NT = S // TT
    NCH = S // 128
    NB = CAP // 128
    NSLOT = NE * CAP
    NG = NQ * 4
    assert S % TT == 0 and CAP % 128 == 0

    nc = bass.Bass("TRN2", target_bir_lowering=False)
    dt = nc.dram_tensor

    def din(name, shape, dtype=F32):
        return dt(name, list(shape), dtype, kind="ExternalInput").ap()

    x_d = din("x", [S, D])
    win_d = din("w_in", [D, DIN])
    wout_d = din("w_out", [D, D])
    g1_d = din("g1", [128, 8])
    bfm_d = din("bfm", [128, 16])
    btm_d = din("btm", [1, 1032])
    cw_d = din("cw", [128, 4 * 31])
    cb_d = din("cb", [128, 4])
    lng_d = din("lng", [128, 4])
    lnb_d = din("lnb", [128, 4])
    qkw_d = din("qkw", [128, 8 * 4])
    qkb_d = din("qkb", [128, 8])
    mg_d = din("mg", [128, 4])
    g2_d = din("g2", [1, D])
    gf_d = din("gf", [1, D])
    rw_d = din("rw", [D, 36])
    rb_d = din("rb", [1, 36])
    ecap_d = din("ecap", [1, 32])
    wg_d = din("wg", [NE, D, DE])
    wu_d = din("wu", [NE, D, DE])
    wd_d = din("wd", [NE, DE, D])
    ident_d = din("ident", [128, 128])
    triI_d = din("tri_incl", [128, 128])
    triS_d = din("tri_strict", [128, 128])
    out_d = dt("out", [S, D], F32, kind="ExternalOutput").ap()
    x1_d = dt("x1s", [S, D], F32, kind="Internal").ap()
    xs_d = dt("xs", [NSLOT, D], BF16, kind="ExternalOutput" if debug else "Internal").ap()
    ys_d = dt("ys", [NSLOT, D], F32, kind="ExternalOutput" if debug else "Internal").ap()
    dbg = {}
    if debug:
        dbg["x1"] = dt("dbg_x1", [S, D], F32, kind="ExternalOutput").ap()
        dbg["idx"] = dt("dbg_idx", [128, NCH * 2], I32, kind="ExternalOutput").ap()
        dbg["gw"] = dt("dbg_gw", [128, NCH * 2], F32, kind="ExternalOutput").ap()
        dbg["yT"] = dt("dbg_yT", [128, 8 * S], F32, kind="ExternalOutput").ap()

    with contextlib.ExitStack() as st:
        P = Prog(nc, st)

        def sb(name, shape, dtype=F32):
            return st.enter_context(nc.sbuf_tensor("s_" + name, list(shape), dtype))

        def mk(eng, fn, reads=(), writes=(), accum=(), dma=None):
            return P.op(eng, fn, reads, writes, accum, dma)

        dumps = {}

        def dump(name, ap, reads):
            if not debug or name in dumps:
                return
            dumps[name] = dt("dmp_" + name, list(ap.shape), ap.dtype, kind="ExternalOutput").ap()
            mk("sp", lambda e: e.dma_start(out=dumps[name], in_=ap), reads=reads, accum=[R_dbg], dma=P.dsem())

        def emit_rsqrt(y, v, t, R):
            mk("dve", lambda e: e.tensor_scalar(out=y.bitcast(I32), in0=v.bitcast(I32), scalar1=-0.5, scalar2=1597463007.0, op0=ALU.mult, op1=ALU.add),
               reads=R, writes=R)
            for _ in range(2):
                mk("dve", lambda e: e.scalar_tensor_tensor(out=t, in0=y, scalar=-0.5, in1=y, op0=ALU.mult, op1=ALU.mult), reads=R, writes=R)
                mk("dve", lambda e: e.tensor_tensor(out=t, in0=t, in1=v, op=ALU.mult), reads=R, writes=R)
                mk("dve", lambda e: e.scalar_tensor_tensor(out=y, in0=t, scalar=1.5, in1=y, op0=ALU.add, op1=ALU.mult), reads=R, writes=R)

        banks = [st.enter_context(nc.psum_tensor("bank%d" % i, [128, 512], F32)) for i in range(8)]
        bank_res = [Res("bank%d" % i) for i in range(8)]
        bank_ctr = [0]

        def getbank():
            i = bank_ctr[0] % 8
            bank_ctr[0] += 1
            return banks[i], bank_res[i]

        def b16(bank):
            return bank[:, :].bitcast(BF16)

        didx = sb("didx", [128, NCH * 2], I32)
        gwt = sb("gwt", [128, NCH * 2], F32)
        R_didx = Res("didx")
        R_gwt = Res("gwt")
        R_xs = Res("xs")
        R_ys = Res("ys")
        R_x1 = Res("x1d")
        R_out = Res("out")
        R_dbg = Res("dbg")
        identb = sb("identb", [128, 128], BF16)
        R_const = Res("const")

        ld = P.dsem("ld_const")

        with contextlib.ExitStack() as st2:
            def sb2(name, shape, dtype=F32):
                return st2.enter_context(nc.sbuf_tensor("s_" + name, list(shape), dtype))

            winb = sb2("winb", [128, 8, DIN], BF16)
            woutb = sb2("woutb", [128, 8, D], BF16)
            dconv = sb2("dconv", [128, 4 * 31, 128], BF16)
            dqk = sb2("dqk", [128, 8 * 4, 128], BF16)
            identf = sb2("identf", [128, 128], F32)
            triI = sb2("triI", [128, 128], F32)
            onesf = sb2("onesf", [128, 128], F32)
            maskk = sb2("maskk", [128, 128], F32)
            triSb = sb2("triSb", [128, 128], BF16)
            onesb = sb2("onesb", [128, 128], BF16)
            o512b = sb2("o512b", [128, 128], BF16)
            g1 = sb2("g1", [128, 8])
            bfm = sb2("bfm", [128, 16])
            hbfm = sb2("hbfm", [128, 16])
            btm = sb2("btm", [128, 1032])
            cw = sb2("cw", [128, 124])
            cb = sb2("cb", [128, 4])
            lng = sb2("lng", [128, 4])
            lnb = sb2("lnb", [128, 4])
            hlng = sb2("hlng", [128, 4])
            hlnb = sb2("hlnb", [128, 4])
            qkw = sb2("qkw", [128, 32])
            qkb = sb2("qkb", [128, 8])
            hqkb = sb2("hqkb", [128, 8])
            mg = sb2("mg", [128, 4])
            g2bc = sb2("g2bc", [128, D])
            rwf = sb2("rwf", [128, 8, 36])
            rwb = sb2("rwb", [128, 8, 36], BF16)
            rbbc = sb2("rbbc", [128, 36])
            basep = sb2("basep", [128, 32])
            halfc = sb2("halfc", [128, 1])
            epsc = sb2("epsc", [128, 1])

            xt = [sb2("xt%d" % i, [128, NQ, D]) for i in range(2)]
            R_xt = [Res("xt0"), Res("xt1")]
            xsem = [P.dsem("x0"), P.dsem("x1")]
            hb = sb2("hb", [128, NQ, D], BF16); R_hb = Res("hb")
            h2 = sb2("h2", [128, NQ, D], BF16); R_h2 = Res("h2")
            hT = sb2("hT", [128, 8, TT], BF16); R_hT = Res("hT")
            h2T = sb2("h2T", [128, 8, TT], BF16); R_h2T = Res("h2T")
            ubuf = sb2("ubuf", [128, 4, TT + 32], BF16); R_ubuf = Res("ubuf")
            qkbuf = sb2("qkbuf", [128, 8, TT + 4], BF16); R_qkbuf = Res("qkbuf")
            NTMP = 6
            tmp = [sb2("tmp%d" % i, [128, TT]) for i in range(NTMP)]
            R_tmp = [Res("tmp%d" % i) for i in range(NTMP)]
            tmp_ctr = [0]

            def gettmp():
                i = tmp_ctr[0] % NTMP
                tmp_ctr[0] += 1
                return tmp[i], R_tmp[i]

            vsb = sb2("vsb", [128, NQ, 512]); R_vsb = Res("vsb")
            osb = sb2("osb", [128, NQ, 512]); R_osb = Res("osb")
            gi = sb2("gi", [128, NG]); gfp = sb2("gfp", [128, NG]); R_g = Res("gates")
            gsm = sb2("gsm", [128, 8, NG]); R_gsm = Res("gsm")
            ev = sb2("ev", [128, NG]); fl = sb2("fl", [128, NG]); dec = sb2("dec", [128, NG])
            R_ev = Res("ev")
            cv = sb2("cv", [128, 4, TT]); R_cv = [Res("cv%d" % i) for i in range(4)]
            cvb = sb2("cvb", [128, 4, TT], BF16); R_cvb = Res("cvb")
            sqb = sb2("sqb", [128, 4, TT], BF16); R_sqb = Res("sqb")
            mean_sb = sb2("mean_sb", [128, TT]); rstdt = sb2("rstdt", [128, TT]); msq = sb2("msq", [128, TT])
            R_stats = Res("stats")
            qkT = sb2("qkT", [128, 8, TT], BF16); R_qkT = Res("qkT")
            ktm = sb2("ktm", [128, NQ, 512], BF16); R_ktm = Res("ktm")
            vt = [sb2("vt%d" % i, [128, 4, 129], BF16) for i in range(2)]; R_vt = [Res("vt0"), Res("vt1")]
            stT = [sb2("stT%d" % i, [128, 4, 128], BF16) for i in range(2)]; R_stT = [Res("stT0"), Res("stT1")]
            Chat = sb2("Chat", [128, 4, 129]); Chb = sb2("Chb", [128, 4, 129], BF16)
            R_Chat = Res("Chat"); R_Chb = Res("Chb")
            hm = sb2("hm", [128, 4, 128]); R_hm = Res("hm")
            hmn = sb2("hmn", [128, 4, 128], BF16); R_hmn = Res("hmn")
            sm = sb2("sm", [128, 12, 4]); R_sm = Res("sm")
            junk = sb2("junk", [128, 128]); R_junk = Res("junk")
            yT = sb2("yT", [128, 8, TT], BF16); R_yT = Res("yT")
            ss = sb2("ss", [128, 8, NQ]); R_ss = Res("ssf"); R_ssb = Res("ssb")
            rt = sb2("rt", [128, 16, 36]); R_rt = Res("rt")
            cmbb = sb2("cmbb", [128, 32], BF16)
            x1sem = [P.dsem("x1st0"), P.dsem("x1st1")]
            dbsem = [P.dsem("db0"), P.dsem("db1")]
            scsem = P.dsem("scat")

            def cld(eng, dst, src):
                mk(eng, lambda e: e.dma_start(out=dst, in_=src), accum=[R_const], dma=ld)

            cld("sp", identf[:, :], ident_d)
            cld("sp", triI[:, :], triI_d)
            cld("sp", g1[:, :], g1_d)
            cld("sp", bfm[:, :], bfm_d)
            cld("sp", btm[:, :], btm_d.partition_broadcast(128))
            cld("sp", cw[:, :], cw_d)
            cld("sp", cb[:, :], cb_d)
            cld("sp", lng[:, :], lng_d)
            cld("sp", lnb[:, :], lnb_d)
            cld("sp", qkw[:, :], qkw_d)
            cld("sp", qkb[:, :], qkb_d)
            cld("sp", mg[:, :], mg_d)
            cld("sp", g2bc[:, :], g2_d.partition_broadcast(128))
            cld("sp", rbbc[:, :], rb_d.partition_broadcast(128))
            R_base = Res("basep")
            mk("sp", lambda e: e.dma_start(out=basep[:, :], in_=ecap_d.partition_broadcast(128)), writes=[R_base], dma=P.dsem("base"))
            cld("sp", rwf[:, :, :], rw_d.rearrange("(k p) n -> p k n", p=128))
            cld("pool", triSb[:, :], triS_d)
            cld("pool", identb[:, :], ident_d)
            R_c2 = Res("const2")

            def cop(eng, fn):
                mk(eng, fn, reads=[R_const], accum=[R_c2])

            cop("dve", lambda e: e.memset(onesf[:, :], 1.0))
            cop("dve", lambda e: e.memset(onesb[:, :], 1.0))
            cop("dve", lambda e: e.memset(o512b[:, :], 1.0 / 512.0))
            cop("dve", lambda e: e.memset(halfc[:, :], 0.5))
            cop("dve", lambda e: e.memset(epsc[:, :], EPS))
            cop("dve", lambda e: e.tensor_scalar(out=maskk[:, :], in0=triI[:, :], scalar1=KAPPA, scalar2=None, op0=ALU.mult))
            cop("dve", lambda e: e.tensor_scalar(out=hbfm[:, :], in0=bfm[:, :], scalar1=0.5, scalar2=None, op0=ALU.mult))
            cop("dve", lambda e: e.tensor_scalar(out=hlng[:, :], in0=lng[:, :], scalar1=0.5, scalar2=None, op0=ALU.mult))
            cop("dve", lambda e: e.tensor_scalar(out=hlnb[:, :], in0=lnb[:, :], scalar1=0.5, scalar2=None, op0=ALU.mult))
            cop("dve", lambda e: e.tensor_scalar(out=hqkb[:, :], in0=qkb[:, :], scalar1=0.5, scalar2=None, op0=ALU.mult))
            cop("dve", lambda e: e.tensor_copy(out=rwb[:, :, :], in_=rwf[:, :, :]))
            mk("dve", lambda e: e.memset(Chat[:, :, :], 0.0), writes=[R_Chat])
            mk("dve", lambda e: e.memset(Chb[:, :, :], 0.0), writes=[R_Chb])
            mk("dve", lambda e: e.memset(ubuf[:, :, :], 0.0), writes=[R_ubuf])
            mk("dve", lambda e: e.memset(qkbuf[:, :, :], 0.0), writes=[R_qkbuf])
            for jc in range(4):
                cop("dve", lambda e, jc=jc: e.scalar_tensor_tensor(out=dconv[:, jc * 31:(jc + 1) * 31, :], in0=identf[:, :].unsqueeze(1).broadcast_to([128, 31, 128]), scalar=0.5,
                                                                   in1=cw[:, jc * 31:(jc + 1) * 31].unsqueeze(2).broadcast_to([128, 31, 128]), op0=ALU.mult, op1=ALU.mult))
            cop("pool", lambda e: e.tensor_tensor(out=dqk[:, :, :], in0=identf[:, :].unsqueeze(1).broadcast_to([128, 32, 128]),
                                                  in1=qkw[:, :].unsqueeze(2).broadcast_to([128, 32, 128]), op=ALU.mult))
            R_stage = R_xt
            HW = DIN // 2
            for k in range(8):
                for hf in range(2):
                    s = (2 * k + hf) % 2
                    flat = xt[s][:, :, :].rearrange("p q d -> p (q d)")
                    mk("sp", lambda e, k=k, hf=hf, flat=flat: e.dma_start(out=flat[:, 0:HW], in_=win_d[k * 128:(k + 1) * 128, hf * HW:(hf + 1) * HW]),
                       writes=[R_stage[s]], dma=xsem[s])
                    if s == 0:
                        mk("dve", lambda e, k=k, hf=hf, flat=flat: e.tensor_scalar(out=winb[:, k, hf * HW:(hf + 1) * HW], in0=flat[:, 0:HW],
                                                                                  scalar1=g1[:, k:k + 1], scalar2=None, op0=ALU.mult),
                           reads=[R_stage[s], R_const], accum=[R_c2])
                    else:
                        mk("act", lambda e, k=k, hf=hf, flat=flat: e.activation(out=winb[:, k, hf * HW:(hf + 1) * HW], in_=flat[:, 0:HW],
                                                                               func=AF.Identity, scale=g1[:, k:k + 1]),
                           reads=[R_stage[s], R_const], accum=[R_c2])
            for k in range(8):
                s = k % 2
                mk("sp", lambda e, k=k, s=s: e.dma_start(out=xt[s][:, 0, :], in_=wout_d[k * 128:(k + 1) * 128, :]),
                   writes=[R_stage[s]], dma=xsem[s])
                sc = 0.5 if k < 4 else mg[:, k - 4:k - 3]
                if k % 2 == 0:
                    mk("dve", lambda e, k=k, s=s, sc=sc: e.tensor_scalar(out=woutb[:, k, :], in0=xt[s][:, 0, :], scalar1=sc,
                                                                         scalar2=None, op0=ALU.mult),
                       reads=[R_stage[s], R_const], accum=[R_c2])
                else:
                    mk("act", lambda e, k=k, s=s, sc=sc: e.activation(out=woutb[:, k, :], in_=xt[s][:, 0, :], func=AF.Identity, scale=sc),
                       reads=[R_stage[s], R_const], accum=[R_c2])
            RC = [R_const, R_c2]

            def front1(T):
                s = T % 2
                X = xt[s]
                mk("sp", lambda e: e.dma_start(out=X[:, :, :], in_=x_d[T * TT:(T + 1) * TT, :].rearrange("(q p) d -> p q d", p=128)),
                   writes=[R_xt[s]], dma=xsem[s])
                for q in range(NQ):
                    mk("act", lambda e, q=q: e.activation(out=hb[:, q, :], in_=X[:, q, :], func=AF.Square, accum_out=ss[:, 0, q:q + 1]),
                       reads=[R_xt[s]], writes=[R_hb, R_ss])
                mk("dve", lambda e: e.tensor_scalar(out=ss[:, 1, :], in0=ss[:, 0, :], scalar1=1.0 / D, scalar2=EPS, op0=ALU.mult, op1=ALU.add),
                   reads=[R_ss], writes=[R_ss])
                emit_rsqrt(ss[:, 2, :], ss[:, 1, :], ss[:, 3, :], [R_ss])
                for q in range(NQ):
                    mk("act", lambda e, q=q: e.activation(out=hb[:, q, :], in_=X[:, q, :], func=AF.Identity, scale=ss[:, 2, q:q + 1]),
                       reads=[R_xt[s], R_ss], writes=[R_hb])
                for q in range(NQ):
                    bk, rb_ = getbank()
                    for k in range(8):
                        mk("pe", lambda e, q=q, k=k, bk=bk: e.transpose(out=b16(bk)[:, k * 128:(k + 1) * 128], in_=hb[:, q, k * 128:(k + 1) * 128], identity=identb[:, :]),
                           reads=[R_hb] + RC, writes=[rb_])
                    mk("dve", lambda e, q=q, bk=bk: e.tensor_copy(out=hT[:, :, q * 128:(q + 1) * 128], in_=b16(bk)[:, 0:1024].rearrange("p (k t) -> p k t", k=8)),
                       reads=[rb_], writes=[R_hT])

            def front2(T):
                order = [4, 0, 5, 1, 6, 2, 7, 3] + list(range(8, 16))
                thg = {}
                for j in order:
                    bk, rb_ = getbank()
                    for k in range(8):
                        mk("pe", lambda e, j=j, k=k, bk=bk: e.matmul(bk[:, 0:TT], lhsT=winb[:, k, j * 128:(j + 1) * 128], rhs=hT[:, k, :],
                                                                     start=(k == 0), stop=(k == 7)),
                           reads=[R_hT] + RC, writes=[rb_])
                    if 4 <= j < 8:
                        t, rt_ = gettmp()
                        thg[j - 4] = (t, rt_)
                        mk("act", lambda e, j=j, bk=bk, t=t: e.activation(out=t[:, :], in_=bk[:, 0:TT], func=AF.Tanh, scale=0.5, bias=hbfm[:, j:j + 1]),
                           reads=[rb_] + RC, writes=[rt_])
                    elif j < 4:
                        t, rt_ = thg[j]
                        a, ra_ = gettmp()
                        mk("act", lambda e, j=j, bk=bk, a=a: e.activation(out=a[:, :], in_=bk[:, 0:TT], func=AF.Identity, bias=bfm[:, j:j + 1]),
                           reads=[rb_] + RC, writes=[ra_])
                        mk("dve", lambda e, j=j, t=t, a=a: e.scalar_tensor_tensor(out=ubuf[:, j, 30:30 + TT], in0=t[:, :], scalar=1.0, in1=a[:, :],
                                                                                  op0=ALU.add, op1=ALU.mult),
                           reads=[rt_, ra_], writes=[R_ubuf])
                    else:
                        jp = j - 8
                        if jp % 2 == 0:
                            mk("act", lambda e, j=j, jp=jp, bk=bk: e.activation(out=qkbuf[:, jp, 3:3 + TT], in_=bk[:, 0:TT], func=AF.Identity, bias=bfm[:, j:j + 1]),
                               reads=[rb_] + RC, writes=[R_qkbuf])
                        else:
                            mk("dve", lambda e, j=j, jp=jp, bk=bk: e.tensor_scalar(out=qkbuf[:, jp, 3:3 + TT], in0=bk[:, 0:TT], scalar1=bfm[:, j:j + 1], scalar2=None, op0=ALU.add),
                               reads=[rb_] + RC, writes=[R_qkbuf])
                bg, rbg = getbank()
                for q in range(NQ):
                    bv, rbv = getbank()
                    bo, rbo = getbank()
                    for k in range(8):
                        lt = hT[:, k, q * 128:(q + 1) * 128]
                        mk("pe", lambda e, k=k, bv=bv, lt=lt: e.matmul(bv[:, 0:512], lhsT=lt, rhs=winb[:, k, 2048:2560], start=(k == 0), stop=(k == 7)),
                           reads=[R_hT] + RC, writes=[rbv])
                        mk("pe", lambda e, k=k, bo=bo, lt=lt: e.matmul(bo[:, 0:512], lhsT=lt, rhs=winb[:, k, 2560:3072], start=(k == 0), stop=(k == 7)),
                           reads=[R_hT] + RC, writes=[rbo])
                    for k in range(8):
                        lt = hT[:, k, q * 128:(q + 1) * 128]
                        mk("pe", lambda e, k=k, q=q, lt=lt: e.matmul(bg[:, q * 8:(q + 1) * 8], lhsT=lt, rhs=winb[:, k, 3072:3080], start=(k == 0), stop=(k == 7)),
                           reads=[R_hT] + RC, writes=[rbg])
                    mk("dve", lambda e, q=q, bv=bv: e.tensor_tensor(out=vsb[:, q, :], in0=bv[:, 0:512], in1=btm[:, 0:512], op=ALU.add),
                       reads=[rbv] + RC, writes=[R_vsb])
                    mk("dve", lambda e, q=q, bo=bo: e.tensor_tensor(out=osb[:, q, :], in0=bo[:, 0:512], in1=btm[:, 512:1024], op=ALU.add),
                       reads=[rbo] + RC, writes=[R_osb])
                    mk("act", lambda e, q=q: e.activation(out=osb[:, q, :], in_=osb[:, q, :], func=AF.Tanh, scale=0.5),
                       reads=[R_osb], writes=[R_osb])
                    mk("pool", lambda e, q=q: e.tensor_scalar(out=osb[:, q, :], in0=osb[:, q, :], scalar1=0.5, scalar2=0.5, op0=ALU.mult, op1=ALU.add),
                       reads=[R_osb], writes=[R_osb])
                for q in range(NQ):
                    mk("dve", lambda e, q=q: e.tensor_tensor(out=gi[:, q * 4:(q + 1) * 4], in0=bg[:, q * 8:q * 8 + 4], in1=btm[:, 1024:1028], op=ALU.add),
                       reads=[rbg] + RC, writes=[R_g])
                    mk("dve", lambda e, q=q: e.tensor_tensor(out=gfp[:, q * 4:(q + 1) * 4], in0=bg[:, q * 8 + 4:q * 8 + 8], in1=btm[:, 1028:1032], op=ALU.add),
                       reads=[rbg] + RC, writes=[R_g])

            def gates_a(T):
                G = gsm
                mk("act", lambda e: e.activation(out=G[:, 0, :], in_=gfp[:, :], func=AF.Abs),
                   reads=[R_g], writes=[R_gsm])
                mk("act", lambda e: e.activation(out=G[:, 1, :], in_=G[:, 0, :], func=AF.Exp, scale=-1.0),
                   reads=[R_gsm], writes=[R_gsm])
                mk("act", lambda e: e.activation(out=G[:, 2, :], in_=G[:, 1, :], func=AF.Ln, bias=1.0),
                   reads=[R_gsm], writes=[R_gsm])
                mk("dve", lambda e: e.tensor_scalar(out=G[:, 3, :], in0=gfp[:, :], scalar1=0.0, scalar2=None, op0=ALU.min),
                   reads=[R_g], writes=[R_gsm])
                mk("dve", lambda e: e.tensor_tensor(out=G[:, 4, :], in0=G[:, 2, :], in1=G[:, 3, :], op=ALU.subtract),
                   reads=[R_gsm], writes=[R_gsm])
                bk, rb_ = getbank()
                mk("pe", lambda e: e.matmul(bk[:, 0:NG], lhsT=triI[:, :], rhs=G[:, 4, :], start=True, stop=True),
                   reads=[R_gsm] + RC, writes=[rb_])
                mk("pe", lambda e: e.matmul(bk[:, 16:16 + NG], lhsT=onesf[:, :], rhs=G[:, 4, :], start=True, stop=True),
                   reads=[R_gsm] + RC, writes=[rb_])
                mk("dve", lambda e: e.tensor_tensor(out=G[:, 5, :], in0=bk[:, 0:NG], in1=gi[:, :], op=ALU.add),
                   reads=[rb_, R_g], writes=[R_gsm])
                return bk, rb_

            def gates_b(T, bk, rb_):
                G = gsm
                mk("act", lambda e: e.activation(out=ev[:, :], in_=G[:, 5, :], func=AF.Exp),
                   reads=[R_gsm], writes=[R_ev])
                mk("act", lambda e: e.activation(out=fl[:, :], in_=bk[:, 0:NG], func=AF.Exp),
                   reads=[rb_], writes=[R_ev])
                mk("act", lambda e: e.activation(out=dec[:, :], in_=bk[:, 16:16 + NG], func=AF.Exp, scale=-1.0),
                   reads=[rb_], writes=[R_ev])

            def conv_a(T):
                for jc in range(4):
                    bk, rb_ = getbank()
                    for j in range(31):
                        mk("pe", lambda e, jc=jc, j=j, bk=bk: e.matmul(bk[:, 0:TT], lhsT=dconv[:, jc * 31 + j, :], rhs=ubuf[:, jc, j:j + TT],
                                                                       start=(j == 0), stop=(j == 30)),
                           reads=[R_ubuf] + RC, writes=[rb_])
                    mk("act", lambda e, jc=jc, bk=bk: e.activation(out=cv[:, jc, :], in_=bk[:, 0:TT], func=AF.Identity, bias=cb[:, jc:jc + 1]),
                       reads=[rb_] + RC, writes=[R_cv[jc]])
                    mk("pool", lambda e, jc=jc: e.tensor_copy(out=cvb[:, jc, :], in_=cv[:, jc, :]),
                       reads=[R_cv[jc]], writes=[R_cvb])
                    mk("act", lambda e, jc=jc: e.activation(out=sqb[:, jc, :], in_=cv[:, jc, :], func=AF.Square),
                       reads=[R_cv[jc]], writes=[R_sqb])
                mk("pool", lambda e: e.tensor_copy(out=ubuf[:, :, 0:30], in_=ubuf[:, :, TT:TT + 30]),
                   reads=[], writes=[R_ubuf])
                bm, rbm = getbank()
                be, rbe = getbank()
                for jc in range(4):
                    mk("pe", lambda e, jc=jc: e.matmul(bm[:, 0:TT], lhsT=o512b[:, :], rhs=cvb[:, jc, :], start=(jc == 0), stop=(jc == 3)),
                       reads=[R_cvb] + RC, writes=[rbm])
                for jc in range(4):
                    mk("pe", lambda e, jc=jc: e.matmul(be[:, 0:TT], lhsT=o512b[:, :], rhs=sqb[:, jc, :], start=(jc == 0), stop=(jc == 3)),
                       reads=[R_sqb] + RC, writes=[rbe])
                mk("act", lambda e: e.activation(out=mean_sb[:, :], in_=bm[:, 0:TT], func=AF.Identity),
                   reads=[rbm], writes=[R_stats])
                mk("dve", lambda e: e.tensor_tensor(out=msq[:, :], in0=mean_sb[:, :], in1=mean_sb[:, :], op=ALU.mult),
                   reads=[R_stats], writes=[R_stats])
                mk("dve", lambda e: e.tensor_tensor(out=msq[:, :], in0=be[:, 0:TT], in1=msq[:, :], op=ALU.subtract),
                   reads=[rbe, R_stats], writes=[R_stats])

            def conv_ln(T):
                mk("act", lambda e: e.activation(out=rstdt[:, :], in_=msq[:, :], func=AF.Ln, bias=epsc[:, 0:1]),
                   reads=[R_stats] + RC, writes=[R_stats])
                mk("act", lambda e: e.activation(out=rstdt[:, :], in_=rstdt[:, :], func=AF.Exp, scale=-0.5),
                   reads=[R_stats], writes=[R_stats])

            def conv_b(T):
                for jc in range(4):
                    mk("pool", lambda e, jc=jc: e.tensor_tensor(out=cv[:, jc, :], in0=cv[:, jc, :], in1=mean_sb[:, :], op=ALU.subtract),
                       reads=[R_stats, R_cv[jc]], writes=[R_cv[jc]])
                    mk("dve", lambda e, jc=jc: e.tensor_tensor(out=cv[:, jc, :], in0=cv[:, jc, :], in1=rstdt[:, :], op=ALU.mult),
                       reads=[R_stats, R_cv[jc]], writes=[R_cv[jc]])
                    t, rt_ = gettmp()
                    mk("act", lambda e, jc=jc, t=t: e.activation(out=t[:, :], in_=cv[:, jc, :], func=AF.Tanh, scale=hlng[:, jc:jc + 1], bias=hlnb[:, jc:jc + 1]),
                       reads=[R_cv[jc]] + RC, writes=[rt_])
                    mk("act", lambda e, jc=jc: e.activation(out=cv[:, jc, :], in_=cv[:, jc, :], func=AF.Identity, scale=lng[:, jc:jc + 1], bias=lnb[:, jc:jc + 1]),
                       reads=[R_cv[jc]] + RC, writes=[R_cv[jc]])
                    mk("dve", lambda e, jc=jc, t=t: e.scalar_tensor_tensor(out=yT[:, jc, :], in0=t[:, :], scalar=1.0, in1=cv[:, jc, :], op0=ALU.add, op1=ALU.mult),
                       reads=[rt_, R_cv[jc]], writes=[R_yT])

            def tile_qkconv(T):
                for jp in range(8):
                    bk, rb_ = getbank()
                    for j in range(4):
                        mk("pe", lambda e, jp=jp, j=j, bk=bk: e.matmul(bk[:, 0:TT], lhsT=dqk[:, jp * 4 + j, :], rhs=qkbuf[:, jp, j:j + TT],
                                                                       start=(j == 0), stop=(j == 3)),
                           reads=[R_qkbuf] + RC, writes=[rb_])
                    t, rt_ = gettmp()
                    z, rz_ = gettmp()
                    mk("act", lambda e, jp=jp, bk=bk, t=t: e.activation(out=t[:, :], in_=bk[:, 0:TT], func=AF.Tanh, scale=0.5, bias=hqkb[:, jp:jp + 1]),
                       reads=[rb_] + RC, writes=[rt_])
                    mk("act", lambda e, jp=jp, bk=bk, z=z: e.activation(out=z[:, :], in_=bk[:, 0:TT], func=AF.Identity, bias=qkb[:, jp:jp + 1]),
                       reads=[rb_] + RC, writes=[rz_])
                    mk("dve", lambda e, jp=jp, t=t, z=z: e.scalar_tensor_tensor(out=qkT[:, jp, :], in0=t[:, :], scalar=1.0, in1=z[:, :], op0=ALU.add, op1=ALU.mult),
                       reads=[rt_, rz_], writes=[R_qkT])
                mk("pool", lambda e: e.tensor_copy(out=qkbuf[:, :, 0:3], in_=qkbuf[:, :, TT:TT + 3]),
                   reads=[], writes=[R_qkbuf])
                for q in range(NQ):
                    bk, rb_ = getbank()
                    for h in range(4):
                        mk("pe", lambda e, q=q, h=h, bk=bk: e.transpose(out=b16(bk)[:, h * 128:(h + 1) * 128], in_=qkT[:, 4 + h, q * 128:(q + 1) * 128], identity=identb[:, :]),
                           reads=[R_qkT] + RC, writes=[rb_])
                    mk("act", lambda e, q=q, bk=bk: e.activation(out=ktm[:, q, :], in_=b16(bk)[:, 0:512], func=AF.Identity),
                       reads=[rb_], writes=[R_ktm])

            def chunk_mlstm(T, q):
                c = T * NQ + q
                s = c % 2
                V = vt[s]; ST_ = stT[s]
                evq = ev[:, q * 4:(q + 1) * 4]
                cs = slice(q * 128, (q + 1) * 128)
                mk("dve", lambda e: e.tensor_tensor(out=V[:, :, 0:128], in0=vsb[:, q, :].rearrange("p (h d) -> p h d", h=4),
                                                    in1=evq.unsqueeze(2).broadcast_to([128, 4, 128]), op=ALU.mult),
                   reads=[R_vsb, R_ev], writes=[R_vt[s]])
                mk("pool", lambda e: e.tensor_copy(out=V[:, :, 128:129], in_=evq.unsqueeze(2)),
                   reads=[R_ev], writes=[R_vt[s]])
                bs, rbs = getbank()
                for h in range(4):
                    mk("pe", lambda e, h=h: e.matmul(bs[:, h * 128:(h + 1) * 128], lhsT=qkT[:, 4 + h, cs], rhs=qkT[:, h, cs], start=True, stop=True),
                       reads=[R_qkT], writes=[rbs])
                mk("dve", lambda e: e.tensor_tensor(out=ST_[:, :, :], in0=bs[:, 0:512].rearrange("p (h t) -> p h t", h=4),
                                                    in1=maskk[:, :].unsqueeze(1).broadcast_to([128, 4, 128]), op=ALU.mult),
                   reads=[rbs] + RC, writes=[R_stT[s]])
                bo = [getbank(), getbank()]
                for h in range(4):
                    bk, rb_ = bo[h // 2]
                    off = (h % 2) * 129
                    mk("pe", lambda e, h=h, bk=bk, off=off: e.matmul(bk[:, off:off + 129], lhsT=ST_[:, h, :], rhs=V[:, h, :], start=True, stop=False),
                       reads=[R_stT[s], R_vt[s]], writes=[rb_])
                    mk("pe", lambda e, h=h, bk=bk, off=off: e.matmul(bk[:, off:off + 129], lhsT=qkT[:, h, cs], rhs=Chb[:, h, :], start=False, stop=True),
                       reads=[R_qkT, R_Chb] + RC, writes=[rb_])
                bd = [getbank(), getbank()]
                for h in range(4):
                    bk, rb_ = bd[h // 2]
                    off = (h % 2) * 129
                    mk("pe", lambda e, h=h, bk=bk, off=off: e.matmul(bk[:, off:off + 129], lhsT=ktm[:, q, h * 128:(h + 1) * 128], rhs=V[:, h, :], start=True, stop=True),
                       reads=[R_ktm, R_vt[s]], writes=[rb_])
                for p in range(2):
                    bk, rb_ = bd[p]
                    cview = Chat[:, 2 * p:2 * p + 2, :].rearrange("p h c -> p (h c)")
                    mk("dve", lambda e, bk=bk, cview=cview: e.scalar_tensor_tensor(out=cview, in0=bk[:, 0:258], scalar=KAPPA, in1=cview, op0=ALU.mult, op1=ALU.add),
                       reads=[rb_, R_Chat], writes=[R_Chat])
                mk("pool", lambda e: e.tensor_tensor(out=Chat[:, :, :], in0=Chat[:, :, :], in1=dec[:, q * 4:(q + 1) * 4].unsqueeze(2).broadcast_to([128, 4, 129]), op=ALU.mult),
                   reads=[R_Chat, R_ev], writes=[R_Chat])
                mk("pool", lambda e: e.tensor_copy(out=Chb[:, :, :], in_=Chat[:, :, :]),
                   reads=[R_Chat], writes=[R_Chb])
                for p in range(2):
                    bk, rb_ = bo[p]
                    mk("dve", lambda e, p=p, bk=bk: e.tensor_scalar(out=sm[:, 10, 2 * p:2 * p + 2].unsqueeze(2),
                                                                    in0=bk[:, 0:258].rearrange("p (h c) -> p h c", h=2)[:, :, 128:129],
                                                                    scalar1=-1.0, scalar2=None, op0=ALU.mult),
                       reads=[rb_], writes=[R_sm])
                mk("dve", lambda e: e.scalar_tensor_tensor(out=sm[:, 0, :], in0=sm[:, 10, :], scalar=-1.0, in1=sm[:, 10, :], op0=ALU.mult, op1=ALU.max),
                   reads=[R_sm], writes=[R_sm])
                mk("dve", lambda e: e.tensor_tensor(out=sm[:, 0, :], in0=sm[:, 0, :], in1=fl[:, q * 4:(q + 1) * 4], op=ALU.max),
                   reads=[R_sm, R_ev], writes=[R_sm])
                mk("dve", lambda e: e.reciprocal(out=sm[:, 1, :], in_=sm[:, 0, :]), reads=[R_sm], writes=[R_sm])
                for h in range(4):
                    bk, rb_ = bo[h // 2]
                    off = (h % 2) * 129
                    mk("dve", lambda e, h=h, bk=bk, off=off: e.scalar_tensor_tensor(out=hm[:, h, :], in0=bk[:, off:off + 128], scalar=sm[:, 1, h:h + 1],
                                                                                    in1=osb[:, q, h * 128:(h + 1) * 128], op0=ALU.mult, op1=ALU.mult,
                                                                                    accum_out=sm[:, 2, h:h + 1]),
                       reads=[rb_, R_sm, R_osb], writes=[R_hm, R_sm])
                for h in range(4):
                    mk("act", lambda e, h=h: e.activation(out=junk[:, :], in_=hm[:, h, :], func=AF.Square, accum_out=sm[:, 3, h:h + 1]),
                       reads=[R_hm], writes=[R_junk, R_sm])
                mk("dve", lambda e: e.tensor_scalar(out=sm[:, 4, :], in0=sm[:, 2, :], scalar1=1.0 / 128, scalar2=None, op0=ALU.mult),
                   reads=[R_sm], writes=[R_sm])
                mk("dve", lambda e: e.tensor_tensor(out=sm[:, 5, :], in0=sm[:, 4, :], in1=sm[:, 4, :], op=ALU.mult),
                   reads=[R_sm], writes=[R_sm])
                mk("dve", lambda e: e.scalar_tensor_tensor(out=sm[:, 6, :], in0=sm[:, 3, :], scalar=1.0 / 128, in1=sm[:, 5, :], op0=ALU.mult, op1=ALU.subtract),
                   reads=[R_sm], writes=[R_sm])
                mk("dve", lambda e: e.tensor_scalar(out=sm[:, 8, :], in0=sm[:, 6, :], scalar1=EPS, scalar2=None, op0=ALU.add),
                   reads=[R_sm], writes=[R_sm])
                emit_rsqrt(sm[:, 7, :], sm[:, 8, :], sm[:, 9, :], [R_sm])
                for h in range(4):
                    eng = "dve" if h % 2 == 0 else "pool"
                    mk(eng, lambda e, h=h: e.tensor_scalar(out=hmn[:, h, :], in0=hm[:, h, :], scalar1=sm[:, 4, h:h + 1], scalar2=sm[:, 7, h:h + 1],
                                                           op0=ALU.subtract, op1=ALU.mult),
                       reads=[R_hm, R_sm], writes=[R_hmn])
                bk, rb_ = getbank()
                for h in range(4):
                    mk("pe", lambda e, h=h: e.transpose(out=b16(bk)[:, h * 128:(h + 1) * 128], in_=hmn[:, h, :], identity=identb[:, :]),
                       reads=[R_hmn] + RC, writes=[rb_])
                mk("act", lambda e: e.activation(out=yT[:, 4:8, cs], in_=b16(bk)[:, 0:512].rearrange("p (h t) -> p h t", h=4), func=AF.Identity),
                   reads=[rb_], writes=[R_yT])

            def back1(T):
                s = T % 2
                X = xt[s]
                for q in range(NQ):
                    cs = slice(q * 128, (q + 1) * 128)
                    c = T * NQ + q
                    ba, rba = getbank()
                    bb, rbb = getbank()
                    for k in range(8):
                        mk("pe", lambda e, k=k, ba=ba, cs=cs: e.matmul(ba[:, 0:512], lhsT=yT[:, k, cs], rhs=woutb[:, k, 0:512], start=(k == 0), stop=(k == 7)),
                           reads=[R_yT] + RC, writes=[rba])
                        mk("pe", lambda e, k=k, bb=bb, cs=cs: e.matmul(bb[:, 0:512], lhsT=yT[:, k, cs], rhs=woutb[:, k, 512:1024], start=(k == 0), stop=(k == 7)),
                           reads=[R_yT] + RC, writes=[rbb])
                    mk("dve", lambda e, q=q, ba=ba: e.tensor_tensor(out=X[:, q, 0:512], in0=ba[:, 0:512], in1=X[:, q, 0:512], op=ALU.add),
                       reads=[rba, R_xt[s]], writes=[R_xt[s]])
                    mk("dve", lambda e, q=q, bb=bb: e.tensor_tensor(out=X[:, q, 512:1024], in0=bb[:, 0:512], in1=X[:, q, 512:1024], op=ALU.add),
                       reads=[rbb, R_xt[s]], writes=[R_xt[s]])
                    mk("act", lambda e, q=q: e.activation(out=h2[:, q, :], in_=X[:, q, :], func=AF.Square, accum_out=ss[:, 4, q:q + 1]),
                       reads=[R_xt[s]], writes=[R_h2, R_ssb])
                mk("sp", lambda e: e.dma_start(out=x1_d[T * TT:(T + 1) * TT, :].rearrange("(q p) d -> p q d", p=128), in_=X[:, :, :]),
                   reads=[R_xt[s]], accum=[R_x1], dma=x1sem[s])
                if debug:
                    mk("sp", lambda e: e.dma_start(out=dbg["x1"][T * TT:(T + 1) * TT, :].rearrange("(q p) d -> p q d", p=128), in_=X[:, :, :]),
                       reads=[R_xt[s]], accum=[R_dbg], dma=dbsem[s])

            def back2(T):
                s = T % 2
                X = xt[s]
                mk("dve", lambda e: e.tensor_scalar(out=ss[:, 5, :], in0=ss[:, 4, :], scalar1=1.0 / D, scalar2=EPS, op0=ALU.mult, op1=ALU.add),
                   reads=[R_ssb], writes=[R_ssb])
                emit_rsqrt(ss[:, 6, :], ss[:, 5, :], ss[:, 7, :], [R_ssb])
                for q in range(NQ):
                    mk("dve", lambda e, q=q: e.scalar_tensor_tensor(out=h2[:, q, :], in0=X[:, q, :], scalar=ss[:, 6, q:q + 1], in1=g2bc[:, :], op0=ALU.mult, op1=ALU.mult),
                       reads=[R_xt[s], R_ssb] + RC, writes=[R_h2])
                br, rbr = getbank()
                for q in range(NQ):
                    cs = slice(q * 128, (q + 1) * 128)
                    bk, rb_ = getbank()
                    for k in range(8):
                        mk("pe", lambda e, q=q, k=k, bk=bk: e.transpose(out=b16(bk)[:, k * 128:(k + 1) * 128], in_=h2[:, q, k * 128:(k + 1) * 128], identity=identb[:, :]),
                           reads=[R_h2] + RC, writes=[rb_])
                    mk("act", lambda e, q=q, bk=bk, cs=cs: e.activation(out=h2T[:, :, cs], in_=b16(bk)[:, 0:1024].rearrange("p (k t) -> p k t", k=8), func=AF.Identity),
                       reads=[rb_], writes=[R_h2T])
                    for k in range(8):
                        mk("pe", lambda e, q=q, k=k, cs=cs: e.matmul(br[:, q * 36:(q + 1) * 36], lhsT=h2T[:, k, cs], rhs=rwb[:, k, :], start=(k == 0), stop=(k == 7)),
                           reads=[R_h2T] + RC, writes=[rbr])
                for q in range(NQ):
                    c = T * NQ + q
                    lg = rt[:, 0, :]
                    mk("dve", lambda e, q=q, lg=lg: e.tensor_tensor(out=lg, in0=br[:, q * 36:(q + 1) * 36], in1=rbbc[:, :], op=ALU.add),
                       reads=[rbr] + RC, writes=[R_rt])
                    r1 = [R_rt]
                    mk("dve", lambda e: e.tensor_reduce(out=rt[:, 1, 0:1], in_=rt[:, 0, 0:4], axis=AX.X, op=ALU.max), reads=r1, writes=r1)
                    mk("dve", lambda e: e.tensor_scalar(out=rt[:, 2, 0:4], in0=rt[:, 0, 0:4], scalar1=rt[:, 1, 0:1], scalar2=None, op0=ALU.is_equal), reads=r1, writes=r1)
                    mk("dve", lambda e: e.tensor_scalar(out=rt[:, 1, 1:2], in0=rt[:, 1, 0:1], scalar1=-1.0, scalar2=None, op0=ALU.mult), reads=r1, writes=r1)
                    mk("act", lambda e: e.activation(out=rt[:, 3, 0:4], in_=rt[:, 0, 0:4], func=AF.Exp, bias=rt[:, 1, 1:2], accum_out=rt[:, 1, 2:3]), reads=r1, writes=r1)
                    mk("dve", lambda e: e.reciprocal(out=rt[:, 1, 3:4], in_=rt[:, 1, 2:3]), reads=r1, writes=r1)
                    mk("dve", lambda e: e.tensor_scalar(out=rt[:, 4, 0:4], in0=rt[:, 2, 0:4], scalar1=-1.0, scalar2=1e30, op0=ALU.add, op1=ALU.mult), reads=r1, writes=r1)
                    mk("dve", lambda e: e.tensor_tensor(out=rt[:, 5, 0:32].rearrange("p (g k) -> p g k", g=4), in0=rt[:, 0, 4:36].rearrange("p (g k) -> p g k", g=4),
                                                        in1=rt[:, 4, 0:4].unsqueeze(2).broadcast_to([128, 4, 8]), op=ALU.add), reads=r1, writes=r1)
                    mk("dve", lambda e: e.max(out=rt[:, 6, 0:8], in_=rt[:, 5, 0:32]), reads=r1, writes=r1)
                    mk("dve", lambda e: e.tensor_scalar(out=rt[:, 7, 0:32], in0=rt[:, 5, 0:32], scalar1=rt[:, 6, 0:1], scalar2=None, op0=ALU.is_equal), reads=r1, writes=r1)
                    mk("dve", lambda e: e.tensor_scalar(out=rt[:, 8, 0:32], in0=rt[:, 5, 0:32], scalar1=rt[:, 6, 1:2], scalar2=None, op0=ALU.is_equal), reads=r1, writes=r1)
                    mk("dve", lambda e: e.tensor_tensor(out=rt[:, 1, 4:5], in0=rt[:, 6, 1:2], in1=rt[:, 6, 0:1], op=ALU.subtract), reads=r1, writes=r1)
                    mk("act", lambda e: e.activation(out=rt[:, 1, 5:6], in_=rt[:, 1, 4:5], func=AF.Exp), reads=r1, writes=r1)
                    mk("dve", lambda e: e.tensor_scalar(out=rt[:, 1, 6:7], in0=rt[:, 1, 5:6], scalar1=1.0, scalar2=None, op0=ALU.add), reads=r1, writes=r1)
                    mk("dve", lambda e: e.reciprocal(out=rt[:, 1, 7:8], in_=rt[:, 1, 6:7]), reads=r1, writes=r1)
                    mk("dve", lambda e, c=c: e.tensor_tensor(out=gwt[:, 2 * c:2 * c + 1], in0=rt[:, 1, 7:8], in1=rt[:, 1, 3:4], op=ALU.mult), reads=r1, writes=[R_gwt])
                    mk("dve", lambda e, c=c: e.tensor_tensor(out=gwt[:, 2 * c + 1:2 * c + 2], in0=rt[:, 1, 3:4], in1=gwt[:, 2 * c:2 * c + 1], op=ALU.subtract), reads=r1 + [R_gwt], writes=[R_gwt])
                    mk("dve", lambda e: e.tensor_tensor(out=cmbb[:, :], in0=rt[:, 7, 0:32], in1=rt[:, 8, 0:32], op=ALU.add), reads=r1, writes=r1)
                    bk, rb_ = getbank()
                    mk("pe", lambda e, bk=bk: e.matmul(bk[:, 0:32], lhsT=triSb[:, :], rhs=cmbb[:, :], start=True, stop=True), reads=r1 + RC, writes=[rb_])
                    mk("pe", lambda e, bk=bk: e.matmul(bk[:, 32:64], lhsT=onesb[:, :], rhs=cmbb[:, :], start=True, stop=True), reads=r1 + RC, writes=[rb_])
                    mk("dve", lambda e, bk=bk: e.tensor_tensor(out=rt[:, 9, 0:32], in0=bk[:, 0:32], in1=basep[:, :], op=ALU.add), reads=[rb_, R_base] + r1, writes=r1)
                    mk("dve", lambda e: e.scalar_tensor_tensor(out=rt[:, 10, 0:32], in0=rt[:, 7, 0:32], scalar=1.0, in1=rt[:, 9, 0:32], op0=ALU.mult, op1=ALU.mult, accum_out=rt[:, 1, 8:9]), reads=r1, writes=r1)
                    mk("dve", lambda e: e.scalar_tensor_tensor(out=rt[:, 10, 0:32], in0=rt[:, 8, 0:32], scalar=1.0, in1=rt[:, 9, 0:32], op0=ALU.mult, op1=ALU.mult, accum_out=rt[:, 1, 9:10]), reads=r1, writes=r1)
                    mk("dve", lambda e, c=c: e.tensor_scalar(out=didx[:, 2 * c:2 * c + 2], in0=rt[:, 1, 8:10], scalar1=float(NSLOT - 1), scalar2=None, op0=ALU.min), reads=r1, writes=[R_didx])
                    mk("dve", lambda e, bk=bk: e.tensor_tensor(out=basep[:, :], in0=basep[:, :], in1=bk[:, 32:64], op=ALU.add), reads=[rb_], writes=[R_base])
                    for j in range(2):
                        mk("pool", lambda e, q=q, c=c, j=j: e.indirect_dma_start(out=xs_d, out_offset=bass.IndirectOffsetOnAxis(ap=didx[:, 2 * c + j:2 * c + j + 1], axis=0),
                                                                                 in_=h2[:, q, :], in_offset=None),
                           reads=[R_h2, R_didx], accum=[R_xs], dma=scsem)

            def dumps0():
                dump("winb", winb[:, 0, :], RC)
                dump("woutb", woutb[:, 4, :], RC)
                dump("dconv", dconv[:, 0:4, :], RC)
                dump("ss", ss[:, :, :], [R_ss])
                dump("hb", hb[:, :, :], [R_hb])
                dump("hT", hT[:, :, :], [R_hT])

            front1(0)
            if debug:
                dumps0()
            for T in range(NT):
                front2(T)
                if T == 0:
                    dump("ubuf", ubuf[:, :, :], [R_ubuf])
                    dump("qkbuf", qkbuf[:, :, :], [R_qkbuf])
                    dump("vsb", vsb[:, :, :], [R_vsb])
                    dump("osb", osb[:, :, :], [R_osb])
                    dump("gi", gi[:, :], [R_g])
                    dump("gfp", gfp[:, :], [R_g])
                if T >= 1:
                    back2(T - 1)
                if T + 1 < NT:
                    front1(T + 1)
                conv_a(T)
                gbk = gates_a(T)
                conv_ln(T)
                gates_b(T, *gbk)
                if T == 0:
                    dump("gsm", gsm[:, :, :], [R_gsm])
                    dump("ev", ev[:, :], [R_ev])
                    dump("fl", fl[:, :], [R_ev])
                    dump("dec", dec[:, :], [R_ev])
                tile_qkconv(T)
                conv_b(T)
                if T == 0:
                    dump("cv", cv[:, :, :], R_cv)
                    dump("rstdt", rstdt[:, :], [R_stats])
                    dump("mean", mean_sb[:, :], [R_stats])
                    dump("qkT", qkT[:, :, :], [R_qkT])
                    dump("ktm", ktm[:, :, :], [R_ktm])
                for q in range(NQ):
                    chunk_mlstm(T, q)
                    if T == 0 and q == 0:
                        dump("vt", vt[0][:, :, :], [R_vt[0]])
                        dump("stT", stT[0][:, :, :], [R_stT[0]])
                        dump("Chat", Chat[:, :, :], [R_Chat])
                        dump("hm", hm[:, :, :], [R_hm])
                        dump("hmn", hmn[:, :, :], [R_hmn])
                        dump("sm", sm[:, :, :], [R_sm])
                if debug and S <= 1024:
                    for k in range(8):
                        t, rt_ = gettmp()
                        mk("dve", lambda e, k=k, t=t: e.tensor_copy(out=t[:, :], in_=yT[:, k, :]), reads=[R_yT], writes=[rt_])
                        mk("sp", lambda e, k=k, t=t, T=T: e.dma_start(out=dbg["yT"][:, k * S + T * TT:k * S + (T + 1) * TT], in_=t[:, :]), reads=[rt_], accum=[R_dbg], dma=P.dsem())
                back1(T)
            back2(NT - 1)

            allres = [R_xt[0], R_xt[1], R_hb, R_h2, R_hT, R_h2T, R_ubuf, R_qkbuf, R_vsb, R_osb, R_g, R_gsm, R_ev, R_cvb, R_sqb, R_stats,
                      R_qkT, R_ktm, R_vt[0], R_vt[1], R_stT[0], R_stT[1], R_Chat, R_Chb, R_hm, R_hmn, R_sm, R_junk, R_yT, R_ss, R_ssb, R_rt,
                      R_const, R_c2, R_base] + R_cv + R_tmp + bank_res
            R_phase = Res("phase")
            for eng in ("pe", "act", "dve", "pool", "sp"):
                P.wait_all(eng, allres)

        with contextlib.ExitStack() as st3:
            def sb3(name, shape, dtype=F32):
                return st3.enter_context(nc.sbuf_tensor("s_" + name, list(shape), dtype))

            wgb = [sb3("wgb%d" % i, [128, 8, DE], BF16) for i in range(2)]
            wub = [sb3("wub%d" % i, [128, 8, DE], BF16) for i in range(2)]
            wdb = [sb3("wdb%d" % i, [128, 4, D], BF16) for i in range(2)]
            R_w = [Res("w0"), Res("w1")]
            wsem = [P.dsem("w0"), P.dsem("w1")]
            xg = [sb3("xg%d" % i, [128, D], BF16) for i in range(3)]
            R_xg = [Res("xg%d" % i) for i in range(3)]
            xgsem = [P.dsem("xg%d" % i) for i in range(3)]
            xTe = [sb3("xTe%d" % i, [128, 8, CAP], BF16) for i in range(2)]
            R_xTe = [Res("xTe0"), Res("xTe1")]
            actT = [sb3("actT%d" % i, [128, 4, CAP], BF16) for i in range(2)]
            R_actT = [Res("actT0"), Res("actT1")]
            sil = [sb3("sil%d" % i, [128, 512]) for i in range(2)]
            R_sil = [Res("sil0"), Res("sil1")]
            yo = [sb3("yo%d" % i, [128, D]) for i in range(3)]
            R_yo = [Res("yo%d" % i) for i in range(3)]
            yosem = [P.dsem("yo%d" % i) for i in range(3)]
            def load_w(e_):
                s = e_ % 2
                for (dst, src) in ((wgb[s], wg_d), (wub[s], wu_d)):
                    for k in range(8):
                        mk("pool", lambda e, dst=dst, src=src, k=k: e.dma_start(out=dst[:, k, :], in_=src[e_, k * 128:(k + 1) * 128, :]),
                           accum=[R_w[s]], dma=wsem[s])
                for k in range(4):
                    mk("pool", lambda e, k=k: e.dma_start(out=wdb[s][:, k, :], in_=wd_d[e_, k * 128:(k + 1) * 128, :]),
                       accum=[R_w[s]], dma=wsem[s])

            sil_ctr = [0]
            xg_ctr = [0]
            yo_ctr = [0]
            load_w(0)
            segs = []
            o = 0
            while o < CAP:
                n = min(512, CAP - o)
                segs.append((o, n))
                o += n
            def expert(e_):
                s = e_ % 2
                if e_ + 1 < NE:
                    load_w(e_ + 1)
                XT = xTe[s]
                for b in range(NB):
                    gi_ = xg_ctr[0] % 3
                    xg_ctr[0] += 1
                    row0 = e_ * CAP + b * 128
                    mk("sp", lambda e, gi_=gi_, row0=row0: e.dma_start(out=xg[gi_][:, :], in_=xs_d[row0:row0 + 128, :]),
                       reads=[R_xs], writes=[R_xg[gi_]], dma=xgsem[gi_])
                    bk, rb_ = getbank()
                    for k in range(8):
                        mk("pe", lambda e, gi_=gi_, k=k, bk=bk: e.transpose(out=b16(bk)[:, k * 128:(k + 1) * 128], in_=xg[gi_][:, k * 128:(k + 1) * 128], identity=identb[:, :]),
                           reads=[R_xg[gi_], R_const], writes=[rb_])
                    eng = "dve" if b % 2 == 0 else "act"
                    if eng == "dve":
                        mk("dve", lambda e, b=b, bk=bk: e.tensor_copy(out=XT[:, :, b * 128:(b + 1) * 128], in_=b16(bk)[:, 0:1024].rearrange("p (k t) -> p k t", k=8)),
                           reads=[rb_], writes=[R_xTe[s]])
                    else:
                        mk("act", lambda e, b=b, bk=bk: e.activation(out=XT[:, :, b * 128:(b + 1) * 128], in_=b16(bk)[:, 0:1024].rearrange("p (k t) -> p k t", k=8), func=AF.Identity),
                           reads=[rb_], writes=[R_xTe[s]])
                for j in range(4):
                    for (o, n) in segs:
                        bg_, rbg_ = getbank()
                        bu_, rbu_ = getbank()
                        for k in range(8):
                            mk("pe", lambda e, j=j, k=k, o=o, n=n, bg_=bg_: e.matmul(bg_[:, 0:n], lhsT=wgb[s][:, k, j * 128:(j + 1) * 128], rhs=XT[:, k, o:o + n], start=(k == 0), stop=(k == 7)),
                               reads=[R_w[s], R_xTe[s]], writes=[rbg_])
                        for k in range(8):
                            mk("pe", lambda e, j=j, k=k, o=o, n=n, bu_=bu_: e.matmul(bu_[:, 0:n], lhsT=wub[s][:, k, j * 128:(j + 1) * 128], rhs=XT[:, k, o:o + n], start=(k == 0), stop=(k == 7)),
                               reads=[R_w[s], R_xTe[s]], writes=[rbu_])
                        si = sil_ctr[0] % 2
                        sil_ctr[0] += 1
                        mk("act", lambda e, si=si, n=n, bg_=bg_: e.activation(out=sil[si][:, 0:n], in_=bg_[:, 0:n], func=AF.Silu),
                           reads=[rbg_], writes=[R_sil[si]])
                        mk("dve", lambda e, si=si, j=j, o=o, n=n, bu_=bu_: e.tensor_tensor(out=actT[s][:, j, o:o + n], in0=sil[si][:, 0:n], in1=bu_[:, 0:n], op=ALU.mult),
                           reads=[R_sil[si], rbu_], writes=[R_actT[s]])
                for b in range(NB):
                    ba, rba = getbank()
                    bb, rbb = getbank()
                    for j in range(4):
                        mk("pe", lambda e, j=j, b=b, ba=ba: e.matmul(ba[:, 0:512], lhsT=actT[s][:, j, b * 128:(b + 1) * 128], rhs=wdb[s][:, j, 0:512], start=(j == 0), stop=(j == 3)),
                           reads=[R_w[s], R_actT[s]], writes=[rba])
                        mk("pe", lambda e, j=j, b=b, bb=bb: e.matmul(bb[:, 0:512], lhsT=actT[s][:, j, b * 128:(b + 1) * 128], rhs=wdb[s][:, j, 512:1024], start=(j == 0), stop=(j == 3)),
                           reads=[R_w[s], R_actT[s]], writes=[rbb])
                    yi = yo_ctr[0] % 3
                    yo_ctr[0] += 1
                    mk("act", lambda e, yi=yi, ba=ba: e.activation(out=yo[yi][:, 0:512], in_=ba[:, 0:512], func=AF.Identity),
                       reads=[rba], writes=[R_yo[yi]])
                    mk("dve", lambda e, yi=yi, bb=bb: e.tensor_copy(out=yo[yi][:, 512:1024], in_=bb[:, 0:512]),
                       reads=[rbb], writes=[R_yo[yi]])
                    row0 = e_ * CAP + b * 128
                    mk("sp", lambda e, yi=yi, row0=row0: e.dma_start(out=ys_d[row0:row0 + 128, :], in_=yo[yi][:, :]),
                       reads=[R_yo[yi]], accum=[R_ys], dma=yosem[yi])

            for e_ in range(NE):
                expert(e_)

            finC = [R_w[0], R_w[1]] + R_xg + R_xTe + R_actT + R_sil + R_yo + bank_res
            for eng in ("sp", "pe", "act", "dve", "pool"):
                P.wait_all(eng, finC)

        with contextlib.ExitStack() as st4:
            def sb4(name, shape, dtype=F32):
                return st4.enter_context(nc.sbuf_tensor("s_" + name, list(shape), dtype))

            GD = 4
            NSL = 2 * GD
            gfbc = sb4("gfbc", [128, D])
            R_gf = Res("gfbc")
            mk("sp", lambda e: e.dma_start(out=gfbc[:, :], in_=gf_d.partition_broadcast(128)), writes=[R_gf], dma=P.dsem("gf"))
            xa = [sb4("xa%d" % i, [128, D]) for i in range(NSL)]
            ya = [sb4("ya%d" % i, [128, D]) for i in range(NSL)]
            yb = [sb4("yb%d" % i, [128, D]) for i in range(NSL)]
            ob = [sb4("ob%d" % i, [128, D]) for i in range(2)]
            R_xa = [Res("xa%d" % i) for i in range(NSL)]; R_ya = [Res("ya%d" % i) for i in range(NSL)]; R_yb = [Res("yb%d" % i) for i in range(NSL)]
            R_ob = [Res("ob0"), Res("ob1")]
            xasem = [P.dsem() for _ in range(NSL)]; yasem = [P.dsem() for _ in range(NSL)]; ybsem = [P.dsem() for _ in range(NSL)]; obsem = [P.dsem(), P.dsem()]
            fs = sb4("fs", [128, 4, NCH]); R_fs = Res("fs")
            jk = sb4("jk", [128, D], BF16); R_jk = Res("jk")

            def d_load(c):
                s = c % NSL
                mk("sp", lambda e: e.dma_start(out=xa[s][:, :], in_=x1_d[c * 128:(c + 1) * 128, :]), reads=[R_x1], writes=[R_xa[s]], dma=xasem[s])
                mk("pool", lambda e: e.indirect_dma_start(out=ya[s][:, :], out_offset=None, in_=ys_d,
                                                          in_offset=bass.IndirectOffsetOnAxis(ap=didx[:, 2 * c:2 * c + 1], axis=0)),
                   reads=[R_ys, R_didx], writes=[R_ya[s]], dma=yasem[s])
                mk("pool", lambda e: e.indirect_dma_start(out=yb[s][:, :], out_offset=None, in_=ys_d,
                                                          in_offset=bass.IndirectOffsetOnAxis(ap=didx[:, 2 * c + 1:2 * c + 2], axis=0)),
                   reads=[R_ys, R_didx], writes=[R_yb[s]], dma=ybsem[s])

            def d_combine(c):
                s = c % NSL
                mk("dve", lambda e: e.scalar_tensor_tensor(out=xa[s][:, :], in0=ya[s][:, :], scalar=gwt[:, 2 * c:2 * c + 1], in1=xa[s][:, :], op0=ALU.mult, op1=ALU.add),
                   reads=[R_ya[s], R_xa[s], R_gwt], writes=[R_xa[s]])
                mk("dve", lambda e: e.scalar_tensor_tensor(out=xa[s][:, :], in0=yb[s][:, :], scalar=gwt[:, 2 * c + 1:2 * c + 2], in1=xa[s][:, :], op0=ALU.mult, op1=ALU.add),
                   reads=[R_yb[s], R_xa[s], R_gwt], writes=[R_xa[s]])
                mk("act", lambda e: e.activation(out=jk[:, :], in_=xa[s][:, :], func=AF.Square, accum_out=fs[:, 0, c:c + 1]),
                   reads=[R_xa[s]], writes=[R_jk, R_fs])

            def d_finish(c):
                s = c % NSL
                o = c % 2
                mk("act", lambda e: e.activation(out=xa[s][:, :], in_=xa[s][:, :], func=AF.Identity, scale=fs[:, 2, c:c + 1]),
                   reads=[R_xa[s], R_fs], writes=[R_xa[s]])
                mk("pool", lambda e: e.tensor_tensor(out=ob[o][:, :], in0=xa[s][:, :], in1=gfbc[:, :], op=ALU.mult),
                   reads=[R_xa[s], R_gf], writes=[R_ob[o]])
                mk("sp", lambda e: e.dma_start(out=out_d[c * 128:(c + 1) * 128, :], in_=ob[o][:, :]),
                   reads=[R_ob[o]], accum=[R_out], dma=obsem[o])

            NGD = NCH // GD
            for c in range(min(GD, NCH)):
                d_load(c)
            for g in range(NGD):
                if g + 1 < NGD:
                    for c in range((g + 1) * GD, (g + 2) * GD):
                        d_load(c)
                cs_ = slice(g * GD, (g + 1) * GD)
                for c in range(g * GD, (g + 1) * GD):
                    d_combine(c)
                mk("dve", lambda e, cs_=cs_: e.tensor_scalar(out=fs[:, 1, cs_], in0=fs[:, 0, cs_], scalar1=1.0 / D, scalar2=EPS, op0=ALU.mult, op1=ALU.add),
                   reads=[R_fs], writes=[R_fs])
                emit_rsqrt(fs[:, 2, cs_], fs[:, 1, cs_], fs[:, 3, cs_], [R_fs])
                for c in range(g * GD, (g + 1) * GD):
                    d_finish(c)
            if debug:
                mk("sp", lambda e: e.dma_start(out=dbg["idx"], in_=didx[:, :]), reads=[R_didx], accum=[R_dbg], dma=obsem[0])
                mk("sp", lambda e: e.dma_start(out=dbg["gw"], in_=gwt[:, :]), reads=[R_gwt], accum=[R_dbg], dma=obsem[1])
            fin = [R_out, R_dbg, R_x1, R_xs, R_ys, R_didx, R_gwt, R_fs, R_jk, R_gf] + R_xa + R_ya + R_yb + R_ob + bank_res
            for eng in ("sp", "pe", "act", "dve", "pool"):
                P.wait_all(eng, fin)

        P.emit()
    return nc


def make_shared(inp, CAP):
    f = np.float32

    def a(v):
        return np.ascontiguousarray(v, dtype=f)

    def pk(vec, n):
        return a(np.asarray(vec).reshape(n, 128).T)

    b_in = np.asarray(inp["b_in"][0])
    sh = {
        "w_in": a(inp["w_in"][0]),
        "w_out": a(inp["w_out"][0]),
        "g1": pk(inp["norm_mix_g"][0], 8),
        "bfm": pk(b_in[0:2048], 16),
        "btm": a(b_in[2048:3080].reshape(1, 1032)),
        "cw": a(np.asarray(inp["conv_w"][0]).reshape(31, 4, 128).transpose(2, 1, 0).reshape(128, 124)),
        "cb": pk(inp["conv_b"][0], 4),
        "lng": pk(inp["conv_ln_g"][0], 4),
        "lnb": pk(inp["conv_ln_b"][0], 4),
        "qkw": a(np.asarray(inp["qk_conv_w"][0]).reshape(4, 8, 128).transpose(2, 1, 0).reshape(128, 32)),
        "qkb": pk(inp["qk_conv_b"][0], 8),
        "mg": pk(inp["mlstm_norm_g"][0], 4),
        "g2": a(np.asarray(inp["norm_ffn_g"][0]).reshape(1, D)),
        "gf": a(np.asarray(inp["final_norm_g"]).reshape(1, D)),
        "rw": a(np.concatenate([inp["router_group_w"][0], inp["router_expert_w"][0]], axis=1)),
        "rb": a(np.concatenate([inp["router_group_b"][0], inp["router_expert_b"][0]]).reshape(1, 36)),
        "ecap": a((np.arange(32) * CAP).reshape(1, 32)),
        "wg": a(inp["expert_w_gate"][0]),
        "wu": a(inp["expert_w_up"][0]),
        "wd": a(inp["expert_w_down"][0]),
        "ident": a(np.eye(128)),
        "tri_incl": a(np.triu(np.ones((128, 128)))),
        "tri_strict": a(np.triu(np.ones((128, 128)), 1)),
    }
    return sh


_CACHE = {}


def kernel(**inputs):
    x = np.asarray(inputs["x"], dtype=np.float32)
    B, S, _ = x.shape
    CAP = 1024
    key = (S, CAP)
    if key not in _CACHE:
        _CACHE[key] = build_program(S=S, CAP=CAP, NQ=2)
    nc = _CACHE[key]
    sh = make_shared(inputs, CAP)
    in_maps = []
    for b in range(B):
        m = dict(sh)
        m["x"] = np.ascontiguousarray(x[b])
        in_maps.append(m)
    res = run_bass_kernel_spmd(nc, in_maps, core_ids=list(range(B)))
    return np.stack([np.asarray(r["out"]) for r in res.results], axis=0).astype(np.float32)
```

```python
import contextlib
import numpy as np
import concourse.bass as bass
import concourse.mybir as mybir
from concourse.bass_utils import run_bass_kernel_spmd
from concourse.alu_op_type import AluOpType as ALU

F32 = mybir.dt.float32
BF16 = mybir.dt.bfloat16
I32 = mybir.dt.int32
AF = mybir.ActivationFunctionType
AX = mybir.AxisListType

D = 1024
DIN = 3080
NE = 32
DE = 512
EPS = 1e-6
KAPPA = 0.25 * (128.0 ** -0.5)


class Res:
    __slots__ = ("name", "w", "rd", "acc")

    def __init__(self, name):
        self.name = name
        self.w = None
        self.rd = []
        self.acc = []


class DmaSem:
    def __init__(self, sem):
        self.sem = sem
        self.count = 0


class Prog:
    ENGS = ("pe", "act", "dve", "pool", "sp")

    def __init__(self, nc, stack):
        self.nc = nc
        self.stack = stack
        self.items = {e: [] for e in self.ENGS}
        self.count = {e: 0 for e in self.ENGS}
        self.sem = {e: stack.enter_context(nc.semaphore("c_" + e)) for e in self.ENGS}
        self.known = {e: {} for e in self.ENGS}
        self.ndma = 0
        self.sync_same_engine = True
        self._frames = []

    def dsem(self, name=None):
        self.ndma += 1
        return DmaSem(self.stack.enter_context(self.nc.semaphore(name or ("d%d" % self.ndma))))

    def _deps(self, eng, reads, writes, accum):
        toks = []
        for r in reads:
            if r.w is not None:
                toks.append(r.w)
            toks.extend(r.acc)
        for w in writes:
            if w.w is not None:
                toks.append(w.w)
            toks.extend(w.rd)
            toks.extend(w.acc)
        for a in accum:
            if a.w is not None:
                toks.append(a.w)
            toks.extend(a.rd)
        waits = []
        kn = self.known[eng]
        own = self.sem[eng]
        best = {}
        for (s, v) in toks:
            if s is own and (eng == "pe" or not self.sync_same_engine):
                continue
            if kn.get(id(s), 0) >= v:
                continue
            if id(s) not in best or best[id(s)][1] < v:
                best[id(s)] = (s, v)
        for (s, v) in best.values():
            kn[id(s)] = v
            waits.append((s, v))
        return waits

    def op(self, eng, fn, reads=(), writes=(), accum=(), dma=None):
        waits = self._deps(eng, reads, writes, accum)
        if dma is None:
            self.count[eng] += 1
            tok = (self.sem[eng], self.count[eng])
            inc = 1
        else:
            for fr in self._frames:
                d = fr["reg"][eng]["dma"].setdefault(id(dma), [dma.sem, dma.count, 0])
                d[2] += 16
            dma.count += 16
            tok = (dma.sem, dma.count)
            inc = 16
        self.items[eng].append((waits, fn, tok[0], inc))
        for r in reads:
            r.rd.append(tok)
        for w in writes:
            w.w = tok
            w.rd = []
            w.acc = []
        for a in accum:
            a.acc.append(tok)
        return tok

    def cond_begin(self, cnt_ap, thresh):
        frame = {"snap": {e: dict(k) for e, k in self.known.items()},
                 "reg": {e: {"start": len(self.items[e]), "cnt0": self.count[e], "dma": {}} for e in self.ENGS},
                 "cond": (cnt_ap, thresh)}
        self._frames.append(frame)

    def cond_end(self):
        frame = self._frames.pop()
        cnt_ap, thresh = frame["cond"]
        for e in self.ENGS:
            r = frame["reg"][e]
            body = self.items[e][r["start"]:]
            n_ops = self.count[e] - r["cnt0"]
            del self.items[e][r["start"]:]
            if body:
                self.items[e].append(("cond", cnt_ap, thresh, body, r["cnt0"], n_ops, [tuple(v) for v in r["dma"].values()]))
        self.known = frame["snap"]

    def wait_all(self, eng, ress):
        waits = self._deps(eng, ress, ress, ())
        self.items[eng].append((waits, None, None, 0))

    def emit(self):
        nc = self.nc
        with nc.Block() as block:
            def run_items(name, handle, items, reg):
                own = self.sem[name]
                for it in items:
                    if it[0] == "cond":
                        _, cnt_ap, thresh, body, cnt0, n_ops, dmas = it
                        handle.reg_load(reg, cnt_ap)
                        with handle.If_lt(reg, thresh + 1):
                            if n_ops:
                                handle.wait_ge(own, cnt0)
                                left = n_ops
                                while left > 0:
                                    k = min(left, 15)
                                    handle.sem_inc(own, k)
                                    left -= k
                            for (dsem_, before, inc) in dmas:
                                handle.wait_ge(dsem_, before)
                                left = inc
                                while left > 0:
                                    k = min(left, 15)
                                    handle.sem_inc(dsem_, k)
                                    left -= k
                        with handle.Else():
                            run_items(name, handle, body, reg)
                        continue
                    (waits, fn, sem, inc) = it
                    for (s_, v) in waits:
                        handle.wait_ge(s_, v)
                    if fn is not None:
                        fn(handle).then_inc(sem, inc)

            def run(name, handle):
                with handle.register("rc_" + name) as reg:
                    run_items(name, handle, self.items[name], reg)

            @block.tensor
            def _(e):
                run("pe", e)

            @block.scalar
            def _(e):
                run("act", e)

            @block.vector
            def _(e):
                run("dve", e)

            @block.gpsimd
            def _(e):
                run("pool", e)

            @block.sync
            def _(e):
                run("sp", e)


def build_program(S=8192, CAP=640, NQ=2, debug=False):
    TT = NQ * 128
    NT = S // TT
    NCH = S // 128
    NB = CAP // 128
    NSLOT = NE * CAP
    NG = NQ * 4
    assert S % TT == 0 and CAP % 256 == 0

    nc = bass.Bass("TRN2", target_bir_lowering=False)
    dt = nc.dram_tensor

    def din(name, shape, dtype=F32):
        return dt(name, list(shape), dtype, kind="ExternalInput").ap()

    x_d = din("x", [S, D])
    win_d = din("w_in", [D, DIN])
    wout_d = din("w_out", [D, D])
    g1_d = din("g1", [128, 8])
    bfm_d = din("bfm", [128, 16])
    btm_d = din("btm", [1, 1032])
    cw_d = din("cw", [128, 4 * 31])
    cb_d = din("cb", [128, 4])
    lng_d = din("lng", [128, 4])
    lnb_d = din("lnb", [128, 4])
    qkw_d = din("qkw", [128, 8 * 4])
    qkb_d = din("qkb", [128, 8])
    mg_d = din("mg", [128, 4])
    g2_d = din("g2", [1, D])
    gf_d = din("gf", [1, D])
    rw_d = din("rw", [D, 36])
    rb_d = din("rb", [1, 36])
    ecap_d = din("ecap", [1, 32])
    wg_d = din("wg", [NE, D, DE])
    wu_d = din("wu", [NE, D, DE])
    wd_d = din("wd", [NE, DE, D])
    ident_d = din("ident", [128, 128])
    triI_d = din("tri_incl", [128, 128])
    triS_d = din("tri_strict", [128, 128])
    out_d = dt("out", [S, D], F32, kind="ExternalOutput").ap()
    x1_d = dt("x1s", [S, D], F32, kind="Internal").ap()
    xs_d = dt("xs", [NSLOT, D], BF16, kind="ExternalOutput" if debug else "Internal").ap()
    ys_d = dt("ys", [NSLOT, D], F32, kind="ExternalOutput" if debug else "Internal").ap()
    dbg = {}
    if debug:
        dbg["x1"] = dt("dbg_x1", [S, D], F32, kind="ExternalOutput").ap()
        dbg["idx"] = dt("dbg_idx", [128, NCH * 2], I32, kind="ExternalOutput").ap()
        dbg["gw"] = dt("dbg_gw", [128, NCH * 2], F32, kind="ExternalOutput").ap()
        dbg["yT"] = dt("dbg_yT", [128, 8 * S], F32, kind="ExternalOutput").ap()

    with contextlib.ExitStack() as st:
        P = Prog(nc, st)

        def sb(name, shape, dtype=F32):
            return st.enter_context(nc.sbuf_tensor("s_" + name, list(shape), dtype))

        def mk(eng, fn, reads=(), writes=(), accum=(), dma=None):
            return P.op(eng, fn, reads, writes, accum, dma)

        dumps = {}

        def dump(name, ap, reads):
            if not debug or name in dumps:
                return
            dumps[name] = dt("dmp_" + name, list(ap.shape), ap.dtype, kind="ExternalOutput").ap()
            mk("sp", lambda e: e.dma_start(out=dumps[name], in_=ap), reads=reads, accum=[R_dbg], dma=P.dsem())

        def emit_rsqrt(y, v, t, R):
            mk("dve", lambda e: e.tensor_scalar(out=y.bitcast(I32), in0=v.bitcast(I32), scalar1=-0.5, scalar2=1597463007.0, op0=ALU.mult, op1=ALU.add),
               reads=R, writes=R)
            for _ in range(2):
                mk("dve", lambda e: e.scalar_tensor_tensor(out=t, in0=y, scalar=-0.5, in1=y, op0=ALU.mult, op1=ALU.mult), reads=R, writes=R)
                mk("dve", lambda e: e.tensor_tensor(out=t, in0=t, in1=v, op=ALU.mult), reads=R, writes=R)
                mk("dve", lambda e: e.scalar_tensor_tensor(out=y, in0=t, scalar=1.5, in1=y, op0=ALU.add, op1=ALU.mult), reads=R, writes=R)

        banks = [st.enter_context(nc.psum_tensor("bank%d" % i, [128, 512], F32)) for i in range(8)]
        bank_res = [Res("bank%d" % i) for i in range(8)]
        bank_ctr = [0]

        def getbank():
            i = bank_ctr[0] % 8
            bank_ctr[0] += 1
            return banks[i], bank_res[i]

        def b16(bank):
            return bank[:, :].bitcast(BF16)

        didx = sb("didx", [128, NCH * 2], I32)
        gwt = sb("gwt", [128, NCH * 2], F32)
        R_didx = Res("didx")
        R_gwt = Res("gwt")
        R_xs = Res("xs")
        R_ys = Res("ys")
        R_x1 = Res("x1d")
        R_out = Res("out")
        R_dbg = Res("dbg")
        identb = sb("identb", [128, 128], BF16)
        cnti = sb("cnti", [128, 32], I32)
        ecapsb = sb("ecapsb", [128, 32], F32)
        R_cnt = Res("cnti")
        R_const = Res("const")

        ld = P.dsem("ld_const")

        with contextlib.ExitStack() as st2:
            def sb2(name, shape, dtype=F32):
                return st2.enter_context(nc.sbuf_tensor("s_" + name, list(shape), dtype))

            winb = sb2("winb", [128, 8, DIN], BF16)
            woutb = sb2("woutb", [128, 8, D], BF16)
            dconv = sb2("dconv", [128, 4 * 31, 128], BF16)
            dqk = sb2("dqk", [128, 8 * 4, 128], BF16)
            identf = sb2("identf", [128, 128], F32)
            triI = sb2("triI", [128, 128], F32)
            onesf = sb2("onesf", [128, 128], F32)
            maskk = sb2("maskk", [128, 128], F32)
            triSb = sb2("triSb", [128, 128], BF16)
            onesb = sb2("onesb", [128, 128], BF16)
            o512b = sb2("o512b", [128, 128], BF16)
            g1 = sb2("g1", [128, 8])
            bfm = sb2("bfm", [128, 16])
            hbfm = sb2("hbfm", [128, 16])
            btm = sb2("btm", [128, 1032])
            cw = sb2("cw", [128, 124])
            cb = sb2("cb", [128, 4])
            lng = sb2("lng", [128, 4])
            lnb = sb2("lnb", [128, 4])
            hlng = sb2("hlng", [128, 4])
            hlnb = sb2("hlnb", [128, 4])
            qkw = sb2("qkw", [128, 32])
            qkb = sb2("qkb", [128, 8])
            hqkb = sb2("hqkb", [128, 8])
            mg = sb2("mg", [128, 4])
            g2bc = sb2("g2bc", [128, D])
            rwf = sb2("rwf", [128, 8, 36])
            rwb = sb2("rwb", [128, 8, 36], BF16)
            rbbc = sb2("rbbc", [128, 36])
            basep = sb2("basep", [128, 32])
            halfc = sb2("halfc", [128, 1])
            epsc = sb2("epsc", [128, 1])

            xt = [sb2("xt%d" % i, [128, NQ, D]) for i in range(2)]
            R_xt = [Res("xt0"), Res("xt1")]
            xsem = [P.dsem("x0"), P.dsem("x1")]
            hb = sb2("hb", [128, NQ, D], BF16); R_hb = Res("hb")
            h2 = sb2("h2", [128, NQ, D], BF16); R_h2 = Res("h2")
            hT = sb2("hT", [128, 8, TT], BF16); R_hT = Res("hT")
            h2T = sb2("h2T", [128, 8, TT], BF16); R_h2T = Res("h2T")
            ubuf = sb2("ubuf", [128, 4, TT + 32], BF16); R_ubuf = Res("ubuf")
            qkbuf = sb2("qkbuf", [128, 8, TT + 4], BF16); R_qkbuf = Res("qkbuf")
            NTMP = 6
            tmp = [sb2("tmp%d" % i, [128, TT]) for i in range(NTMP)]
            R_tmp = [Res("tmp%d" % i) for i in range(NTMP)]
            tmp_ctr = [0]

            def gettmp():
                i = tmp_ctr[0] % NTMP
                tmp_ctr[0] += 1
                return tmp[i], R_tmp[i]

            vsb = sb2("vsb", [128, NQ, 512]); R_vsb = Res("vsb")
            osb = sb2("osb", [128, NQ, 512]); R_osb = Res("osb")
            gi = sb2("gi", [128, NG]); gfp = sb2("gfp", [128, NG]); R_g = Res("gates")
            gsm = sb2("gsm", [128, 8, NG]); R_gsm = Res("gsm")
            ev = sb2("ev", [128, NG]); fl = sb2("fl", [128, NG]); dec = sb2("dec", [128, NG])
            R_ev = Res("ev")
            cv = sb2("cv", [128, 4, TT]); R_cv = [Res("cv%d" % i) for i in range(4)]
            cvb = sb2("cvb", [128, 4, TT], BF16); R_cvb = Res("cvb")
            sqb = sb2("sqb", [128, 4, TT], BF16); R_sqb = Res("sqb")
            mean_sb = sb2("mean_sb", [128, TT]); rstdt = sb2("rstdt", [128, TT]); msq = sb2("msq", [128, TT])
            R_stats = Res("stats")
            qkT = sb2("qkT", [128, 8, TT], BF16); R_qkT = Res("qkT")
            ktm = sb2("ktm", [128, NQ, 512], BF16); R_ktm = Res("ktm")
            vt = [sb2("vt%d" % i, [128, 4, 129], BF16) for i in range(2)]; R_vt = [Res("vt0"), Res("vt1")]
            stT = [sb2("stT%d" % i, [128, 4, 128], BF16) for i in range(2)]; R_stT = [Res("stT0"), Res("stT1")]
            Chat = sb2("Chat", [128, 4, 129]); Chb = sb2("Chb", [128, 4, 129], BF16)
            R_Chat = Res("Chat"); R_Chb = Res("Chb")
            hm = sb2("hm", [128, 4, 128]); R_hm = Res("hm")
            hmn = sb2("hmn", [128, 4, 128], BF16); R_hmn = Res("hmn")
            sm = sb2("sm", [128, 12, 4]); R_sm = Res("sm")
            junk = sb2("junk", [128, 128]); R_junk = Res("junk")
            yT = sb2("yT", [128, 8, TT], BF16); R_yT = Res("yT")
            ss = sb2("ss", [128, 8, NQ]); R_ss = Res("ssf"); R_ssb = Res("ssb")
            rt = sb2("rt", [128, 16, 36]); R_rt = Res("rt")
            cmbb = sb2("cmbb", [128, 32], BF16)
            x1sem = [P.dsem("x1st0"), P.dsem("x1st1")]
            dbsem = [P.dsem("db0"), P.dsem("db1")]
            scsem = P.dsem("scat")

            def cld(eng, dst, src):
                mk(eng, lambda e: e.dma_start(out=dst, in_=src), accum=[R_const], dma=ld)

            cld("sp", identf[:, :], ident_d)
            cld("sp", triI[:, :], triI_d)
            cld("sp", g1[:, :], g1_d)
            cld("sp", bfm[:, :], bfm_d)
            cld("sp", btm[:, :], btm_d.partition_broadcast(128))
            cld("sp", cw[:, :], cw_d)
            cld("sp", cb[:, :], cb_d)
            cld("sp", lng[:, :], lng_d)
            cld("sp", lnb[:, :], lnb_d)
            cld("sp", qkw[:, :], qkw_d)
            cld("sp", qkb[:, :], qkb_d)
            cld("sp", mg[:, :], mg_d)
            cld("sp", g2bc[:, :], g2_d.partition_broadcast(128))
            cld("sp", rbbc[:, :], rb_d.partition_broadcast(128))
            R_base = Res("basep")
            mk("sp", lambda e: e.dma_start(out=basep[:, :], in_=ecap_d.partition_broadcast(128)), writes=[R_base], dma=P.dsem("base"))
            cld("sp", rwf[:, :, :], rw_d.rearrange("(k p) n -> p k n", p=128))
            cld("sp", ecapsb[:, :], ecap_d.partition_broadcast(128))
            cld("pool", triSb[:, :], triS_d)
            cld("pool", identb[:, :], ident_d)
            R_c2 = Res("const2")

            def cop(eng, fn):
                mk(eng, fn, reads=[R_const], accum=[R_c2])

            cop("dve", lambda e: e.memset(onesf[:, :], 1.0))
            cop("dve", lambda e: e.memset(onesb[:, :], 1.0))
            cop("dve", lambda e: e.memset(o512b[:, :], 1.0 / 512.0))
            cop("dve", lambda e: e.memset(halfc[:, :], 0.5))
            cop("dve", lambda e: e.memset(epsc[:, :], EPS))
            cop("dve", lambda e: e.tensor_scalar(out=maskk[:, :], in0=triI[:, :], scalar1=KAPPA, scalar2=None, op0=ALU.mult))
            cop("dve", lambda e: e.tensor_scalar(out=hbfm[:, :], in0=bfm[:, :], scalar1=0.5, scalar2=None, op0=ALU.mult))
            cop("dve", lambda e: e.tensor_scalar(out=hlng[:, :], in0=lng[:, :], scalar1=0.5, scalar2=None, op0=ALU.mult))
            cop("dve", lambda e: e.tensor_scalar(out=hlnb[:, :], in0=lnb[:, :], scalar1=0.5, scalar2=None, op0=ALU.mult))
            cop("dve", lambda e: e.tensor_scalar(out=hqkb[:, :], in0=qkb[:, :], scalar1=0.5, scalar2=None, op0=ALU.mult))
            cop("dve", lambda e: e.tensor_copy(out=rwb[:, :, :], in_=rwf[:, :, :]))
            mk("dve", lambda e: e.memset(Chat[:, :, :], 0.0), writes=[R_Chat])
            mk("dve", lambda e: e.memset(Chb[:, :, :], 0.0), writes=[R_Chb])
            mk("dve", lambda e: e.memset(ubuf[:, :, :], 0.0), writes=[R_ubuf])
            mk("dve", lambda e: e.memset(qkbuf[:, :, :], 0.0), writes=[R_qkbuf])
            for jc in range(4):
                cop("dve", lambda e, jc=jc: e.scalar_tensor_tensor(out=dconv[:, jc * 31:(jc + 1) * 31, :], in0=identf[:, :].unsqueeze(1).broadcast_to([128, 31, 128]), scalar=0.5,
                                                                   in1=cw[:, jc * 31:(jc + 1) * 31].unsqueeze(2).broadcast_to([128, 31, 128]), op0=ALU.mult, op1=ALU.mult))
            cop("pool", lambda e: e.tensor_tensor(out=dqk[:, :, :], in0=identf[:, :].unsqueeze(1).broadcast_to([128, 32, 128]),
                                                  in1=qkw[:, :].unsqueeze(2).broadcast_to([128, 32, 128]), op=ALU.mult))
            R_stage = R_xt
            HW = DIN // 2
            for k in range(8):
                for hf in range(2):
                    s = (2 * k + hf) % 2
                    flat = xt[s][:, :, :].rearrange("p q d -> p (q d)")
                    mk("sp", lambda e, k=k, hf=hf, flat=flat: e.dma_start(out=flat[:, 0:HW], in_=win_d[k * 128:(k + 1) * 128, hf * HW:(hf + 1) * HW]),
                       writes=[R_stage[s]], dma=xsem[s])
                    if s == 0:
                        mk("dve", lambda e, k=k, hf=hf, flat=flat: e.tensor_scalar(out=winb[:, k, hf * HW:(hf + 1) * HW], in0=flat[:, 0:HW],
                                                                                  scalar1=g1[:, k:k + 1], scalar2=None, op0=ALU.mult),
                           reads=[R_stage[s], R_const], accum=[R_c2])
                    else:
                        mk("act", lambda e, k=k, hf=hf, flat=flat: e.activation(out=winb[:, k, hf * HW:(hf + 1) * HW], in_=flat[:, 0:HW],
                                                                               func=AF.Identity, scale=g1[:, k:k + 1]),
                           reads=[R_stage[s], R_const], accum=[R_c2])
            for k in range(8):
                s = k % 2
                mk("sp", lambda e, k=k, s=s: e.dma_start(out=xt[s][:, 0, :], in_=wout_d[k * 128:(k + 1) * 128, :]),
                   writes=[R_stage[s]], dma=xsem[s])
                sc = 0.5 if k < 4 else mg[:, k - 4:k - 3]
                if k % 2 == 0:
                    mk("dve", lambda e, k=k, s=s, sc=sc: e.tensor_scalar(out=woutb[:, k, :], in0=xt[s][:, 0, :], scalar1=sc,
                                                                         scalar2=None, op0=ALU.mult),
                       reads=[R_stage[s], R_const], accum=[R_c2])
                else:
                    mk("act", lambda e, k=k, s=s, sc=sc: e.activation(out=woutb[:, k, :], in_=xt[s][:, 0, :], func=AF.Identity, scale=sc),
                       reads=[R_stage[s], R_const], accum=[R_c2])
            RC = [R_const, R_c2]

            def front1(T):
                s = T % 2
                X = xt[s]
                mk("sp", lambda e: e.dma_start(out=X[:, :, :], in_=x_d[T * TT:(T + 1) * TT, :].rearrange("(q p) d -> p q d", p=128)),
                   writes=[R_xt[s]], dma=xsem[s])
                for q in range(NQ):
                    mk("act", lambda e, q=q: e.activation(out=hb[:, q, :], in_=X[:, q, :], func=AF.Square, accum_out=ss[:, 0, q:q + 1]),
                       reads=[R_xt[s]], writes=[R_hb, R_ss])
                mk("dve", lambda e: e.tensor_scalar(out=ss[:, 1, :], in0=ss[:, 0, :], scalar1=1.0 / D, scalar2=EPS, op0=ALU.mult, op1=ALU.add),
                   reads=[R_ss], writes=[R_ss])
                emit_rsqrt(ss[:, 2, :], ss[:, 1, :], ss[:, 3, :], [R_ss])
                for q in range(NQ):
                    mk("act", lambda e, q=q: e.activation(out=hb[:, q, :], in_=X[:, q, :], func=AF.Identity, scale=ss[:, 2, q:q + 1]),
                       reads=[R_xt[s], R_ss], writes=[R_hb])
                for q in range(NQ):
                    bk, rb_ = getbank()
                    for k in range(8):
                        mk("pe", lambda e, q=q, k=k, bk=bk: e.transpose(out=b16(bk)[:, k * 128:(k + 1) * 128], in_=hb[:, q, k * 128:(k + 1) * 128], identity=identb[:, :]),
                           reads=[R_hb] + RC, writes=[rb_])
                    mk("dve", lambda e, q=q, bk=bk: e.tensor_copy(out=hT[:, :, q * 128:(q + 1) * 128], in_=b16(bk)[:, 0:1024].rearrange("p (k t) -> p k t", k=8)),
                       reads=[rb_], writes=[R_hT])

            def front2(T):
                order = [4, 0, 5, 1, 6, 2, 7, 3] + list(range(8, 16))
                thg = {}
                for j in order:
                    bk, rb_ = getbank()
                    for k in range(8):
                        mk("pe", lambda e, j=j, k=k, bk=bk: e.matmul(bk[:, 0:TT], lhsT=winb[:, k, j * 128:(j + 1) * 128], rhs=hT[:, k, :],
                                                                     start=(k == 0), stop=(k == 7)),
                           reads=[R_hT] + RC, writes=[rb_])
                    if 4 <= j < 8:
                        t, rt_ = gettmp()
                        thg[j - 4] = (t, rt_)
                        mk("act", lambda e, j=j, bk=bk, t=t: e.activation(out=t[:, :], in_=bk[:, 0:TT], func=AF.Tanh, scale=0.5, bias=hbfm[:, j:j + 1]),
                           reads=[rb_] + RC, writes=[rt_])
                    elif j < 4:
                        t, rt_ = thg[j]
                        a, ra_ = gettmp()
                        mk("act", lambda e, j=j, bk=bk, a=a: e.activation(out=a[:, :], in_=bk[:, 0:TT], func=AF.Identity, bias=bfm[:, j:j + 1]),
                           reads=[rb_] + RC, writes=[ra_])
                        mk("dve", lambda e, j=j, t=t, a=a: e.scalar_tensor_tensor(out=ubuf[:, j, 30:30 + TT], in0=t[:, :], scalar=1.0, in1=a[:, :],
                                                                                  op0=ALU.add, op1=ALU.mult),
                           reads=[rt_, ra_], writes=[R_ubuf])
                    else:
                        jp = j - 8
                        if jp % 2 == 0:
                            mk("act", lambda e, j=j, jp=jp, bk=bk: e.activation(out=qkbuf[:, jp, 3:3 + TT], in_=bk[:, 0:TT], func=AF.Identity, bias=bfm[:, j:j + 1]),
                               reads=[rb_] + RC, writes=[R_qkbuf])
                        else:
                            mk("dve", lambda e, j=j, jp=jp, bk=bk: e.tensor_scalar(out=qkbuf[:, jp, 3:3 + TT], in0=bk[:, 0:TT], scalar1=bfm[:, j:j + 1], scalar2=None, op0=ALU.add),
                               reads=[rb_] + RC, writes=[R_qkbuf])
                bg, rbg = getbank()
                for q in range(NQ):
                    bv, rbv = getbank()
                    bo, rbo = getbank()
                    for k in range(8):
                        lt = hT[:, k, q * 128:(q + 1) * 128]
                        mk("pe", lambda e, k=k, bv=bv, lt=lt: e.matmul(bv[:, 0:512], lhsT=lt, rhs=winb[:, k, 2048:2560], start=(k == 0), stop=(k == 7)),
                           reads=[R_hT] + RC, writes=[rbv])
                        mk("pe", lambda e, k=k, bo=bo, lt=lt: e.matmul(bo[:, 0:512], lhsT=lt, rhs=winb[:, k, 2560:3072], start=(k == 0), stop=(k == 7)),
                           reads=[R_hT] + RC, writes=[rbo])
                    for k in range(8):
                        lt = hT[:, k, q * 128:(q + 1) * 128]
                        mk("pe", lambda e, k=k, q=q, lt=lt: e.matmul(bg[:, q * 8:(q + 1) * 8], lhsT=lt, rhs=winb[:, k, 3072:3080], start=(k == 0), stop=(k == 7)),
                           reads=[R_hT] + RC, writes=[rbg])
                    mk("dve", lambda e, q=q, bv=bv: e.tensor_tensor(out=vsb[:, q, :], in0=bv[:, 0:512], in1=btm[:, 0:512], op=ALU.add),
                       reads=[rbv] + RC, writes=[R_vsb])
                    mk("dve", lambda e, q=q, bo=bo: e.tensor_tensor(out=osb[:, q, :], in0=bo[:, 0:512], in1=btm[:, 512:1024], op=ALU.add),
                       reads=[rbo] + RC, writes=[R_osb])
                    mk("act", lambda e, q=q: e.activation(out=osb[:, q, :], in_=osb[:, q, :], func=AF.Tanh, scale=0.5),
                       reads=[R_osb], writes=[R_osb])
                    mk("pool", lambda e, q=q: e.tensor_scalar(out=osb[:, q, :], in0=osb[:, q, :], scalar1=0.5, scalar2=0.5, op0=ALU.mult, op1=ALU.add),
                       reads=[R_osb], writes=[R_osb])
                for q in range(NQ):
                    mk("dve", lambda e, q=q: e.tensor_tensor(out=gi[:, q * 4:(q + 1) * 4], in0=bg[:, q * 8:q * 8 + 4], in1=btm[:, 1024:1028], op=ALU.add),
                       reads=[rbg] + RC, writes=[R_g])
                    mk("dve", lambda e, q=q: e.tensor_tensor(out=gfp[:, q * 4:(q + 1) * 4], in0=bg[:, q * 8 + 4:q * 8 + 8], in1=btm[:, 1028:1032], op=ALU.add),
                       reads=[rbg] + RC, writes=[R_g])

            def gates_a(T):
                G = gsm
                mk("act", lambda e: e.activation(out=G[:, 0, :], in_=gfp[:, :], func=AF.Abs),
                   reads=[R_g], writes=[R_gsm])
                mk("act", lambda e: e.activation(out=G[:, 1, :], in_=G[:, 0, :], func=AF.Exp, scale=-1.0),
                   reads=[R_gsm], writes=[R_gsm])
                mk("act", lambda e: e.activation(out=G[:, 2, :], in_=G[:, 1, :], func=AF.Ln, bias=1.0),
                   reads=[R_gsm], writes=[R_gsm])
                mk("dve", lambda e: e.tensor_scalar(out=G[:, 3, :], in0=gfp[:, :], scalar1=0.0, scalar2=None, op0=ALU.min),
                   reads=[R_g], writes=[R_gsm])
                mk("dve", lambda e: e.tensor_tensor(out=G[:, 4, :], in0=G[:, 2, :], in1=G[:, 3, :], op=ALU.subtract),
                   reads=[R_gsm], writes=[R_gsm])
                bk, rb_ = getbank()
                mk("pe", lambda e: e.matmul(bk[:, 0:NG], lhsT=triI[:, :], rhs=G[:, 4, :], start=True, stop=True),
                   reads=[R_gsm] + RC, writes=[rb_])
                mk("pe", lambda e: e.matmul(bk[:, 16:16 + NG], lhsT=onesf[:, :], rhs=G[:, 4, :], start=True, stop=True),
                   reads=[R_gsm] + RC, writes=[rb_])
                mk("dve", lambda e: e.tensor_tensor(out=G[:, 5, :], in0=bk[:, 0:NG], in1=gi[:, :], op=ALU.add),
                   reads=[rb_, R_g], writes=[R_gsm])
                return bk, rb_

            def gates_b(T, bk, rb_):
                G = gsm
                mk("act", lambda e: e.activation(out=ev[:, :], in_=G[:, 5, :], func=AF.Exp),
                   reads=[R_gsm], writes=[R_ev])
                mk("act", lambda e: e.activation(out=fl[:, :], in_=bk[:, 0:NG], func=AF.Exp),
                   reads=[rb_], writes=[R_ev])
                mk("act", lambda e: e.activation(out=dec[:, :], in_=bk[:, 16:16 + NG], func=AF.Exp, scale=-1.0),
                   reads=[rb_], writes=[R_ev])

            def conv_a(T):
                for jc in range(4):
                    bk, rb_ = getbank()
                    for j in range(31):
                        mk("pe", lambda e, jc=jc, j=j, bk=bk: e.matmul(bk[:, 0:TT], lhsT=dconv[:, jc * 31 + j, :], rhs=ubuf[:, jc, j:j + TT],
                                                                       start=(j == 0), stop=(j == 30)),
                           reads=[R_ubuf] + RC, writes=[rb_])
                    mk("act", lambda e, jc=jc, bk=bk: e.activation(out=cv[:, jc, :], in_=bk[:, 0:TT], func=AF.Identity, bias=cb[:, jc:jc + 1]),
                       reads=[rb_] + RC, writes=[R_cv[jc]])
                    mk("pool", lambda e, jc=jc: e.tensor_copy(out=cvb[:, jc, :], in_=cv[:, jc, :]),
                       reads=[R_cv[jc]], writes=[R_cvb])
                    mk("act", lambda e, jc=jc: e.activation(out=sqb[:, jc, :], in_=cv[:, jc, :], func=AF.Square),
                       reads=[R_cv[jc]], writes=[R_sqb])
                mk("pool", lambda e: e.tensor_copy(out=ubuf[:, :, 0:30], in_=ubuf[:, :, TT:TT + 30]),
                   reads=[], writes=[R_ubuf])
                bm, rbm = getbank()
                be, rbe = getbank()
                for jc in range(4):
                    mk("pe", lambda e, jc=jc: e.matmul(bm[:, 0:TT], lhsT=o512b[:, :], rhs=cvb[:, jc, :], start=(jc == 0), stop=(jc == 3)),
                       reads=[R_cvb] + RC, writes=[rbm])
                for jc in range(4):
                    mk("pe", lambda e, jc=jc: e.matmul(be[:, 0:TT], lhsT=o512b[:, :], rhs=sqb[:, jc, :], start=(jc == 0), stop=(jc == 3)),
                       reads=[R_sqb] + RC, writes=[rbe])
                mk("act", lambda e: e.activation(out=mean_sb[:, :], in_=bm[:, 0:TT], func=AF.Identity),
                   reads=[rbm], writes=[R_stats])
                mk("dve", lambda e: e.tensor_tensor(out=msq[:, :], in0=mean_sb[:, :], in1=mean_sb[:, :], op=ALU.mult),
                   reads=[R_stats], writes=[R_stats])
                mk("dve", lambda e: e.tensor_tensor(out=msq[:, :], in0=be[:, 0:TT], in1=msq[:, :], op=ALU.subtract),
                   reads=[rbe, R_stats], writes=[R_stats])

            def conv_ln(T):
                mk("act", lambda e: e.activation(out=rstdt[:, :], in_=msq[:, :], func=AF.Ln, bias=epsc[:, 0:1]),
                   reads=[R_stats] + RC, writes=[R_stats])
                mk("act", lambda e: e.activation(out=rstdt[:, :], in_=rstdt[:, :], func=AF.Exp, scale=-0.5),
                   reads=[R_stats], writes=[R_stats])

            def conv_b(T):
                for jc in range(4):
                    mk("pool", lambda e, jc=jc: e.tensor_tensor(out=cv[:, jc, :], in0=cv[:, jc, :], in1=mean_sb[:, :], op=ALU.subtract),
                       reads=[R_stats, R_cv[jc]], writes=[R_cv[jc]])
                    mk("dve", lambda e, jc=jc: e.tensor_tensor(out=cv[:, jc, :], in0=cv[:, jc, :], in1=rstdt[:, :], op=ALU.mult),
                       reads=[R_stats, R_cv[jc]], writes=[R_cv[jc]])
                    t, rt_ = gettmp()
                    mk("act", lambda e, jc=jc, t=t: e.activation(out=t[:, :], in_=cv[:, jc, :], func=AF.Tanh, scale=hlng[:, jc:jc + 1], bias=hlnb[:, jc:jc + 1]),
                       reads=[R_cv[jc]] + RC, writes=[rt_])
                    mk("act", lambda e, jc=jc: e.activation(out=cv[:, jc, :], in_=cv[:, jc, :], func=AF.Identity, scale=lng[:, jc:jc + 1], bias=lnb[:, jc:jc + 1]),
                       reads=[R_cv[jc]] + RC, writes=[R_cv[jc]])
                    mk("dve", lambda e, jc=jc, t=t: e.scalar_tensor_tensor(out=yT[:, jc, :], in0=t[:, :], scalar=1.0, in1=cv[:, jc, :], op0=ALU.add, op1=ALU.mult),
                       reads=[rt_, R_cv[jc]], writes=[R_yT])

            def tile_qkconv(T):
                for jp in range(8):
                    bk, rb_ = getbank()
                    for j in range(4):
                        mk("pe", lambda e, jp=jp, j=j, bk=bk: e.matmul(bk[:, 0:TT], lhsT=dqk[:, jp * 4 + j, :], rhs=qkbuf[:, jp, j:j + TT],
                                                                       start=(j == 0), stop=(j == 3)),
                           reads=[R_qkbuf] + RC, writes=[rb_])
                    t, rt_ = gettmp()
                    z, rz_ = gettmp()
                    mk("act", lambda e, jp=jp, bk=bk, t=t: e.activation(out=t[:, :], in_=bk[:, 0:TT], func=AF.Tanh, scale=0.5, bias=hqkb[:, jp:jp + 1]),
                       reads=[rb_] + RC, writes=[rt_])
                    mk("act", lambda e, jp=jp, bk=bk, z=z: e.activation(out=z[:, :], in_=bk[:, 0:TT], func=AF.Identity, bias=qkb[:, jp:jp + 1]),
                       reads=[rb_] + RC, writes=[rz_])
                    mk("dve", lambda e, jp=jp, t=t, z=z: e.scalar_tensor_tensor(out=qkT[:, jp, :], in0=t[:, :], scalar=1.0, in1=z[:, :], op0=ALU.add, op1=ALU.mult),
                       reads=[rt_, rz_], writes=[R_qkT])
                mk("pool", lambda e: e.tensor_copy(out=qkbuf[:, :, 0:3], in_=qkbuf[:, :, TT:TT + 3]),
                   reads=[], writes=[R_qkbuf])
                for q in range(NQ):
                    bk, rb_ = getbank()
                    for h in range(4):
                        mk("pe", lambda e, q=q, h=h, bk=bk: e.transpose(out=b16(bk)[:, h * 128:(h + 1) * 128], in_=qkT[:, 4 + h, q * 128:(q + 1) * 128], identity=identb[:, :]),
                           reads=[R_qkT] + RC, writes=[rb_])
                    mk("act", lambda e, q=q, bk=bk: e.activation(out=ktm[:, q, :], in_=b16(bk)[:, 0:512], func=AF.Identity),
                       reads=[rb_], writes=[R_ktm])

            def chunk_mlstm(T, q):
                c = T * NQ + q
                s = c % 2
                V = vt[s]; ST_ = stT[s]
                evq = ev[:, q * 4:(q + 1) * 4]
                cs = slice(q * 128, (q + 1) * 128)
                mk("dve", lambda e: e.tensor_tensor(out=V[:, :, 0:128], in0=vsb[:, q, :].rearrange("p (h d) -> p h d", h=4),
                                                    in1=evq.unsqueeze(2).broadcast_to([128, 4, 128]), op=ALU.mult),
                   reads=[R_vsb, R_ev], writes=[R_vt[s]])
                mk("pool", lambda e: e.tensor_copy(out=V[:, :, 128:129], in_=evq.unsqueeze(2)),
                   reads=[R_ev], writes=[R_vt[s]])
                bs, rbs = getbank()
                for h in range(4):
                    mk("pe", lambda e, h=h: e.matmul(bs[:, h * 128:(h + 1) * 128], lhsT=qkT[:, 4 + h, cs], rhs=qkT[:, h, cs], start=True, stop=True),
                       reads=[R_qkT], writes=[rbs])
                mk("dve", lambda e: e.tensor_tensor(out=ST_[:, :, :], in0=bs[:, 0:512].rearrange("p (h t) -> p h t", h=4),
                                                    in1=maskk[:, :].unsqueeze(1).broadcast_to([128, 4, 128]), op=ALU.mult),
                   reads=[rbs] + RC, writes=[R_stT[s]])
                bo = [getbank(), getbank()]
                for h in range(4):
                    bk, rb_ = bo[h // 2]
                    off = (h % 2) * 129
                    mk("pe", lambda e, h=h, bk=bk, off=off: e.matmul(bk[:, off:off + 129], lhsT=ST_[:, h, :], rhs=V[:, h, :], start=True, stop=False),
                       reads=[R_stT[s], R_vt[s]], writes=[rb_])
                    mk("pe", lambda e, h=h, bk=bk, off=off: e.matmul(bk[:, off:off + 129], lhsT=qkT[:, h, cs], rhs=Chb[:, h, :], start=False, stop=True),
                       reads=[R_qkT, R_Chb] + RC, writes=[rb_])
                bd = [getbank(), getbank()]
                for h in range(4):
                    bk, rb_ = bd[h // 2]
                    off = (h % 2) * 129
                    mk("pe", lambda e, h=h, bk=bk, off=off: e.matmul(bk[:, off:off + 129], lhsT=ktm[:, q, h * 128:(h + 1) * 128], rhs=V[:, h, :], start=True, stop=True),
                       reads=[R_ktm, R_vt[s]], writes=[rb_])
                for p in range(2):
                    bk, rb_ = bd[p]
                    cview = Chat[:, 2 * p:2 * p + 2, :].rearrange("p h c -> p (h c)")
                    mk("dve", lambda e, bk=bk, cview=cview: e.scalar_tensor_tensor(out=cview, in0=bk[:, 0:258], scalar=KAPPA, in1=cview, op0=ALU.mult, op1=ALU.add),
                       reads=[rb_, R_Chat], writes=[R_Chat])
                mk("pool", lambda e: e.tensor_tensor(out=Chat[:, :, :], in0=Chat[:, :, :], in1=dec[:, q * 4:(q + 1) * 4].unsqueeze(2).broadcast_to([128, 4, 129]), op=ALU.mult),
                   reads=[R_Chat, R_ev], writes=[R_Chat])
                mk("pool", lambda e: e.tensor_copy(out=Chb[:, :, :], in_=Chat[:, :, :]),
                   reads=[R_Chat], writes=[R_Chb])
                for p in range(2):
                    bk, rb_ = bo[p]
                    mk("dve", lambda e, p=p, bk=bk: e.tensor_scalar(out=sm[:, 10, 2 * p:2 * p + 2].unsqueeze(2),
                                                                    in0=bk[:, 0:258].rearrange("p (h c) -> p h c", h=2)[:, :, 128:129],
                                                                    scalar1=-1.0, scalar2=None, op0=ALU.mult),
                       reads=[rb_], writes=[R_sm])
                mk("dve", lambda e: e.scalar_tensor_tensor(out=sm[:, 0, :], in0=sm[:, 10, :], scalar=-1.0, in1=sm[:, 10, :], op0=ALU.mult, op1=ALU.max),
                   reads=[R_sm], writes=[R_sm])
                mk("dve", lambda e: e.tensor_tensor(out=sm[:, 0, :], in0=sm[:, 0, :], in1=fl[:, q * 4:(q + 1) * 4], op=ALU.max),
                   reads=[R_sm, R_ev], writes=[R_sm])
                mk("dve", lambda e: e.reciprocal(out=sm[:, 1, :], in_=sm[:, 0, :]), reads=[R_sm], writes=[R_sm])
                for h in range(4):
                    bk, rb_ = bo[h // 2]
                    off = (h % 2) * 129
                    mk("dve", lambda e, h=h, bk=bk, off=off: e.scalar_tensor_tensor(out=hm[:, h, :], in0=bk[:, off:off + 128], scalar=sm[:, 1, h:h + 1],
                                                                                    in1=osb[:, q, h * 128:(h + 1) * 128], op0=ALU.mult, op1=ALU.mult,
                                                                                    accum_out=sm[:, 2, h:h + 1]),
                       reads=[rb_, R_sm, R_osb], writes=[R_hm, R_sm])
                for h in range(4):
                    mk("act", lambda e, h=h: e.activation(out=junk[:, :], in_=hm[:, h, :], func=AF.Square, accum_out=sm[:, 3, h:h + 1]),
                       reads=[R_hm], writes=[R_junk, R_sm])
                mk("dve", lambda e: e.tensor_scalar(out=sm[:, 4, :], in0=sm[:, 2, :], scalar1=1.0 / 128, scalar2=None, op0=ALU.mult),
                   reads=[R_sm], writes=[R_sm])
                mk("dve", lambda e: e.tensor_tensor(out=sm[:, 5, :], in0=sm[:, 4, :], in1=sm[:, 4, :], op=ALU.mult),
                   reads=[R_sm], writes=[R_sm])
                mk("dve", lambda e: e.scalar_tensor_tensor(out=sm[:, 6, :], in0=sm[:, 3, :], scalar=1.0 / 128, in1=sm[:, 5, :], op0=ALU.mult, op1=ALU.subtract),
                   reads=[R_sm], writes=[R_sm])
                mk("dve", lambda e: e.tensor_scalar(out=sm[:, 8, :], in0=sm[:, 6, :], scalar1=EPS, scalar2=None, op0=ALU.add),
                   reads=[R_sm], writes=[R_sm])
                emit_rsqrt(sm[:, 7, :], sm[:, 8, :], sm[:, 9, :], [R_sm])
                for h in range(4):
                    eng = "dve" if h % 2 == 0 else "pool"
                    mk(eng, lambda e, h=h: e.tensor_scalar(out=hmn[:, h, :], in0=hm[:, h, :], scalar1=sm[:, 4, h:h + 1], scalar2=sm[:, 7, h:h + 1],
                                                           op0=ALU.subtract, op1=ALU.mult),
                       reads=[R_hm, R_sm], writes=[R_hmn])
                bk, rb_ = getbank()
                for h in range(4):
                    mk("pe", lambda e, h=h: e.transpose(out=b16(bk)[:, h * 128:(h + 1) * 128], in_=hmn[:, h, :], identity=identb[:, :]),
                       reads=[R_hmn] + RC, writes=[rb_])
                mk("act", lambda e: e.activation(out=yT[:, 4:8, cs], in_=b16(bk)[:, 0:512].rearrange("p (h t) -> p h t", h=4), func=AF.Identity),
                   reads=[rb_], writes=[R_yT])

            def back1(T):
                s = T % 2
                X = xt[s]
                for q in range(NQ):
                    cs = slice(q * 128, (q + 1) * 128)
                    c = T * NQ + q
                    ba, rba = getbank()
                    bb, rbb = getbank()
                    for k in range(8):
                        mk("pe", lambda e, k=k, ba=ba, cs=cs: e.matmul(ba[:, 0:512], lhsT=yT[:, k, cs], rhs=woutb[:, k, 0:512], start=(k == 0), stop=(k == 7)),
                           reads=[R_yT] + RC, writes=[rba])
                        mk("pe", lambda e, k=k, bb=bb, cs=cs: e.matmul(bb[:, 0:512], lhsT=yT[:, k, cs], rhs=woutb[:, k, 512:1024], start=(k == 0), stop=(k == 7)),
                           reads=[R_yT] + RC, writes=[rbb])
                    mk("dve", lambda e, q=q, ba=ba: e.tensor_tensor(out=X[:, q, 0:512], in0=ba[:, 0:512], in1=X[:, q, 0:512], op=ALU.add),
                       reads=[rba, R_xt[s]], writes=[R_xt[s]])
                    mk("dve", lambda e, q=q, bb=bb: e.tensor_tensor(out=X[:, q, 512:1024], in0=bb[:, 0:512], in1=X[:, q, 512:1024], op=ALU.add),
                       reads=[rbb, R_xt[s]], writes=[R_xt[s]])
                    mk("act", lambda e, q=q: e.activation(out=h2[:, q, :], in_=X[:, q, :], func=AF.Square, accum_out=ss[:, 4, q:q + 1]),
                       reads=[R_xt[s]], writes=[R_h2, R_ssb])
                mk("sp", lambda e: e.dma_start(out=x1_d[T * TT:(T + 1) * TT, :].rearrange("(q p) d -> p q d", p=128), in_=X[:, :, :]),
                   reads=[R_xt[s]], accum=[R_x1], dma=x1sem[s])
                if debug:
                    mk("sp", lambda e: e.dma_start(out=dbg["x1"][T * TT:(T + 1) * TT, :].rearrange("(q p) d -> p q d", p=128), in_=X[:, :, :]),
                       reads=[R_xt[s]], accum=[R_dbg], dma=dbsem[s])

            def back2(T):
                s = T % 2
                X = xt[s]
                mk("dve", lambda e: e.tensor_scalar(out=ss[:, 5, :], in0=ss[:, 4, :], scalar1=1.0 / D, scalar2=EPS, op0=ALU.mult, op1=ALU.add),
                   reads=[R_ssb], writes=[R_ssb])
                emit_rsqrt(ss[:, 6, :], ss[:, 5, :], ss[:, 7, :], [R_ssb])
                for q in range(NQ):
                    mk("dve", lambda e, q=q: e.scalar_tensor_tensor(out=h2[:, q, :], in0=X[:, q, :], scalar=ss[:, 6, q:q + 1], in1=g2bc[:, :], op0=ALU.mult, op1=ALU.mult),
                       reads=[R_xt[s], R_ssb] + RC, writes=[R_h2])
                br, rbr = getbank()
                for q in range(NQ):
                    cs = slice(q * 128, (q + 1) * 128)
                    bk, rb_ = getbank()
                    for k in range(8):
                        mk("pe", lambda e, q=q, k=k, bk=bk: e.transpose(out=b16(bk)[:, k * 128:(k + 1) * 128], in_=h2[:, q, k * 128:(k + 1) * 128], identity=identb[:, :]),
                           reads=[R_h2] + RC, writes=[rb_])
                    mk("act", lambda e, q=q, bk=bk, cs=cs: e.activation(out=h2T[:, :, cs], in_=b16(bk)[:, 0:1024].rearrange("p (k t) -> p k t", k=8), func=AF.Identity),
                       reads=[rb_], writes=[R_h2T])
                    for k in range(8):
                        mk("pe", lambda e, q=q, k=k, cs=cs: e.matmul(br[:, q * 36:(q + 1) * 36], lhsT=h2T[:, k, cs], rhs=rwb[:, k, :], start=(k == 0), stop=(k == 7)),
                           reads=[R_h2T] + RC, writes=[rbr])
                for q in range(NQ):
                    c = T * NQ + q
                    lg = rt[:, 0, :]
                    mk("dve", lambda e, q=q, lg=lg: e.tensor_tensor(out=lg, in0=br[:, q * 36:(q + 1) * 36], in1=rbbc[:, :], op=ALU.add),
                       reads=[rbr] + RC, writes=[R_rt])
                    r1 = [R_rt]
                    mk("dve", lambda e: e.tensor_reduce(out=rt[:, 1, 0:1], in_=rt[:, 0, 0:4], axis=AX.X, op=ALU.max), reads=r1, writes=r1)
                    mk("dve", lambda e: e.tensor_scalar(out=rt[:, 2, 0:4], in0=rt[:, 0, 0:4], scalar1=rt[:, 1, 0:1], scalar2=None, op0=ALU.is_equal), reads=r1, writes=r1)
                    mk("dve", lambda e: e.tensor_scalar(out=rt[:, 1, 1:2], in0=rt[:, 1, 0:1], scalar1=-1.0, scalar2=None, op0=ALU.mult), reads=r1, writes=r1)
                    mk("act", lambda e: e.activation(out=rt[:, 3, 0:4], in_=rt[:, 0, 0:4], func=AF.Exp, bias=rt[:, 1, 1:2], accum_out=rt[:, 1, 2:3]), reads=r1, writes=r1)
                    mk("dve", lambda e: e.reciprocal(out=rt[:, 1, 3:4], in_=rt[:, 1, 2:3]), reads=r1, writes=r1)
                    mk("dve", lambda e: e.tensor_scalar(out=rt[:, 4, 0:4], in0=rt[:, 2, 0:4], scalar1=-1.0, scalar2=1e30, op0=ALU.add, op1=ALU.mult), reads=r1, writes=r1)
                    mk("dve", lambda e: e.tensor_tensor(out=rt[:, 5, 0:32].rearrange("p (g k) -> p g k", g=4), in0=rt[:, 0, 4:36].rearrange("p (g k) -> p g k", g=4),
                                                        in1=rt[:, 4, 0:4].unsqueeze(2).broadcast_to([128, 4, 8]), op=ALU.add), reads=r1, writes=r1)
                    mk("dve", lambda e: e.max(out=rt[:, 6, 0:8], in_=rt[:, 5, 0:32]), reads=r1, writes=r1)
                    mk("dve", lambda e: e.tensor_scalar(out=rt[:, 7, 0:32], in0=rt[:, 5, 0:32], scalar1=rt[:, 6, 0:1], scalar2=None, op0=ALU.is_equal), reads=r1, writes=r1)
                    mk("dve", lambda e: e.tensor_scalar(out=rt[:, 8, 0:32], in0=rt[:, 5, 0:32], scalar1=rt[:, 6, 1:2], scalar2=None, op0=ALU.is_equal), reads=r1, writes=r1)
                    mk("dve", lambda e: e.tensor_tensor(out=rt[:, 1, 4:5], in0=rt[:, 6, 1:2], in1=rt[:, 6, 0:1], op=ALU.subtract), reads=r1, writes=r1)
                    mk("act", lambda e: e.activation(out=rt[:, 1, 5:6], in_=rt[:, 1, 4:5], func=AF.Exp), reads=r1, writes=r1)
                    mk("dve", lambda e: e.tensor_scalar(out=rt[:, 1, 6:7], in0=rt[:, 1, 5:6], scalar1=1.0, scalar2=None, op0=ALU.add), reads=r1, writes=r1)
                    mk("dve", lambda e: e.reciprocal(out=rt[:, 1, 7:8], in_=rt[:, 1, 6:7]), reads=r1, writes=r1)
                    mk("dve", lambda e, c=c: e.tensor_tensor(out=gwt[:, 2 * c:2 * c + 1], in0=rt[:, 1, 7:8], in1=rt[:, 1, 3:4], op=ALU.mult), reads=r1, writes=[R_gwt])
                    mk("dve", lambda e, c=c: e.tensor_tensor(out=gwt[:, 2 * c + 1:2 * c + 2], in0=rt[:, 1, 3:4], in1=gwt[:, 2 * c:2 * c + 1], op=ALU.subtract), reads=r1 + [R_gwt], writes=[R_gwt])
                    mk("dve", lambda e: e.tensor_tensor(out=cmbb[:, :], in0=rt[:, 7, 0:32], in1=rt[:, 8, 0:32], op=ALU.add), reads=r1, writes=r1)
                    bk, rb_ = getbank()
                    mk("pe", lambda e, bk=bk: e.matmul(bk[:, 0:32], lhsT=triSb[:, :], rhs=cmbb[:, :], start=True, stop=True), reads=r1 + RC, writes=[rb_])
                    mk("pe", lambda e, bk=bk: e.matmul(bk[:, 32:64], lhsT=onesb[:, :], rhs=cmbb[:, :], start=True, stop=True), reads=r1 + RC, writes=[rb_])
                    mk("dve", lambda e, bk=bk: e.tensor_tensor(out=rt[:, 9, 0:32], in0=bk[:, 0:32], in1=basep[:, :], op=ALU.add), reads=[rb_, R_base] + r1, writes=r1)
                    mk("dve", lambda e: e.scalar_tensor_tensor(out=rt[:, 10, 0:32], in0=rt[:, 7, 0:32], scalar=1.0, in1=rt[:, 9, 0:32], op0=ALU.mult, op1=ALU.mult, accum_out=rt[:, 1, 8:9]), reads=r1, writes=r1)
                    mk("dve", lambda e: e.scalar_tensor_tensor(out=rt[:, 10, 0:32], in0=rt[:, 8, 0:32], scalar=1.0, in1=rt[:, 9, 0:32], op0=ALU.mult, op1=ALU.mult, accum_out=rt[:, 1, 9:10]), reads=r1, writes=r1)
                    mk("dve", lambda e, c=c: e.tensor_scalar(out=didx[:, 2 * c:2 * c + 2], in0=rt[:, 1, 8:10], scalar1=float(NSLOT - 1), scalar2=None, op0=ALU.min), reads=r1, writes=[R_didx])
                    mk("dve", lambda e, bk=bk: e.tensor_tensor(out=basep[:, :], in0=basep[:, :], in1=bk[:, 32:64], op=ALU.add), reads=[rb_], writes=[R_base])
                    for j in range(2):
                        mk("pool", lambda e, q=q, c=c, j=j: e.indirect_dma_start(out=xs_d, out_offset=bass.IndirectOffsetOnAxis(ap=didx[:, 2 * c + j:2 * c + j + 1], axis=0),
                                                                                 in_=h2[:, q, :], in_offset=None),
                           reads=[R_h2, R_didx], accum=[R_xs], dma=scsem)

            def dumps0():
                dump("winb", winb[:, 0, :], RC)
                dump("woutb", woutb[:, 4, :], RC)
                dump("dconv", dconv[:, 0:4, :], RC)
                dump("ss", ss[:, :, :], [R_ss])
                dump("hb", hb[:, :, :], [R_hb])
                dump("hT", hT[:, :, :], [R_hT])

            front1(0)
            if debug:
                dumps0()
            for T in range(NT):
                front2(T)
                if T == 0:
                    dump("ubuf", ubuf[:, :, :], [R_ubuf])
                    dump("qkbuf", qkbuf[:, :, :], [R_qkbuf])
                    dump("vsb", vsb[:, :, :], [R_vsb])
                    dump("osb", osb[:, :, :], [R_osb])
                    dump("gi", gi[:, :], [R_g])
                    dump("gfp", gfp[:, :], [R_g])
                if T >= 1:
                    back2(T - 1)
                if T + 1 < NT:
                    front1(T + 1)
                conv_a(T)
                gbk = gates_a(T)
                conv_ln(T)
                gates_b(T, *gbk)
                if T == 0:
                    dump("gsm", gsm[:, :, :], [R_gsm])
                    dump("ev", ev[:, :], [R_ev])
                    dump("fl", fl[:, :], [R_ev])
                    dump("dec", dec[:, :], [R_ev])
                tile_qkconv(T)
                conv_b(T)
                if T == 0:
                    dump("cv", cv[:, :, :], R_cv)
                    dump("rstdt", rstdt[:, :], [R_stats])
                    dump("mean", mean_sb[:, :], [R_stats])
                    dump("qkT", qkT[:, :, :], [R_qkT])
                    dump("ktm", ktm[:, :, :], [R_ktm])
                for q in range(NQ):
                    chunk_mlstm(T, q)
                    if T == 0 and q == 0:
                        dump("vt", vt[0][:, :, :], [R_vt[0]])
                        dump("stT", stT[0][:, :, :], [R_stT[0]])
                        dump("Chat", Chat[:, :, :], [R_Chat])
                        dump("hm", hm[:, :, :], [R_hm])
                        dump("hmn", hmn[:, :, :], [R_hmn])
                        dump("sm", sm[:, :, :], [R_sm])
                if debug and S <= 1024:
                    for k in range(8):
                        t, rt_ = gettmp()
                        mk("dve", lambda e, k=k, t=t: e.tensor_copy(out=t[:, :], in_=yT[:, k, :]), reads=[R_yT], writes=[rt_])
                        mk("sp", lambda e, k=k, t=t, T=T: e.dma_start(out=dbg["yT"][:, k * S + T * TT:k * S + (T + 1) * TT], in_=t[:, :]), reads=[rt_], accum=[R_dbg], dma=P.dsem())
                back1(T)
            back2(NT - 1)

            mk("dve", lambda e: e.tensor_tensor(out=cnti[:, :], in0=basep[:, :], in1=ecapsb[:, :], op=ALU.subtract),
               reads=[R_base, R_const], writes=[R_cnt])
            allres = [R_xt[0], R_xt[1], R_hb, R_h2, R_hT, R_h2T, R_ubuf, R_qkbuf, R_vsb, R_osb, R_g, R_gsm, R_ev, R_cvb, R_sqb, R_stats,
                      R_qkT, R_ktm, R_vt[0], R_vt[1], R_stT[0], R_stT[1], R_Chat, R_Chb, R_hm, R_hmn, R_sm, R_junk, R_yT, R_ss, R_ssb, R_rt,
                      R_const, R_c2, R_base, R_cnt] + R_cv + R_tmp + bank_res
            R_phase = Res("phase")
            for eng in ("pe", "act", "dve", "pool", "sp"):
                P.wait_all(eng, allres)

        with contextlib.ExitStack() as st3:
            def sb3(name, shape, dtype=F32):
                return st3.enter_context(nc.sbuf_tensor("s_" + name, list(shape), dtype))

            wgb = [sb3("wgb%d" % i, [128, 8, DE], BF16) for i in range(2)]
            wub = [sb3("wub%d" % i, [128, 8, DE], BF16) for i in range(2)]
            wdb = [sb3("wdb%d" % i, [128, 4, D], BF16) for i in range(2)]
            R_w = [Res("w0"), Res("w1")]
            wsem = [P.dsem("w0"), P.dsem("w1")]
            NXG = 4
            xg = [sb3("xg%d" % i, [128, D], BF16) for i in range(NXG)]
            R_xg = [Res("xg%d" % i) for i in range(NXG)]
            xgsem = [P.dsem("xg%d" % i) for i in range(NXG)]
            xTb = [sb3("xTb%d" % i, [128, 8, 128], BF16) for i in range(2)]
            R_xTb = [Res("xTb0"), Res("xTb1")]
            actTb = [sb3("actTb%d" % i, [128, 4, 128], BF16) for i in range(2)]
            R_actTb = [Res("actTb0"), Res("actTb1")]
            sil = [sb3("sil%d" % i, [128, 512]) for i in range(2)]
            R_sil = [Res("sil0"), Res("sil1")]
            actm = [sb3("actm%d" % i, [128, 512], BF16) for i in range(2)]
            R_actm = [Res("actm0"), Res("actm1")]
            yo = [sb3("yo%d" % i, [128, D]) for i in range(3)]
            R_yo = [Res("yo%d" % i) for i in range(3)]
            yosem = [P.dsem("yo%d" % i) for i in range(3)]

            def load_w(e_):
                s = e_ % 2
                for (dst, src) in ((wgb[s], wg_d), (wub[s], wu_d)):
                    for k in range(8):
                        mk("pool", lambda e, dst=dst, src=src, k=k: e.dma_start(out=dst[:, k, :], in_=src[e_, k * 128:(k + 1) * 128, :]),
                           accum=[R_w[s]], dma=wsem[s])
                for k in range(4):
                    mk("pool", lambda e, k=k: e.dma_start(out=wdb[s][:, k, :], in_=wd_d[e_, k * 128:(k + 1) * 128, :]),
                       accum=[R_w[s]], dma=wsem[s])

            blk_ctr = [0]

            def stage1(e_, b, i):
                s = e_ % 2
                gi_ = i % NXG
                t2 = i % 2
                yi = i % 3
                row0 = e_ * CAP + b * 128
                XT = xTb[t2]
                AT = actTb[t2]
                mk("sp", lambda e: e.dma_start(out=xg[gi_][:, :], in_=xs_d[row0:row0 + 128, :]),
                   reads=[R_xs], writes=[R_xg[gi_]], dma=xgsem[gi_])
                bk, rb_ = getbank()
                for k in range(8):
                    mk("pe", lambda e, k=k: e.transpose(out=b16(bk)[:, k * 128:(k + 1) * 128], in_=xg[gi_][:, k * 128:(k + 1) * 128], identity=identb[:, :]),
                       reads=[R_xg[gi_], R_const], writes=[rb_])
                mk("dve", lambda e: e.tensor_copy(out=XT[:, :, :], in_=b16(bk)[:, 0:1024].rearrange("p (k t) -> p k t", k=8)),
                   reads=[rb_], writes=[R_xTb[t2]])
                bg_, rbg_ = getbank()
                bu_, rbu_ = getbank()
                for k in range(8):
                    mk("pe", lambda e, k=k: e.matmul(bg_[:, 0:512], lhsT=XT[:, k, :], rhs=wgb[s][:, k, :], start=(k == 0), stop=(k == 7)),
                       reads=[R_w[s], R_xTb[t2]], writes=[rbg_])
                for k in range(8):
                    mk("pe", lambda e, k=k: e.matmul(bu_[:, 0:512], lhsT=XT[:, k, :], rhs=wub[s][:, k, :], start=(k == 0), stop=(k == 7)),
                       reads=[R_w[s], R_xTb[t2]], writes=[rbu_])
                mk("act", lambda e: e.activation(out=sil[t2][:, :], in_=bg_[:, 0:512], func=AF.Silu),
                   reads=[rbg_], writes=[R_sil[t2]])
                mk("dve", lambda e: e.tensor_tensor(out=actm[t2][:, :], in0=sil[t2][:, :], in1=bu_[:, 0:512], op=ALU.mult),
                   reads=[R_sil[t2], rbu_], writes=[R_actm[t2]])

            def stage2(e_, b, i):
                s = e_ % 2
                gi_ = i % NXG
                t2 = i % 2
                yi = i % 3
                row0 = e_ * CAP + b * 128
                XT = xTb[t2]
                AT = actTb[t2]
                bt_, rbt_ = getbank()
                for j in range(4):
                    mk("pe", lambda e, j=j: e.transpose(out=b16(bt_)[:, j * 128:(j + 1) * 128], in_=actm[t2][:, j * 128:(j + 1) * 128], identity=identb[:, :]),
                       reads=[R_actm[t2], R_const], writes=[rbt_])
                mk("dve", lambda e: e.tensor_copy(out=AT[:, :, :].rearrange("p j t -> p (j t)"), in_=b16(bt_)[:, 0:512]),
                   reads=[rbt_], writes=[R_actTb[t2]])
                ba, rba = getbank()
                bb, rbb = getbank()
                for j in range(4):
                    mk("pe", lambda e, j=j: e.matmul(ba[:, 0:512], lhsT=AT[:, j, :], rhs=wdb[s][:, j, 0:512], start=(j == 0), stop=(j == 3)),
                       reads=[R_w[s], R_actTb[t2]], writes=[rba])
                    mk("pe", lambda e, j=j: e.matmul(bb[:, 0:512], lhsT=AT[:, j, :], rhs=wdb[s][:, j, 512:1024], start=(j == 0), stop=(j == 3)),
                       reads=[R_w[s], R_actTb[t2]], writes=[rbb])
                mk("dve", lambda e: e.tensor_copy(out=yo[yi][:, 0:512], in_=ba[:, 0:512]),
                   reads=[rba], writes=[R_yo[yi]])
                mk("dve", lambda e: e.tensor_copy(out=yo[yi][:, 512:1024], in_=bb[:, 0:512]),
                   reads=[rbb], writes=[R_yo[yi]])
                mk("pool", lambda e: e.dma_start(out=ys_d[row0:row0 + 128, :], in_=yo[yi][:, :]),
                   reads=[R_yo[yi]], accum=[R_ys], dma=yosem[yi])

            load_w(0)
            NP = NB // 2
            for e_ in range(NE):
                if e_ + 1 < NE:
                    load_w(e_ + 1)
                for bp in range(NP):
                    P.cond_begin(cnti[0:1, e_:e_ + 1], bp * 256)
                    i0 = blk_ctr[0]
                    blk_ctr[0] += 2
                    stage1(e_, 2 * bp, i0)
                    stage1(e_, 2 * bp + 1, i0 + 1)
                    stage2(e_, 2 * bp, i0)
                    stage2(e_, 2 * bp + 1, i0 + 1)
                for bp in range(NP):
                    P.cond_end()

            finC = [R_w[0], R_w[1]] + R_xg + R_xTb + R_actTb + R_actm + R_sil + R_yo + bank_res
            for eng in ("sp", "pe", "act", "dve", "pool"):
                P.wait_all(eng, finC)

        with contextlib.ExitStack() as st4:
            def sb4(name, shape, dtype=F32):
                return st4.enter_context(nc.sbuf_tensor("s_" + name, list(shape), dtype))

            GD = 4
            NSL = 2 * GD
            gfbc = sb4("gfbc", [128, D])
            R_gf = Res("gfbc")
            mk("sp", lambda e: e.dma_start(out=gfbc[:, :], in_=gf_d.partition_broadcast(128)), writes=[R_gf], dma=P.dsem("gf"))
            xa = [sb4("xa%d" % i, [128, D]) for i in range(NSL)]
            ya = [sb4("ya%d" % i, [128, D]) for i in range(NSL)]
            yb = [sb4("yb%d" % i, [128, D]) for i in range(NSL)]
            ob = [sb4("ob%d" % i, [128, D]) for i in range(2)]
            R_xa = [Res("xa%d" % i) for i in range(NSL)]; R_ya = [Res("ya%d" % i) for i in range(NSL)]; R_yb = [Res("yb%d" % i) for i in range(NSL)]
            R_ob = [Res("ob0"), Res("ob1")]
            xasem = [P.dsem() for _ in range(NSL)]; yasem = [P.dsem() for _ in range(NSL)]; ybsem = [P.dsem() for _ in range(NSL)]; obsem = [P.dsem(), P.dsem()]
            fs = sb4("fs", [128, 4, NCH]); R_fs = Res("fs")
            jk = sb4("jk", [128, D], BF16); R_jk = Res("jk")

            def d_load(c):
                s = c % NSL
                mk("sp", lambda e: e.dma_start(out=xa[s][:, :], in_=x1_d[c * 128:(c + 1) * 128, :]), reads=[R_x1], writes=[R_xa[s]], dma=xasem[s])
                mk("pool", lambda e: e.indirect_dma_start(out=ya[s][:, :], out_offset=None, in_=ys_d,
                                                          in_offset=bass.IndirectOffsetOnAxis(ap=didx[:, 2 * c:2 * c + 1], axis=0)),
                   reads=[R_ys, R_didx], writes=[R_ya[s]], dma=yasem[s])
                mk("pool", lambda e: e.indirect_dma_start(out=yb[s][:, :], out_offset=None, in_=ys_d,
                                                          in_offset=bass.IndirectOffsetOnAxis(ap=didx[:, 2 * c + 1:2 * c + 2], axis=0)),
                   reads=[R_ys, R_didx], writes=[R_yb[s]], dma=ybsem[s])

            def d_combine(c):
                s = c % NSL
                mk("dve", lambda e: e.scalar_tensor_tensor(out=xa[s][:, :], in0=ya[s][:, :], scalar=gwt[:, 2 * c:2 * c + 1], in1=xa[s][:, :], op0=ALU.mult, op1=ALU.add),
                   reads=[R_ya[s], R_xa[s], R_gwt], writes=[R_xa[s]])
                mk("dve", lambda e: e.scalar_tensor_tensor(out=xa[s][:, :], in0=yb[s][:, :], scalar=gwt[:, 2 * c + 1:2 * c + 2], in1=xa[s][:, :], op0=ALU.mult, op1=ALU.add),
                   reads=[R_yb[s], R_xa[s], R_gwt], writes=[R_xa[s]])
                mk("act", lambda e: e.activation(out=jk[:, :], in_=xa[s][:, :], func=AF.Square, accum_out=fs[:, 0, c:c + 1]),
                   reads=[R_xa[s]], writes=[R_jk, R_fs])

            def d_finish(c):
                s = c % NSL
                o = c % 2
                mk("act", lambda e: e.activation(out=xa[s][:, :], in_=xa[s][:, :], func=AF.Identity, scale=fs[:, 2, c:c + 1]),
                   reads=[R_xa[s], R_fs], writes=[R_xa[s]])
                mk("pool", lambda e: e.tensor_tensor(out=ob[o][:, :], in0=xa[s][:, :], in1=gfbc[:, :], op=ALU.mult),
                   reads=[R_xa[s], R_gf], writes=[R_ob[o]])
                mk("sp", lambda e: e.dma_start(out=out_d[c * 128:(c + 1) * 128, :], in_=ob[o][:, :]),
                   reads=[R_ob[o]], accum=[R_out], dma=obsem[o])

            NGD = NCH // GD
            for c in range(min(GD, NCH)):
                d_load(c)
            for g in range(NGD):
                if g + 1 < NGD:
                    for c in range((g + 1) * GD, (g + 2) * GD):
                        d_load(c)
                cs_ = slice(g * GD, (g + 1) * GD)
                for c in range(g * GD, (g + 1) * GD):
                    d_combine(c)
                mk("dve", lambda e, cs_=cs_: e.tensor_scalar(out=fs[:, 1, cs_], in0=fs[:, 0, cs_], scalar1=1.0 / D, scalar2=EPS, op0=ALU.mult, op1=ALU.add),
                   reads=[R_fs], writes=[R_fs])
                emit_rsqrt(fs[:, 2, cs_], fs[:, 1, cs_], fs[:, 3, cs_], [R_fs])
                for c in range(g * GD, (g + 1) * GD):
                    d_finish(c)
            if debug:
                mk("sp", lambda e: e.dma_start(out=dbg["idx"], in_=didx[:, :]), reads=[R_didx], accum=[R_dbg], dma=obsem[0])
                mk("sp", lambda e: e.dma_start(out=dbg["gw"], in_=gwt[:, :]), reads=[R_gwt], accum=[R_dbg], dma=obsem[1])
            fin = [R_out, R_dbg, R_x1, R_xs, R_ys, R_didx, R_gwt, R_fs, R_jk, R_gf] + R_xa + R_ya + R_yb + R_ob + bank_res
            for eng in ("sp", "pe", "act", "dve", "pool"):
                P.wait_all(eng, fin)

        P.emit()
    return nc


def make_shared(inp, CAP):
    f = np.float32

    def a(v):
        return np.ascontiguousarray(v, dtype=f)

    def pk(vec, n):
        return a(np.asarray(vec).reshape(n, 128).T)

    b_in = np.asarray(inp["b_in"][0])
    sh = {
        "w_in": a(inp["w_in"][0]),
        "w_out": a(inp["w_out"][0]),
        "g1": pk(inp["norm_mix_g"][0], 8),
        "bfm": pk(b_in[0:2048], 16),
        "btm": a(b_in[2048:3080].reshape(1, 1032)),
        "cw": a(np.asarray(inp["conv_w"][0]).reshape(31, 4, 128).transpose(2, 1, 0).reshape(128, 124)),
        "cb": pk(inp["conv_b"][0], 4),
        "lng": pk(inp["conv_ln_g"][0], 4),
        "lnb": pk(inp["conv_ln_b"][0], 4),
        "qkw": a(np.asarray(inp["qk_conv_w"][0]).reshape(4, 8, 128).transpose(2, 1, 0).reshape(128, 32)),
        "qkb": pk(inp["qk_conv_b"][0], 8),
        "mg": pk(inp["mlstm_norm_g"][0], 4),
        "g2": a(np.asarray(inp["norm_ffn_g"][0]).reshape(1, D)),
        "gf": a(np.asarray(inp["final_norm_g"]).reshape(1, D)),
        "rw": a(np.concatenate([inp["router_group_w"][0], inp["router_expert_w"][0]], axis=1)),
        "rb": a(np.concatenate([inp["router_group_b"][0], inp["router_expert_b"][0]]).reshape(1, 36)),
        "ecap": a((np.arange(32) * CAP).reshape(1, 32)),
        "wg": a(inp["expert_w_gate"][0]),
        "wu": a(inp["expert_w_up"][0]),
        "wd": a(inp["expert_w_down"][0]),
        "ident": a(np.eye(128)),
        "tri_incl": a(np.triu(np.ones((128, 128)))),
        "tri_strict": a(np.triu(np.ones((128, 128)), 1)),
    }
    return sh


_CACHE = {}


def kernel(**inputs):
    x = np.asarray(inputs["x"], dtype=np.float32)
    B, S, _ = x.shape
    CAP = 1536
    key = (S, CAP)
    if key not in _CACHE:
        _CACHE[key] = build_program(S=S, CAP=CAP, NQ=2)
    nc = _CACHE[key]
    sh = make_shared(inputs, CAP)
    in_maps = []
    for b in range(B):
        m = dict(sh)
        m["x"] = np.ascontiguousarray(x[b])
        in_maps.append(m)
    res = run_bass_kernel_spmd(nc, in_maps, core_ids=list(range(B)))
    return np.stack([np.asarray(r["out"]) for r in res.results], axis=0).astype(np.float32)
```

```python
import contextlib
import numpy as np
import concourse.bass as bass
import concourse.mybir as mybir
from concourse.bass_utils import run_bass_kernel_spmd
from concourse.alu_op_type import AluOpType as ALU

F32 = mybir.dt.float32
BF16 = mybir.dt.bfloat16
I32 = mybir.dt.int32
AF = mybir.ActivationFunctionType
AX = mybir.AxisListType

D = 1024
DIN = 3080
NE = 32
DE = 512
EPS = 1e-6
KAPPA = 0.25 * (128.0 ** -0.5)


class Res:
    __slots__ = ("name", "w", "rd", "acc")

    def __init__(self, name):
        self.name = name
        self.w = None
        self.rd = []
        self.acc = []


class DmaSem:
    def __init__(self, sem):
        self.sem = sem
        self.count = 0


class Prog:
    ENGS = ("pe", "act", "dve", "pool", "sp")

    def __init__(self, nc, stack):
        self.nc = nc
        self.stack = stack
        self.items = {e: [] for e in self.ENGS}
        self.count = {e: 0 for e in self.ENGS}
        self.sem = {e: stack.enter_context(nc.semaphore("c_" + e)) for e in self.ENGS}
        self.known = {e: {} for e in self.ENGS}
        self.ndma = 0
        self.sync_same_engine = True
        self._frames = []

    def dsem(self, name=None):
        self.ndma += 1
        return DmaSem(self.stack.enter_context(self.nc.semaphore(name or ("d%d" % self.ndma))))

    def _deps(self, eng, reads, writes, accum):
        toks = []
        for r in reads:
            if r.w is not None:
                toks.append(r.w)
            toks.extend(r.acc)
        for w in writes:
            if w.w is not None:
                toks.append(w.w)
            toks.extend(w.rd)
            toks.extend(w.acc)
        for a in accum:
            if a.w is not None:
                toks.append(a.w)
            toks.extend(a.rd)
        waits = []
        kn = self.known[eng]
        own = self.sem[eng]
        best = {}
        for (s, v) in toks:
            if s is own and (eng == "pe" or not self.sync_same_engine):
                continue
            if kn.get(id(s), 0) >= v:
                continue
            if id(s) not in best or best[id(s)][1] < v:
                best[id(s)] = (s, v)
        for (s, v) in best.values():
            kn[id(s)] = v
            waits.append((s, v))
        return waits

    def op(self, eng, fn, reads=(), writes=(), accum=(), dma=None):
        waits = self._deps(eng, reads, writes, accum)
        if dma is None:
            self.count[eng] += 1
            tok = (self.sem[eng], self.count[eng])
            inc = 1
        else:
            for fr in self._frames:
                d = fr["reg"][eng]["dma"].setdefault(id(dma), [dma.sem, dma.count, 0])
                d[2] += 16
            dma.count += 16
            tok = (dma.sem, dma.count)
            inc = 16
        self.items[eng].append((waits, fn, tok[0], inc))
        for r in reads:
            r.rd.append(tok)
        for w in writes:
            w.w = tok
            w.rd = []
            w.acc = []
        for a in accum:
            a.acc.append(tok)
        return tok

    def cond_begin(self, cnt_ap, thresh):
        frame = {"snap": {e: dict(k) for e, k in self.known.items()},
                 "reg": {e: {"start": len(self.items[e]), "cnt0": self.count[e], "dma": {}} for e in self.ENGS},
                 "cond": (cnt_ap, thresh)}
        self._frames.append(frame)

    def cond_end(self):
        frame = self._frames.pop()
        cnt_ap, thresh = frame["cond"]
        for e in self.ENGS:
            r = frame["reg"][e]
            body = self.items[e][r["start"]:]
            n_ops = self.count[e] - r["cnt0"]
            del self.items[e][r["start"]:]
            if body:
                self.items[e].append(("cond", cnt_ap, thresh, body, r["cnt0"], n_ops, [tuple(v) for v in r["dma"].values()]))
        self.known = frame["snap"]

    def wait_all(self, eng, ress):
        waits = self._deps(eng, ress, ress, ())
        self.items[eng].append((waits, None, None, 0))

    def emit(self):
        nc = self.nc
        with nc.Block() as block:
            def run_items(name, handle, items, reg):
                own = self.sem[name]
                for it in items:
                    if it[0] == "cond":
                        _, cnt_ap, thresh, body, cnt0, n_ops, dmas = it
                        handle.reg_load(reg, cnt_ap)
                        with handle.If_lt(reg, thresh + 1):
                            if n_ops:
                                handle.wait_ge(own, cnt0)
                                left = n_ops
                                while left > 0:
                                    k = min(left, 15)
                                    handle.sem_inc(own, k)
                                    left -= k
                            for (dsem_, before, inc) in dmas:
                                handle.wait_ge(dsem_, before)
                                left = inc
                                while left > 0:
                                    k = min(left, 15)
                                    handle.sem_inc(dsem_, k)
                                    left -= k
                        with handle.Else():
                            run_items(name, handle, body, reg)
                        continue
                    (waits, fn, sem, inc) = it
                    for (s_, v) in waits:
                        handle.wait_ge(s_, v)
                    if fn is not None:
                        fn(handle).then_inc(sem, inc)

            def run(name, handle):
                with handle.register("rc_" + name) as reg:
                    run_items(name, handle, self.items[name], reg)

            @block.tensor
            def _(e):
                run("pe", e)

            @block.scalar
            def _(e):
                run("act", e)

            @block.vector
            def _(e):
                run("dve", e)

            @block.gpsimd
            def _(e):
                run("pool", e)

            @block.sync
            def _(e):
                run("sp", e)


def build_program(S=8192, CAP=640, NQ=2, debug=False):
    TT = NQ * 128
    NT = S // TT
    NCH = S // 128
    NB = CAP // 128
    NSLOT = NE * CAP
    NG = NQ * 4
    assert S % TT == 0 and CAP % 256 == 0

    nc = bass.Bass("TRN2", target_bir_lowering=False)
    dt = nc.dram_tensor

    def din(name, shape, dtype=F32):
        return dt(name, list(shape), dtype, kind="ExternalInput").ap()

    x_d = din("x", [S, D])
    win_d = din("w_in", [D, DIN])
    wout_d = din("w_out", [D, D])
    g1_d = din("g1", [128, 8])
    bfm_d = din("bfm", [128, 16])
    btm_d = din("btm", [1, 1032])
    cw_d = din("cw", [128, 4 * 31])
    cb_d = din("cb", [128, 4])
    lng_d = din("lng", [128, 4])
    lnb_d = din("lnb", [128, 4])
    qkw_d = din("qkw", [128, 8 * 4])
    qkb_d = din("qkb", [128, 8])
    mg_d = din("mg", [128, 4])
    g2_d = din("g2", [1, D])
    gf_d = din("gf", [1, D])
    rw_d = din("rw", [D, 36])
    rb_d = din("rb", [1, 36])
    ecap_d = din("ecap", [1, 32])
    wg_d = din("wg", [NE, D, DE])
    wu_d = din("wu", [NE, D, DE])
    wd_d = din("wd", [NE, DE, D])
    ident_d = din("ident", [128, 128])
    triI_d = din("tri_incl", [128, 128])
    triS_d = din("tri_strict", [128, 128])
    out_d = dt("out", [S, D], F32, kind="ExternalOutput").ap()
    x1_d = dt("x1s", [S, D], F32, kind="Internal").ap()
    xs_d = dt("xs", [NSLOT, D], BF16, kind="ExternalOutput" if debug else "Internal").ap()
    ys_d = dt("ys", [NSLOT, D], F32, kind="ExternalOutput" if debug else "Internal").ap()
    wgs_d = dt("wgs", [NE, D, DE], BF16, kind="Internal").ap()
    wus_d = dt("wus", [NE, D, DE], BF16, kind="Internal").ap()
    wds_d = dt("wds", [NE, DE, D], BF16, kind="Internal").ap()
    dbg = {}
    if debug:
        dbg["x1"] = dt("dbg_x1", [S, D], F32, kind="ExternalOutput").ap()
        dbg["idx"] = dt("dbg_idx", [128, NCH * 2], I32, kind="ExternalOutput").ap()
        dbg["gw"] = dt("dbg_gw", [128, NCH * 2], F32, kind="ExternalOutput").ap()
        dbg["yT"] = dt("dbg_yT", [128, 8 * S], F32, kind="ExternalOutput").ap()

    with contextlib.ExitStack() as st:
        P = Prog(nc, st)

        def sb(name, shape, dtype=F32):
            return st.enter_context(nc.sbuf_tensor("s_" + name, list(shape), dtype))

        def mk(eng, fn, reads=(), writes=(), accum=(), dma=None):
            return P.op(eng, fn, reads, writes, accum, dma)

        dumps = {}

        def dump(name, ap, reads):
            if not debug or name in dumps:
                return
            dumps[name] = dt("dmp_" + name, list(ap.shape), ap.dtype, kind="ExternalOutput").ap()
            mk("sp", lambda e: e.dma_start(out=dumps[name], in_=ap), reads=reads, accum=[R_dbg], dma=P.dsem())

        def emit_rsqrt(y, v, t, R):
            mk("dve", lambda e: e.tensor_scalar(out=y.bitcast(I32), in0=v.bitcast(I32), scalar1=-0.5, scalar2=1597463007.0, op0=ALU.mult, op1=ALU.add),
               reads=R, writes=R)
            for _ in range(2):
                mk("dve", lambda e: e.scalar_tensor_tensor(out=t, in0=y, scalar=-0.5, in1=y, op0=ALU.mult, op1=ALU.mult), reads=R, writes=R)
                mk("dve", lambda e: e.tensor_tensor(out=t, in0=t, in1=v, op=ALU.mult), reads=R, writes=R)
                mk("dve", lambda e: e.scalar_tensor_tensor(out=y, in0=t, scalar=1.5, in1=y, op0=ALU.add, op1=ALU.mult), reads=R, writes=R)

        banks = [st.enter_context(nc.psum_tensor("bank%d" % i, [128, 512], F32)) for i in range(8)]
        bank_res = [Res("bank%d" % i) for i in range(8)]
        bank_ctr = [0]

        def getbank():
            i = bank_ctr[0] % 8
            bank_ctr[0] += 1
            return banks[i], bank_res[i]

        def b16(bank):
            return bank[:, :].bitcast(BF16)

        didx = sb("didx", [128, NCH * 2], I32)
        gwt = sb("gwt", [128, NCH * 2], F32)
        R_didx = Res("didx")
        R_gwt = Res("gwt")
        R_xs = Res("xs")
        R_ys = Res("ys")
        R_x1 = Res("x1d")
        R_out = Res("out")
        R_dbg = Res("dbg")
        identb = sb("identb", [128, 128], BF16)
        cnti = sb("cnti", [128, 32], I32)
        ecapsb = sb("ecapsb", [128, 32], F32)
        R_cnt = Res("cnti")
        R_wscr = Res("wscr")
        wcsem = P.dsem("wcast")
        R_const = Res("const")

        ld = P.dsem("ld_const")

        with contextlib.ExitStack() as st2:
            def sb2(name, shape, dtype=F32):
                return st2.enter_context(nc.sbuf_tensor("s_" + name, list(shape), dtype))

            winb = sb2("winb", [128, 8, DIN], BF16)
            woutb = sb2("woutb", [128, 8, D], BF16)
            dconv = sb2("dconv", [128, 4 * 31, 128], BF16)
            dqk = sb2("dqk", [128, 8 * 4, 128], BF16)
            identf = sb2("identf", [128, 128], F32)
            triI = sb2("triI", [128, 128], F32)
            onesf = sb2("onesf", [128, 128], F32)
            maskk = sb2("maskk", [128, 128], F32)
            triSb = sb2("triSb", [128, 128], BF16)
            onesb = sb2("onesb", [128, 128], BF16)
            o512b = sb2("o512b", [128, 128], BF16)
            g1 = sb2("g1", [128, 8])
            bfm = sb2("bfm", [128, 16])
            hbfm = sb2("hbfm", [128, 16])
            btm = sb2("btm", [128, 1032])
            cw = sb2("cw", [128, 124])
            cb = sb2("cb", [128, 4])
            lng = sb2("lng", [128, 4])
            lnb = sb2("lnb", [128, 4])
            hlng = sb2("hlng", [128, 4])
            hlnb = sb2("hlnb", [128, 4])
            qkw = sb2("qkw", [128, 32])
            qkb = sb2("qkb", [128, 8])
            hqkb = sb2("hqkb", [128, 8])
            mg = sb2("mg", [128, 4])
            g2bc = sb2("g2bc", [128, D])
            rwf = sb2("rwf", [128, 8, 36])
            rwb = sb2("rwb", [128, 8, 36], BF16)
            rbbc = sb2("rbbc", [128, 36])
            basep = sb2("basep", [128, 32])
            halfc = sb2("halfc", [128, 1])
            epsc = sb2("epsc", [128, 1])

            xt = [sb2("xt%d" % i, [128, NQ, D]) for i in range(2)]
            R_xt = [Res("xt0"), Res("xt1")]
            xsem = [P.dsem("x0"), P.dsem("x1")]
            hb = sb2("hb", [128, NQ, D], BF16); R_hb = Res("hb")
            h2 = sb2("h2", [128, NQ, D], BF16); R_h2 = Res("h2")
            hT = sb2("hT", [128, 8, TT], BF16); R_hT = Res("hT")
            h2T = sb2("h2T", [128, 8, TT], BF16); R_h2T = Res("h2T")
            ubuf = sb2("ubuf", [128, 4, TT + 32], BF16); R_ubuf = Res("ubuf")
            qkbuf = sb2("qkbuf", [128, 8, TT + 4], BF16); R_qkbuf = Res("qkbuf")
            NTMP = 6
            tmp = [sb2("tmp%d" % i, [128, TT]) for i in range(NTMP)]
            R_tmp = [Res("tmp%d" % i) for i in range(NTMP)]
            tmp_ctr = [0]

            def gettmp():
                i = tmp_ctr[0] % NTMP
                tmp_ctr[0] += 1
                return tmp[i], R_tmp[i]

            vsb = sb2("vsb", [128, NQ, 512]); R_vsb = Res("vsb")
            osb = sb2("osb", [128, NQ, 512]); R_osb = Res("osb")
            gi = sb2("gi", [128, NG]); gfp = sb2("gfp", [128, NG]); R_g = Res("gates")
            gsm = sb2("gsm", [128, 8, NG]); R_gsm = Res("gsm")
            ev = sb2("ev", [128, NG]); fl = sb2("fl", [128, NG]); dec = sb2("dec", [128, NG])
            R_ev = Res("ev")
            cv = sb2("cv", [128, 4, TT]); R_cv = [Res("cv%d" % i) for i in range(4)]
            cvb = sb2("cvb", [128, 4, TT], BF16); R_cvb = Res("cvb")
            sqb = sb2("sqb", [128, 4, TT], BF16); R_sqb = Res("sqb")
            mean_sb = sb2("mean_sb", [128, TT]); rstdt = sb2("rstdt", [128, TT]); msq = sb2("msq", [128, TT])
            R_stats = Res("stats")
            qkT = sb2("qkT", [128, 8, TT], BF16); R_qkT = Res("qkT")
            ktm = sb2("ktm", [128, NQ, 512], BF16); R_ktm = Res("ktm")
            vt = [sb2("vt%d" % i, [128, 4, 129], BF16) for i in range(2)]; R_vt = [Res("vt0"), Res("vt1")]
            stT = [sb2("stT%d" % i, [128, 4, 128], BF16) for i in range(2)]; R_stT = [Res("stT0"), Res("stT1")]
            Chat = sb2("Chat", [128, 4, 129]); Chb = sb2("Chb", [128, 4, 129], BF16)
            R_Chat = Res("Chat"); R_Chb = Res("Chb")
            hm = sb2("hm", [128, 4, 128]); R_hm = Res("hm")
            hmn = sb2("hmn", [128, 4, 128], BF16); R_hmn = Res("hmn")
            sm = sb2("sm", [128, 12, 4]); R_sm = Res("sm")
            junk = sb2("junk", [128, 128]); R_junk = Res("junk")
            yT = sb2("yT", [128, 8, TT], BF16); R_yT = Res("yT")
            ss = sb2("ss", [128, 8, NQ]); R_ss = Res("ssf"); R_ssb = Res("ssb")
            rt = sb2("rt", [128, 16, 36]); R_rt = Res("rt")
            cmbb = sb2("cmbb", [128, 32], BF16)
            x1sem = [P.dsem("x1st0"), P.dsem("x1st1")]
            dbsem = [P.dsem("db0"), P.dsem("db1")]
            scsem = P.dsem("scat")

            ld2 = P.dsem("ld_const_sw")

            def cld(eng, dst, src):
                mk(eng, lambda e: e.dma_start(out=dst, in_=src), accum=[R_const], dma=(ld if eng == "sp" else ld2))

            cld("sp", identf[:, :], ident_d)
            cld("sp", triI[:, :], triI_d)
            cld("sp", g1[:, :], g1_d)
            cld("sp", bfm[:, :], bfm_d)
            cld("sp", btm[:, :], btm_d.partition_broadcast(128))
            cld("sp", cw[:, :], cw_d)
            cld("sp", cb[:, :], cb_d)
            cld("sp", lng[:, :], lng_d)
            cld("sp", lnb[:, :], lnb_d)
            cld("sp", qkw[:, :], qkw_d)
            cld("sp", qkb[:, :], qkb_d)
            cld("sp", mg[:, :], mg_d)
            cld("sp", g2bc[:, :], g2_d.partition_broadcast(128))
            cld("sp", rbbc[:, :], rb_d.partition_broadcast(128))
            R_base = Res("basep")
            mk("sp", lambda e: e.dma_start(out=basep[:, :], in_=ecap_d.partition_broadcast(128)), writes=[R_base], dma=P.dsem("base"))
            cld("sp", rwf[:, :, :], rw_d.rearrange("(k p) n -> p k n", p=128))
            cld("sp", ecapsb[:, :], ecap_d.partition_broadcast(128))
            cld("pool", triSb[:, :], triS_d)
            cld("pool", identb[:, :], ident_d)
            R_c2 = Res("const2")

            def cop(eng, fn):
                mk(eng, fn, reads=[R_const], accum=[R_c2])

            cop("dve", lambda e: e.memset(onesf[:, :], 1.0))
            cop("dve", lambda e: e.memset(onesb[:, :], 1.0))
            cop("dve", lambda e: e.memset(o512b[:, :], 1.0 / 512.0))
            cop("dve", lambda e: e.memset(halfc[:, :], 0.5))
            cop("dve", lambda e: e.memset(epsc[:, :], EPS))
            cop("dve", lambda e: e.tensor_scalar(out=maskk[:, :], in0=triI[:, :], scalar1=KAPPA, scalar2=None, op0=ALU.mult))
            cop("dve", lambda e: e.tensor_scalar(out=hbfm[:, :], in0=bfm[:, :], scalar1=0.5, scalar2=None, op0=ALU.mult))
            cop("dve", lambda e: e.tensor_scalar(out=hlng[:, :], in0=lng[:, :], scalar1=0.5, scalar2=None, op0=ALU.mult))
            cop("dve", lambda e: e.tensor_scalar(out=hlnb[:, :], in0=lnb[:, :], scalar1=0.5, scalar2=None, op0=ALU.mult))
            cop("dve", lambda e: e.tensor_scalar(out=hqkb[:, :], in0=qkb[:, :], scalar1=0.5, scalar2=None, op0=ALU.mult))
            cop("dve", lambda e: e.tensor_copy(out=rwb[:, :, :], in_=rwf[:, :, :]))
            mk("dve", lambda e: e.memset(Chat[:, :, :], 0.0), writes=[R_Chat])
            mk("dve", lambda e: e.memset(Chb[:, :, :], 0.0), writes=[R_Chb])
            mk("dve", lambda e: e.memset(ubuf[:, :, :], 0.0), writes=[R_ubuf])
            mk("dve", lambda e: e.memset(qkbuf[:, :, :], 0.0), writes=[R_qkbuf])
            for jc in range(4):
                cop("dve", lambda e, jc=jc: e.scalar_tensor_tensor(out=dconv[:, jc * 31:(jc + 1) * 31, :], in0=identf[:, :].unsqueeze(1).broadcast_to([128, 31, 128]), scalar=0.5,
                                                                   in1=cw[:, jc * 31:(jc + 1) * 31].unsqueeze(2).broadcast_to([128, 31, 128]), op0=ALU.mult, op1=ALU.mult))
            cop("pool", lambda e: e.tensor_tensor(out=dqk[:, :, :], in0=identf[:, :].unsqueeze(1).broadcast_to([128, 32, 128]),
                                                  in1=qkw[:, :].unsqueeze(2).broadcast_to([128, 32, 128]), op=ALU.mult))
            R_stage = R_xt
            HW = DIN // 2
            for k in range(8):
                for hf in range(2):
                    s = (2 * k + hf) % 2
                    flat = xt[s][:, :, :].rearrange("p q d -> p (q d)")
                    mk("sp", lambda e, k=k, hf=hf, flat=flat: e.dma_start(out=flat[:, 0:HW], in_=win_d[k * 128:(k + 1) * 128, hf * HW:(hf + 1) * HW]),
                       writes=[R_stage[s]], dma=xsem[s])
                    if s == 0:
                        mk("dve", lambda e, k=k, hf=hf, flat=flat: e.tensor_scalar(out=winb[:, k, hf * HW:(hf + 1) * HW], in0=flat[:, 0:HW],
                                                                                  scalar1=g1[:, k:k + 1], scalar2=None, op0=ALU.mult),
                           reads=[R_stage[s], R_const], accum=[R_c2])
                    else:
                        mk("act", lambda e, k=k, hf=hf, flat=flat: e.activation(out=winb[:, k, hf * HW:(hf + 1) * HW], in_=flat[:, 0:HW],
                                                                               func=AF.Identity, scale=g1[:, k:k + 1]),
                           reads=[R_stage[s], R_const], accum=[R_c2])
            for k in range(8):
                s = k % 2
                mk("sp", lambda e, k=k, s=s: e.dma_start(out=xt[s][:, 0, :], in_=wout_d[k * 128:(k + 1) * 128, :]),
                   writes=[R_stage[s]], dma=xsem[s])
                sc = 0.5 if k < 4 else mg[:, k - 4:k - 3]
                if k % 2 == 0:
                    mk("dve", lambda e, k=k, s=s, sc=sc: e.tensor_scalar(out=woutb[:, k, :], in0=xt[s][:, 0, :], scalar1=sc,
                                                                         scalar2=None, op0=ALU.mult),
                       reads=[R_stage[s], R_const], accum=[R_c2])
                else:
                    mk("act", lambda e, k=k, s=s, sc=sc: e.activation(out=woutb[:, k, :], in_=xt[s][:, 0, :], func=AF.Identity, scale=sc),
                       reads=[R_stage[s], R_const], accum=[R_c2])
            RC = [R_const, R_c2]

            def front1(T):
                s = T % 2
                X = xt[s]
                mk("sp", lambda e: e.dma_start(out=X[:, :, :], in_=x_d[T * TT:(T + 1) * TT, :].rearrange("(q p) d -> p q d", p=128)),
                   writes=[R_xt[s]], dma=xsem[s])
                for q in range(NQ):
                    mk("act", lambda e, q=q: e.activation(out=hb[:, q, :], in_=X[:, q, :], func=AF.Square, accum_out=ss[:, 0, q:q + 1]),
                       reads=[R_xt[s]], writes=[R_hb, R_ss])
                mk("dve", lambda e: e.tensor_scalar(out=ss[:, 1, :], in0=ss[:, 0, :], scalar1=1.0 / D, scalar2=EPS, op0=ALU.mult, op1=ALU.add),
                   reads=[R_ss], writes=[R_ss])
                emit_rsqrt(ss[:, 2, :], ss[:, 1, :], ss[:, 3, :], [R_ss])
                for q in range(NQ):
                    mk("act", lambda e, q=q: e.activation(out=hb[:, q, :], in_=X[:, q, :], func=AF.Identity, scale=ss[:, 2, q:q + 1]),
                       reads=[R_xt[s], R_ss], writes=[R_hb])
                for q in range(NQ):
                    bk, rb_ = getbank()
                    for k in range(8):
                        mk("pe", lambda e, q=q, k=k, bk=bk: e.transpose(out=b16(bk)[:, k * 128:(k + 1) * 128], in_=hb[:, q, k * 128:(k + 1) * 128], identity=identb[:, :]),
                           reads=[R_hb] + RC, writes=[rb_])
                    mk("dve", lambda e, q=q, bk=bk: e.tensor_copy(out=hT[:, :, q * 128:(q + 1) * 128], in_=b16(bk)[:, 0:1024].rearrange("p (k t) -> p k t", k=8)),
                       reads=[rb_], writes=[R_hT])

            def front2(T):
                order = [4, 0, 5, 1, 6, 2, 7, 3] + list(range(8, 16))
                thg = {}
                for j in order:
                    bk, rb_ = getbank()
                    for k in range(8):
                        mk("pe", lambda e, j=j, k=k, bk=bk: e.matmul(bk[:, 0:TT], lhsT=winb[:, k, j * 128:(j + 1) * 128], rhs=hT[:, k, :],
                                                                     start=(k == 0), stop=(k == 7)),
                           reads=[R_hT] + RC, writes=[rb_])
                    if 4 <= j < 8:
                        t, rt_ = gettmp()
                        thg[j - 4] = (t, rt_)
                        mk("act", lambda e, j=j, bk=bk, t=t: e.activation(out=t[:, :], in_=bk[:, 0:TT], func=AF.Tanh, scale=0.5, bias=hbfm[:, j:j + 1]),
                           reads=[rb_] + RC, writes=[rt_])
                    elif j < 4:
                        t, rt_ = thg[j]
                        a, ra_ = gettmp()
                        mk("act", lambda e, j=j, bk=bk, a=a: e.activation(out=a[:, :], in_=bk[:, 0:TT], func=AF.Identity, bias=bfm[:, j:j + 1]),
                           reads=[rb_] + RC, writes=[ra_])
                        mk("dve", lambda e, j=j, t=t, a=a: e.scalar_tensor_tensor(out=ubuf[:, j, 30:30 + TT], in0=t[:, :], scalar=1.0, in1=a[:, :],
                                                                                  op0=ALU.add, op1=ALU.mult),
                           reads=[rt_, ra_], writes=[R_ubuf])
                    else:
                        jp = j - 8
                        if jp % 2 == 0:
                            mk("act", lambda e, j=j, jp=jp, bk=bk: e.activation(out=qkbuf[:, jp, 3:3 + TT], in_=bk[:, 0:TT], func=AF.Identity, bias=bfm[:, j:j + 1]),
                               reads=[rb_] + RC, writes=[R_qkbuf])
                        else:
                            mk("dve", lambda e, j=j, jp=jp, bk=bk: e.tensor_scalar(out=qkbuf[:, jp, 3:3 + TT], in0=bk[:, 0:TT], scalar1=bfm[:, j:j + 1], scalar2=None, op0=ALU.add),
                               reads=[rb_] + RC, writes=[R_qkbuf])
                bg, rbg = getbank()
                for q in range(NQ):
                    bv, rbv = getbank()
                    bo, rbo = getbank()
                    for k in range(8):
                        lt = hT[:, k, q * 128:(q + 1) * 128]
                        mk("pe", lambda e, k=k, bv=bv, lt=lt: e.matmul(bv[:, 0:512], lhsT=lt, rhs=winb[:, k, 2048:2560], start=(k == 0), stop=(k == 7)),
                           reads=[R_hT] + RC, writes=[rbv])
                        mk("pe", lambda e, k=k, bo=bo, lt=lt: e.matmul(bo[:, 0:512], lhsT=lt, rhs=winb[:, k, 2560:3072], start=(k == 0), stop=(k == 7)),
                           reads=[R_hT] + RC, writes=[rbo])
                    for k in range(8):
                        lt = hT[:, k, q * 128:(q + 1) * 128]
                        mk("pe", lambda e, k=k, q=q, lt=lt: e.matmul(bg[:, q * 8:(q + 1) * 8], lhsT=lt, rhs=winb[:, k, 3072:3080], start=(k == 0), stop=(k == 7)),
                           reads=[R_hT] + RC, writes=[rbg])
                    mk("dve", lambda e, q=q, bv=bv: e.tensor_tensor(out=vsb[:, q, :], in0=bv[:, 0:512], in1=btm[:, 0:512], op=ALU.add),
                       reads=[rbv] + RC, writes=[R_vsb])
                    mk("dve", lambda e, q=q, bo=bo: e.tensor_tensor(out=osb[:, q, :], in0=bo[:, 0:512], in1=btm[:, 512:1024], op=ALU.add),
                       reads=[rbo] + RC, writes=[R_osb])
                    mk("act", lambda e, q=q: e.activation(out=osb[:, q, :], in_=osb[:, q, :], func=AF.Tanh, scale=0.5),
                       reads=[R_osb], writes=[R_osb])
                    mk("pool", lambda e, q=q: e.tensor_scalar(out=osb[:, q, :], in0=osb[:, q, :], scalar1=0.5, scalar2=0.5, op0=ALU.mult, op1=ALU.add),
                       reads=[R_osb], writes=[R_osb])
                for q in range(NQ):
                    mk("dve", lambda e, q=q: e.tensor_tensor(out=gi[:, q * 4:(q + 1) * 4], in0=bg[:, q * 8:q * 8 + 4], in1=btm[:, 1024:1028], op=ALU.add),
                       reads=[rbg] + RC, writes=[R_g])
                    mk("dve", lambda e, q=q: e.tensor_tensor(out=gfp[:, q * 4:(q + 1) * 4], in0=bg[:, q * 8 + 4:q * 8 + 8], in1=btm[:, 1028:1032], op=ALU.add),
                       reads=[rbg] + RC, writes=[R_g])

            def gates_a(T):
                G = gsm
                mk("act", lambda e: e.activation(out=G[:, 0, :], in_=gfp[:, :], func=AF.Abs),
                   reads=[R_g], writes=[R_gsm])
                mk("act", lambda e: e.activation(out=G[:, 1, :], in_=G[:, 0, :], func=AF.Exp, scale=-1.0),
                   reads=[R_gsm], writes=[R_gsm])
                mk("act", lambda e: e.activation(out=G[:, 2, :], in_=G[:, 1, :], func=AF.Ln, bias=1.0),
                   reads=[R_gsm], writes=[R_gsm])
                mk("dve", lambda e: e.tensor_scalar(out=G[:, 3, :], in0=gfp[:, :], scalar1=0.0, scalar2=None, op0=ALU.min),
                   reads=[R_g], writes=[R_gsm])
                mk("dve", lambda e: e.tensor_tensor(out=G[:, 4, :], in0=G[:, 2, :], in1=G[:, 3, :], op=ALU.subtract),
                   reads=[R_gsm], writes=[R_gsm])
                bk, rb_ = getbank()
                mk("pe", lambda e: e.matmul(bk[:, 0:NG], lhsT=triI[:, :], rhs=G[:, 4, :], start=True, stop=True),
                   reads=[R_gsm] + RC, writes=[rb_])
                mk("pe", lambda e: e.matmul(bk[:, 16:16 + NG], lhsT=onesf[:, :], rhs=G[:, 4, :], start=True, stop=True),
                   reads=[R_gsm] + RC, writes=[rb_])
                mk("dve", lambda e: e.tensor_tensor(out=G[:, 5, :], in0=bk[:, 0:NG], in1=gi[:, :], op=ALU.add),
                   reads=[rb_, R_g], writes=[R_gsm])
                return bk, rb_

            def gates_b(T, bk, rb_):
                G = gsm
                mk("act", lambda e: e.activation(out=ev[:, :], in_=G[:, 5, :], func=AF.Exp),
                   reads=[R_gsm], writes=[R_ev])
                mk("act", lambda e: e.activation(out=fl[:, :], in_=bk[:, 0:NG], func=AF.Exp),
                   reads=[rb_], writes=[R_ev])
                mk("act", lambda e: e.activation(out=dec[:, :], in_=bk[:, 16:16 + NG], func=AF.Exp, scale=-1.0),
                   reads=[rb_], writes=[R_ev])

            def conv_a(T):
                for jc in range(4):
                    bk, rb_ = getbank()
                    for j in range(31):
                        mk("pe", lambda e, jc=jc, j=j, bk=bk: e.matmul(bk[:, 0:TT], lhsT=dconv[:, jc * 31 + j, :], rhs=ubuf[:, jc, j:j + TT],
                                                                       start=(j == 0), stop=(j == 30)),
                           reads=[R_ubuf] + RC, writes=[rb_])
                    mk("act", lambda e, jc=jc, bk=bk: e.activation(out=cv[:, jc, :], in_=bk[:, 0:TT], func=AF.Identity, bias=cb[:, jc:jc + 1]),
                       reads=[rb_] + RC, writes=[R_cv[jc]])
                    mk("pool", lambda e, jc=jc: e.tensor_copy(out=cvb[:, jc, :], in_=cv[:, jc, :]),
                       reads=[R_cv[jc]], writes=[R_cvb])
                    mk("act", lambda e, jc=jc: e.activation(out=sqb[:, jc, :], in_=cv[:, jc, :], func=AF.Square),
                       reads=[R_cv[jc]], writes=[R_sqb])
                mk("pool", lambda e: e.tensor_copy(out=ubuf[:, :, 0:30], in_=ubuf[:, :, TT:TT + 30]),
                   reads=[], writes=[R_ubuf])
                bm, rbm = getbank()
                be, rbe = getbank()
                for jc in range(4):
                    mk("pe", lambda e, jc=jc: e.matmul(bm[:, 0:TT], lhsT=o512b[:, :], rhs=cvb[:, jc, :], start=(jc == 0), stop=(jc == 3)),
                       reads=[R_cvb] + RC, writes=[rbm])
                for jc in range(4):
                    mk("pe", lambda e, jc=jc: e.matmul(be[:, 0:TT], lhsT=o512b[:, :], rhs=sqb[:, jc, :], start=(jc == 0), stop=(jc == 3)),
                       reads=[R_sqb] + RC, writes=[rbe])
                mk("act", lambda e: e.activation(out=mean_sb[:, :], in_=bm[:, 0:TT], func=AF.Identity),
                   reads=[rbm], writes=[R_stats])
                mk("dve", lambda e: e.tensor_tensor(out=msq[:, :], in0=mean_sb[:, :], in1=mean_sb[:, :], op=ALU.mult),
                   reads=[R_stats], writes=[R_stats])
                mk("dve", lambda e: e.tensor_tensor(out=msq[:, :], in0=be[:, 0:TT], in1=msq[:, :], op=ALU.subtract),
                   reads=[rbe, R_stats], writes=[R_stats])

            def conv_ln(T):
                mk("act", lambda e: e.activation(out=rstdt[:, :], in_=msq[:, :], func=AF.Ln, bias=epsc[:, 0:1]),
                   reads=[R_stats] + RC, writes=[R_stats])
                mk("act", lambda e: e.activation(out=rstdt[:, :], in_=rstdt[:, :], func=AF.Exp, scale=-0.5),
                   reads=[R_stats], writes=[R_stats])

            def conv_b(T):
                for jc in range(4):
                    mk("pool", lambda e, jc=jc: e.tensor_tensor(out=cv[:, jc, :], in0=cv[:, jc, :], in1=mean_sb[:, :], op=ALU.subtract),
                       reads=[R_stats, R_cv[jc]], writes=[R_cv[jc]])
                    mk("dve", lambda e, jc=jc: e.tensor_tensor(out=cv[:, jc, :], in0=cv[:, jc, :], in1=rstdt[:, :], op=ALU.mult),
                       reads=[R_stats, R_cv[jc]], writes=[R_cv[jc]])
                    t, rt_ = gettmp()
                    mk("act", lambda e, jc=jc, t=t: e.activation(out=t[:, :], in_=cv[:, jc, :], func=AF.Tanh, scale=hlng[:, jc:jc + 1], bias=hlnb[:, jc:jc + 1]),
                       reads=[R_cv[jc]] + RC, writes=[rt_])
                    mk("act", lambda e, jc=jc: e.activation(out=cv[:, jc, :], in_=cv[:, jc, :], func=AF.Identity, scale=lng[:, jc:jc + 1], bias=lnb[:, jc:jc + 1]),
                       reads=[R_cv[jc]] + RC, writes=[R_cv[jc]])
                    mk("dve", lambda e, jc=jc, t=t: e.scalar_tensor_tensor(out=yT[:, jc, :], in0=t[:, :], scalar=1.0, in1=cv[:, jc, :], op0=ALU.add, op1=ALU.mult),
                       reads=[rt_, R_cv[jc]], writes=[R_yT])

            def tile_qkconv(T):
                for jp in range(8):
                    bk, rb_ = getbank()
                    for j in range(4):
                        mk("pe", lambda e, jp=jp, j=j, bk=bk: e.matmul(bk[:, 0:TT], lhsT=dqk[:, jp * 4 + j, :], rhs=qkbuf[:, jp, j:j + TT],
                                                                       start=(j == 0), stop=(j == 3)),
                           reads=[R_qkbuf] + RC, writes=[rb_])
                    t, rt_ = gettmp()
                    z, rz_ = gettmp()
                    mk("act", lambda e, jp=jp, bk=bk, t=t: e.activation(out=t[:, :], in_=bk[:, 0:TT], func=AF.Tanh, scale=0.5, bias=hqkb[:, jp:jp + 1]),
                       reads=[rb_] + RC, writes=[rt_])
                    mk("act", lambda e, jp=jp, bk=bk, z=z: e.activation(out=z[:, :], in_=bk[:, 0:TT], func=AF.Identity, bias=qkb[:, jp:jp + 1]),
                       reads=[rb_] + RC, writes=[rz_])
                    mk("dve", lambda e, jp=jp, t=t, z=z: e.scalar_tensor_tensor(out=qkT[:, jp, :], in0=t[:, :], scalar=1.0, in1=z[:, :], op0=ALU.add, op1=ALU.mult),
                       reads=[rt_, rz_], writes=[R_qkT])
                mk("pool", lambda e: e.tensor_copy(out=qkbuf[:, :, 0:3], in_=qkbuf[:, :, TT:TT + 3]),
                   reads=[], writes=[R_qkbuf])
                for q in range(NQ):
                    bk, rb_ = getbank()
                    for h in range(4):
                        mk("pe", lambda e, q=q, h=h, bk=bk: e.transpose(out=b16(bk)[:, h * 128:(h + 1) * 128], in_=qkT[:, 4 + h, q * 128:(q + 1) * 128], identity=identb[:, :]),
                           reads=[R_qkT] + RC, writes=[rb_])
                    mk("act", lambda e, q=q, bk=bk: e.activation(out=ktm[:, q, :], in_=b16(bk)[:, 0:512], func=AF.Identity),
                       reads=[rb_], writes=[R_ktm])

            def chunk_mlstm(T, q):
                c = T * NQ + q
                s = c % 2
                V = vt[s]; ST_ = stT[s]
                evq = ev[:, q * 4:(q + 1) * 4]
                cs = slice(q * 128, (q + 1) * 128)
                mk("dve", lambda e: e.tensor_tensor(out=V[:, :, 0:128], in0=vsb[:, q, :].rearrange("p (h d) -> p h d", h=4),
                                                    in1=evq.unsqueeze(2).broadcast_to([128, 4, 128]), op=ALU.mult),
                   reads=[R_vsb, R_ev], writes=[R_vt[s]])
                mk("pool", lambda e: e.tensor_copy(out=V[:, :, 128:129], in_=evq.unsqueeze(2)),
                   reads=[R_ev], writes=[R_vt[s]])
                bs, rbs = getbank()
                for h in range(4):
                    mk("pe", lambda e, h=h: e.matmul(bs[:, h * 128:(h + 1) * 128], lhsT=qkT[:, 4 + h, cs], rhs=qkT[:, h, cs], start=True, stop=True),
                       reads=[R_qkT], writes=[rbs])
                mk("dve", lambda e: e.tensor_tensor(out=ST_[:, :, :], in0=bs[:, 0:512].rearrange("p (h t) -> p h t", h=4),
                                                    in1=maskk[:, :].unsqueeze(1).broadcast_to([128, 4, 128]), op=ALU.mult),
                   reads=[rbs] + RC, writes=[R_stT[s]])
                bo = [getbank(), getbank()]
                for h in range(4):
                    bk, rb_ = bo[h // 2]
                    off = (h % 2) * 129
                    mk("pe", lambda e, h=h, bk=bk, off=off: e.matmul(bk[:, off:off + 129], lhsT=ST_[:, h, :], rhs=V[:, h, :], start=True, stop=False),
                       reads=[R_stT[s], R_vt[s]], writes=[rb_])
                    mk("pe", lambda e, h=h, bk=bk, off=off: e.matmul(bk[:, off:off + 129], lhsT=qkT[:, h, cs], rhs=Chb[:, h, :], start=False, stop=True),
                       reads=[R_qkT, R_Chb] + RC, writes=[rb_])
                bd = [getbank(), getbank()]
                for h in range(4):
                    bk, rb_ = bd[h // 2]
                    off = (h % 2) * 129
                    mk("pe", lambda e, h=h, bk=bk, off=off: e.matmul(bk[:, off:off + 129], lhsT=ktm[:, q, h * 128:(h + 1) * 128], rhs=V[:, h, :], start=True, stop=True),
                       reads=[R_ktm, R_vt[s]], writes=[rb_])
                for p in range(2):
                    bk, rb_ = bd[p]
                    cview = Chat[:, 2 * p:2 * p + 2, :].rearrange("p h c -> p (h c)")
                    mk("dve", lambda e, bk=bk, cview=cview: e.scalar_tensor_tensor(out=cview, in0=bk[:, 0:258], scalar=KAPPA, in1=cview, op0=ALU.mult, op1=ALU.add),
                       reads=[rb_, R_Chat], writes=[R_Chat])
                mk("pool", lambda e: e.tensor_tensor(out=Chat[:, :, :], in0=Chat[:, :, :], in1=dec[:, q * 4:(q + 1) * 4].unsqueeze(2).broadcast_to([128, 4, 129]), op=ALU.mult),
                   reads=[R_Chat, R_ev], writes=[R_Chat])
                mk("pool", lambda e: e.tensor_copy(out=Chb[:, :, :], in_=Chat[:, :, :]),
                   reads=[R_Chat], writes=[R_Chb])
                for p in range(2):
                    bk, rb_ = bo[p]
                    mk("dve", lambda e, p=p, bk=bk: e.tensor_scalar(out=sm[:, 10, 2 * p:2 * p + 2].unsqueeze(2),
                                                                    in0=bk[:, 0:258].rearrange("p (h c) -> p h c", h=2)[:, :, 128:129],
                                                                    scalar1=-1.0, scalar2=None, op0=ALU.mult),
                       reads=[rb_], writes=[R_sm])
                mk("dve", lambda e: e.scalar_tensor_tensor(out=sm[:, 0, :], in0=sm[:, 10, :], scalar=-1.0, in1=sm[:, 10, :], op0=ALU.mult, op1=ALU.max),
                   reads=[R_sm], writes=[R_sm])
                mk("dve", lambda e: e.tensor_tensor(out=sm[:, 0, :], in0=sm[:, 0, :], in1=fl[:, q * 4:(q + 1) * 4], op=ALU.max),
                   reads=[R_sm, R_ev], writes=[R_sm])
                mk("dve", lambda e: e.reciprocal(out=sm[:, 1, :], in_=sm[:, 0, :]), reads=[R_sm], writes=[R_sm])
                for h in range(4):
                    bk, rb_ = bo[h // 2]
                    off = (h % 2) * 129
                    mk("dve", lambda e, h=h, bk=bk, off=off: e.scalar_tensor_tensor(out=hm[:, h, :], in0=bk[:, off:off + 128], scalar=sm[:, 1, h:h + 1],
                                                                                    in1=osb[:, q, h * 128:(h + 1) * 128], op0=ALU.mult, op1=ALU.mult,
                                                                                    accum_out=sm[:, 2, h:h + 1]),
                       reads=[rb_, R_sm, R_osb], writes=[R_hm, R_sm])
                for h in range(4):
                    mk("act", lambda e, h=h: e.activation(out=junk[:, :], in_=hm[:, h, :], func=AF.Square, accum_out=sm[:, 3, h:h + 1]),
                       reads=[R_hm], writes=[R_junk, R_sm])
                mk("dve", lambda e: e.tensor_scalar(out=sm[:, 4, :], in0=sm[:, 2, :], scalar1=1.0 / 128, scalar2=None, op0=ALU.mult),
                   reads=[R_sm], writes=[R_sm])
                mk("dve", lambda e: e.tensor_tensor(out=sm[:, 5, :], in0=sm[:, 4, :], in1=sm[:, 4, :], op=ALU.mult),
                   reads=[R_sm], writes=[R_sm])
                mk("dve", lambda e: e.scalar_tensor_tensor(out=sm[:, 6, :], in0=sm[:, 3, :], scalar=1.0 / 128, in1=sm[:, 5, :], op0=ALU.mult, op1=ALU.subtract),
                   reads=[R_sm], writes=[R_sm])
                mk("dve", lambda e: e.tensor_scalar(out=sm[:, 8, :], in0=sm[:, 6, :], scalar1=EPS, scalar2=None, op0=ALU.add),
                   reads=[R_sm], writes=[R_sm])
                emit_rsqrt(sm[:, 7, :], sm[:, 8, :], sm[:, 9, :], [R_sm])
                for h in range(4):
                    eng = "dve" if h % 2 == 0 else "pool"
                    mk(eng, lambda e, h=h: e.tensor_scalar(out=hmn[:, h, :], in0=hm[:, h, :], scalar1=sm[:, 4, h:h + 1], scalar2=sm[:, 7, h:h + 1],
                                                           op0=ALU.subtract, op1=ALU.mult),
                       reads=[R_hm, R_sm], writes=[R_hmn])
                bk, rb_ = getbank()
                for h in range(4):
                    mk("pe", lambda e, h=h: e.transpose(out=b16(bk)[:, h * 128:(h + 1) * 128], in_=hmn[:, h, :], identity=identb[:, :]),
                       reads=[R_hmn] + RC, writes=[rb_])
                mk("act", lambda e: e.activation(out=yT[:, 4:8, cs], in_=b16(bk)[:, 0:512].rearrange("p (h t) -> p h t", h=4), func=AF.Identity),
                   reads=[rb_], writes=[R_yT])

            def back1(T):
                s = T % 2
                X = xt[s]
                for q in range(NQ):
                    cs = slice(q * 128, (q + 1) * 128)
                    c = T * NQ + q
                    ba, rba = getbank()
                    bb, rbb = getbank()
                    for k in range(8):
                        mk("pe", lambda e, k=k, ba=ba, cs=cs: e.matmul(ba[:, 0:512], lhsT=yT[:, k, cs], rhs=woutb[:, k, 0:512], start=(k == 0), stop=(k == 7)),
                           reads=[R_yT] + RC, writes=[rba])
                        mk("pe", lambda e, k=k, bb=bb, cs=cs: e.matmul(bb[:, 0:512], lhsT=yT[:, k, cs], rhs=woutb[:, k, 512:1024], start=(k == 0), stop=(k == 7)),
                           reads=[R_yT] + RC, writes=[rbb])
                    mk("dve", lambda e, q=q, ba=ba: e.tensor_tensor(out=X[:, q, 0:512], in0=ba[:, 0:512], in1=X[:, q, 0:512], op=ALU.add),
                       reads=[rba, R_xt[s]], writes=[R_xt[s]])
                    mk("dve", lambda e, q=q, bb=bb: e.tensor_tensor(out=X[:, q, 512:1024], in0=bb[:, 0:512], in1=X[:, q, 512:1024], op=ALU.add),
                       reads=[rbb, R_xt[s]], writes=[R_xt[s]])
                    mk("act", lambda e, q=q: e.activation(out=h2[:, q, :], in_=X[:, q, :], func=AF.Square, accum_out=ss[:, 4, q:q + 1]),
                       reads=[R_xt[s]], writes=[R_h2, R_ssb])
                mk("sp", lambda e: e.dma_start(out=x1_d[T * TT:(T + 1) * TT, :].rearrange("(q p) d -> p q d", p=128), in_=X[:, :, :]),
                   reads=[R_xt[s]], accum=[R_x1], dma=x1sem[s])
                if debug:
                    mk("sp", lambda e: e.dma_start(out=dbg["x1"][T * TT:(T + 1) * TT, :].rearrange("(q p) d -> p q d", p=128), in_=X[:, :, :]),
                       reads=[R_xt[s]], accum=[R_dbg], dma=dbsem[s])

            def back2(T):
                s = T % 2
                X = xt[s]
                mk("dve", lambda e: e.tensor_scalar(out=ss[:, 5, :], in0=ss[:, 4, :], scalar1=1.0 / D, scalar2=EPS, op0=ALU.mult, op1=ALU.add),
                   reads=[R_ssb], writes=[R_ssb])
                emit_rsqrt(ss[:, 6, :], ss[:, 5, :], ss[:, 7, :], [R_ssb])
                for q in range(NQ):
                    mk("dve", lambda e, q=q: e.scalar_tensor_tensor(out=h2[:, q, :], in0=X[:, q, :], scalar=ss[:, 6, q:q + 1], in1=g2bc[:, :], op0=ALU.mult, op1=ALU.mult),
                       reads=[R_xt[s], R_ssb] + RC, writes=[R_h2])
                br, rbr = getbank()
                for q in range(NQ):
                    cs = slice(q * 128, (q + 1) * 128)
                    bk, rb_ = getbank()
                    for k in range(8):
                        mk("pe", lambda e, q=q, k=k, bk=bk: e.transpose(out=b16(bk)[:, k * 128:(k + 1) * 128], in_=h2[:, q, k * 128:(k + 1) * 128], identity=identb[:, :]),
                           reads=[R_h2] + RC, writes=[rb_])
                    mk("act", lambda e, q=q, bk=bk, cs=cs: e.activation(out=h2T[:, :, cs], in_=b16(bk)[:, 0:1024].rearrange("p (k t) -> p k t", k=8), func=AF.Identity),
                       reads=[rb_], writes=[R_h2T])
                    for k in range(8):
                        mk("pe", lambda e, q=q, k=k, cs=cs: e.matmul(br[:, q * 36:(q + 1) * 36], lhsT=h2T[:, k, cs], rhs=rwb[:, k, :], start=(k == 0), stop=(k == 7)),
                           reads=[R_h2T] + RC, writes=[rbr])
                for q in range(NQ):
                    c = T * NQ + q
                    lg = rt[:, 0, :]
                    mk("dve", lambda e, q=q, lg=lg: e.tensor_tensor(out=lg, in0=br[:, q * 36:(q + 1) * 36], in1=rbbc[:, :], op=ALU.add),
                       reads=[rbr] + RC, writes=[R_rt])
                    r1 = [R_rt]
                    mk("dve", lambda e: e.tensor_reduce(out=rt[:, 1, 0:1], in_=rt[:, 0, 0:4], axis=AX.X, op=ALU.max), reads=r1, writes=r1)
                    mk("dve", lambda e: e.tensor_scalar(out=rt[:, 2, 0:4], in0=rt[:, 0, 0:4], scalar1=rt[:, 1, 0:1], scalar2=None, op0=ALU.is_equal), reads=r1, writes=r1)
                    mk("dve", lambda e: e.tensor_scalar(out=rt[:, 1, 1:2], in0=rt[:, 1, 0:1], scalar1=-1.0, scalar2=None, op0=ALU.mult), reads=r1, writes=r1)
                    mk("act", lambda e: e.activation(out=rt[:, 3, 0:4], in_=rt[:, 0, 0:4], func=AF.Exp, bias=rt[:, 1, 1:2], accum_out=rt[:, 1, 2:3]), reads=r1, writes=r1)
                    mk("dve", lambda e: e.reciprocal(out=rt[:, 1, 3:4], in_=rt[:, 1, 2:3]), reads=r1, writes=r1)
                    mk("dve", lambda e: e.tensor_scalar(out=rt[:, 4, 0:4], in0=rt[:, 2, 0:4], scalar1=-1.0, scalar2=1e30, op0=ALU.add, op1=ALU.mult), reads=r1, writes=r1)
                    mk("dve", lambda e: e.tensor_tensor(out=rt[:, 5, 0:32].rearrange("p (g k) -> p g k", g=4), in0=rt[:, 0, 4:36].rearrange("p (g k) -> p g k", g=4),
                                                        in1=rt[:, 4, 0:4].unsqueeze(2).broadcast_to([128, 4, 8]), op=ALU.add), reads=r1, writes=r1)
                    mk("dve", lambda e: e.max(out=rt[:, 6, 0:8], in_=rt[:, 5, 0:32]), reads=r1, writes=r1)
                    mk("dve", lambda e: e.tensor_scalar(out=rt[:, 7, 0:32], in0=rt[:, 5, 0:32], scalar1=rt[:, 6, 0:1], scalar2=None, op0=ALU.is_equal), reads=r1, writes=r1)
                    mk("dve", lambda e: e.tensor_scalar(out=rt[:, 8, 0:32], in0=rt[:, 5, 0:32], scalar1=rt[:, 6, 1:2], scalar2=None, op0=ALU.is_equal), reads=r1, writes=r1)
                    mk("dve", lambda e: e.tensor_tensor(out=rt[:, 1, 4:5], in0=rt[:, 6, 1:2], in1=rt[:, 6, 0:1], op=ALU.subtract), reads=r1, writes=r1)
                    mk("act", lambda e: e.activation(out=rt[:, 1, 5:6], in_=rt[:, 1, 4:5], func=AF.Exp), reads=r1, writes=r1)
                    mk("dve", lambda e: e.tensor_scalar(out=rt[:, 1, 6:7], in0=rt[:, 1, 5:6], scalar1=1.0, scalar2=None, op0=ALU.add), reads=r1, writes=r1)
                    mk("dve", lambda e: e.reciprocal(out=rt[:, 1, 7:8], in_=rt[:, 1, 6:7]), reads=r1, writes=r1)
                    mk("dve", lambda e, c=c: e.tensor_tensor(out=gwt[:, 2 * c:2 * c + 1], in0=rt[:, 1, 7:8], in1=rt[:, 1, 3:4], op=ALU.mult), reads=r1, writes=[R_gwt])
                    mk("dve", lambda e, c=c: e.tensor_tensor(out=gwt[:, 2 * c + 1:2 * c + 2], in0=rt[:, 1, 3:4], in1=gwt[:, 2 * c:2 * c + 1], op=ALU.subtract), reads=r1 + [R_gwt], writes=[R_gwt])
                    mk("dve", lambda e: e.tensor_tensor(out=cmbb[:, :], in0=rt[:, 7, 0:32], in1=rt[:, 8, 0:32], op=ALU.add), reads=r1, writes=r1)
                    bk, rb_ = getbank()
                    mk("pe", lambda e, bk=bk: e.matmul(bk[:, 0:32], lhsT=triSb[:, :], rhs=cmbb[:, :], start=True, stop=True), reads=r1 + RC, writes=[rb_])
                    mk("pe", lambda e, bk=bk: e.matmul(bk[:, 32:64], lhsT=onesb[:, :], rhs=cmbb[:, :], start=True, stop=True), reads=r1 + RC, writes=[rb_])
                    mk("dve", lambda e, bk=bk: e.tensor_tensor(out=rt[:, 9, 0:32], in0=bk[:, 0:32], in1=basep[:, :], op=ALU.add), reads=[rb_, R_base] + r1, writes=r1)
                    mk("dve", lambda e: e.scalar_tensor_tensor(out=rt[:, 10, 0:32], in0=rt[:, 7, 0:32], scalar=1.0, in1=rt[:, 9, 0:32], op0=ALU.mult, op1=ALU.mult, accum_out=rt[:, 1, 8:9]), reads=r1, writes=r1)
                    mk("dve", lambda e: e.scalar_tensor_tensor(out=rt[:, 10, 0:32], in0=rt[:, 8, 0:32], scalar=1.0, in1=rt[:, 9, 0:32], op0=ALU.mult, op1=ALU.mult, accum_out=rt[:, 1, 9:10]), reads=r1, writes=r1)
                    mk("dve", lambda e, c=c: e.tensor_scalar(out=didx[:, 2 * c:2 * c + 2], in0=rt[:, 1, 8:10], scalar1=float(NSLOT - 1), scalar2=None, op0=ALU.min), reads=r1, writes=[R_didx])
                    mk("dve", lambda e, bk=bk: e.tensor_tensor(out=basep[:, :], in0=basep[:, :], in1=bk[:, 32:64], op=ALU.add), reads=[rb_], writes=[R_base])
                    for j in range(2):
                        mk("pool", lambda e, q=q, c=c, j=j: e.indirect_dma_start(out=xs_d, out_offset=bass.IndirectOffsetOnAxis(ap=didx[:, 2 * c + j:2 * c + j + 1], axis=0),
                                                                                 in_=h2[:, q, :], in_offset=None),
                           reads=[R_h2, R_didx], accum=[R_xs], dma=scsem)

            def dumps0():
                dump("winb", winb[:, 0, :], RC)
                dump("woutb", woutb[:, 4, :], RC)
                dump("dconv", dconv[:, 0:4, :], RC)
                dump("ss", ss[:, :, :], [R_ss])
                dump("hb", hb[:, :, :], [R_hb])
                dump("hT", hT[:, :, :], [R_hT])

            front1(0)
            if debug:
                dumps0()
            epT = -(-NE // NT)
            for T in range(NT):
                for e_ in range(T * epT, min(NE, (T + 1) * epT)):
                    for (dst_, src_) in ((wgs_d, wg_d), (wus_d, wu_d), (wds_d, wd_d)):
                        mk("pool", lambda e, dst_=dst_, src_=src_, e_=e_: e.dma_start(out=dst_[e_], in_=src_[e_]), accum=[R_wscr], dma=wcsem)
                front2(T)
                if T == 0:
                    dump("ubuf", ubuf[:, :, :], [R_ubuf])
                    dump("qkbuf", qkbuf[:, :, :], [R_qkbuf])
                    dump("vsb", vsb[:, :, :], [R_vsb])
                    dump("osb", osb[:, :, :], [R_osb])
                    dump("gi", gi[:, :], [R_g])
                    dump("gfp", gfp[:, :], [R_g])
                if T >= 1:
                    back2(T - 1)
                if T + 1 < NT:
                    front1(T + 1)
                conv_a(T)
                gbk = gates_a(T)
                conv_ln(T)
                gates_b(T, *gbk)
                if T == 0:
                    dump("gsm", gsm[:, :, :], [R_gsm])
                    dump("ev", ev[:, :], [R_ev])
                    dump("fl", fl[:, :], [R_ev])
                    dump("dec", dec[:, :], [R_ev])
                tile_qkconv(T)
                conv_b(T)
                if T == 0:
                    dump("cv", cv[:, :, :], R_cv)
                    dump("rstdt", rstdt[:, :], [R_stats])
                    dump("mean", mean_sb[:, :], [R_stats])
                    dump("qkT", qkT[:, :, :], [R_qkT])
                    dump("ktm", ktm[:, :, :], [R_ktm])
                for q in range(NQ):
                    chunk_mlstm(T, q)
                    if T == 0 and q == 0:
                        dump("vt", vt[0][:, :, :], [R_vt[0]])
                        dump("stT", stT[0][:, :, :], [R_stT[0]])
                        dump("Chat", Chat[:, :, :], [R_Chat])
                        dump("hm", hm[:, :, :], [R_hm])
                        dump("hmn", hmn[:, :, :], [R_hmn])
                        dump("sm", sm[:, :, :], [R_sm])
                if debug and S <= 1024:
                    for k in range(8):
                        t, rt_ = gettmp()
                        mk("dve", lambda e, k=k, t=t: e.tensor_copy(out=t[:, :], in_=yT[:, k, :]), reads=[R_yT], writes=[rt_])
                        mk("sp", lambda e, k=k, t=t, T=T: e.dma_start(out=dbg["yT"][:, k * S + T * TT:k * S + (T + 1) * TT], in_=t[:, :]), reads=[rt_], accum=[R_dbg], dma=P.dsem())
                back1(T)
            back2(NT - 1)

            mk("dve", lambda e: e.tensor_tensor(out=cnti[:, :], in0=basep[:, :], in1=ecapsb[:, :], op=ALU.subtract),
               reads=[R_base, R_const], writes=[R_cnt])
            allres = [R_xt[0], R_xt[1], R_hb, R_h2, R_hT, R_h2T, R_ubuf, R_qkbuf, R_vsb, R_osb, R_g, R_gsm, R_ev, R_cvb, R_sqb, R_stats,
                      R_qkT, R_ktm, R_vt[0], R_vt[1], R_stT[0], R_stT[1], R_Chat, R_Chb, R_hm, R_hmn, R_sm, R_junk, R_yT, R_ss, R_ssb, R_rt,
                      R_const, R_c2, R_base, R_cnt] + R_cv + R_tmp + bank_res
            R_phase = Res("phase")
            for eng in ("pe", "act", "dve", "pool", "sp"):
                P.wait_all(eng, allres)

        with contextlib.ExitStack() as st3:
            def sb3(name, shape, dtype=F32):
                return st3.enter_context(nc.sbuf_tensor("s_" + name, list(shape), dtype))

            wgb = [sb3("wgb%d" % i, [128, 8, DE], BF16) for i in range(2)]
            wub = [sb3("wub%d" % i, [128, 8, DE], BF16) for i in range(2)]
            wdb = [sb3("wdb%d" % i, [128, 4, D], BF16) for i in range(2)]
            R_w = [Res("w0"), Res("w1")]
            wsem = [P.dsem("w0"), P.dsem("w1")]
            NXG = 4
            xg = [sb3("xg%d" % i, [128, D], BF16) for i in range(NXG)]
            R_xg = [Res("xg%d" % i) for i in range(NXG)]
            xgsem = [P.dsem("xg%d" % i) for i in range(NXG)]
            xTb = [sb3("xTb%d" % i, [128, 8, 128], BF16) for i in range(2)]
            R_xTb = [Res("xTb0"), Res("xTb1")]
            actTb = [sb3("actTb%d" % i, [128, 4, 128], BF16) for i in range(2)]
            R_actTb = [Res("actTb0"), Res("actTb1")]
            sil = [sb3("sil%d" % i, [128, 512]) for i in range(2)]
            R_sil = [Res("sil0"), Res("sil1")]
            actm = [sb3("actm%d" % i, [128, 512], BF16) for i in range(2)]
            R_actm = [Res("actm0"), Res("actm1")]
            yo = [sb3("yo%d" % i, [128, D]) for i in range(3)]
            R_yo = [Res("yo%d" % i) for i in range(3)]
            yosem = [P.dsem("yo%d" % i) for i in range(3)]

            def load_w(e_):
                s = e_ % 2
                mk("sp", lambda e: e.dma_start(out=wgb[s][:, :, :], in_=wgs_d[e_].rearrange("(k p) n -> p k n", p=128)),
                   reads=[R_wscr], accum=[R_w[s]], dma=wsem[s])
                mk("sp", lambda e: e.dma_start(out=wub[s][:, :, :], in_=wus_d[e_].rearrange("(k p) n -> p k n", p=128)),
                   reads=[R_wscr], accum=[R_w[s]], dma=wsem[s])
                mk("sp", lambda e: e.dma_start(out=wdb[s][:, :, :], in_=wds_d[e_].rearrange("(k p) n -> p k n", p=128)),
                   reads=[R_wscr], accum=[R_w[s]], dma=wsem[s])

            blk_ctr = [0]

            def stage1(e_, b, i):
                s = e_ % 2
                gi_ = i % NXG
                t2 = i % 2
                yi = i % 3
                row0 = e_ * CAP + b * 128
                XT = xTb[t2]
                AT = actTb[t2]
                mk("sp", lambda e: e.dma_start(out=xg[gi_][:, :], in_=xs_d[row0:row0 + 128, :]),
                   reads=[R_xs], writes=[R_xg[gi_]], dma=xgsem[gi_])
                bk, rb_ = getbank()
                for k in range(8):
                    mk("pe", lambda e, k=k: e.transpose(out=b16(bk)[:, k * 128:(k + 1) * 128], in_=xg[gi_][:, k * 128:(k + 1) * 128], identity=identb[:, :]),
                       reads=[R_xg[gi_], R_const], writes=[rb_])
                mk("dve", lambda e: e.tensor_copy(out=XT[:, :, :], in_=b16(bk)[:, 0:1024].rearrange("p (k t) -> p k t", k=8)),
                   reads=[rb_], writes=[R_xTb[t2]])

            def stage1b(e_, b, i):
                s = e_ % 2
                t2 = i % 2
                XT = xTb[t2]
                bg_, rbg_ = getbank()
                bu_, rbu_ = getbank()
                for k in range(8):
                    mk("pe", lambda e, k=k: e.matmul(bg_[:, 0:512], lhsT=XT[:, k, :], rhs=wgb[s][:, k, :], start=(k == 0), stop=(k == 7)),
                       reads=[R_w[s], R_xTb[t2]], writes=[rbg_])
                for k in range(8):
                    mk("pe", lambda e, k=k: e.matmul(bu_[:, 0:512], lhsT=XT[:, k, :], rhs=wub[s][:, k, :], start=(k == 0), stop=(k == 7)),
                       reads=[R_w[s], R_xTb[t2]], writes=[rbu_])
                mk("act", lambda e: e.activation(out=sil[t2][:, :], in_=bg_[:, 0:512], func=AF.Silu),
                   reads=[rbg_], writes=[R_sil[t2]])
                mk("dve", lambda e: e.tensor_tensor(out=actm[t2][:, :], in0=sil[t2][:, :], in1=bu_[:, 0:512], op=ALU.mult),
                   reads=[R_sil[t2], rbu_], writes=[R_actm[t2]])

            def stage2(e_, b, i):
                s = e_ % 2
                gi_ = i % NXG
                t2 = i % 2
                yi = i % 3
                row0 = e_ * CAP + b * 128
                XT = xTb[t2]
                AT = actTb[t2]
                bt_, rbt_ = getbank()
                for j in range(4):
                    mk("pe", lambda e, j=j: e.transpose(out=b16(bt_)[:, j * 128:(j + 1) * 128], in_=actm[t2][:, j * 128:(j + 1) * 128], identity=identb[:, :]),
                       reads=[R_actm[t2], R_const], writes=[rbt_])
                mk("dve", lambda e: e.tensor_copy(out=AT[:, :, :].rearrange("p j t -> p (j t)"), in_=b16(bt_)[:, 0:512]),
                   reads=[rbt_], writes=[R_actTb[t2]])

            def stage2b(e_, b, i):
                s = e_ % 2
                t2 = i % 2
                yi = i % 3
                row0 = e_ * CAP + b * 128
                AT = actTb[t2]
                ba, rba = getbank()
                bb, rbb = getbank()
                for j in range(4):
                    mk("pe", lambda e, j=j: e.matmul(ba[:, 0:512], lhsT=AT[:, j, :], rhs=wdb[s][:, j, 0:512], start=(j == 0), stop=(j == 3)),
                       reads=[R_w[s], R_actTb[t2]], writes=[rba])
                    mk("pe", lambda e, j=j: e.matmul(bb[:, 0:512], lhsT=AT[:, j, :], rhs=wdb[s][:, j, 512:1024], start=(j == 0), stop=(j == 3)),
                       reads=[R_w[s], R_actTb[t2]], writes=[rbb])
                mk("dve", lambda e: e.tensor_copy(out=yo[yi][:, 0:512], in_=ba[:, 0:512]),
                   reads=[rba], writes=[R_yo[yi]])
                mk("dve", lambda e: e.tensor_copy(out=yo[yi][:, 512:1024], in_=bb[:, 0:512]),
                   reads=[rbb], writes=[R_yo[yi]])
                mk("pool", lambda e: e.dma_start(out=ys_d[row0:row0 + 128, :], in_=yo[yi][:, :]),
                   reads=[R_yo[yi]], accum=[R_ys], dma=yosem[yi])

            def pair(e_, bp):
                i0 = blk_ctr[0]
                blk_ctr[0] += 2
                stage1(e_, 2 * bp, i0)
                stage1(e_, 2 * bp + 1, i0 + 1)
                stage1b(e_, 2 * bp, i0)
                stage1b(e_, 2 * bp + 1, i0 + 1)
                stage2(e_, 2 * bp, i0)
                stage2(e_, 2 * bp + 1, i0 + 1)
                stage2b(e_, 2 * bp, i0)
                stage2b(e_, 2 * bp + 1, i0 + 1)

            load_w(0)
            NP = NB // 2
            NPM = min(NP, 4)
            for e_ in range(NE):
                if e_ + 1 < NE:
                    load_w(e_ + 1)
                for bp in range(NPM):
                    P.cond_begin(cnti[0:1, e_:e_ + 1], bp * 256)
                    pair(e_, bp)
                for bp in range(NPM):
                    P.cond_end()
            if NP > NPM:
                for e_ in range(NE):
                    P.cond_begin(cnti[0:1, e_:e_ + 1], NPM * 256)
                    load_w(e_)
                    pair(e_, NPM)
                    for bp in range(NPM + 1, NP):
                        P.cond_begin(cnti[0:1, e_:e_ + 1], bp * 256)
                        pair(e_, bp)
                    for bp in range(NPM + 1, NP):
                        P.cond_end()
                    P.cond_end()

            finC = [R_w[0], R_w[1]] + R_xg + R_xTb + R_actTb + R_actm + R_sil + R_yo + bank_res
            for eng in ("sp", "pe", "act", "dve", "pool"):
                P.wait_all(eng, finC)

        with contextlib.ExitStack() as st4:
            def sb4(name, shape, dtype=F32):
                return st4.enter_context(nc.sbuf_tensor("s_" + name, list(shape), dtype))

            GD = 4
            NSL = 2 * GD
            gfbc = sb4("gfbc", [128, D])
            R_gf = Res("gfbc")
            mk("sp", lambda e: e.dma_start(out=gfbc[:, :], in_=gf_d.partition_broadcast(128)), writes=[R_gf], dma=P.dsem("gf"))
            xa = [sb4("xa%d" % i, [128, D]) for i in range(NSL)]
            ya = [sb4("ya%d" % i, [128, D]) for i in range(NSL)]
            yb = [sb4("yb%d" % i, [128, D]) for i in range(NSL)]
            ob = [sb4("ob%d" % i, [128, D]) for i in range(2)]
            R_xa = [Res("xa%d" % i) for i in range(NSL)]; R_ya = [Res("ya%d" % i) for i in range(NSL)]; R_yb = [Res("yb%d" % i) for i in range(NSL)]
            R_ob = [Res("ob0"), Res("ob1")]
            xasem = [P.dsem() for _ in range(NSL)]; yasem = [P.dsem() for _ in range(NSL)]; ybsem = [P.dsem() for _ in range(NSL)]; obsem = [P.dsem(), P.dsem()]
            fs = sb4("fs", [128, 4, NCH]); R_fs = Res("fs")
            jk = sb4("jk", [128, D], BF16); R_jk = Res("jk")

            def d_load(c):
                s = c % NSL
                mk("sp", lambda e: e.dma_start(out=xa[s][:, :], in_=x1_d[c * 128:(c + 1) * 128, :]), reads=[R_x1], writes=[R_xa[s]], dma=xasem[s])
                mk("pool", lambda e: e.indirect_dma_start(out=ya[s][:, :], out_offset=None, in_=ys_d,
                                                          in_offset=bass.IndirectOffsetOnAxis(ap=didx[:, 2 * c:2 * c + 1], axis=0)),
                   reads=[R_ys, R_didx], writes=[R_ya[s]], dma=yasem[s])
                mk("pool", lambda e: e.indirect_dma_start(out=yb[s][:, :], out_offset=None, in_=ys_d,
                                                          in_offset=bass.IndirectOffsetOnAxis(ap=didx[:, 2 * c + 1:2 * c + 2], axis=0)),
                   reads=[R_ys, R_didx], writes=[R_yb[s]], dma=ybsem[s])

            def d_combine(c):
                s = c % NSL
                mk("dve", lambda e: e.scalar_tensor_tensor(out=xa[s][:, :], in0=ya[s][:, :], scalar=gwt[:, 2 * c:2 * c + 1], in1=xa[s][:, :], op0=ALU.mult, op1=ALU.add),
                   reads=[R_ya[s], R_xa[s], R_gwt], writes=[R_xa[s]])
                mk("dve", lambda e: e.scalar_tensor_tensor(out=xa[s][:, :], in0=yb[s][:, :], scalar=gwt[:, 2 * c + 1:2 * c + 2], in1=xa[s][:, :], op0=ALU.mult, op1=ALU.add),
                   reads=[R_yb[s], R_xa[s], R_gwt], writes=[R_xa[s]])
                mk("act", lambda e: e.activation(out=jk[:, :], in_=xa[s][:, :], func=AF.Square, accum_out=fs[:, 0, c:c + 1]),
                   reads=[R_xa[s]], writes=[R_jk, R_fs])

            def d_finish(c):
                s = c % NSL
                o = c % 2
                mk("act", lambda e: e.activation(out=xa[s][:, :], in_=xa[s][:, :], func=AF.Identity, scale=fs[:, 2, c:c + 1]),
                   reads=[R_xa[s], R_fs], writes=[R_xa[s]])
                mk("pool", lambda e: e.tensor_tensor(out=ob[o][:, :], in0=xa[s][:, :], in1=gfbc[:, :], op=ALU.mult),
                   reads=[R_xa[s], R_gf], writes=[R_ob[o]])
                mk("sp", lambda e: e.dma_start(out=out_d[c * 128:(c + 1) * 128, :], in_=ob[o][:, :]),
                   reads=[R_ob[o]], accum=[R_out], dma=obsem[o])

            NGD = NCH // GD
            for c in range(min(GD, NCH)):
                d_load(c)
            for g in range(NGD):
                if g + 1 < NGD:
                    for c in range((g + 1) * GD, (g + 2) * GD):
                        d_load(c)
                cs_ = slice(g * GD, (g + 1) * GD)
                for c in range(g * GD, (g + 1) * GD):
                    d_combine(c)
                mk("dve", lambda e, cs_=cs_: e.tensor_scalar(out=fs[:, 1, cs_], in0=fs[:, 0, cs_], scalar1=1.0 / D, scalar2=EPS, op0=ALU.mult, op1=ALU.add),
                   reads=[R_fs], writes=[R_fs])
                emit_rsqrt(fs[:, 2, cs_], fs[:, 1, cs_], fs[:, 3, cs_], [R_fs])
                for c in range(g * GD, (g + 1) * GD):
                    d_finish(c)
            if debug:
                mk("sp", lambda e: e.dma_start(out=dbg["idx"], in_=didx[:, :]), reads=[R_didx], accum=[R_dbg], dma=obsem[0])
                mk("sp", lambda e: e.dma_start(out=dbg["gw"], in_=gwt[:, :]), reads=[R_gwt], accum=[R_dbg], dma=obsem[1])
            fin = [R_out, R_dbg, R_x1, R_xs, R_ys, R_wscr, R_didx, R_gwt, R_fs, R_jk, R_gf] + R_xa + R_ya + R_yb + R_ob + bank_res
            for eng in ("sp", "pe", "act", "dve", "pool"):
                P.wait_all(eng, fin)

        P.emit()
    return nc


def make_shared(inp, CAP):
    f = np.float32

    def a(v):
        return np.ascontiguousarray(v, dtype=f)

    def pk(vec, n):
        return a(np.asarray(vec).reshape(n, 128).T)

    b_in = np.asarray(inp["b_in"][0])
    sh = {
        "w_in": a(inp["w_in"][0]),
        "w_out": a(inp["w_out"][0]),
        "g1": pk(inp["norm_mix_g"][0], 8),
        "bfm": pk(b_in[0:2048], 16),
        "btm": a(b_in[2048:3080].reshape(1, 1032)),
        "cw": a(np.asarray(inp["conv_w"][0]).reshape(31, 4, 128).transpose(2, 1, 0).reshape(128, 124)),
        "cb": pk(inp["conv_b"][0], 4),
        "lng": pk(inp["conv_ln_g"][0], 4),
        "lnb": pk(inp["conv_ln_b"][0], 4),
        "qkw": a(np.asarray(inp["qk_conv_w"][0]).reshape(4, 8, 128).transpose(2, 1, 0).reshape(128, 32)),
        "qkb": pk(inp["qk_conv_b"][0], 8),
        "mg": pk(inp["mlstm_norm_g"][0], 4),
        "g2": a(np.asarray(inp["norm_ffn_g"][0]).reshape(1, D)),
        "gf": a(np.asarray(inp["final_norm_g"]).reshape(1, D)),
        "rw": a(np.concatenate([inp["router_group_w"][0], inp["router_expert_w"][0]], axis=1)),
        "rb": a(np.concatenate([inp["router_group_b"][0], inp["router_expert_b"][0]]).reshape(1, 36)),
        "ecap": a((np.arange(32) * CAP).reshape(1, 32)),
        "wg": a(inp["expert_w_gate"][0]),
        "wu": a(inp["expert_w_up"][0]),
        "wd": a(inp["expert_w_down"][0]),
        "ident": a(np.eye(128)),
        "tri_incl": a(np.triu(np.ones((128, 128)))),
        "tri_strict": a(np.triu(np.ones((128, 128)), 1)),
    }
    return sh


_CACHE = {}


def kernel(**inputs):
    x = np.asarray(inputs["x"], dtype=np.float32)
    B, S, _ = x.shape
    CAP = 1536
    key = (S, CAP)
    if key not in _CACHE:
        _CACHE[key] = build_program(S=S, CAP=CAP, NQ=2)
    nc = _CACHE[key]
    sh = make_shared(inputs, CAP)
    in_maps = []
    for b in range(B):
        m = dict(sh)
        m["x"] = np.ascontiguousarray(x[b])
        in_maps.append(m)
    res = run_bass_kernel_spmd(nc, in_maps, core_ids=list(range(B)))
    return np.stack([np.asarray(r["out"]) for r in res.results], axis=0).astype(np.float32)
```

```python
import contextlib
import numpy as np
import concourse.bass as bass
import concourse.mybir as mybir
from concourse.bass_utils import run_bass_kernel_spmd
from concourse.alu_op_type import AluOpType as ALU

F32 = mybir.dt.float32
BF16 = mybir.dt.bfloat16
I32 = mybir.dt.int32
AF = mybir.ActivationFunctionType
AX = mybir.AxisListType

D = 1024
DIN = 3080
NE = 32
DE = 512
EPS = 1e-6
KAPPA = 0.25 * (128.0 ** -0.5)


class Res:
    __slots__ = ("name", "w", "rd", "acc")

    def __init__(self, name):
        self.name = name
        self.w = None
        self.rd = []
        self.acc = []


class DmaSem:
    def __init__(self, sem):
        self.sem = sem
        self.count = 0


class Prog:
    ENGS = ("pe", "act", "dve", "pool", "sp")

    def __init__(self, nc, stack):
        self.nc = nc
        self.stack = stack
        self.items = {e: [] for e in self.ENGS}
        self.count = {e: 0 for e in self.ENGS}
        self.sem = {e: stack.enter_context(nc.semaphore("c_" + e)) for e in self.ENGS}
        self.known = {e: {} for e in self.ENGS}
        self.ndma = 0
        self.sync_same_engine = True
        self._frames = []

    def dsem(self, name=None):
        self.ndma += 1
        return DmaSem(self.stack.enter_context(self.nc.semaphore(name or ("d%d" % self.ndma))))

    def _deps(self, eng, reads, writes, accum):
        toks = []
        for r in reads:
            if r.w is not None:
                toks.append(r.w)
            toks.extend(r.acc)
        for w in writes:
            if w.w is not None:
                toks.append(w.w)
            toks.extend(w.rd)
            toks.extend(w.acc)
        for a in accum:
            if a.w is not None:
                toks.append(a.w)
            toks.extend(a.rd)
        waits = []
        kn = self.known[eng]
        own = self.sem[eng]
        best = {}
        for (s, v) in toks:
            if s is own and (eng == "pe" or not self.sync_same_engine):
                continue
            if kn.get(id(s), 0) >= v:
                continue
            if id(s) not in best or best[id(s)][1] < v:
                best[id(s)] = (s, v)
        for (s, v) in best.values():
            kn[id(s)] = v
            waits.append((s, v))
        return waits

    def op(self, eng, fn, reads=(), writes=(), accum=(), dma=None):
        waits = self._deps(eng, reads, writes, accum)
        if dma is None:
            self.count[eng] += 1
            tok = (self.sem[eng], self.count[eng])
            inc = 1
        else:
            for fr in self._frames:
                d = fr["reg"][eng]["dma"].setdefault(id(dma), [dma.sem, dma.count, 0])
                d[2] += 16
            dma.count += 16
            tok = (dma.sem, dma.count)
            inc = 16
        self.items[eng].append((waits, fn, tok[0], inc))
        for r in reads:
            r.rd.append(tok)
        for w in writes:
            w.w = tok
            w.rd = []
            w.acc = []
        for a in accum:
            a.acc.append(tok)
        return tok

    def cond_begin(self, cnt_ap, thresh):
        frame = {"snap": {e: dict(k) for e, k in self.known.items()},
                 "reg": {e: {"start": len(self.items[e]), "cnt0": self.count[e], "dma": {}} for e in self.ENGS},
                 "cond": (cnt_ap, thresh)}
        self._frames.append(frame)

    def cond_end(self):
        frame = self._frames.pop()
        cnt_ap, thresh = frame["cond"]
        for e in self.ENGS:
            r = frame["reg"][e]
            body = self.items[e][r["start"]:]
            n_ops = self.count[e] - r["cnt0"]
            del self.items[e][r["start"]:]
            if body:
                self.items[e].append(("cond", cnt_ap, thresh, body, r["cnt0"], n_ops, [tuple(v) for v in r["dma"].values()]))
        self.known = frame["snap"]

    def wait_all(self, eng, ress):
        waits = self._deps(eng, ress, ress, ())
        self.items[eng].append((waits, None, None, 0))

    def emit(self):
        nc = self.nc
        with nc.Block() as block:
            def run_items(name, handle, items, reg):
                own = self.sem[name]
                for it in items:
                    if it[0] == "cond":
                        _, cnt_ap, thresh, body, cnt0, n_ops, dmas = it
                        handle.reg_load(reg, cnt_ap)
                        with handle.If_lt(reg, thresh + 1):
                            if n_ops:
                                handle.wait_ge(own, cnt0)
                                left = n_ops
                                while left > 0:
                                    k = min(left, 15)
                                    handle.sem_inc(own, k)
                                    left -= k
                            for (dsem_, before, inc) in dmas:
                                handle.wait_ge(dsem_, before)
                                left = inc
                                while left > 0:
                                    k = min(left, 15)
                                    handle.sem_inc(dsem_, k)
                                    left -= k
                        with handle.Else():
                            run_items(name, handle, body, reg)
                        continue
                    (waits, fn, sem, inc) = it
                    for (s_, v) in waits:
                        handle.wait_ge(s_, v)
                    if fn is not None:
                        fn(handle).then_inc(sem, inc)

            def run(name, handle):
                with handle.register("rc_" + name) as reg:
                    run_items(name, handle, self.items[name], reg)

            @block.tensor
            def _(e):
                run("pe", e)

            @block.scalar
            def _(e):
                run("act", e)

            @block.vector
            def _(e):
                run("dve", e)

            @block.gpsimd
            def _(e):
                run("pool", e)

            @block.sync
            def _(e):
                run("sp", e)


def build_program(S=8192, CAP=640, NQ=2, debug=False):
    TT = NQ * 128
    NT = S // TT
    NCH = S // 128
    NB = CAP // 128
    NSLOT = NE * CAP
    NG = NQ * 4
    assert S % TT == 0 and CAP % 256 == 0

    nc = bass.Bass("TRN2", target_bir_lowering=False)
    dt = nc.dram_tensor

    def din(name, shape, dtype=F32):
        return dt(name, list(shape), dtype, kind="ExternalInput").ap()

    x_d = din("x", [S, D])
    win_d = din("w_in", [D, DIN])
    wout_d = din("w_out", [D, D])
    g1_d = din("g1", [128, 8])
    bfm_d = din("bfm", [128, 16])
    btm_d = din("btm", [1, 1032])
    cw_d = din("cw", [128, 4 * 31])
    cb_d = din("cb", [128, 4])
    lng_d = din("lng", [128, 4])
    lnb_d = din("lnb", [128, 4])
    qkw_d = din("qkw", [128, 8 * 4])
    qkb_d = din("qkb", [128, 8])
    mg_d = din("mg", [128, 4])
    g2_d = din("g2", [1, D])
    gf_d = din("gf", [1, D])
    rw_d = din("rw", [D, 36])
    rb_d = din("rb", [1, 36])
    ecap_d = din("ecap", [1, 32])
    wg_d = din("wg", [NE, D, DE])
    wu_d = din("wu", [NE, D, DE])
    wd_d = din("wd", [NE, DE, D])
    ident_d = din("ident", [128, 128])
    triI_d = din("tri_incl", [128, 128])
    triS_d = din("tri_strict", [128, 128])
    out_d = dt("out", [S, D], F32, kind="ExternalOutput").ap()
    x1_d = dt("x1s", [S, D], F32, kind="Internal").ap()
    xs_d = dt("xs", [NSLOT, D], BF16, kind="ExternalOutput" if debug else "Internal").ap()
    ys_d = dt("ys", [NSLOT, D], F32, kind="ExternalOutput" if debug else "Internal").ap()
    wgs_d = dt("wgs", [NE, D, DE], BF16, kind="Internal").ap()
    wus_d = dt("wus", [NE, D, DE], BF16, kind="Internal").ap()
    wds_d = dt("wds", [NE, DE, D], BF16, kind="Internal").ap()
    dbg = {}
    if debug:
        dbg["x1"] = dt("dbg_x1", [S, D], F32, kind="ExternalOutput").ap()
        dbg["idx"] = dt("dbg_idx", [128, NCH * 2], I32, kind="ExternalOutput").ap()
        dbg["gw"] = dt("dbg_gw", [128, NCH * 2], F32, kind="ExternalOutput").ap()
        dbg["yT"] = dt("dbg_yT", [128, 8 * S], F32, kind="ExternalOutput").ap()

    with contextlib.ExitStack() as st:
        P = Prog(nc, st)

        def sb(name, shape, dtype=F32):
            return st.enter_context(nc.sbuf_tensor("s_" + name, list(shape), dtype))

        def mk(eng, fn, reads=(), writes=(), accum=(), dma=None):
            return P.op(eng, fn, reads, writes, accum, dma)

        dumps = {}

        def dump(name, ap, reads):
            if not debug or name in dumps:
                return
            dumps[name] = dt("dmp_" + name, list(ap.shape), ap.dtype, kind="ExternalOutput").ap()
            mk("sp", lambda e: e.dma_start(out=dumps[name], in_=ap), reads=reads, accum=[R_dbg], dma=P.dsem())

        def emit_rsqrt(y, v, t, R):
            mk("dve", lambda e: e.tensor_scalar(out=y.bitcast(I32), in0=v.bitcast(I32), scalar1=-0.5, scalar2=1597463007.0, op0=ALU.mult, op1=ALU.add),
               reads=R, writes=R)
            for _ in range(2):
                mk("dve", lambda e: e.scalar_tensor_tensor(out=t, in0=y, scalar=-0.5, in1=y, op0=ALU.mult, op1=ALU.mult), reads=R, writes=R)
                mk("dve", lambda e: e.tensor_tensor(out=t, in0=t, in1=v, op=ALU.mult), reads=R, writes=R)
                mk("dve", lambda e: e.scalar_tensor_tensor(out=y, in0=t, scalar=1.5, in1=y, op0=ALU.add, op1=ALU.mult), reads=R, writes=R)

        banks = [st.enter_context(nc.psum_tensor("bank%d" % i, [128, 512], F32)) for i in range(8)]
        bank_res = [Res("bank%d" % i) for i in range(8)]
        bank_ctr = [0]

        pool_ctr = {"m": 0, "f": 0}

        def getbank(pool=None):
            if pool == "m":
                i = pool_ctr["m"] % 5
                pool_ctr["m"] += 1
            elif pool == "f":
                i = 5 + pool_ctr["f"] % 3
                pool_ctr["f"] += 1
            else:
                i = bank_ctr[0] % 8
                bank_ctr[0] += 1
            return banks[i], bank_res[i]

        def b16(bank):
            return bank[:, :].bitcast(BF16)

        didx = sb("didx", [128, NCH * 2], I32)
        gwt = sb("gwt", [128, NCH * 2], F32)
        R_didx = Res("didx")
        R_gwt = Res("gwt")
        R_xs = Res("xs")
        R_ys = Res("ys")
        R_x1 = Res("x1d")
        R_out = Res("out")
        R_dbg = Res("dbg")
        identb = sb("identb", [128, 128], BF16)
        cnti = sb("cnti", [128, 32], I32)
        ecapsb = sb("ecapsb", [128, 32], F32)
        R_cnt = Res("cnti")
        R_wscr = Res("wscr")
        wcsem = P.dsem("wcast")
        R_const = Res("const")

        ld = P.dsem("ld_const")

        with contextlib.ExitStack() as st2:
            def sb2(name, shape, dtype=F32):
                return st2.enter_context(nc.sbuf_tensor("s_" + name, list(shape), dtype))

            winb = sb2("winb", [128, 8, DIN], BF16)
            woutb = sb2("woutb", [128, 8, D], BF16)
            dconv = sb2("dconv", [128, 4 * 31, 128], BF16)
            dqk = sb2("dqk", [128, 8 * 4, 128], BF16)
            identf = sb2("identf", [128, 128], F32)
            triI = sb2("triI", [128, 128], F32)
            onesf = sb2("onesf", [128, 128], F32)
            maskk = sb2("maskk", [128, 128], F32)
            triSb = sb2("triSb", [128, 128], BF16)
            onesb = sb2("onesb", [128, 128], BF16)
            o512b = sb2("o512b", [128, 128], BF16)
            g1 = sb2("g1", [128, 8])
            bfm = sb2("bfm", [128, 16])
            hbfm = sb2("hbfm", [128, 16])
            btm = sb2("btm", [128, 1032])
            cw = sb2("cw", [128, 124])
            cb = sb2("cb", [128, 4])
            lng = sb2("lng", [128, 4])
            lnb = sb2("lnb", [128, 4])
            hlng = sb2("hlng", [128, 4])
            hlnb = sb2("hlnb", [128, 4])
            qkw = sb2("qkw", [128, 32])
            qkb = sb2("qkb", [128, 8])
            hqkb = sb2("hqkb", [128, 8])
            mg = sb2("mg", [128, 4])
            g2bc = sb2("g2bc", [128, D])
            rwf = sb2("rwf", [128, 8, 36])
            rwb = sb2("rwb", [128, 8, 36], BF16)
            rbbc = sb2("rbbc", [128, 36])
            basep = sb2("basep", [128, 32])
            halfc = sb2("halfc", [128, 1])
            epsc = sb2("epsc", [128, 1])

            xt = [sb2("xt%d" % i, [128, NQ, D]) for i in range(2)]
            R_xt = [Res("xt0"), Res("xt1")]
            xsem = [P.dsem("x0"), P.dsem("x1")]
            hb = sb2("hb", [128, NQ, D], BF16); R_hb = Res("hb")
            h2 = sb2("h2", [128, NQ, D], BF16); R_h2 = Res("h2")
            hT = sb2("hT", [128, 8, TT], BF16); R_hT = Res("hT")
            h2T = hT; R_h2T = R_hT
            ubuf = sb2("ubuf", [128, 4, TT + 32], BF16); R_ubuf = Res("ubuf")
            qkbuf = sb2("qkbuf", [128, 8, TT + 4], BF16); R_qkbuf = Res("qkbuf")
            NTMP = 6
            tmp = [sb2("tmp%d" % i, [128, TT]) for i in range(NTMP)]
            R_tmp = [Res("tmp%d" % i) for i in range(NTMP)]
            tmp_ctr = [0]

            def gettmp():
                i = tmp_ctr[0] % NTMP
                tmp_ctr[0] += 1
                return tmp[i], R_tmp[i]

            vsb = sb2("vsb", [128, NQ, 512]); R_vsb = Res("vsb")
            osb = sb2("osb", [128, NQ, 512]); R_osb = Res("osb")
            gi = sb2("gi", [128, NG]); gfp = sb2("gfp", [128, NG]); R_g = Res("gates")
            gsm = sb2("gsm", [128, 8, NG]); R_gsm = Res("gsm")
            ev = sb2("ev", [128, NG]); fl = sb2("fl", [128, NG]); dec = sb2("dec", [128, NG])
            R_ev = Res("ev")
            cv = sb2("cv", [128, 4, TT]); R_cv = [Res("cv%d" % i) for i in range(4)]
            cvb = sb2("cvb", [128, 4, TT], BF16); R_cvb = Res("cvb")
            sqb = sb2("sqb", [128, 4, TT], BF16); R_sqb = Res("sqb")
            mean_sb = sb2("mean_sb", [128, TT]); rstdt = sb2("rstdt", [128, TT]); msq = sb2("msq", [128, TT])
            R_stats = Res("stats")
            qkT = sb2("qkT", [128, 8, TT], BF16); R_qkT = Res("qkT")
            ktm = sb2("ktm", [128, NQ, 512], BF16); R_ktm = Res("ktm")
            vt = [sb2("vt%d" % i, [128, 4, 129], BF16) for i in range(2)]; R_vt = [Res("vt0"), Res("vt1")]
            stT = [sb2("stT%d" % i, [128, 4, 128], BF16) for i in range(2)]; R_stT = [Res("stT0"), Res("stT1")]
            Chat = sb2("Chat", [128, 4, 129]); Chb2 = [sb2("Chb%d" % i, [128, 4, 129], BF16) for i in range(2)]
            R_Chat = Res("Chat"); R_Chb2 = [Res("Chb0"), Res("Chb1")]
            hm2 = [sb2("hm%d" % i, [128, 4, 128]) for i in range(2)]; R_hm2 = [Res("hm0"), Res("hm1")]
            hmn2 = [sb2("hmn%d" % i, [128, 4, 128], BF16) for i in range(2)]; R_hmn2 = [Res("hmn0"), Res("hmn1")]
            sm2 = [sb2("sm%d" % i, [128, 12, 4]) for i in range(2)]; R_sm2 = [Res("sm0"), Res("sm1")]
            junk = sb2("junk", [128, 128]); R_junk = Res("junk")
            yT = sb2("yT", [128, 8, TT], BF16); R_yT = Res("yT")
            ss = sb2("ss", [128, 8, NQ]); R_ss = Res("ssf"); R_ssb = Res("ssb")
            rt = sb2("rt", [128, 11, 36]); R_rt = Res("rt")
            cmbb = sb2("cmbb", [128, 32], BF16)
            x1sem = [P.dsem("x1st0"), P.dsem("x1st1")]
            dbsem = [P.dsem("db0"), P.dsem("db1")]
            scsem = P.dsem("scat")

            ld2 = P.dsem("ld_const_sw")

            def cld(eng, dst, src):
                mk(eng, lambda e: e.dma_start(out=dst, in_=src), accum=[R_const], dma=(ld if eng == "sp" else ld2))

            cld("sp", identf[:, :], ident_d)
            cld("sp", triI[:, :], triI_d)
            cld("sp", g1[:, :], g1_d)
            cld("sp", bfm[:, :], bfm_d)
            cld("sp", btm[:, :], btm_d.partition_broadcast(128))
            cld("sp", cw[:, :], cw_d)
            cld("sp", cb[:, :], cb_d)
            cld("sp", lng[:, :], lng_d)
            cld("sp", lnb[:, :], lnb_d)
            cld("sp", qkw[:, :], qkw_d)
            cld("sp", qkb[:, :], qkb_d)
            cld("sp", mg[:, :], mg_d)
            cld("sp", g2bc[:, :], g2_d.partition_broadcast(128))
            cld("sp", rbbc[:, :], rb_d.partition_broadcast(128))
            R_base = Res("basep")
            mk("sp", lambda e: e.dma_start(out=basep[:, :], in_=ecap_d.partition_broadcast(128)), writes=[R_base], dma=P.dsem("base"))
            cld("sp", rwf[:, :, :], rw_d.rearrange("(k p) n -> p k n", p=128))
            cld("sp", ecapsb[:, :], ecap_d.partition_broadcast(128))
            cld("pool", triSb[:, :], triS_d)
            cld("pool", identb[:, :], ident_d)
            R_c2 = Res("const2")

            def cop(eng, fn):
                mk(eng, fn, reads=[R_const], accum=[R_c2])

            cop("dve", lambda e: e.memset(onesf[:, :], 1.0))
            cop("dve", lambda e: e.memset(onesb[:, :], 1.0))
            cop("dve", lambda e: e.memset(o512b[:, :], 1.0 / 512.0))
            cop("dve", lambda e: e.memset(halfc[:, :], 0.5))
            cop("dve", lambda e: e.memset(epsc[:, :], EPS))
            cop("dve", lambda e: e.tensor_scalar(out=maskk[:, :], in0=triI[:, :], scalar1=KAPPA, scalar2=None, op0=ALU.mult))
            cop("dve", lambda e: e.tensor_scalar(out=hbfm[:, :], in0=bfm[:, :], scalar1=0.5, scalar2=None, op0=ALU.mult))
            cop("dve", lambda e: e.tensor_scalar(out=hlng[:, :], in0=lng[:, :], scalar1=0.5, scalar2=None, op0=ALU.mult))
            cop("dve", lambda e: e.tensor_scalar(out=hlnb[:, :], in0=lnb[:, :], scalar1=0.5, scalar2=None, op0=ALU.mult))
            cop("dve", lambda e: e.tensor_scalar(out=hqkb[:, :], in0=qkb[:, :], scalar1=0.5, scalar2=None, op0=ALU.mult))
            cop("dve", lambda e: e.tensor_copy(out=rwb[:, :, :], in_=rwf[:, :, :]))
            mk("dve", lambda e: e.memset(Chat[:, :, :], 0.0), writes=[R_Chat])
            mk("dve", lambda e: e.memset(Chb2[0][:, :, :], 0.0), writes=[R_Chb2[0]])
            mk("dve", lambda e: e.memset(Chb2[1][:, :, :], 0.0), writes=[R_Chb2[1]])
            mk("dve", lambda e: e.memset(ubuf[:, :, :], 0.0), writes=[R_ubuf])
            mk("dve", lambda e: e.memset(qkbuf[:, :, :], 0.0), writes=[R_qkbuf])
            for jc in range(4):
                cop("dve", lambda e, jc=jc: e.scalar_tensor_tensor(out=dconv[:, jc * 31:(jc + 1) * 31, :], in0=identf[:, :].unsqueeze(1).broadcast_to([128, 31, 128]), scalar=0.5,
                                                                   in1=cw[:, jc * 31:(jc + 1) * 31].unsqueeze(2).broadcast_to([128, 31, 128]), op0=ALU.mult, op1=ALU.mult))
            cop("pool", lambda e: e.tensor_tensor(out=dqk[:, :, :], in0=identf[:, :].unsqueeze(1).broadcast_to([128, 32, 128]),
                                                  in1=qkw[:, :].unsqueeze(2).broadcast_to([128, 32, 128]), op=ALU.mult))
            R_stage = R_xt
            HW = DIN // 2
            for k in range(8):
                for hf in range(2):
                    s = (2 * k + hf) % 2
                    flat = xt[s][:, :, :].rearrange("p q d -> p (q d)")
                    mk("sp", lambda e, k=k, hf=hf, flat=flat: e.dma_start(out=flat[:, 0:HW], in_=win_d[k * 128:(k + 1) * 128, hf * HW:(hf + 1) * HW]),
                       writes=[R_stage[s]], dma=xsem[s])
                    if s == 0:
                        mk("dve", lambda e, k=k, hf=hf, flat=flat: e.tensor_scalar(out=winb[:, k, hf * HW:(hf + 1) * HW], in0=flat[:, 0:HW],
                                                                                  scalar1=g1[:, k:k + 1], scalar2=None, op0=ALU.mult),
                           reads=[R_stage[s], R_const], accum=[R_c2])
                    else:
                        mk("act", lambda e, k=k, hf=hf, flat=flat: e.activation(out=winb[:, k, hf * HW:(hf + 1) * HW], in_=flat[:, 0:HW],
                                                                               func=AF.Identity, scale=g1[:, k:k + 1]),
                           reads=[R_stage[s], R_const], accum=[R_c2])
            for k in range(8):
                s = k % 2
                mk("sp", lambda e, k=k, s=s: e.dma_start(out=xt[s][:, 0, :], in_=wout_d[k * 128:(k + 1) * 128, :]),
                   writes=[R_stage[s]], dma=xsem[s])
                sc = 0.5 if k < 4 else mg[:, k - 4:k - 3]
                if k % 2 == 0:
                    mk("dve", lambda e, k=k, s=s, sc=sc: e.tensor_scalar(out=woutb[:, k, :], in0=xt[s][:, 0, :], scalar1=sc,
                                                                         scalar2=None, op0=ALU.mult),
                       reads=[R_stage[s], R_const], accum=[R_c2])
                else:
                    mk("act", lambda e, k=k, s=s, sc=sc: e.activation(out=woutb[:, k, :], in_=xt[s][:, 0, :], func=AF.Identity, scale=sc),
                       reads=[R_stage[s], R_const], accum=[R_c2])
            RC = [R_const, R_c2]

            def front1(T):
                s = T % 2
                X = xt[s]
                mk("sp", lambda e: e.dma_start(out=X[:, :, :], in_=x_d[T * TT:(T + 1) * TT, :].rearrange("(q p) d -> p q d", p=128)),
                   writes=[R_xt[s]], dma=xsem[s])
                for q in range(NQ):
                    mk("act", lambda e, q=q: e.activation(out=hb[:, q, :], in_=X[:, q, :], func=AF.Square, accum_out=ss[:, 0, q:q + 1]),
                       reads=[R_xt[s]], writes=[R_hb, R_ss])
                mk("dve", lambda e: e.tensor_scalar(out=ss[:, 1, :], in0=ss[:, 0, :], scalar1=1.0 / D, scalar2=EPS, op0=ALU.mult, op1=ALU.add),
                   reads=[R_ss], writes=[R_ss])
                emit_rsqrt(ss[:, 2, :], ss[:, 1, :], ss[:, 3, :], [R_ss])
                for q in range(NQ):
                    mk("act", lambda e, q=q: e.activation(out=hb[:, q, :], in_=X[:, q, :], func=AF.Identity, scale=ss[:, 2, q:q + 1]),
                       reads=[R_xt[s], R_ss], writes=[R_hb])
                for q in range(NQ):
                    bk, rb_ = getbank()
                    for k in range(8):
                        mk("pe", lambda e, q=q, k=k, bk=bk: e.transpose(out=b16(bk)[:, k * 128:(k + 1) * 128], in_=hb[:, q, k * 128:(k + 1) * 128], identity=identb[:, :]),
                           reads=[R_hb] + RC, writes=[rb_])
                    mk("dve", lambda e, q=q, bk=bk: e.tensor_copy(out=hT[:, :, q * 128:(q + 1) * 128], in_=b16(bk)[:, 0:1024].rearrange("p (k t) -> p k t", k=8)),
                       reads=[rb_], writes=[R_hT])

            def front2_fm(T, pool=None):
                order = [4, 0, 5, 1, 6, 2, 7, 3] + list(range(8, 16))
                thg = {}
                for j in order:
                    if j != order[0]:
                        yield
                    bk, rb_ = getbank(pool)
                    for k in range(8):
                        mk("pe", lambda e, j=j, k=k, bk=bk: e.matmul(bk[:, 0:TT], lhsT=winb[:, k, j * 128:(j + 1) * 128], rhs=hT[:, k, :],
                                                                     start=(k == 0), stop=(k == 7)),
                           reads=[R_hT] + RC, writes=[rb_])
                    if 4 <= j < 8:
                        t, rt_ = gettmp()
                        thg[j - 4] = (t, rt_)
                        mk("act", lambda e, j=j, bk=bk, t=t: e.activation(out=t[:, :], in_=bk[:, 0:TT], func=AF.Tanh, scale=0.5, bias=hbfm[:, j:j + 1]),
                           reads=[rb_] + RC, writes=[rt_])
                    elif j < 4:
                        t, rt_ = thg[j]
                        a, ra_ = gettmp()
                        mk("act", lambda e, j=j, bk=bk, a=a: e.activation(out=a[:, :], in_=bk[:, 0:TT], func=AF.Identity, bias=bfm[:, j:j + 1]),
                           reads=[rb_] + RC, writes=[ra_])
                        mk("dve", lambda e, j=j, t=t, a=a: e.scalar_tensor_tensor(out=ubuf[:, j, 30:30 + TT], in0=t[:, :], scalar=1.0, in1=a[:, :],
                                                                                  op0=ALU.add, op1=ALU.mult),
                           reads=[rt_, ra_], writes=[R_ubuf])
                    else:
                        jp = j - 8
                        if True:
                            mk("act", lambda e, j=j, jp=jp, bk=bk: e.activation(out=qkbuf[:, jp, 3:3 + TT], in_=bk[:, 0:TT], func=AF.Identity, bias=bfm[:, j:j + 1]),
                               reads=[rb_] + RC, writes=[R_qkbuf])
                        else:
                            mk("dve", lambda e, j=j, jp=jp, bk=bk: e.tensor_scalar(out=qkbuf[:, jp, 3:3 + TT], in0=bk[:, 0:TT], scalar1=bfm[:, j:j + 1], scalar2=None, op0=ALU.add),
                               reads=[rb_] + RC, writes=[R_qkbuf])
                yield

            def front2_tm(T):
                bg, rbg = getbank()
                for q in range(NQ):
                    bv, rbv = getbank()
                    bo, rbo = getbank()
                    for k in range(8):
                        lt = hT[:, k, q * 128:(q + 1) * 128]
                        mk("pe", lambda e, k=k, bv=bv, lt=lt: e.matmul(bv[:, 0:512], lhsT=lt, rhs=winb[:, k, 2048:2560], start=(k == 0), stop=(k == 7)),
                           reads=[R_hT] + RC, writes=[rbv])
                        mk("pe", lambda e, k=k, bo=bo, lt=lt: e.matmul(bo[:, 0:512], lhsT=lt, rhs=winb[:, k, 2560:3072], start=(k == 0), stop=(k == 7)),
                           reads=[R_hT] + RC, writes=[rbo])
                    for k in range(8):
                        lt = hT[:, k, q * 128:(q + 1) * 128]
                        mk("pe", lambda e, k=k, q=q, lt=lt: e.matmul(bg[:, q * 8:(q + 1) * 8], lhsT=lt, rhs=winb[:, k, 3072:3080], start=(k == 0), stop=(k == 7)),
                           reads=[R_hT] + RC, writes=[rbg])
                    mk("dve", lambda e, q=q, bv=bv: e.tensor_tensor(out=vsb[:, q, :], in0=bv[:, 0:512], in1=btm[:, 0:512], op=ALU.add),
                       reads=[rbv] + RC, writes=[R_vsb])
                    mk("dve", lambda e, q=q, bo=bo: e.tensor_tensor(out=osb[:, q, :], in0=bo[:, 0:512], in1=btm[:, 512:1024], op=ALU.add),
                       reads=[rbo] + RC, writes=[R_osb])
                    mk("act", lambda e, q=q: e.activation(out=osb[:, q, :], in_=osb[:, q, :], func=AF.Tanh, scale=0.5),
                       reads=[R_osb], writes=[R_osb])
                    mk("pool", lambda e, q=q: e.tensor_scalar(out=osb[:, q, :], in0=osb[:, q, :], scalar1=0.5, scalar2=0.5, op0=ALU.mult, op1=ALU.add),
                       reads=[R_osb], writes=[R_osb])
                for q in range(NQ):
                    mk("dve", lambda e, q=q: e.tensor_tensor(out=gi[:, q * 4:(q + 1) * 4], in0=bg[:, q * 8:q * 8 + 4], in1=btm[:, 1024:1028], op=ALU.add),
                       reads=[rbg] + RC, writes=[R_g])
                    mk("dve", lambda e, q=q: e.tensor_tensor(out=gfp[:, q * 4:(q + 1) * 4], in0=bg[:, q * 8 + 4:q * 8 + 8], in1=btm[:, 1028:1032], op=ALU.add),
                       reads=[rbg] + RC, writes=[R_g])

            def gates_a(T):
                G = gsm
                mk("act", lambda e: e.activation(out=G[:, 0, :], in_=gfp[:, :], func=AF.Abs),
                   reads=[R_g], writes=[R_gsm])
                mk("act", lambda e: e.activation(out=G[:, 1, :], in_=G[:, 0, :], func=AF.Exp, scale=-1.0),
                   reads=[R_gsm], writes=[R_gsm])
                mk("act", lambda e: e.activation(out=G[:, 2, :], in_=G[:, 1, :], func=AF.Ln, bias=1.0),
                   reads=[R_gsm], writes=[R_gsm])
                mk("dve", lambda e: e.tensor_scalar(out=G[:, 3, :], in0=gfp[:, :], scalar1=0.0, scalar2=None, op0=ALU.min),
                   reads=[R_g], writes=[R_gsm])
                mk("dve", lambda e: e.tensor_tensor(out=G[:, 4, :], in0=G[:, 2, :], in1=G[:, 3, :], op=ALU.subtract),
                   reads=[R_gsm], writes=[R_gsm])
                bk, rb_ = getbank()
                mk("pe", lambda e: e.matmul(bk[:, 0:NG], lhsT=triI[:, :], rhs=G[:, 4, :], start=True, stop=True),
                   reads=[R_gsm] + RC, writes=[rb_])
                mk("pe", lambda e: e.matmul(bk[:, 16:16 + NG], lhsT=onesf[:, :], rhs=G[:, 4, :], start=True, stop=True),
                   reads=[R_gsm] + RC, writes=[rb_])
                mk("dve", lambda e: e.tensor_tensor(out=G[:, 5, :], in0=bk[:, 0:NG], in1=gi[:, :], op=ALU.add),
                   reads=[rb_, R_g], writes=[R_gsm])
                return bk, rb_

            def gates_b(T, bk, rb_):
                G = gsm
                mk("act", lambda e: e.activation(out=ev[:, :], in_=G[:, 5, :], func=AF.Exp),
                   reads=[R_gsm], writes=[R_ev])
                mk("act", lambda e: e.activation(out=fl[:, :], in_=bk[:, 0:NG], func=AF.Exp),
                   reads=[rb_], writes=[R_ev])
                mk("act", lambda e: e.activation(out=dec[:, :], in_=bk[:, 16:16 + NG], func=AF.Exp, scale=-1.0),
                   reads=[rb_], writes=[R_ev])

            def conv_a(T, pool=None):
                for jc in range(4):
                    if jc:
                        yield
                    bk, rb_ = getbank(pool)
                    for j in range(31):
                        mk("pe", lambda e, jc=jc, j=j, bk=bk: e.matmul(bk[:, 0:TT], lhsT=dconv[:, jc * 31 + j, :], rhs=ubuf[:, jc, j:j + TT],
                                                                       start=(j == 0), stop=(j == 30)),
                           reads=[R_ubuf] + RC, writes=[rb_])
                    mk("act", lambda e, jc=jc, bk=bk: e.activation(out=cv[:, jc, :], in_=bk[:, 0:TT], func=AF.Identity, bias=cb[:, jc:jc + 1]),
                       reads=[rb_] + RC, writes=[R_cv[jc]])
                    mk("pool", lambda e, jc=jc: e.tensor_copy(out=cvb[:, jc, :], in_=cv[:, jc, :]),
                       reads=[R_cv[jc]], writes=[R_cvb])
                    mk("act", lambda e, jc=jc: e.activation(out=sqb[:, jc, :], in_=cv[:, jc, :], func=AF.Square),
                       reads=[R_cv[jc]], writes=[R_sqb])
                mk("pool", lambda e: e.tensor_copy(out=ubuf[:, :, 0:30], in_=ubuf[:, :, TT:TT + 30]),
                   reads=[], writes=[R_ubuf])
                yield
                bm, rbm = getbank(pool)
                be, rbe = getbank(pool)
                for jc in range(4):
                    mk("pe", lambda e, jc=jc: e.matmul(bm[:, 0:TT], lhsT=o512b[:, :], rhs=cvb[:, jc, :], start=(jc == 0), stop=(jc == 3)),
                       reads=[R_cvb] + RC, writes=[rbm])
                for jc in range(4):
                    mk("pe", lambda e, jc=jc: e.matmul(be[:, 0:TT], lhsT=o512b[:, :], rhs=sqb[:, jc, :], start=(jc == 0), stop=(jc == 3)),
                       reads=[R_sqb] + RC, writes=[rbe])
                mk("act", lambda e: e.activation(out=mean_sb[:, :], in_=bm[:, 0:TT], func=AF.Identity),
                   reads=[rbm], writes=[R_stats])
                mk("dve", lambda e: e.tensor_tensor(out=msq[:, :], in0=mean_sb[:, :], in1=mean_sb[:, :], op=ALU.mult),
                   reads=[R_stats], writes=[R_stats])
                mk("dve", lambda e: e.tensor_tensor(out=msq[:, :], in0=be[:, 0:TT], in1=msq[:, :], op=ALU.subtract),
                   reads=[rbe, R_stats], writes=[R_stats])
                yield

            def conv_ln(T):
                mk("act", lambda e: e.activation(out=rstdt[:, :], in_=msq[:, :], func=AF.Ln, bias=epsc[:, 0:1]),
                   reads=[R_stats] + RC, writes=[R_stats])
                mk("act", lambda e: e.activation(out=rstdt[:, :], in_=rstdt[:, :], func=AF.Exp, scale=-0.5),
                   reads=[R_stats], writes=[R_stats])

            def conv_b(T):
                for jc in range(4):
                    mk("pool", lambda e, jc=jc: e.tensor_tensor(out=cv[:, jc, :], in0=cv[:, jc, :], in1=mean_sb[:, :], op=ALU.subtract),
                       reads=[R_stats, R_cv[jc]], writes=[R_cv[jc]])
                    mk("dve", lambda e, jc=jc: e.tensor_tensor(out=cv[:, jc, :], in0=cv[:, jc, :], in1=rstdt[:, :], op=ALU.mult),
                       reads=[R_stats, R_cv[jc]], writes=[R_cv[jc]])
                    t, rt_ = gettmp()
                    mk("act", lambda e, jc=jc, t=t: e.activation(out=t[:, :], in_=cv[:, jc, :], func=AF.Tanh, scale=hlng[:, jc:jc + 1], bias=hlnb[:, jc:jc + 1]),
                       reads=[R_cv[jc]] + RC, writes=[rt_])
                    mk("act", lambda e, jc=jc: e.activation(out=cv[:, jc, :], in_=cv[:, jc, :], func=AF.Identity, scale=lng[:, jc:jc + 1], bias=lnb[:, jc:jc + 1]),
                       reads=[R_cv[jc]] + RC, writes=[R_cv[jc]])
                    mk("dve", lambda e, jc=jc, t=t: e.scalar_tensor_tensor(out=yT[:, jc, :], in0=t[:, :], scalar=1.0, in1=cv[:, jc, :], op0=ALU.add, op1=ALU.mult),
                       reads=[rt_, R_cv[jc]], writes=[R_yT])

            def tile_qkconv(T):
                for jp in range(8):
                    bk, rb_ = getbank()
                    for j in range(4):
                        mk("pe", lambda e, jp=jp, j=j, bk=bk: e.matmul(bk[:, 0:TT], lhsT=dqk[:, jp * 4 + j, :], rhs=qkbuf[:, jp, j:j + TT],
                                                                       start=(j == 0), stop=(j == 3)),
                           reads=[R_qkbuf] + RC, writes=[rb_])
                    t, rt_ = gettmp()
                    z, rz_ = gettmp()
                    mk("act", lambda e, jp=jp, bk=bk, t=t: e.activation(out=t[:, :], in_=bk[:, 0:TT], func=AF.Tanh, scale=0.5, bias=hqkb[:, jp:jp + 1]),
                       reads=[rb_] + RC, writes=[rt_])
                    mk("act", lambda e, jp=jp, bk=bk, z=z: e.activation(out=z[:, :], in_=bk[:, 0:TT], func=AF.Identity, bias=qkb[:, jp:jp + 1]),
                       reads=[rb_] + RC, writes=[rz_])
                    mk("dve", lambda e, jp=jp, t=t, z=z: e.scalar_tensor_tensor(out=qkT[:, jp, :], in0=t[:, :], scalar=1.0, in1=z[:, :], op0=ALU.add, op1=ALU.mult),
                       reads=[rt_, rz_], writes=[R_qkT])
                mk("pool", lambda e: e.tensor_copy(out=qkbuf[:, :, 0:3], in_=qkbuf[:, :, TT:TT + 3]),
                   reads=[], writes=[R_qkbuf])
                for q in range(NQ):
                    bk, rb_ = getbank()
                    for h in range(4):
                        mk("pe", lambda e, q=q, h=h, bk=bk: e.transpose(out=b16(bk)[:, h * 128:(h + 1) * 128], in_=qkT[:, 4 + h, q * 128:(q + 1) * 128], identity=identb[:, :]),
                           reads=[R_qkT] + RC, writes=[rb_])
                    mk("act", lambda e, q=q, bk=bk: e.activation(out=ktm[:, q, :], in_=b16(bk)[:, 0:512], func=AF.Identity),
                       reads=[rb_], writes=[R_ktm])

            def chunk_mlstm(T, q):
                c = T * NQ + q
                s = c % 2
                V = vt[s]; ST_ = stT[s]
                CBi = Chb2[s]; R_CBi = R_Chb2[s]; CBo = Chb2[1 - s]; R_CBo = R_Chb2[1 - s]
                sm = sm2[s]; R_sm = R_sm2[s]; hm = hm2[s]; R_hm = R_hm2[s]; hmn = hmn2[s]; R_hmn = R_hmn2[s]
                evq = ev[:, q * 4:(q + 1) * 4]
                cs = slice(q * 128, (q + 1) * 128)
                mk("dve", lambda e: e.tensor_tensor(out=V[:, :, 0:128], in0=vsb[:, q, :].rearrange("p (h d) -> p h d", h=4),
                                                    in1=evq.unsqueeze(2).broadcast_to([128, 4, 128]), op=ALU.mult),
                   reads=[R_vsb, R_ev], writes=[R_vt[s]])
                mk("pool", lambda e: e.tensor_copy(out=V[:, :, 128:129], in_=evq.unsqueeze(2)),
                   reads=[R_ev], writes=[R_vt[s]])
                bs, rbs = getbank("m")
                for h in range(4):
                    mk("pe", lambda e, h=h: e.matmul(bs[:, h * 128:(h + 1) * 128], lhsT=qkT[:, 4 + h, cs], rhs=qkT[:, h, cs], start=True, stop=True),
                       reads=[R_qkT], writes=[rbs])
                bd = [getbank("m"), getbank("m")]
                for h in range(4):
                    bk, rb_ = bd[h // 2]
                    off = (h % 2) * 129
                    mk("pe", lambda e, h=h, bk=bk, off=off: e.matmul(bk[:, off:off + 129], lhsT=ktm[:, q, h * 128:(h + 1) * 128], rhs=V[:, h, :], start=True, stop=True),
                       reads=[R_ktm, R_vt[s]], writes=[rb_])
                mk("dve", lambda e: e.tensor_tensor(out=ST_[:, :, :], in0=bs[:, 0:512].rearrange("p (h t) -> p h t", h=4),
                                                    in1=maskk[:, :].unsqueeze(1).broadcast_to([128, 4, 128]), op=ALU.mult),
                   reads=[rbs] + RC, writes=[R_stT[s]])
                for p in range(2):
                    bk, rb_ = bd[p]
                    cview = Chat[:, 2 * p:2 * p + 2, :].rearrange("p h c -> p (h c)")
                    mk("dve", lambda e, bk=bk, cview=cview: e.scalar_tensor_tensor(out=cview, in0=bk[:, 0:258], scalar=KAPPA, in1=cview, op0=ALU.mult, op1=ALU.add),
                       reads=[rb_, R_Chat], writes=[R_Chat])
                mk("pool", lambda e: e.tensor_tensor(out=Chat[:, :, :], in0=Chat[:, :, :], in1=dec[:, q * 4:(q + 1) * 4].unsqueeze(2).broadcast_to([128, 4, 129]), op=ALU.mult),
                   reads=[R_Chat, R_ev], writes=[R_Chat])
                mk("pool", lambda e: e.tensor_copy(out=CBo[:, :, :], in_=Chat[:, :, :]),
                   reads=[R_Chat], writes=[R_CBo])
                yield
                bo = [getbank("m"), getbank("m")]
                for h in range(4):
                    bk, rb_ = bo[h // 2]
                    off = (h % 2) * 129
                    mk("pe", lambda e, h=h, bk=bk, off=off: e.matmul(bk[:, off:off + 129], lhsT=ST_[:, h, :], rhs=V[:, h, :], start=True, stop=False),
                       reads=[R_stT[s], R_vt[s]], writes=[rb_])
                    mk("pe", lambda e, h=h, bk=bk, off=off: e.matmul(bk[:, off:off + 129], lhsT=qkT[:, h, cs], rhs=CBi[:, h, :], start=False, stop=True),
                       reads=[R_qkT, R_CBi] + RC, writes=[rb_])
                for p in range(2):
                    bk, rb_ = bo[p]
                    mk("dve", lambda e, p=p, bk=bk: e.tensor_scalar(out=sm[:, 10, 2 * p:2 * p + 2].unsqueeze(2),
                                                                    in0=bk[:, 0:258].rearrange("p (h c) -> p h c", h=2)[:, :, 128:129],
                                                                    scalar1=-1.0, scalar2=None, op0=ALU.mult),
                       reads=[rb_], writes=[R_sm])
                mk("dve", lambda e: e.scalar_tensor_tensor(out=sm[:, 0, :], in0=sm[:, 10, :], scalar=-1.0, in1=sm[:, 10, :], op0=ALU.mult, op1=ALU.max),
                   reads=[R_sm], writes=[R_sm])
                mk("dve", lambda e: e.tensor_tensor(out=sm[:, 0, :], in0=sm[:, 0, :], in1=fl[:, q * 4:(q + 1) * 4], op=ALU.max),
                   reads=[R_sm, R_ev], writes=[R_sm])
                mk("dve", lambda e: e.reciprocal(out=sm[:, 1, :], in_=sm[:, 0, :]), reads=[R_sm], writes=[R_sm])
                for h in range(4):
                    bk, rb_ = bo[h // 2]
                    off = (h % 2) * 129
                    mk("dve", lambda e, h=h, bk=bk, off=off: e.scalar_tensor_tensor(out=hm[:, h, :], in0=bk[:, off:off + 128], scalar=sm[:, 1, h:h + 1],
                                                                                    in1=osb[:, q, h * 128:(h + 1) * 128], op0=ALU.mult, op1=ALU.mult,
                                                                                    accum_out=sm[:, 2, h:h + 1]),
                       reads=[rb_, R_sm, R_osb], writes=[R_hm, R_sm])
                for h in range(4):
                    mk("act", lambda e, h=h: e.activation(out=junk[:, :], in_=hm[:, h, :], func=AF.Square, accum_out=sm[:, 3, h:h + 1]),
                       reads=[R_hm], writes=[R_junk, R_sm])
                mk("dve", lambda e: e.tensor_scalar(out=sm[:, 4, :], in0=sm[:, 2, :], scalar1=1.0 / 128, scalar2=None, op0=ALU.mult),
                   reads=[R_sm], writes=[R_sm])
                mk("dve", lambda e: e.tensor_tensor(out=sm[:, 5, :], in0=sm[:, 4, :], in1=sm[:, 4, :], op=ALU.mult),
                   reads=[R_sm], writes=[R_sm])
                mk("dve", lambda e: e.scalar_tensor_tensor(out=sm[:, 6, :], in0=sm[:, 3, :], scalar=1.0 / 128, in1=sm[:, 5, :], op0=ALU.mult, op1=ALU.subtract),
                   reads=[R_sm], writes=[R_sm])
                mk("dve", lambda e: e.tensor_scalar(out=sm[:, 8, :], in0=sm[:, 6, :], scalar1=EPS, scalar2=None, op0=ALU.add),
                   reads=[R_sm], writes=[R_sm])
                emit_rsqrt(sm[:, 7, :], sm[:, 8, :], sm[:, 9, :], [R_sm])
                for h in range(4):
                    eng = "dve" if h % 2 == 0 else "pool"
                    mk(eng, lambda e, h=h: e.tensor_scalar(out=hmn[:, h, :], in0=hm[:, h, :], scalar1=sm[:, 4, h:h + 1], scalar2=sm[:, 7, h:h + 1],
                                                           op0=ALU.subtract, op1=ALU.mult),
                       reads=[R_hm, R_sm], writes=[R_hmn])
                yield
                bk, rb_ = getbank("m")
                for h in range(4):
                    mk("pe", lambda e, h=h: e.transpose(out=b16(bk)[:, h * 128:(h + 1) * 128], in_=hmn[:, h, :], identity=identb[:, :]),
                       reads=[R_hmn] + RC, writes=[rb_])
                mk("act", lambda e: e.activation(out=yT[:, 4:8, cs], in_=b16(bk)[:, 0:512].rearrange("p (h t) -> p h t", h=4), func=AF.Identity),
                   reads=[rb_], writes=[R_yT])

            def back1(T):
                s = T % 2
                X = xt[s]
                for q in range(NQ):
                    cs = slice(q * 128, (q + 1) * 128)
                    c = T * NQ + q
                    ba, rba = getbank()
                    bb, rbb = getbank()
                    for k in range(8):
                        mk("pe", lambda e, k=k, ba=ba, cs=cs: e.matmul(ba[:, 0:512], lhsT=yT[:, k, cs], rhs=woutb[:, k, 0:512], start=(k == 0), stop=(k == 7)),
                           reads=[R_yT] + RC, writes=[rba])
                        mk("pe", lambda e, k=k, bb=bb, cs=cs: e.matmul(bb[:, 0:512], lhsT=yT[:, k, cs], rhs=woutb[:, k, 512:1024], start=(k == 0), stop=(k == 7)),
                           reads=[R_yT] + RC, writes=[rbb])
                    mk("dve", lambda e, q=q, ba=ba: e.tensor_tensor(out=X[:, q, 0:512], in0=ba[:, 0:512], in1=X[:, q, 0:512], op=ALU.add),
                       reads=[rba, R_xt[s]], writes=[R_xt[s]])
                    mk("dve", lambda e, q=q, bb=bb: e.tensor_tensor(out=X[:, q, 512:1024], in0=bb[:, 0:512], in1=X[:, q, 512:1024], op=ALU.add),
                       reads=[rbb, R_xt[s]], writes=[R_xt[s]])
                    mk("act", lambda e, q=q: e.activation(out=h2[:, q, :], in_=X[:, q, :], func=AF.Square, accum_out=ss[:, 4, q:q + 1]),
                       reads=[R_xt[s]], writes=[R_h2, R_ssb])
                mk("sp", lambda e: e.dma_start(out=x1_d[T * TT:(T + 1) * TT, :].rearrange("(q p) d -> p q d", p=128), in_=X[:, :, :]),
                   reads=[R_xt[s]], accum=[R_x1], dma=x1sem[s])
                if debug:
                    mk("sp", lambda e: e.dma_start(out=dbg["x1"][T * TT:(T + 1) * TT, :].rearrange("(q p) d -> p q d", p=128), in_=X[:, :, :]),
                       reads=[R_xt[s]], accum=[R_dbg], dma=dbsem[s])

            def back2(T):
                s = T % 2
                X = xt[s]
                mk("dve", lambda e: e.tensor_scalar(out=ss[:, 5, :], in0=ss[:, 4, :], scalar1=1.0 / D, scalar2=EPS, op0=ALU.mult, op1=ALU.add),
                   reads=[R_ssb], writes=[R_ssb])
                emit_rsqrt(ss[:, 6, :], ss[:, 5, :], ss[:, 7, :], [R_ssb])
                for q in range(NQ):
                    mk("dve", lambda e, q=q: e.scalar_tensor_tensor(out=h2[:, q, :], in0=X[:, q, :], scalar=ss[:, 6, q:q + 1], in1=g2bc[:, :], op0=ALU.mult, op1=ALU.mult),
                       reads=[R_xt[s], R_ssb] + RC, writes=[R_h2])
                br, rbr = getbank()
                for q in range(NQ):
                    cs = slice(q * 128, (q + 1) * 128)
                    bk, rb_ = getbank()
                    for k in range(8):
                        mk("pe", lambda e, q=q, k=k, bk=bk: e.transpose(out=b16(bk)[:, k * 128:(k + 1) * 128], in_=h2[:, q, k * 128:(k + 1) * 128], identity=identb[:, :]),
                           reads=[R_h2] + RC, writes=[rb_])
                    mk("act", lambda e, q=q, bk=bk, cs=cs: e.activation(out=h2T[:, :, cs], in_=b16(bk)[:, 0:1024].rearrange("p (k t) -> p k t", k=8), func=AF.Identity),
                       reads=[rb_], writes=[R_h2T])
                    for k in range(8):
                        mk("pe", lambda e, q=q, k=k, cs=cs: e.matmul(br[:, q * 36:(q + 1) * 36], lhsT=h2T[:, k, cs], rhs=rwb[:, k, :], start=(k == 0), stop=(k == 7)),
                           reads=[R_h2T] + RC, writes=[rbr])
                for q in range(NQ):
                    c = T * NQ + q
                    lg = rt[:, 0, :]
                    mk("dve", lambda e, q=q, lg=lg: e.tensor_tensor(out=lg, in0=br[:, q * 36:(q + 1) * 36], in1=rbbc[:, :], op=ALU.add),
                       reads=[rbr] + RC, writes=[R_rt])
                    r1 = [R_rt]
                    mk("dve", lambda e: e.tensor_reduce(out=rt[:, 1, 0:1], in_=rt[:, 0, 0:4], axis=AX.X, op=ALU.max), reads=r1, writes=r1)
                    mk("dve", lambda e: e.tensor_scalar(out=rt[:, 2, 0:4], in0=rt[:, 0, 0:4], scalar1=rt[:, 1, 0:1], scalar2=None, op0=ALU.is_equal), reads=r1, writes=r1)
                    mk("dve", lambda e: e.tensor_scalar(out=rt[:, 1, 1:2], in0=rt[:, 1, 0:1], scalar1=-1.0, scalar2=None, op0=ALU.mult), reads=r1, writes=r1)
                    mk("act", lambda e: e.activation(out=rt[:, 3, 0:4], in_=rt[:, 0, 0:4], func=AF.Exp, bias=rt[:, 1, 1:2], accum_out=rt[:, 1, 2:3]), reads=r1, writes=r1)
                    mk("dve", lambda e: e.reciprocal(out=rt[:, 1, 3:4], in_=rt[:, 1, 2:3]), reads=r1, writes=r1)
                    mk("dve", lambda e: e.tensor_scalar(out=rt[:, 4, 0:4], in0=rt[:, 2, 0:4], scalar1=-1.0, scalar2=1e30, op0=ALU.add, op1=ALU.mult), reads=r1, writes=r1)
                    mk("dve", lambda e: e.tensor_tensor(out=rt[:, 5, 0:32].rearrange("p (g k) -> p g k", g=4), in0=rt[:, 0, 4:36].rearrange("p (g k) -> p g k", g=4),
                                                        in1=rt[:, 4, 0:4].unsqueeze(2).broadcast_to([128, 4, 8]), op=ALU.add), reads=r1, writes=r1)
                    mk("dve", lambda e: e.max(out=rt[:, 6, 0:8], in_=rt[:, 5, 0:32]), reads=r1, writes=r1)
                    mk("dve", lambda e: e.tensor_scalar(out=rt[:, 7, 0:32], in0=rt[:, 5, 0:32], scalar1=rt[:, 6, 0:1], scalar2=None, op0=ALU.is_equal), reads=r1, writes=r1)
                    mk("dve", lambda e: e.tensor_scalar(out=rt[:, 8, 0:32], in0=rt[:, 5, 0:32], scalar1=rt[:, 6, 1:2], scalar2=None, op0=ALU.is_equal), reads=r1, writes=r1)
                    mk("dve", lambda e: e.tensor_tensor(out=rt[:, 1, 4:5], in0=rt[:, 6, 1:2], in1=rt[:, 6, 0:1], op=ALU.subtract), reads=r1, writes=r1)
                    mk("act", lambda e: e.activation(out=rt[:, 1, 5:6], in_=rt[:, 1, 4:5], func=AF.Exp), reads=r1, writes=r1)
                    mk("dve", lambda e: e.tensor_scalar(out=rt[:, 1, 6:7], in0=rt[:, 1, 5:6], scalar1=1.0, scalar2=None, op0=ALU.add), reads=r1, writes=r1)
                    mk("dve", lambda e: e.reciprocal(out=rt[:, 1, 7:8], in_=rt[:, 1, 6:7]), reads=r1, writes=r1)
                    mk("dve", lambda e, c=c: e.tensor_tensor(out=gwt[:, 2 * c:2 * c + 1], in0=rt[:, 1, 7:8], in1=rt[:, 1, 3:4], op=ALU.mult), reads=r1, writes=[R_gwt])
                    mk("dve", lambda e, c=c: e.tensor_tensor(out=gwt[:, 2 * c + 1:2 * c + 2], in0=rt[:, 1, 3:4], in1=gwt[:, 2 * c:2 * c + 1], op=ALU.subtract), reads=r1 + [R_gwt], writes=[R_gwt])
                    mk("dve", lambda e: e.tensor_tensor(out=cmbb[:, :], in0=rt[:, 7, 0:32], in1=rt[:, 8, 0:32], op=ALU.add), reads=r1, writes=r1)
                    bk, rb_ = getbank()
                    mk("pe", lambda e, bk=bk: e.matmul(bk[:, 0:32], lhsT=triSb[:, :], rhs=cmbb[:, :], start=True, stop=True), reads=r1 + RC, writes=[rb_])
                    mk("pe", lambda e, bk=bk: e.matmul(bk[:, 32:64], lhsT=onesb[:, :], rhs=cmbb[:, :], start=True, stop=True), reads=r1 + RC, writes=[rb_])
                    mk("dve", lambda e, bk=bk: e.tensor_tensor(out=rt[:, 9, 0:32], in0=bk[:, 0:32], in1=basep[:, :], op=ALU.add), reads=[rb_, R_base] + r1, writes=r1)
                    mk("dve", lambda e: e.scalar_tensor_tensor(out=rt[:, 10, 0:32], in0=rt[:, 7, 0:32], scalar=1.0, in1=rt[:, 9, 0:32], op0=ALU.mult, op1=ALU.mult, accum_out=rt[:, 1, 8:9]), reads=r1, writes=r1)
                    mk("dve", lambda e: e.scalar_tensor_tensor(out=rt[:, 10, 0:32], in0=rt[:, 8, 0:32], scalar=1.0, in1=rt[:, 9, 0:32], op0=ALU.mult, op1=ALU.mult, accum_out=rt[:, 1, 9:10]), reads=r1, writes=r1)
                    mk("dve", lambda e, c=c: e.tensor_scalar(out=didx[:, 2 * c:2 * c + 2], in0=rt[:, 1, 8:10], scalar1=float(NSLOT - 1), scalar2=None, op0=ALU.min), reads=r1, writes=[R_didx])
                    mk("dve", lambda e, bk=bk: e.tensor_tensor(out=basep[:, :], in0=basep[:, :], in1=bk[:, 32:64], op=ALU.add), reads=[rb_], writes=[R_base])
                    for j in range(2):
                        mk("pool", lambda e, q=q, c=c, j=j: e.indirect_dma_start(out=xs_d, out_offset=bass.IndirectOffsetOnAxis(ap=didx[:, 2 * c + j:2 * c + j + 1], axis=0),
                                                                                 in_=h2[:, q, :], in_offset=None),
                           reads=[R_h2, R_didx], accum=[R_xs], dma=scsem)

            def dumps0():
                dump("winb", winb[:, 0, :], RC)
                dump("woutb", woutb[:, 4, :], RC)
                dump("dconv", dconv[:, 0:4, :], RC)
                dump("ss", ss[:, :, :], [R_ss])
                dump("hb", hb[:, :, :], [R_hb])
                dump("hT", hT[:, :, :], [R_hT])

            def run_all(gen):
                for _ in gen:
                    pass

            def interleave(main, fill, sched):
                fill_done = False
                i = 0
                for _ in main:
                    for _ in range(sched[i % len(sched)]):
                        if not fill_done:
                            try:
                                next(fill)
                            except StopIteration:
                                fill_done = True
                    i += 1
                if not fill_done:
                    run_all(fill)

            def chain(*gens):
                for g in gens:
                    yield from g

            front1(0)
            if debug:
                dumps0()
            run_all(front2_fm(0))
            run_all(conv_a(0))
            epT = -(-NE // NT)
            for T in range(NT):
                for e_ in range(T * epT, min(NE, (T + 1) * epT)):
                    for (dst_, src_) in ((wgs_d, wg_d), (wus_d, wu_d), (wds_d, wd_d)):
                        mk("pool", lambda e, dst_=dst_, src_=src_, e_=e_: e.dma_start(out=dst_[e_], in_=src_[e_]), accum=[R_wscr], dma=wcsem)
                front2_tm(T)
                if T == 0:
                    dump("ubuf", ubuf[:, :, :], [R_ubuf])
                    dump("qkbuf", qkbuf[:, :, :], [R_qkbuf])
                    dump("vsb", vsb[:, :, :], [R_vsb])
                    dump("osb", osb[:, :, :], [R_osb])
                    dump("gi", gi[:, :], [R_g])
                    dump("gfp", gfp[:, :], [R_g])
                gbk = gates_a(T)
                conv_ln(T)
                gates_b(T, *gbk)
                if T >= 1:
                    back2(T - 1)
                tile_qkconv(T)
                conv_b(T)
                if T == 0:
                    dump("gsm", gsm[:, :, :], [R_gsm])
                    dump("ev", ev[:, :], [R_ev])
                    dump("fl", fl[:, :], [R_ev])
                    dump("dec", dec[:, :], [R_ev])
                    dump("cv", cv[:, :, :], R_cv)
                    dump("rstdt", rstdt[:, :], [R_stats])
                    dump("mean", mean_sb[:, :], [R_stats])
                    dump("qkT", qkT[:, :, :], [R_qkT])
                    dump("ktm", ktm[:, :, :], [R_ktm])
                main = chain(*[chunk_mlstm(T, q) for q in range(NQ)])
                if T + 1 < NT:
                    front1(T + 1)
                    fill = chain(front2_fm(T + 1, "f"), conv_a(T + 1, "f"))
                    interleave(main, fill, [2, 9])
                else:
                    run_all(main)
                if debug and S <= 1024:
                    for k in range(8):
                        t, rt_ = gettmp()
                        mk("dve", lambda e, k=k, t=t: e.tensor_copy(out=t[:, :], in_=yT[:, k, :]), reads=[R_yT], writes=[rt_])
                        mk("sp", lambda e, k=k, t=t, T=T: e.dma_start(out=dbg["yT"][:, k * S + T * TT:k * S + (T + 1) * TT], in_=t[:, :]), reads=[rt_], accum=[R_dbg], dma=P.dsem())
                back1(T)
            back2(NT - 1)

            mk("dve", lambda e: e.tensor_tensor(out=cnti[:, :], in0=basep[:, :], in1=ecapsb[:, :], op=ALU.subtract),
               reads=[R_base, R_const], writes=[R_cnt])
            allres = [R_xt[0], R_xt[1], R_hb, R_h2, R_hT, R_ubuf, R_qkbuf, R_vsb, R_osb, R_g, R_gsm, R_ev, R_cvb, R_sqb, R_stats,
                      R_qkT, R_ktm, R_vt[0], R_vt[1], R_stT[0], R_stT[1], R_Chat, R_junk, R_yT, R_ss, R_ssb, R_rt,
                      R_const, R_c2, R_base, R_cnt] + R_cv + R_tmp + bank_res + R_Chb2 + R_hm2 + R_hmn2 + R_sm2
            R_phase = Res("phase")
            for eng in ("pe", "act", "dve", "pool", "sp"):
                P.wait_all(eng, allres)

        with contextlib.ExitStack() as st3:
            def sb3(name, shape, dtype=F32):
                return st3.enter_context(nc.sbuf_tensor("s_" + name, list(shape), dtype))

            wgb = [sb3("wgb%d" % i, [128, 8, DE], BF16) for i in range(2)]
            wub = [sb3("wub%d" % i, [128, 8, DE], BF16) for i in range(2)]
            wdb = [sb3("wdb%d" % i, [128, 4, D], BF16) for i in range(2)]
            R_w = [Res("w0"), Res("w1")]
            wsem = [P.dsem("w0"), P.dsem("w1")]
            NXG = 4
            xg = [sb3("xg%d" % i, [128, D], BF16) for i in range(NXG)]
            R_xg = [Res("xg%d" % i) for i in range(NXG)]
            xgsem = [P.dsem("xg%d" % i) for i in range(NXG)]
            xTb = [sb3("xTb%d" % i, [128, 8, 128], BF16) for i in range(2)]
            R_xTb = [Res("xTb0"), Res("xTb1")]
            actTb = [sb3("actTb%d" % i, [128, 4, 128], BF16) for i in range(2)]
            R_actTb = [Res("actTb0"), Res("actTb1")]
            sil = [sb3("sil%d" % i, [128, 512]) for i in range(2)]
            R_sil = [Res("sil0"), Res("sil1")]
            actm = [sb3("actm%d" % i, [128, 512], BF16) for i in range(2)]
            R_actm = [Res("actm0"), Res("actm1")]
            yo = [sb3("yo%d" % i, [128, D]) for i in range(3)]
            R_yo = [Res("yo%d" % i) for i in range(3)]
            yosem = [P.dsem("yo%d" % i) for i in range(3)]

            def load_w(e_):
                s = e_ % 2
                mk("sp", lambda e: e.dma_start(out=wgb[s][:, :, :], in_=wgs_d[e_].rearrange("(k p) n -> p k n", p=128)),
                   reads=[R_wscr], accum=[R_w[s]], dma=wsem[s])
                mk("sp", lambda e: e.dma_start(out=wub[s][:, :, :], in_=wus_d[e_].rearrange("(k p) n -> p k n", p=128)),
                   reads=[R_wscr], accum=[R_w[s]], dma=wsem[s])
                mk("sp", lambda e: e.dma_start(out=wdb[s][:, :, :], in_=wds_d[e_].rearrange("(k p) n -> p k n", p=128)),
                   reads=[R_wscr], accum=[R_w[s]], dma=wsem[s])

            blk_ctr = [0]

            def stage1(e_, b, i):
                s = e_ % 2
                gi_ = i % NXG
                t2 = i % 2
                yi = i % 3
                row0 = e_ * CAP + b * 128
                XT = xTb[t2]
                AT = actTb[t2]
                mk("sp", lambda e: e.dma_start(out=xg[gi_][:, :], in_=xs_d[row0:row0 + 128, :]),
                   reads=[R_xs], writes=[R_xg[gi_]], dma=xgsem[gi_])
                bk, rb_ = getbank()
                for k in range(8):
                    mk("pe", lambda e, k=k: e.transpose(out=b16(bk)[:, k * 128:(k + 1) * 128], in_=xg[gi_][:, k * 128:(k + 1) * 128], identity=identb[:, :]),
                       reads=[R_xg[gi_], R_const], writes=[rb_])
                mk("dve", lambda e: e.tensor_copy(out=XT[:, :, :], in_=b16(bk)[:, 0:1024].rearrange("p (k t) -> p k t", k=8)),
                   reads=[rb_], writes=[R_xTb[t2]])

            def stage1b(e_, b, i):
                s = e_ % 2
                t2 = i % 2
                XT = xTb[t2]
                bg_, rbg_ = getbank()
                bu_, rbu_ = getbank()
                for k in range(8):
                    mk("pe", lambda e, k=k: e.matmul(bg_[:, 0:512], lhsT=XT[:, k, :], rhs=wgb[s][:, k, :], start=(k == 0), stop=(k == 7)),
                       reads=[R_w[s], R_xTb[t2]], writes=[rbg_])
                for k in range(8):
                    mk("pe", lambda e, k=k: e.matmul(bu_[:, 0:512], lhsT=XT[:, k, :], rhs=wub[s][:, k, :], start=(k == 0), stop=(k == 7)),
                       reads=[R_w[s], R_xTb[t2]], writes=[rbu_])
                mk("act", lambda e: e.activation(out=sil[t2][:, :], in_=bg_[:, 0:512], func=AF.Silu),
                   reads=[rbg_], writes=[R_sil[t2]])
                mk("dve", lambda e: e.tensor_tensor(out=actm[t2][:, :], in0=sil[t2][:, :], in1=bu_[:, 0:512], op=ALU.mult),
                   reads=[R_sil[t2], rbu_], writes=[R_actm[t2]])

            def stage2(e_, b, i):
                s = e_ % 2
                gi_ = i % NXG
                t2 = i % 2
                yi = i % 3
                row0 = e_ * CAP + b * 128
                XT = xTb[t2]
                AT = actTb[t2]
                bt_, rbt_ = getbank()
                for j in range(4):
                    mk("pe", lambda e, j=j: e.transpose(out=b16(bt_)[:, j * 128:(j + 1) * 128], in_=actm[t2][:, j * 128:(j + 1) * 128], identity=identb[:, :]),
                       reads=[R_actm[t2], R_const], writes=[rbt_])
                mk("dve", lambda e: e.tensor_copy(out=AT[:, :, :].rearrange("p j t -> p (j t)"), in_=b16(bt_)[:, 0:512]),
                   reads=[rbt_], writes=[R_actTb[t2]])

            def stage2b(e_, b, i):
                s = e_ % 2
                t2 = i % 2
                yi = i % 3
                row0 = e_ * CAP + b * 128
                AT = actTb[t2]
                ba, rba = getbank()
                bb, rbb = getbank()
                for j in range(4):
                    mk("pe", lambda e, j=j: e.matmul(ba[:, 0:512], lhsT=AT[:, j, :], rhs=wdb[s][:, j, 0:512], start=(j == 0), stop=(j == 3)),
                       reads=[R_w[s], R_actTb[t2]], writes=[rba])
                    mk("pe", lambda e, j=j: e.matmul(bb[:, 0:512], lhsT=AT[:, j, :], rhs=wdb[s][:, j, 512:1024], start=(j == 0), stop=(j == 3)),
                       reads=[R_w[s], R_actTb[t2]], writes=[rbb])
                mk("dve", lambda e: e.tensor_copy(out=yo[yi][:, 0:512], in_=ba[:, 0:512]),
                   reads=[rba], writes=[R_yo[yi]])
                mk("dve", lambda e: e.tensor_copy(out=yo[yi][:, 512:1024], in_=bb[:, 0:512]),
                   reads=[rbb], writes=[R_yo[yi]])
                mk("pool", lambda e: e.dma_start(out=ys_d[row0:row0 + 128, :], in_=yo[yi][:, :]),
                   reads=[R_yo[yi]], accum=[R_ys], dma=yosem[yi])

            def pair(e_, bp):
                i0 = blk_ctr[0]
                blk_ctr[0] += 2
                stage1(e_, 2 * bp, i0)
                stage1(e_, 2 * bp + 1, i0 + 1)
                stage1b(e_, 2 * bp, i0)
                stage1b(e_, 2 * bp + 1, i0 + 1)
                stage2(e_, 2 * bp, i0)
                stage2(e_, 2 * bp + 1, i0 + 1)
                stage2b(e_, 2 * bp, i0)
                stage2b(e_, 2 * bp + 1, i0 + 1)

            load_w(0)
            NP = NB // 2
            NPM = min(NP, 4)
            for e_ in range(NE):
                if e_ + 1 < NE:
                    load_w(e_ + 1)
                for bp in range(NPM):
                    P.cond_begin(cnti[0:1, e_:e_ + 1], bp * 256)
                    pair(e_, bp)
                for bp in range(NPM):
                    P.cond_end()
            if NP > NPM:
                for e_ in range(NE):
                    P.cond_begin(cnti[0:1, e_:e_ + 1], NPM * 256)
                    load_w(e_)
                    pair(e_, NPM)
                    for bp in range(NPM + 1, NP):
                        P.cond_begin(cnti[0:1, e_:e_ + 1], bp * 256)
                        pair(e_, bp)
                    for bp in range(NPM + 1, NP):
                        P.cond_end()
                    P.cond_end()

            finC = [R_w[0], R_w[1]] + R_xg + R_xTb + R_actTb + R_actm + R_sil + R_yo + bank_res
            for eng in ("sp", "pe", "act", "dve", "pool"):
                P.wait_all(eng, finC)

        with contextlib.ExitStack() as st4:
            def sb4(name, shape, dtype=F32):
                return st4.enter_context(nc.sbuf_tensor("s_" + name, list(shape), dtype))

            GD = 4
            NSL = 2 * GD
            gfbc = sb4("gfbc", [128, D])
            R_gf = Res("gfbc")
            mk("sp", lambda e: e.dma_start(out=gfbc[:, :], in_=gf_d.partition_broadcast(128)), writes=[R_gf], dma=P.dsem("gf"))
            xa = [sb4("xa%d" % i, [128, D]) for i in range(NSL)]
            ya = [sb4("ya%d" % i, [128, D]) for i in range(NSL)]
            yb = [sb4("yb%d" % i, [128, D]) for i in range(NSL)]
            ob = [sb4("ob%d" % i, [128, D]) for i in range(2)]
            R_xa = [Res("xa%d" % i) for i in range(NSL)]; R_ya = [Res("ya%d" % i) for i in range(NSL)]; R_yb = [Res("yb%d" % i) for i in range(NSL)]
            R_ob = [Res("ob0"), Res("ob1")]
            xasem = [P.dsem() for _ in range(NSL)]; yasem = [P.dsem() for _ in range(NSL)]; ybsem = [P.dsem() for _ in range(NSL)]; obsem = [P.dsem(), P.dsem()]
            fs = sb4("fs", [128, 4, NCH]); R_fs = Res("fs")
            jk = sb4("jk", [128, D], BF16); R_jk = Res("jk")

            def d_load(c):
                s = c % NSL
                mk("sp", lambda e: e.dma_start(out=xa[s][:, :], in_=x1_d[c * 128:(c + 1) * 128, :]), reads=[R_x1], writes=[R_xa[s]], dma=xasem[s])
                mk("pool", lambda e: e.indirect_dma_start(out=ya[s][:, :], out_offset=None, in_=ys_d,
                                                          in_offset=bass.IndirectOffsetOnAxis(ap=didx[:, 2 * c:2 * c + 1], axis=0)),
                   reads=[R_ys, R_didx], writes=[R_ya[s]], dma=yasem[s])
                mk("pool", lambda e: e.indirect_dma_start(out=yb[s][:, :], out_offset=None, in_=ys_d,
                                                          in_offset=bass.IndirectOffsetOnAxis(ap=didx[:, 2 * c + 1:2 * c + 2], axis=0)),
                   reads=[R_ys, R_didx], writes=[R_yb[s]], dma=ybsem[s])

            def d_combine(c):
                s = c % NSL
                mk("dve", lambda e: e.scalar_tensor_tensor(out=xa[s][:, :], in0=ya[s][:, :], scalar=gwt[:, 2 * c:2 * c + 1], in1=xa[s][:, :], op0=ALU.mult, op1=ALU.add),
                   reads=[R_ya[s], R_xa[s], R_gwt], writes=[R_xa[s]])
                mk("dve", lambda e: e.scalar_tensor_tensor(out=xa[s][:, :], in0=yb[s][:, :], scalar=gwt[:, 2 * c + 1:2 * c + 2], in1=xa[s][:, :], op0=ALU.mult, op1=ALU.add),
                   reads=[R_yb[s], R_xa[s], R_gwt], writes=[R_xa[s]])
                mk("act", lambda e: e.activation(out=jk[:, :], in_=xa[s][:, :], func=AF.Square, accum_out=fs[:, 0, c:c + 1]),
                   reads=[R_xa[s]], writes=[R_jk, R_fs])

            def d_finish(c):
                s = c % NSL
                o = c % 2
                mk("act", lambda e: e.activation(out=xa[s][:, :], in_=xa[s][:, :], func=AF.Identity, scale=fs[:, 2, c:c + 1]),
                   reads=[R_xa[s], R_fs], writes=[R_xa[s]])
                mk("pool", lambda e: e.tensor_tensor(out=ob[o][:, :], in0=xa[s][:, :], in1=gfbc[:, :], op=ALU.mult),
                   reads=[R_xa[s], R_gf], writes=[R_ob[o]])
                mk("sp", lambda e: e.dma_start(out=out_d[c * 128:(c + 1) * 128, :], in_=ob[o][:, :]),
                   reads=[R_ob[o]], accum=[R_out], dma=obsem[o])

            NGD = NCH // GD
            for c in range(min(GD, NCH)):
                d_load(c)
            for g in range(NGD):
                if g + 1 < NGD:
                    for c in range((g + 1) * GD, (g + 2) * GD):
                        d_load(c)
                cs_ = slice(g * GD, (g + 1) * GD)
                for c in range(g * GD, (g + 1) * GD):
                    d_combine(c)
                mk("dve", lambda e, cs_=cs_: e.tensor_scalar(out=fs[:, 1, cs_], in0=fs[:, 0, cs_], scalar1=1.0 / D, scalar2=EPS, op0=ALU.mult, op1=ALU.add),
                   reads=[R_fs], writes=[R_fs])
                emit_rsqrt(fs[:, 2, cs_], fs[:, 1, cs_], fs[:, 3, cs_], [R_fs])
                for c in range(g * GD, (g + 1) * GD):
                    d_finish(c)
            if debug:
                mk("sp", lambda e: e.dma_start(out=dbg["idx"], in_=didx[:, :]), reads=[R_didx], accum=[R_dbg], dma=obsem[0])
                mk("sp", lambda e: e.dma_start(out=dbg["gw"], in_=gwt[:, :]), reads=[R_gwt], accum=[R_dbg], dma=obsem[1])
            fin = [R_out, R_dbg, R_x1, R_xs, R_ys, R_wscr, R_didx, R_gwt, R_fs, R_jk, R_gf] + R_xa + R_ya + R_yb + R_ob + bank_res
            for eng in ("sp", "pe", "act", "dve", "pool"):
                P.wait_all(eng, fin)

        P.emit()
    return nc


def make_shared(inp, CAP):
    f = np.float32

    def a(v):
        return np.ascontiguousarray(v, dtype=f)

    def pk(vec, n):
        return a(np.asarray(vec).reshape(n, 128).T)

    b_in = np.asarray(inp["b_in"][0])
    sh = {
        "w_in": a(inp["w_in"][0]),
        "w_out": a(inp["w_out"][0]),
        "g1": pk(inp["norm_mix_g"][0], 8),
        "bfm": pk(b_in[0:2048], 16),
        "btm": a(b_in[2048:3080].reshape(1, 1032)),
        "cw": a(np.asarray(inp["conv_w"][0]).reshape(31, 4, 128).transpose(2, 1, 0).reshape(128, 124)),
        "cb": pk(inp["conv_b"][0], 4),
        "lng": pk(inp["conv_ln_g"][0], 4),
        "lnb": pk(inp["conv_ln_b"][0], 4),
        "qkw": a(np.asarray(inp["qk_conv_w"][0]).reshape(4, 8, 128).transpose(2, 1, 0).reshape(128, 32)),
        "qkb": pk(inp["qk_conv_b"][0], 8),
        "mg": pk(inp["mlstm_norm_g"][0], 4),
        "g2": a(np.asarray(inp["norm_ffn_g"][0]).reshape(1, D)),
        "gf": a(np.asarray(inp["final_norm_g"]).reshape(1, D)),
        "rw": a(np.concatenate([inp["router_group_w"][0], inp["router_expert_w"][0]], axis=1)),
        "rb": a(np.concatenate([inp["router_group_b"][0], inp["router_expert_b"][0]]).reshape(1, 36)),
        "ecap": a((np.arange(32) * CAP).reshape(1, 32)),
        "wg": a(inp["expert_w_gate"][0]),
        "wu": a(inp["expert_w_up"][0]),
        "wd": a(inp["expert_w_down"][0]),
        "ident": a(np.eye(128)),
        "tri_incl": a(np.triu(np.ones((128, 128)))),
        "tri_strict": a(np.triu(np.ones((128, 128)), 1)),
    }
    return sh


_CACHE = {}


def kernel(**inputs):
    x = np.asarray(inputs["x"], dtype=np.float32)
    B, S, _ = x.shape
    CAP = 1536
    key = (S, CAP)
    if key not in _CACHE:
        _CACHE[key] = build_program(S=S, CAP=CAP, NQ=2)
    nc = _CACHE[key]
    sh = make_shared(inputs, CAP)
    in_maps = []
    for b in range(B):
        m = dict(sh)
        m["x"] = np.ascontiguousarray(x[b])
        in_maps.append(m)
    res = run_bass_kernel_spmd(nc, in_maps, core_ids=list(range(B)))
    return np.stack([np.asarray(r["out"]) for r in res.results], axis=0).astype(np.float32)
```

```python
import contextlib
import numpy as np
import concourse.bass as bass
import concourse.mybir as mybir
from concourse.bass_utils import run_bass_kernel_spmd
from concourse.alu_op_type import AluOpType as ALU

F32 = mybir.dt.float32
BF16 = mybir.dt.bfloat16
I32 = mybir.dt.int32
AF = mybir.ActivationFunctionType
AX = mybir.AxisListType

D = 1024
DIN = 3080
NE = 32
DE = 512
EPS = 1e-6
KAPPA = 0.25 * (128.0 ** -0.5)


class Res:
    __slots__ = ("name", "w", "rd", "acc")

    def __init__(self, name):
        self.name = name
        self.w = None
        self.rd = []
        self.acc = []


class DmaSem:
    def __init__(self, sem):
        self.sem = sem
        self.count = 0


class Prog:
    ENGS = ("pe", "act", "dve", "pool", "sp")

    def __init__(self, nc, stack):
        self.nc = nc
        self.stack = stack
        self.items = {e: [] for e in self.ENGS}
        self.count = {e: 0 for e in self.ENGS}
        self.sem = {e: stack.enter_context(nc.semaphore("c_" + e)) for e in self.ENGS}
        self.known = {e: {} for e in self.ENGS}
        self.ndma = 0
        self.sync_same_engine = True
        self._frames = []

    def dsem(self, name=None):
        self.ndma += 1
        return DmaSem(self.stack.enter_context(self.nc.semaphore(name or ("d%d" % self.ndma))))

    def _deps(self, eng, reads, writes, accum):
        toks = []
        for r in reads:
            if r.w is not None:
                toks.append(r.w)
            toks.extend(r.acc)
        for w in writes:
            if w.w is not None:
                toks.append(w.w)
            toks.extend(w.rd)
            toks.extend(w.acc)
        for a in accum:
            if a.w is not None:
                toks.append(a.w)
            toks.extend(a.rd)
        waits = []
        kn = self.known[eng]
        own = self.sem[eng]
        best = {}
        for (s, v) in toks:
            if s is own and (eng == "pe" or not self.sync_same_engine):
                continue
            if kn.get(id(s), 0) >= v:
                continue
            if id(s) not in best or best[id(s)][1] < v:
                best[id(s)] = (s, v)
        for (s, v) in best.values():
            kn[id(s)] = v
            waits.append((s, v))
        return waits

    def op(self, eng, fn, reads=(), writes=(), accum=(), dma=None):
        waits = self._deps(eng, reads, writes, accum)
        if dma is None:
            self.count[eng] += 1
            tok = (self.sem[eng], self.count[eng])
            inc = 1
        else:
            for fr in self._frames:
                d = fr["reg"][eng]["dma"].setdefault(id(dma), [dma.sem, dma.count, 0])
                d[2] += 16
            dma.count += 16
            tok = (dma.sem, dma.count)
            inc = 16
        self.items[eng].append((waits, fn, tok[0], inc))
        for r in reads:
            r.rd.append(tok)
        for w in writes:
            w.w = tok
            w.rd = []
            w.acc = []
        for a in accum:
            a.acc.append(tok)
        return tok

    def cond_begin(self, cnt_ap, thresh):
        frame = {"snap": {e: dict(k) for e, k in self.known.items()},
                 "reg": {e: {"start": len(self.items[e]), "cnt0": self.count[e], "dma": {}} for e in self.ENGS},
                 "cond": (cnt_ap, thresh)}
        self._frames.append(frame)

    def cond_end(self):
        frame = self._frames.pop()
        cnt_ap, thresh = frame["cond"]
        for e in self.ENGS:
            r = frame["reg"][e]
            body = self.items[e][r["start"]:]
            n_ops = self.count[e] - r["cnt0"]
            del self.items[e][r["start"]:]
            if body:
                self.items[e].append(("cond", cnt_ap, thresh, body, r["cnt0"], n_ops, [tuple(v) for v in r["dma"].values()]))
        self.known = frame["snap"]

    def wait_all(self, eng, ress):
        waits = self._deps(eng, ress, ress, ())
        self.items[eng].append((waits, None, None, 0))

    def emit(self):
        nc = self.nc
        with nc.Block() as block:
            def run_items(name, handle, items, reg):
                own = self.sem[name]
                for it in items:
                    if it[0] == "cond":
                        _, cnt_ap, thresh, body, cnt0, n_ops, dmas = it
                        handle.reg_load(reg, cnt_ap)
                        with handle.If_lt(reg, thresh + 1):
                            if n_ops:
                                handle.wait_ge(own, cnt0)
                                left = n_ops
                                while left > 0:
                                    k = min(left, 15)
                                    handle.sem_inc(own, k)
                                    left -= k
                            for (dsem_, before, inc) in dmas:
                                handle.wait_ge(dsem_, before)
                                left = inc
                                while left > 0:
                                    k = min(left, 15)
                                    handle.sem_inc(dsem_, k)
                                    left -= k
                        with handle.Else():
                            run_items(name, handle, body, reg)
                        continue
                    (waits, fn, sem, inc) = it
                    for (s_, v) in waits:
                        handle.wait_ge(s_, v)
                    if fn is not None:
                        fn(handle).then_inc(sem, inc)

            def run(name, handle):
                with handle.register("rc_" + name) as reg:
                    run_items(name, handle, self.items[name], reg)

            @block.tensor
            def _(e):
                run("pe", e)

            @block.scalar
            def _(e):
                run("act", e)

            @block.vector
            def _(e):
                run("dve", e)

            @block.gpsimd
            def _(e):
                run("pool", e)

            @block.sync
            def _(e):
                run("sp", e)


def build_program(S=8192, CAP=640, NQ=2, debug=False):
    TT = NQ * 128
    NT = S // TT
    NCH = S // 128
    NB = CAP // 128
    NSLOT = NE * CAP
    NG = NQ * 4
    assert S % TT == 0 and CAP % 256 == 0

    nc = bass.Bass("TRN2", target_bir_lowering=False)
    dt = nc.dram_tensor

    def din(name, shape, dtype=F32):
        return dt(name, list(shape), dtype, kind="ExternalInput").ap()

    x_d = din("x", [S, D])
    win_d = din("w_in", [D, DIN])
    wout_d = din("w_out", [D, D])
    g1_d = din("g1", [128, 8])
    bfm_d = din("bfm", [128, 16])
    btm_d = din("btm", [1, 1032])
    cw_d = din("cw", [128, 4 * 31])
    cb_d = din("cb", [128, 4])
    lng_d = din("lng", [128, 4])
    lnb_d = din("lnb", [128, 4])
    qkw_d = din("qkw", [128, 8 * 4])
    qkb_d = din("qkb", [128, 8])
    mg_d = din("mg", [128, 4])
    g2_d = din("g2", [1, D])
    gf_d = din("gf", [1, D])
    rw_d = din("rw", [D, 36])
    rb_d = din("rb", [1, 36])
    ecap_d = din("ecap", [1, 32])
    wg_d = din("wg", [NE, D, DE])
    wu_d = din("wu", [NE, D, DE])
    wd_d = din("wd", [NE, DE, D])
    ident_d = din("ident", [128, 128])
    triI_d = din("tri_incl", [128, 128])
    triS_d = din("tri_strict", [128, 128])
    out_d = dt("out", [S, D], F32, kind="ExternalOutput").ap()
    x1_d = dt("x1s", [S, D], F32, kind="Internal").ap()
    xs_d = dt("xs", [NSLOT, D], BF16, kind="ExternalOutput" if debug else "Internal").ap()
    ys_d = dt("ys", [NSLOT, D], F32, kind="ExternalOutput" if debug else "Internal").ap()
    wgs_d = dt("wgs", [NE, D, DE], BF16, kind="Internal").ap()
    wus_d = dt("wus", [NE, D, DE], BF16, kind="Internal").ap()
    wds_d = dt("wds", [NE, DE, D], BF16, kind="Internal").ap()
    dbg = {}
    if debug:
        dbg["x1"] = dt("dbg_x1", [S, D], F32, kind="ExternalOutput").ap()
        dbg["idx"] = dt("dbg_idx", [128, NCH * 2], I32, kind="ExternalOutput").ap()
        dbg["gw"] = dt("dbg_gw", [128, NCH * 2], F32, kind="ExternalOutput").ap()
        dbg["yT"] = dt("dbg_yT", [128, 8 * S], F32, kind="ExternalOutput").ap()

    with contextlib.ExitStack() as st:
        P = Prog(nc, st)

        def sb(name, shape, dtype=F32):
            return st.enter_context(nc.sbuf_tensor("s_" + name, list(shape), dtype))

        def mk(eng, fn, reads=(), writes=(), accum=(), dma=None):
            return P.op(eng, fn, reads, writes, accum, dma)

        dumps = {}

        def dump(name, ap, reads):
            if not debug or name in dumps:
                return
            dumps[name] = dt("dmp_" + name, list(ap.shape), ap.dtype, kind="ExternalOutput").ap()
            mk("sp", lambda e: e.dma_start(out=dumps[name], in_=ap), reads=reads, accum=[R_dbg], dma=P.dsem())

        def emit_rsqrt(y, v, t, R):
            mk("dve", lambda e: e.tensor_scalar(out=y.bitcast(I32), in0=v.bitcast(I32), scalar1=-0.5, scalar2=1597463007.0, op0=ALU.mult, op1=ALU.add),
               reads=R, writes=R)
            for _ in range(2):
                mk("dve", lambda e: e.scalar_tensor_tensor(out=t, in0=y, scalar=-0.5, in1=y, op0=ALU.mult, op1=ALU.mult), reads=R, writes=R)
                mk("dve", lambda e: e.tensor_tensor(out=t, in0=t, in1=v, op=ALU.mult), reads=R, writes=R)
                mk("dve", lambda e: e.scalar_tensor_tensor(out=y, in0=t, scalar=1.5, in1=y, op0=ALU.add, op1=ALU.mult), reads=R, writes=R)

        banks = [st.enter_context(nc.psum_tensor("bank%d" % i, [128, 512], F32)) for i in range(8)]
        bank_res = [Res("bank%d" % i) for i in range(8)]
        bank_ctr = [0]

        pool_ctr = {"m": 0, "f": 0}

        def getbank(pool=None):
            if pool == "m":
                i = pool_ctr["m"] % 5
                pool_ctr["m"] += 1
            elif pool == "f":
                i = 5 + pool_ctr["f"] % 3
                pool_ctr["f"] += 1
            else:
                i = bank_ctr[0] % 8
                bank_ctr[0] += 1
            return banks[i], bank_res[i]

        def b16(bank):
            return bank[:, :].bitcast(BF16)

        didx = sb("didx", [128, NCH * 2], I32)
        gwt = sb("gwt", [128, NCH * 2], F32)
        R_didx = Res("didx")
        R_gwt = Res("gwt")
        R_xs = Res("xs")
        R_ys = Res("ys")
        R_x1 = Res("x1d")
        R_out = Res("out")
        R_dbg = Res("dbg")
        identb = sb("identb", [128, 128], BF16)
        cnti = sb("cnti", [128, 32], I32)
        ecapsb = sb("ecapsb", [128, 32], F32)
        R_cnt = Res("cnti")
        R_wscr = Res("wscr")
        wcsem = P.dsem("wcast")
        R_const = Res("const")

        ld = P.dsem("ld_const")

        with contextlib.ExitStack() as st2:
            def sb2(name, shape, dtype=F32):
                return st2.enter_context(nc.sbuf_tensor("s_" + name, list(shape), dtype))

            winb = sb2("winb", [128, 8, DIN], BF16)
            woutb = sb2("woutb", [128, 8, D], BF16)
            dconv = sb2("dconv", [128, 4 * 31, 128], BF16)
            dqk = sb2("dqk", [128, 8 * 4, 128], BF16)
            identf = sb2("identf", [128, 128], F32)
            triI = sb2("triI", [128, 128], F32)
            onesf = sb2("onesf", [128, 128], F32)
            maskk = sb2("maskk", [128, 128], F32)
            triSb = sb2("triSb", [128, 128], BF16)
            onesb = sb2("onesb", [128, 128], BF16)
            o512b = sb2("o512b", [128, 128], BF16)
            g1 = sb2("g1", [128, 8])
            bfm = sb2("bfm", [128, 16])
            hbfm = sb2("hbfm", [128, 16])
            btm = sb2("btm", [128, 1032])
            cw = sb2("cw", [128, 124])
            cb = sb2("cb", [128, 4])
            lng = sb2("lng", [128, 4])
            lnb = sb2("lnb", [128, 4])
            hlng = sb2("hlng", [128, 4])
            hlnb = sb2("hlnb", [128, 4])
            qkw = sb2("qkw", [128, 32])
            qkb = sb2("qkb", [128, 8])
            hqkb = sb2("hqkb", [128, 8])
            mg = sb2("mg", [128, 4])
            g2bc = sb2("g2bc", [128, D])
            rwf = sb2("rwf", [128, 8, 36])
            rwb = sb2("rwb", [128, 8, 36], BF16)
            rbbc = sb2("rbbc", [128, 36])
            basep = sb2("basep", [128, 32])
            halfc = sb2("halfc", [128, 1])
            epsc = sb2("epsc", [128, 1])

            xt = [sb2("xt%d" % i, [128, NQ, D]) for i in range(2)]
            R_xt = [Res("xt0"), Res("xt1")]
            xsem = [P.dsem("x0"), P.dsem("x1")]
            hb = sb2("hb", [128, NQ, D], BF16); R_hb = Res("hb")
            h2 = sb2("h2", [128, NQ, D], BF16); R_h2 = Res("h2")
            hT = sb2("hT", [128, 8, TT], BF16); R_hT = Res("hT")
            h2Tq = sb2("h2Tq", [128, 8, 128], BF16); R_h2Tq = Res("h2Tq")
            lgall = sb2("lgall", [128, NQ, 36]); R_lgall = Res("lgall")
            ubuf = sb2("ubuf", [128, 4, TT + 32], BF16); R_ubuf = Res("ubuf")
            qkbuf = sb2("qkbuf", [128, 8, TT + 4], BF16); R_qkbuf = Res("qkbuf")
            NTMP = 6
            tmp = [sb2("tmp%d" % i, [128, TT]) for i in range(NTMP)]
            R_tmp = [Res("tmp%d" % i) for i in range(NTMP)]
            tmp_ctr = [0]

            def gettmp():
                i = tmp_ctr[0] % NTMP
                tmp_ctr[0] += 1
                return tmp[i], R_tmp[i]

            vsb = sb2("vsb", [128, NQ, 512]); R_vsb = Res("vsb")
            osb = sb2("osb", [128, NQ, 512]); R_osb = Res("osb")
            gi = sb2("gi", [128, NG]); gfp = sb2("gfp", [128, NG]); R_g = Res("gates")
            gsm = sb2("gsm", [128, 8, NG]); R_gsm = Res("gsm")
            ev = sb2("ev", [128, NG]); fl = sb2("fl", [128, NG]); dec = sb2("dec", [128, NG])
            R_ev = Res("ev")
            cv = sb2("cv", [128, 4, TT]); R_cv = [Res("cv%d" % i) for i in range(4)]
            cvb = sb2("cvb", [128, 4, TT], BF16); R_cvb = Res("cvb")
            sqb = sb2("sqb", [128, 4, TT], BF16); R_sqb = Res("sqb")
            mean_sb = sb2("mean_sb", [128, TT]); rstdt = sb2("rstdt", [128, TT]); msq = sb2("msq", [128, TT])
            R_stats = Res("stats")
            qkT = sb2("qkT", [128, 8, TT], BF16); R_qkT = Res("qkT")
            ktm = sb2("ktm", [128, NQ, 512], BF16); R_ktm = Res("ktm")
            vt = [sb2("vt%d" % i, [128, 4, 129], BF16) for i in range(2)]; R_vt = [Res("vt0"), Res("vt1")]
            stT = [sb2("stT%d" % i, [128, 4, 128], BF16) for i in range(2)]; R_stT = [Res("stT0"), Res("stT1")]
            Chat = sb2("Chat", [128, 4, 129]); Chb2 = [sb2("Chb%d" % i, [128, 4, 129], BF16) for i in range(2)]
            R_Chat = Res("Chat"); R_Chb2 = [Res("Chb0"), Res("Chb1")]
            hm2 = [sb2("hm%d" % i, [128, 4, 128]) for i in range(2)]; R_hm2 = [Res("hm0"), Res("hm1")]
            hmn2 = [sb2("hmn%d" % i, [128, 4, 128], BF16) for i in range(2)]; R_hmn2 = [Res("hmn0"), Res("hmn1")]
            sm2 = [sb2("sm%d" % i, [128, 12, 4]) for i in range(2)]; R_sm2 = [Res("sm0"), Res("sm1")]
            junk = sb2("junk", [128, 128]); R_junk = Res("junk")
            yT = sb2("yT", [128, 8, TT], BF16); R_yT = Res("yT")
            ss = sb2("ss", [128, 8, NQ]); R_ss = Res("ssf"); R_ssb = Res("ssb")
            rt = sb2("rt", [128, 11, 36]); R_rt = Res("rt")
            cmbb = sb2("cmbb", [128, 32], BF16)
            x1sem = [P.dsem("x1st0"), P.dsem("x1st1")]
            dbsem = [P.dsem("db0"), P.dsem("db1")]
            scsem = P.dsem("scat")

            ld2 = P.dsem("ld_const_sw")

            def cld(eng, dst, src):
                mk(eng, lambda e: e.dma_start(out=dst, in_=src), accum=[R_const], dma=(ld if eng == "sp" else ld2))

            cld("sp", identf[:, :], ident_d)
            cld("sp", triI[:, :], triI_d)
            cld("sp", g1[:, :], g1_d)
            cld("sp", bfm[:, :], bfm_d)
            cld("sp", btm[:, :], btm_d.partition_broadcast(128))
            cld("sp", cw[:, :], cw_d)
            cld("sp", cb[:, :], cb_d)
            cld("sp", lng[:, :], lng_d)
            cld("sp", lnb[:, :], lnb_d)
            cld("sp", qkw[:, :], qkw_d)
            cld("sp", qkb[:, :], qkb_d)
            cld("sp", mg[:, :], mg_d)
            cld("sp", g2bc[:, :], g2_d.partition_broadcast(128))
            cld("sp", rbbc[:, :], rb_d.partition_broadcast(128))
            R_base = Res("basep")
            mk("sp", lambda e: e.dma_start(out=basep[:, :], in_=ecap_d.partition_broadcast(128)), writes=[R_base], dma=P.dsem("base"))
            cld("sp", rwf[:, :, :], rw_d.rearrange("(k p) n -> p k n", p=128))
            cld("sp", ecapsb[:, :], ecap_d.partition_broadcast(128))
            cld("pool", triSb[:, :], triS_d)
            cld("pool", identb[:, :], ident_d)
            R_c2 = Res("const2")

            def cop(eng, fn):
                mk(eng, fn, reads=[R_const], accum=[R_c2])

            cop("dve", lambda e: e.memset(onesf[:, :], 1.0))
            cop("dve", lambda e: e.memset(onesb[:, :], 1.0))
            cop("dve", lambda e: e.memset(o512b[:, :], 1.0 / 512.0))
            cop("dve", lambda e: e.memset(halfc[:, :], 0.5))
            cop("dve", lambda e: e.memset(epsc[:, :], EPS))
            cop("dve", lambda e: e.tensor_scalar(out=maskk[:, :], in0=triI[:, :], scalar1=KAPPA, scalar2=None, op0=ALU.mult))
            cop("dve", lambda e: e.tensor_scalar(out=hbfm[:, :], in0=bfm[:, :], scalar1=0.5, scalar2=None, op0=ALU.mult))
            cop("dve", lambda e: e.tensor_scalar(out=hlng[:, :], in0=lng[:, :], scalar1=0.5, scalar2=None, op0=ALU.mult))
            cop("dve", lambda e: e.tensor_scalar(out=hlnb[:, :], in0=lnb[:, :], scalar1=0.5, scalar2=None, op0=ALU.mult))
            cop("dve", lambda e: e.tensor_scalar(out=hqkb[:, :], in0=qkb[:, :], scalar1=0.5, scalar2=None, op0=ALU.mult))
            cop("dve", lambda e: e.tensor_copy(out=rwb[:, :, :], in_=rwf[:, :, :]))
            mk("dve", lambda e: e.memset(Chat[:, :, :], 0.0), writes=[R_Chat])
            mk("dve", lambda e: e.memset(Chb2[0][:, :, :], 0.0), writes=[R_Chb2[0]])
            mk("dve", lambda e: e.memset(Chb2[1][:, :, :], 0.0), writes=[R_Chb2[1]])
            mk("dve", lambda e: e.memset(ubuf[:, :, :], 0.0), writes=[R_ubuf])
            mk("dve", lambda e: e.memset(qkbuf[:, :, :], 0.0), writes=[R_qkbuf])
            for jc in range(4):
                cop("dve", lambda e, jc=jc: e.scalar_tensor_tensor(out=dconv[:, jc * 31:(jc + 1) * 31, :], in0=identf[:, :].unsqueeze(1).broadcast_to([128, 31, 128]), scalar=0.5,
                                                                   in1=cw[:, jc * 31:(jc + 1) * 31].unsqueeze(2).broadcast_to([128, 31, 128]), op0=ALU.mult, op1=ALU.mult))
            cop("pool", lambda e: e.tensor_tensor(out=dqk[:, :, :], in0=identf[:, :].unsqueeze(1).broadcast_to([128, 32, 128]),
                                                  in1=qkw[:, :].unsqueeze(2).broadcast_to([128, 32, 128]), op=ALU.mult))
            R_stage = R_xt
            HW = DIN // 2
            for k in range(8):
                for hf in range(2):
                    s = (2 * k + hf) % 2
                    flat = xt[s][:, :, :].rearrange("p q d -> p (q d)")
                    mk("sp", lambda e, k=k, hf=hf, flat=flat: e.dma_start(out=flat[:, 0:HW], in_=win_d[k * 128:(k + 1) * 128, hf * HW:(hf + 1) * HW]),
                       writes=[R_stage[s]], dma=xsem[s])
                    if s == 0:
                        mk("dve", lambda e, k=k, hf=hf, flat=flat: e.tensor_scalar(out=winb[:, k, hf * HW:(hf + 1) * HW], in0=flat[:, 0:HW],
                                                                                  scalar1=g1[:, k:k + 1], scalar2=None, op0=ALU.mult),
                           reads=[R_stage[s], R_const], accum=[R_c2])
                    else:
                        mk("act", lambda e, k=k, hf=hf, flat=flat: e.activation(out=winb[:, k, hf * HW:(hf + 1) * HW], in_=flat[:, 0:HW],
                                                                               func=AF.Identity, scale=g1[:, k:k + 1]),
                           reads=[R_stage[s], R_const], accum=[R_c2])
            for k in range(8):
                s = k % 2
                mk("sp", lambda e, k=k, s=s: e.dma_start(out=xt[s][:, 0, :], in_=wout_d[k * 128:(k + 1) * 128, :]),
                   writes=[R_stage[s]], dma=xsem[s])
                sc = 0.5 if k < 4 else mg[:, k - 4:k - 3]
                if k % 2 == 0:
                    mk("dve", lambda e, k=k, s=s, sc=sc: e.tensor_scalar(out=woutb[:, k, :], in0=xt[s][:, 0, :], scalar1=sc,
                                                                         scalar2=None, op0=ALU.mult),
                       reads=[R_stage[s], R_const], accum=[R_c2])
                else:
                    mk("act", lambda e, k=k, s=s, sc=sc: e.activation(out=woutb[:, k, :], in_=xt[s][:, 0, :], func=AF.Identity, scale=sc),
                       reads=[R_stage[s], R_const], accum=[R_c2])
            RC = [R_const, R_c2]

            def front1(T):
                s = T % 2
                X = xt[s]
                mk("sp", lambda e: e.dma_start(out=X[:, :, :], in_=x_d[T * TT:(T + 1) * TT, :].rearrange("(q p) d -> p q d", p=128)),
                   writes=[R_xt[s]], dma=xsem[s])
                for q in range(NQ):
                    mk("act", lambda e, q=q: e.activation(out=hb[:, q, :], in_=X[:, q, :], func=AF.Square, accum_out=ss[:, 0, q:q + 1]),
                       reads=[R_xt[s]], writes=[R_hb, R_ss])
                mk("dve", lambda e: e.tensor_scalar(out=ss[:, 1, :], in0=ss[:, 0, :], scalar1=1.0 / D, scalar2=EPS, op0=ALU.mult, op1=ALU.add),
                   reads=[R_ss], writes=[R_ss])
                emit_rsqrt(ss[:, 2, :], ss[:, 1, :], ss[:, 3, :], [R_ss])
                for q in range(NQ):
                    mk("act", lambda e, q=q: e.activation(out=hb[:, q, :], in_=X[:, q, :], func=AF.Identity, scale=ss[:, 2, q:q + 1]),
                       reads=[R_xt[s], R_ss], writes=[R_hb])
                for q in range(NQ):
                    bk, rb_ = getbank()
                    for k in range(8):
                        mk("pe", lambda e, q=q, k=k, bk=bk: e.transpose(out=b16(bk)[:, k * 128:(k + 1) * 128], in_=hb[:, q, k * 128:(k + 1) * 128], identity=identb[:, :]),
                           reads=[R_hb] + RC, writes=[rb_])
                    mk("dve", lambda e, q=q, bk=bk: e.tensor_copy(out=hT[:, :, q * 128:(q + 1) * 128], in_=b16(bk)[:, 0:1024].rearrange("p (k t) -> p k t", k=8)),
                       reads=[rb_], writes=[R_hT])

            def front2_fm(T, pool=None):
                order = [4, 0, 5, 1, 6, 2, 7, 3] + list(range(8, 16))
                thg = {}
                for j in order:
                    if j != order[0]:
                        yield
                    bk, rb_ = getbank(pool)
                    for k in range(8):
                        mk("pe", lambda e, j=j, k=k, bk=bk: e.matmul(bk[:, 0:TT], lhsT=winb[:, k, j * 128:(j + 1) * 128], rhs=hT[:, k, :],
                                                                     start=(k == 0), stop=(k == 7)),
                           reads=[R_hT] + RC, writes=[rb_])
                    if 4 <= j < 8:
                        t, rt_ = gettmp()
                        thg[j - 4] = (t, rt_)
                        mk("act", lambda e, j=j, bk=bk, t=t: e.activation(out=t[:, :], in_=bk[:, 0:TT], func=AF.Tanh, scale=0.5, bias=hbfm[:, j:j + 1]),
                           reads=[rb_] + RC, writes=[rt_])
                    elif j < 4:
                        t, rt_ = thg[j]
                        a, ra_ = gettmp()
                        mk("act", lambda e, j=j, bk=bk, a=a: e.activation(out=a[:, :], in_=bk[:, 0:TT], func=AF.Identity, bias=bfm[:, j:j + 1]),
                           reads=[rb_] + RC, writes=[ra_])
                        mk("dve", lambda e, j=j, t=t, a=a: e.scalar_tensor_tensor(out=ubuf[:, j, 30:30 + TT], in0=t[:, :], scalar=1.0, in1=a[:, :],
                                                                                  op0=ALU.add, op1=ALU.mult),
                           reads=[rt_, ra_], writes=[R_ubuf])
                    else:
                        jp = j - 8
                        if True:
                            mk("act", lambda e, j=j, jp=jp, bk=bk: e.activation(out=qkbuf[:, jp, 3:3 + TT], in_=bk[:, 0:TT], func=AF.Identity, bias=bfm[:, j:j + 1]),
                               reads=[rb_] + RC, writes=[R_qkbuf])
                        else:
                            mk("dve", lambda e, j=j, jp=jp, bk=bk: e.tensor_scalar(out=qkbuf[:, jp, 3:3 + TT], in0=bk[:, 0:TT], scalar1=bfm[:, j:j + 1], scalar2=None, op0=ALU.add),
                               reads=[rb_] + RC, writes=[R_qkbuf])
                yield

            def front2_tm(T):
                bg, rbg = getbank()
                for q in range(NQ):
                    bv, rbv = getbank()
                    bo, rbo = getbank()
                    for k in range(8):
                        lt = hT[:, k, q * 128:(q + 1) * 128]
                        mk("pe", lambda e, k=k, bv=bv, lt=lt: e.matmul(bv[:, 0:512], lhsT=lt, rhs=winb[:, k, 2048:2560], start=(k == 0), stop=(k == 7)),
                           reads=[R_hT] + RC, writes=[rbv])
                        mk("pe", lambda e, k=k, bo=bo, lt=lt: e.matmul(bo[:, 0:512], lhsT=lt, rhs=winb[:, k, 2560:3072], start=(k == 0), stop=(k == 7)),
                           reads=[R_hT] + RC, writes=[rbo])
                    for k in range(8):
                        lt = hT[:, k, q * 128:(q + 1) * 128]
                        mk("pe", lambda e, k=k, q=q, lt=lt: e.matmul(bg[:, q * 8:(q + 1) * 8], lhsT=lt, rhs=winb[:, k, 3072:3080], start=(k == 0), stop=(k == 7)),
                           reads=[R_hT] + RC, writes=[rbg])
                    mk("dve", lambda e, q=q, bv=bv: e.tensor_tensor(out=vsb[:, q, :], in0=bv[:, 0:512], in1=btm[:, 0:512], op=ALU.add),
                       reads=[rbv] + RC, writes=[R_vsb])
                    mk("dve", lambda e, q=q, bo=bo: e.tensor_tensor(out=osb[:, q, :], in0=bo[:, 0:512], in1=btm[:, 512:1024], op=ALU.add),
                       reads=[rbo] + RC, writes=[R_osb])
                    mk("act", lambda e, q=q: e.activation(out=osb[:, q, :], in_=osb[:, q, :], func=AF.Tanh, scale=0.5),
                       reads=[R_osb], writes=[R_osb])
                    mk("pool", lambda e, q=q: e.tensor_scalar(out=osb[:, q, :], in0=osb[:, q, :], scalar1=0.5, scalar2=0.5, op0=ALU.mult, op1=ALU.add),
                       reads=[R_osb], writes=[R_osb])
                for q in range(NQ):
                    mk("dve", lambda e, q=q: e.tensor_tensor(out=gi[:, q * 4:(q + 1) * 4], in0=bg[:, q * 8:q * 8 + 4], in1=btm[:, 1024:1028], op=ALU.add),
                       reads=[rbg] + RC, writes=[R_g])
                    mk("dve", lambda e, q=q: e.tensor_tensor(out=gfp[:, q * 4:(q + 1) * 4], in0=bg[:, q * 8 + 4:q * 8 + 8], in1=btm[:, 1028:1032], op=ALU.add),
                       reads=[rbg] + RC, writes=[R_g])

            def gates_a(T):
                G = gsm
                mk("act", lambda e: e.activation(out=G[:, 0, :], in_=gfp[:, :], func=AF.Abs),
                   reads=[R_g], writes=[R_gsm])
                mk("act", lambda e: e.activation(out=G[:, 1, :], in_=G[:, 0, :], func=AF.Exp, scale=-1.0),
                   reads=[R_gsm], writes=[R_gsm])
                mk("act", lambda e: e.activation(out=G[:, 2, :], in_=G[:, 1, :], func=AF.Ln, bias=1.0),
                   reads=[R_gsm], writes=[R_gsm])
                mk("dve", lambda e: e.tensor_scalar(out=G[:, 3, :], in0=gfp[:, :], scalar1=0.0, scalar2=None, op0=ALU.min),
                   reads=[R_g], writes=[R_gsm])
                mk("dve", lambda e: e.tensor_tensor(out=G[:, 4, :], in0=G[:, 2, :], in1=G[:, 3, :], op=ALU.subtract),
                   reads=[R_gsm], writes=[R_gsm])
                bk, rb_ = getbank()
                mk("pe", lambda e: e.matmul(bk[:, 0:NG], lhsT=triI[:, :], rhs=G[:, 4, :], start=True, stop=True),
                   reads=[R_gsm] + RC, writes=[rb_])
                mk("pe", lambda e: e.matmul(bk[:, 16:16 + NG], lhsT=onesf[:, :], rhs=G[:, 4, :], start=True, stop=True),
                   reads=[R_gsm] + RC, writes=[rb_])
                mk("dve", lambda e: e.tensor_tensor(out=G[:, 5, :], in0=bk[:, 0:NG], in1=gi[:, :], op=ALU.add),
                   reads=[rb_, R_g], writes=[R_gsm])
                return bk, rb_

            def gates_b(T, bk, rb_):
                G = gsm
                mk("act", lambda e: e.activation(out=ev[:, :], in_=G[:, 5, :], func=AF.Exp),
                   reads=[R_gsm], writes=[R_ev])
                mk("act", lambda e: e.activation(out=fl[:, :], in_=bk[:, 0:NG], func=AF.Exp),
                   reads=[rb_], writes=[R_ev])
                mk("act", lambda e: e.activation(out=dec[:, :], in_=bk[:, 16:16 + NG], func=AF.Exp, scale=-1.0),
                   reads=[rb_], writes=[R_ev])

            def conv_a(T, pool=None):
                for jc in range(4):
                    if jc:
                        yield
                    bk, rb_ = getbank(pool)
                    for j in range(31):
                        mk("pe", lambda e, jc=jc, j=j, bk=bk: e.matmul(bk[:, 0:TT], lhsT=dconv[:, jc * 31 + j, :], rhs=ubuf[:, jc, j:j + TT],
                                                                       start=(j == 0), stop=(j == 30)),
                           reads=[R_ubuf] + RC, writes=[rb_])
                    mk("act", lambda e, jc=jc, bk=bk: e.activation(out=cv[:, jc, :], in_=bk[:, 0:TT], func=AF.Identity, bias=cb[:, jc:jc + 1]),
                       reads=[rb_] + RC, writes=[R_cv[jc]])
                    mk("pool", lambda e, jc=jc: e.tensor_copy(out=cvb[:, jc, :], in_=cv[:, jc, :]),
                       reads=[R_cv[jc]], writes=[R_cvb])
                    mk("act", lambda e, jc=jc: e.activation(out=sqb[:, jc, :], in_=cv[:, jc, :], func=AF.Square),
                       reads=[R_cv[jc]], writes=[R_sqb])
                mk("pool", lambda e: e.tensor_copy(out=ubuf[:, :, 0:30], in_=ubuf[:, :, TT:TT + 30]),
                   reads=[], writes=[R_ubuf])
                yield
                bm, rbm = getbank(pool)
                be, rbe = getbank(pool)
                for jc in range(4):
                    mk("pe", lambda e, jc=jc: e.matmul(bm[:, 0:TT], lhsT=o512b[:, :], rhs=cvb[:, jc, :], start=(jc == 0), stop=(jc == 3)),
                       reads=[R_cvb] + RC, writes=[rbm])
                for jc in range(4):
                    mk("pe", lambda e, jc=jc: e.matmul(be[:, 0:TT], lhsT=o512b[:, :], rhs=sqb[:, jc, :], start=(jc == 0), stop=(jc == 3)),
                       reads=[R_sqb] + RC, writes=[rbe])
                mk("act", lambda e: e.activation(out=mean_sb[:, :], in_=bm[:, 0:TT], func=AF.Identity),
                   reads=[rbm], writes=[R_stats])
                mk("dve", lambda e: e.tensor_tensor(out=msq[:, :], in0=mean_sb[:, :], in1=mean_sb[:, :], op=ALU.mult),
                   reads=[R_stats], writes=[R_stats])
                mk("dve", lambda e: e.tensor_tensor(out=msq[:, :], in0=be[:, 0:TT], in1=msq[:, :], op=ALU.subtract),
                   reads=[rbe, R_stats], writes=[R_stats])
                yield

            def conv_ln(T):
                mk("act", lambda e: e.activation(out=rstdt[:, :], in_=msq[:, :], func=AF.Ln, bias=epsc[:, 0:1]),
                   reads=[R_stats] + RC, writes=[R_stats])
                mk("act", lambda e: e.activation(out=rstdt[:, :], in_=rstdt[:, :], func=AF.Exp, scale=-0.5),
                   reads=[R_stats], writes=[R_stats])

            def conv_b(T):
                for jc in range(4):
                    mk("pool", lambda e, jc=jc: e.tensor_tensor(out=cv[:, jc, :], in0=cv[:, jc, :], in1=mean_sb[:, :], op=ALU.subtract),
                       reads=[R_stats, R_cv[jc]], writes=[R_cv[jc]])
                    mk("dve", lambda e, jc=jc: e.tensor_tensor(out=cv[:, jc, :], in0=cv[:, jc, :], in1=rstdt[:, :], op=ALU.mult),
                       reads=[R_stats, R_cv[jc]], writes=[R_cv[jc]])
                    t, rt_ = gettmp()
                    mk("act", lambda e, jc=jc, t=t: e.activation(out=t[:, :], in_=cv[:, jc, :], func=AF.Tanh, scale=hlng[:, jc:jc + 1], bias=hlnb[:, jc:jc + 1]),
                       reads=[R_cv[jc]] + RC, writes=[rt_])
                    mk("act", lambda e, jc=jc: e.activation(out=cv[:, jc, :], in_=cv[:, jc, :], func=AF.Identity, scale=lng[:, jc:jc + 1], bias=lnb[:, jc:jc + 1]),
                       reads=[R_cv[jc]] + RC, writes=[R_cv[jc]])
                    mk("dve", lambda e, jc=jc, t=t: e.scalar_tensor_tensor(out=yT[:, jc, :], in0=t[:, :], scalar=1.0, in1=cv[:, jc, :], op0=ALU.add, op1=ALU.mult),
                       reads=[rt_, R_cv[jc]], writes=[R_yT])

            def tile_qkconv(T):
                for jp in range(8):
                    bk, rb_ = getbank()
                    for j in range(4):
                        mk("pe", lambda e, jp=jp, j=j, bk=bk: e.matmul(bk[:, 0:TT], lhsT=dqk[:, jp * 4 + j, :], rhs=qkbuf[:, jp, j:j + TT],
                                                                       start=(j == 0), stop=(j == 3)),
                           reads=[R_qkbuf] + RC, writes=[rb_])
                    t, rt_ = gettmp()
                    z, rz_ = gettmp()
                    mk("act", lambda e, jp=jp, bk=bk, t=t: e.activation(out=t[:, :], in_=bk[:, 0:TT], func=AF.Tanh, scale=0.5, bias=hqkb[:, jp:jp + 1]),
                       reads=[rb_] + RC, writes=[rt_])
                    mk("act", lambda e, jp=jp, bk=bk, z=z: e.activation(out=z[:, :], in_=bk[:, 0:TT], func=AF.Identity, bias=qkb[:, jp:jp + 1]),
                       reads=[rb_] + RC, writes=[rz_])
                    mk("dve", lambda e, jp=jp, t=t, z=z: e.scalar_tensor_tensor(out=qkT[:, jp, :], in0=t[:, :], scalar=1.0, in1=z[:, :], op0=ALU.add, op1=ALU.mult),
                       reads=[rt_, rz_], writes=[R_qkT])
                mk("pool", lambda e: e.tensor_copy(out=qkbuf[:, :, 0:3], in_=qkbuf[:, :, TT:TT + 3]),
                   reads=[], writes=[R_qkbuf])
                for q in range(NQ):
                    bk, rb_ = getbank()
                    for h in range(4):
                        mk("pe", lambda e, q=q, h=h, bk=bk: e.transpose(out=b16(bk)[:, h * 128:(h + 1) * 128], in_=qkT[:, 4 + h, q * 128:(q + 1) * 128], identity=identb[:, :]),
                           reads=[R_qkT] + RC, writes=[rb_])
                    mk("act", lambda e, q=q, bk=bk: e.activation(out=ktm[:, q, :], in_=b16(bk)[:, 0:512], func=AF.Identity),
                       reads=[rb_], writes=[R_ktm])

            def chunk_mlstm(T, q):
                c = T * NQ + q
                s = c % 2
                V = vt[s]; ST_ = stT[s]
                CBi = Chb2[s]; R_CBi = R_Chb2[s]; CBo = Chb2[1 - s]; R_CBo = R_Chb2[1 - s]
                sm = sm2[s]; R_sm = R_sm2[s]; hm = hm2[s]; R_hm = R_hm2[s]; hmn = hmn2[s]; R_hmn = R_hmn2[s]
                evq = ev[:, q * 4:(q + 1) * 4]
                cs = slice(q * 128, (q + 1) * 128)
                mk("dve", lambda e: e.tensor_tensor(out=V[:, :, 0:128], in0=vsb[:, q, :].rearrange("p (h d) -> p h d", h=4),
                                                    in1=evq.unsqueeze(2).broadcast_to([128, 4, 128]), op=ALU.mult),
                   reads=[R_vsb, R_ev], writes=[R_vt[s]])
                mk("pool", lambda e: e.tensor_copy(out=V[:, :, 128:129], in_=evq.unsqueeze(2)),
                   reads=[R_ev], writes=[R_vt[s]])
                bs, rbs = getbank("m")
                for h in range(4):
                    mk("pe", lambda e, h=h: e.matmul(bs[:, h * 128:(h + 1) * 128], lhsT=qkT[:, 4 + h, cs], rhs=qkT[:, h, cs], start=True, stop=True),
                       reads=[R_qkT], writes=[rbs])
                bd = [getbank("m"), getbank("m")]
                for h in range(4):
                    bk, rb_ = bd[h // 2]
                    off = (h % 2) * 129
                    mk("pe", lambda e, h=h, bk=bk, off=off: e.matmul(bk[:, off:off + 129], lhsT=ktm[:, q, h * 128:(h + 1) * 128], rhs=V[:, h, :], start=True, stop=True),
                       reads=[R_ktm, R_vt[s]], writes=[rb_])
                mk("dve", lambda e: e.tensor_tensor(out=ST_[:, :, :], in0=bs[:, 0:512].rearrange("p (h t) -> p h t", h=4),
                                                    in1=maskk[:, :].unsqueeze(1).broadcast_to([128, 4, 128]), op=ALU.mult),
                   reads=[rbs] + RC, writes=[R_stT[s]])
                for p in range(2):
                    bk, rb_ = bd[p]
                    cview = Chat[:, 2 * p:2 * p + 2, :].rearrange("p h c -> p (h c)")
                    mk("dve", lambda e, bk=bk, cview=cview: e.scalar_tensor_tensor(out=cview, in0=bk[:, 0:258], scalar=KAPPA, in1=cview, op0=ALU.mult, op1=ALU.add),
                       reads=[rb_, R_Chat], writes=[R_Chat])
                mk("pool", lambda e: e.tensor_tensor(out=Chat[:, :, :], in0=Chat[:, :, :], in1=dec[:, q * 4:(q + 1) * 4].unsqueeze(2).broadcast_to([128, 4, 129]), op=ALU.mult),
                   reads=[R_Chat, R_ev], writes=[R_Chat])
                mk("pool", lambda e: e.tensor_copy(out=CBo[:, :, :], in_=Chat[:, :, :]),
                   reads=[R_Chat], writes=[R_CBo])
                yield
                bo = [getbank("m"), getbank("m")]
                for h in range(4):
                    bk, rb_ = bo[h // 2]
                    off = (h % 2) * 129
                    mk("pe", lambda e, h=h, bk=bk, off=off: e.matmul(bk[:, off:off + 129], lhsT=ST_[:, h, :], rhs=V[:, h, :], start=True, stop=False),
                       reads=[R_stT[s], R_vt[s]], writes=[rb_])
                    mk("pe", lambda e, h=h, bk=bk, off=off: e.matmul(bk[:, off:off + 129], lhsT=qkT[:, h, cs], rhs=CBi[:, h, :], start=False, stop=True),
                       reads=[R_qkT, R_CBi] + RC, writes=[rb_])
                for p in range(2):
                    bk, rb_ = bo[p]
                    mk("dve", lambda e, p=p, bk=bk: e.tensor_scalar(out=sm[:, 10, 2 * p:2 * p + 2].unsqueeze(2),
                                                                    in0=bk[:, 0:258].rearrange("p (h c) -> p h c", h=2)[:, :, 128:129],
                                                                    scalar1=-1.0, scalar2=None, op0=ALU.mult),
                       reads=[rb_], writes=[R_sm])
                mk("dve", lambda e: e.scalar_tensor_tensor(out=sm[:, 0, :], in0=sm[:, 10, :], scalar=-1.0, in1=sm[:, 10, :], op0=ALU.mult, op1=ALU.max),
                   reads=[R_sm], writes=[R_sm])
                mk("dve", lambda e: e.tensor_tensor(out=sm[:, 0, :], in0=sm[:, 0, :], in1=fl[:, q * 4:(q + 1) * 4], op=ALU.max),
                   reads=[R_sm, R_ev], writes=[R_sm])
                mk("dve", lambda e: e.reciprocal(out=sm[:, 1, :], in_=sm[:, 0, :]), reads=[R_sm], writes=[R_sm])
                for h in range(4):
                    bk, rb_ = bo[h // 2]
                    off = (h % 2) * 129
                    mk("dve", lambda e, h=h, bk=bk, off=off: e.scalar_tensor_tensor(out=hm[:, h, :], in0=bk[:, off:off + 128], scalar=sm[:, 1, h:h + 1],
                                                                                    in1=osb[:, q, h * 128:(h + 1) * 128], op0=ALU.mult, op1=ALU.mult,
                                                                                    accum_out=sm[:, 2, h:h + 1]),
                       reads=[rb_, R_sm, R_osb], writes=[R_hm, R_sm])
                for h in range(4):
                    mk("act", lambda e, h=h: e.activation(out=junk[:, :], in_=hm[:, h, :], func=AF.Square, accum_out=sm[:, 3, h:h + 1]),
                       reads=[R_hm], writes=[R_junk, R_sm])
                mk("dve", lambda e: e.tensor_scalar(out=sm[:, 4, :], in0=sm[:, 2, :], scalar1=1.0 / 128, scalar2=None, op0=ALU.mult),
                   reads=[R_sm], writes=[R_sm])
                mk("dve", lambda e: e.tensor_tensor(out=sm[:, 5, :], in0=sm[:, 4, :], in1=sm[:, 4, :], op=ALU.mult),
                   reads=[R_sm], writes=[R_sm])
                mk("dve", lambda e: e.scalar_tensor_tensor(out=sm[:, 6, :], in0=sm[:, 3, :], scalar=1.0 / 128, in1=sm[:, 5, :], op0=ALU.mult, op1=ALU.subtract),
                   reads=[R_sm], writes=[R_sm])
                mk("dve", lambda e: e.tensor_scalar(out=sm[:, 8, :], in0=sm[:, 6, :], scalar1=EPS, scalar2=None, op0=ALU.add),
                   reads=[R_sm], writes=[R_sm])
                emit_rsqrt(sm[:, 7, :], sm[:, 8, :], sm[:, 9, :], [R_sm])
                for h in range(4):
                    eng = "dve" if h % 2 == 0 else "pool"
                    mk(eng, lambda e, h=h: e.tensor_scalar(out=hmn[:, h, :], in0=hm[:, h, :], scalar1=sm[:, 4, h:h + 1], scalar2=sm[:, 7, h:h + 1],
                                                           op0=ALU.subtract, op1=ALU.mult),
                       reads=[R_hm, R_sm], writes=[R_hmn])
                yield
                bk, rb_ = getbank("m")
                for h in range(4):
                    mk("pe", lambda e, h=h: e.transpose(out=b16(bk)[:, h * 128:(h + 1) * 128], in_=hmn[:, h, :], identity=identb[:, :]),
                       reads=[R_hmn] + RC, writes=[rb_])
                mk("act", lambda e: e.activation(out=yT[:, 4:8, cs], in_=b16(bk)[:, 0:512].rearrange("p (h t) -> p h t", h=4), func=AF.Identity),
                   reads=[rb_], writes=[R_yT])

            def back1(T):
                s = T % 2
                X = xt[s]
                for q in range(NQ):
                    cs = slice(q * 128, (q + 1) * 128)
                    c = T * NQ + q
                    ba, rba = getbank()
                    bb, rbb = getbank()
                    for k in range(8):
                        mk("pe", lambda e, k=k, ba=ba, cs=cs: e.matmul(ba[:, 0:512], lhsT=yT[:, k, cs], rhs=woutb[:, k, 0:512], start=(k == 0), stop=(k == 7)),
                           reads=[R_yT] + RC, writes=[rba])
                        mk("pe", lambda e, k=k, bb=bb, cs=cs: e.matmul(bb[:, 0:512], lhsT=yT[:, k, cs], rhs=woutb[:, k, 512:1024], start=(k == 0), stop=(k == 7)),
                           reads=[R_yT] + RC, writes=[rbb])
                    mk("dve", lambda e, q=q, ba=ba: e.tensor_tensor(out=X[:, q, 0:512], in0=ba[:, 0:512], in1=X[:, q, 0:512], op=ALU.add),
                       reads=[rba, R_xt[s]], writes=[R_xt[s]])
                    mk("dve", lambda e, q=q, bb=bb: e.tensor_tensor(out=X[:, q, 512:1024], in0=bb[:, 0:512], in1=X[:, q, 512:1024], op=ALU.add),
                       reads=[rbb, R_xt[s]], writes=[R_xt[s]])
                    mk("act", lambda e, q=q: e.activation(out=h2[:, q, :], in_=X[:, q, :], func=AF.Square, accum_out=ss[:, 4, q:q + 1]),
                       reads=[R_xt[s]], writes=[R_h2, R_ssb])
                mk("sp", lambda e: e.dma_start(out=x1_d[T * TT:(T + 1) * TT, :].rearrange("(q p) d -> p q d", p=128), in_=X[:, :, :]),
                   reads=[R_xt[s]], accum=[R_x1], dma=x1sem[s])
                if debug:
                    mk("sp", lambda e: e.dma_start(out=dbg["x1"][T * TT:(T + 1) * TT, :].rearrange("(q p) d -> p q d", p=128), in_=X[:, :, :]),
                       reads=[R_xt[s]], accum=[R_dbg], dma=dbsem[s])

            def back2a(T):
                s = T % 2
                X = xt[s]
                mk("dve", lambda e: e.tensor_scalar(out=ss[:, 5, :], in0=ss[:, 4, :], scalar1=1.0 / D, scalar2=EPS, op0=ALU.mult, op1=ALU.add),
                   reads=[R_ssb], writes=[R_ssb])
                emit_rsqrt(ss[:, 6, :], ss[:, 5, :], ss[:, 7, :], [R_ssb])
                for q in range(NQ):
                    mk("dve", lambda e, q=q: e.scalar_tensor_tensor(out=h2[:, q, :], in0=X[:, q, :], scalar=ss[:, 6, q:q + 1], in1=g2bc[:, :], op0=ALU.mult, op1=ALU.mult),
                       reads=[R_xt[s], R_ssb] + RC, writes=[R_h2])

            def back2b(T, pool=None):
                br, rbr = getbank(pool)
                for q in range(NQ):
                    bk, rb_ = getbank(pool)
                    for k in range(8):
                        mk("pe", lambda e, q=q, k=k, bk=bk: e.transpose(out=b16(bk)[:, k * 128:(k + 1) * 128], in_=h2[:, q, k * 128:(k + 1) * 128], identity=identb[:, :]),
                           reads=[R_h2] + RC, writes=[rb_])
                    mk("act", lambda e, q=q, bk=bk: e.activation(out=h2Tq[:, :, :], in_=b16(bk)[:, 0:1024].rearrange("p (k t) -> p k t", k=8), func=AF.Identity),
                       reads=[rb_], writes=[R_h2Tq])
                    for k in range(8):
                        mk("pe", lambda e, q=q, k=k: e.matmul(br[:, q * 36:(q + 1) * 36], lhsT=h2Tq[:, k, :], rhs=rwb[:, k, :], start=(k == 0), stop=(k == 7)),
                           reads=[R_h2Tq] + RC, writes=[rbr])
                    mk("dve", lambda e, q=q: e.tensor_tensor(out=lgall[:, q, :], in0=br[:, q * 36:(q + 1) * 36], in1=rbbc[:, :], op=ALU.add),
                       reads=[rbr] + RC, writes=[R_lgall])
                    yield
                for q in range(NQ):
                    if q:
                        yield
                    c = T * NQ + q
                    lg = rt[:, 0, :]
                    mk("dve", lambda e, q=q, lg=lg: e.tensor_copy(out=lg, in_=lgall[:, q, :]),
                       reads=[R_lgall], writes=[R_rt])
                    r1 = [R_rt]
                    mk("dve", lambda e: e.tensor_reduce(out=rt[:, 1, 0:1], in_=rt[:, 0, 0:4], axis=AX.X, op=ALU.max), reads=r1, writes=r1)
                    mk("dve", lambda e: e.tensor_scalar(out=rt[:, 2, 0:4], in0=rt[:, 0, 0:4], scalar1=rt[:, 1, 0:1], scalar2=None, op0=ALU.is_equal), reads=r1, writes=r1)
                    mk("dve", lambda e: e.tensor_scalar(out=rt[:, 1, 1:2], in0=rt[:, 1, 0:1], scalar1=-1.0, scalar2=None, op0=ALU.mult), reads=r1, writes=r1)
                    mk("act", lambda e: e.activation(out=rt[:, 3, 0:4], in_=rt[:, 0, 0:4], func=AF.Exp, bias=rt[:, 1, 1:2], accum_out=rt[:, 1, 2:3]), reads=r1, writes=r1)
                    mk("dve", lambda e: e.reciprocal(out=rt[:, 1, 3:4], in_=rt[:, 1, 2:3]), reads=r1, writes=r1)
                    mk("dve", lambda e: e.tensor_scalar(out=rt[:, 4, 0:4], in0=rt[:, 2, 0:4], scalar1=-1.0, scalar2=1e30, op0=ALU.add, op1=ALU.mult), reads=r1, writes=r1)
                    mk("dve", lambda e: e.tensor_tensor(out=rt[:, 5, 0:32].rearrange("p (g k) -> p g k", g=4), in0=rt[:, 0, 4:36].rearrange("p (g k) -> p g k", g=4),
                                                        in1=rt[:, 4, 0:4].unsqueeze(2).broadcast_to([128, 4, 8]), op=ALU.add), reads=r1, writes=r1)
                    mk("dve", lambda e: e.max(out=rt[:, 6, 0:8], in_=rt[:, 5, 0:32]), reads=r1, writes=r1)
                    mk("dve", lambda e: e.tensor_scalar(out=rt[:, 7, 0:32], in0=rt[:, 5, 0:32], scalar1=rt[:, 6, 0:1], scalar2=None, op0=ALU.is_equal), reads=r1, writes=r1)
                    mk("dve", lambda e: e.tensor_scalar(out=rt[:, 8, 0:32], in0=rt[:, 5, 0:32], scalar1=rt[:, 6, 1:2], scalar2=None, op0=ALU.is_equal), reads=r1, writes=r1)
                    mk("dve", lambda e: e.tensor_tensor(out=rt[:, 1, 4:5], in0=rt[:, 6, 1:2], in1=rt[:, 6, 0:1], op=ALU.subtract), reads=r1, writes=r1)
                    mk("act", lambda e: e.activation(out=rt[:, 1, 5:6], in_=rt[:, 1, 4:5], func=AF.Exp), reads=r1, writes=r1)
                    mk("dve", lambda e: e.tensor_scalar(out=rt[:, 1, 6:7], in0=rt[:, 1, 5:6], scalar1=1.0, scalar2=None, op0=ALU.add), reads=r1, writes=r1)
                    mk("dve", lambda e: e.reciprocal(out=rt[:, 1, 7:8], in_=rt[:, 1, 6:7]), reads=r1, writes=r1)
                    mk("dve", lambda e, c=c: e.tensor_tensor(out=gwt[:, 2 * c:2 * c + 1], in0=rt[:, 1, 7:8], in1=rt[:, 1, 3:4], op=ALU.mult), reads=r1, writes=[R_gwt])
                    mk("dve", lambda e, c=c: e.tensor_tensor(out=gwt[:, 2 * c + 1:2 * c + 2], in0=rt[:, 1, 3:4], in1=gwt[:, 2 * c:2 * c + 1], op=ALU.subtract), reads=r1 + [R_gwt], writes=[R_gwt])
                    mk("dve", lambda e: e.tensor_tensor(out=cmbb[:, :], in0=rt[:, 7, 0:32], in1=rt[:, 8, 0:32], op=ALU.add), reads=r1, writes=r1)
                    bk, rb_ = getbank(pool)
                    mk("pe", lambda e, bk=bk: e.matmul(bk[:, 0:32], lhsT=triSb[:, :], rhs=cmbb[:, :], start=True, stop=True), reads=r1 + RC, writes=[rb_])
                    mk("pe", lambda e, bk=bk: e.matmul(bk[:, 32:64], lhsT=onesb[:, :], rhs=cmbb[:, :], start=True, stop=True), reads=r1 + RC, writes=[rb_])
                    mk("dve", lambda e, bk=bk: e.tensor_tensor(out=rt[:, 9, 0:32], in0=bk[:, 0:32], in1=basep[:, :], op=ALU.add), reads=[rb_, R_base] + r1, writes=r1)
                    mk("dve", lambda e: e.scalar_tensor_tensor(out=rt[:, 10, 0:32], in0=rt[:, 7, 0:32], scalar=1.0, in1=rt[:, 9, 0:32], op0=ALU.mult, op1=ALU.mult, accum_out=rt[:, 1, 8:9]), reads=r1, writes=r1)
                    mk("dve", lambda e: e.scalar_tensor_tensor(out=rt[:, 10, 0:32], in0=rt[:, 8, 0:32], scalar=1.0, in1=rt[:, 9, 0:32], op0=ALU.mult, op1=ALU.mult, accum_out=rt[:, 1, 9:10]), reads=r1, writes=r1)
                    mk("dve", lambda e, c=c: e.tensor_scalar(out=didx[:, 2 * c:2 * c + 2], in0=rt[:, 1, 8:10], scalar1=float(NSLOT - 1), scalar2=None, op0=ALU.min), reads=r1, writes=[R_didx])
                    mk("dve", lambda e, bk=bk: e.tensor_tensor(out=basep[:, :], in0=basep[:, :], in1=bk[:, 32:64], op=ALU.add), reads=[rb_], writes=[R_base])
                    for j in range(2):
                        mk("pool", lambda e, q=q, c=c, j=j: e.indirect_dma_start(out=xs_d, out_offset=bass.IndirectOffsetOnAxis(ap=didx[:, 2 * c + j:2 * c + j + 1], axis=0),
                                                                                 in_=h2[:, q, :], in_offset=None),
                           reads=[R_h2, R_didx], accum=[R_xs], dma=scsem)
                yield

            def dumps0():
                dump("winb", winb[:, 0, :], RC)
                dump("woutb", woutb[:, 4, :], RC)
                dump("dconv", dconv[:, 0:4, :], RC)
                dump("ss", ss[:, :, :], [R_ss])
                dump("hb", hb[:, :, :], [R_hb])
                dump("hT", hT[:, :, :], [R_hT])

            def run_all(gen):
                for _ in gen:
                    pass

            def interleave(main, fill, sched):
                fill_done = False
                i = 0
                for _ in main:
                    for _ in range(sched[i % len(sched)]):
                        if not fill_done:
                            try:
                                next(fill)
                            except StopIteration:
                                fill_done = True
                    i += 1
                if not fill_done:
                    run_all(fill)

            def chain(*gens):
                for g in gens:
                    yield from g

            front1(0)
            if debug:
                dumps0()
            run_all(front2_fm(0))
            run_all(conv_a(0))
            epT = -(-NE // NT)
            for T in range(NT):
                for e_ in range(T * epT, min(NE, (T + 1) * epT)):
                    for (dst_, src_) in ((wgs_d, wg_d), (wus_d, wu_d), (wds_d, wd_d)):
                        mk("pool", lambda e, dst_=dst_, src_=src_, e_=e_: e.dma_start(out=dst_[e_], in_=src_[e_]), accum=[R_wscr], dma=wcsem)
                front2_tm(T)
                if T == 0:
                    dump("ubuf", ubuf[:, :, :], [R_ubuf])
                    dump("qkbuf", qkbuf[:, :, :], [R_qkbuf])
                    dump("vsb", vsb[:, :, :], [R_vsb])
                    dump("osb", osb[:, :, :], [R_osb])
                    dump("gi", gi[:, :], [R_g])
                    dump("gfp", gfp[:, :], [R_g])
                gbk = gates_a(T)
                conv_ln(T)
                gates_b(T, *gbk)
                if T >= 1:
                    back2a(T - 1)
                tile_qkconv(T)
                conv_b(T)
                if T == 0:
                    dump("gsm", gsm[:, :, :], [R_gsm])
                    dump("ev", ev[:, :], [R_ev])
                    dump("fl", fl[:, :], [R_ev])
                    dump("dec", dec[:, :], [R_ev])
                    dump("cv", cv[:, :, :], R_cv)
                    dump("rstdt", rstdt[:, :], [R_stats])
                    dump("mean", mean_sb[:, :], [R_stats])
                    dump("qkT", qkT[:, :, :], [R_qkT])
                    dump("ktm", ktm[:, :, :], [R_ktm])
                main = chain(*[chunk_mlstm(T, q) for q in range(NQ)])
                if T + 1 < NT:
                    front1(T + 1)
                    gens = [front2_fm(T + 1, "f"), conv_a(T + 1, "f")]
                    if T >= 1:
                        gens.append(back2b(T - 1, "f"))
                    interleave(main, chain(*gens), [2, 11])
                else:
                    interleave(main, back2b(T - 1, "f"), [1, 1])
                if debug and S <= 1024:
                    for k in range(8):
                        t, rt_ = gettmp()
                        mk("dve", lambda e, k=k, t=t: e.tensor_copy(out=t[:, :], in_=yT[:, k, :]), reads=[R_yT], writes=[rt_])
                        mk("sp", lambda e, k=k, t=t, T=T: e.dma_start(out=dbg["yT"][:, k * S + T * TT:k * S + (T + 1) * TT], in_=t[:, :]), reads=[rt_], accum=[R_dbg], dma=P.dsem())
                back1(T)
            back2a(NT - 1)
            run_all(back2b(NT - 1))

            mk("dve", lambda e: e.tensor_tensor(out=cnti[:, :], in0=basep[:, :], in1=ecapsb[:, :], op=ALU.subtract),
               reads=[R_base, R_const], writes=[R_cnt])
            allres = [R_xt[0], R_xt[1], R_hb, R_h2, R_hT, R_h2Tq, R_lgall, R_ubuf, R_qkbuf, R_vsb, R_osb, R_g, R_gsm, R_ev, R_cvb, R_sqb, R_stats,
                      R_qkT, R_ktm, R_vt[0], R_vt[1], R_stT[0], R_stT[1], R_Chat, R_junk, R_yT, R_ss, R_ssb, R_rt,
                      R_const, R_c2, R_base, R_cnt] + R_cv + R_tmp + bank_res + R_Chb2 + R_hm2 + R_hmn2 + R_sm2
            R_phase = Res("phase")
            for eng in ("pe", "act", "dve", "pool", "sp"):
                P.wait_all(eng, allres)

        with contextlib.ExitStack() as st3:
            def sb3(name, shape, dtype=F32):
                return st3.enter_context(nc.sbuf_tensor("s_" + name, list(shape), dtype))

            wgb = [sb3("wgb%d" % i, [128, 8, DE], BF16) for i in range(2)]
            wub = [sb3("wub%d" % i, [128, 8, DE], BF16) for i in range(2)]
            wdb = [sb3("wdb%d" % i, [128, 4, D], BF16) for i in range(2)]
            R_w = [Res("w0"), Res("w1")]
            wsem = [P.dsem("w0"), P.dsem("w1")]
            NXG = 4
            xg = [sb3("xg%d" % i, [128, D], BF16) for i in range(NXG)]
            R_xg = [Res("xg%d" % i) for i in range(NXG)]
            xgsem = [P.dsem("xg%d" % i) for i in range(NXG)]
            xTb = [sb3("xTb%d" % i, [128, 8, 128], BF16) for i in range(2)]
            R_xTb = [Res("xTb0"), Res("xTb1")]
            actTb = [sb3("actTb%d" % i, [128, 4, 128], BF16) for i in range(2)]
            R_actTb = [Res("actTb0"), Res("actTb1")]
            sil = [sb3("sil%d" % i, [128, 512]) for i in range(2)]
            R_sil = [Res("sil0"), Res("sil1")]
            actm = [sb3("actm%d" % i, [128, 512], BF16) for i in range(2)]
            R_actm = [Res("actm0"), Res("actm1")]
            yo = [sb3("yo%d" % i, [128, D]) for i in range(3)]
            R_yo = [Res("yo%d" % i) for i in range(3)]
            yosem = [P.dsem("yo%d" % i) for i in range(3)]

            def load_w(e_):
                s = e_ % 2
                mk("sp", lambda e: e.dma_start(out=wgb[s][:, :, :], in_=wgs_d[e_].rearrange("(k p) n -> p k n", p=128)),
                   reads=[R_wscr], accum=[R_w[s]], dma=wsem[s])
                mk("sp", lambda e: e.dma_start(out=wub[s][:, :, :], in_=wus_d[e_].rearrange("(k p) n -> p k n", p=128)),
                   reads=[R_wscr], accum=[R_w[s]], dma=wsem[s])
                mk("sp", lambda e: e.dma_start(out=wdb[s][:, :, :], in_=wds_d[e_].rearrange("(k p) n -> p k n", p=128)),
                   reads=[R_wscr], accum=[R_w[s]], dma=wsem[s])

            blk_ctr = [0]

            def stage1(e_, b, i):
                s = e_ % 2
                gi_ = i % NXG
                t2 = i % 2
                yi = i % 3
                row0 = e_ * CAP + b * 128
                XT = xTb[t2]
                AT = actTb[t2]
                mk("sp", lambda e: e.dma_start(out=xg[gi_][:, :], in_=xs_d[row0:row0 + 128, :]),
                   reads=[R_xs], writes=[R_xg[gi_]], dma=xgsem[gi_])
                bk, rb_ = getbank()
                for k in range(8):
                    mk("pe", lambda e, k=k: e.transpose(out=b16(bk)[:, k * 128:(k + 1) * 128], in_=xg[gi_][:, k * 128:(k + 1) * 128], identity=identb[:, :]),
                       reads=[R_xg[gi_], R_const], writes=[rb_])
                mk("dve", lambda e: e.tensor_copy(out=XT[:, :, :], in_=b16(bk)[:, 0:1024].rearrange("p (k t) -> p k t", k=8)),
                   reads=[rb_], writes=[R_xTb[t2]])

            def stage1b(e_, b, i):
                s = e_ % 2
                t2 = i % 2
                XT = xTb[t2]
                bg_, rbg_ = getbank()
                bu_, rbu_ = getbank()
                for k in range(8):
                    mk("pe", lambda e, k=k: e.matmul(bg_[:, 0:512], lhsT=XT[:, k, :], rhs=wgb[s][:, k, :], start=(k == 0), stop=(k == 7)),
                       reads=[R_w[s], R_xTb[t2]], writes=[rbg_])
                for k in range(8):
                    mk("pe", lambda e, k=k: e.matmul(bu_[:, 0:512], lhsT=XT[:, k, :], rhs=wub[s][:, k, :], start=(k == 0), stop=(k == 7)),
                       reads=[R_w[s], R_xTb[t2]], writes=[rbu_])
                mk("act", lambda e: e.activation(out=sil[t2][:, :], in_=bg_[:, 0:512], func=AF.Silu),
                   reads=[rbg_], writes=[R_sil[t2]])
                mk("dve", lambda e: e.tensor_tensor(out=actm[t2][:, :], in0=sil[t2][:, :], in1=bu_[:, 0:512], op=ALU.mult),
                   reads=[R_sil[t2], rbu_], writes=[R_actm[t2]])

            def stage2(e_, b, i):
                s = e_ % 2
                gi_ = i % NXG
                t2 = i % 2
                yi = i % 3
                row0 = e_ * CAP + b * 128
                XT = xTb[t2]
                AT = actTb[t2]
                bt_, rbt_ = getbank()
                for j in range(4):
                    mk("pe", lambda e, j=j: e.transpose(out=b16(bt_)[:, j * 128:(j + 1) * 128], in_=actm[t2][:, j * 128:(j + 1) * 128], identity=identb[:, :]),
                       reads=[R_actm[t2], R_const], writes=[rbt_])
                mk("dve", lambda e: e.tensor_copy(out=AT[:, :, :].rearrange("p j t -> p (j t)"), in_=b16(bt_)[:, 0:512]),
                   reads=[rbt_], writes=[R_actTb[t2]])

            def stage2b(e_, b, i):
                s = e_ % 2
                t2 = i % 2
                yi = i % 3
                row0 = e_ * CAP + b * 128
                AT = actTb[t2]
                ba, rba = getbank()
                bb, rbb = getbank()
                for j in range(4):
                    mk("pe", lambda e, j=j: e.matmul(ba[:, 0:512], lhsT=AT[:, j, :], rhs=wdb[s][:, j, 0:512], start=(j == 0), stop=(j == 3)),
                       reads=[R_w[s], R_actTb[t2]], writes=[rba])
                    mk("pe", lambda e, j=j: e.matmul(bb[:, 0:512], lhsT=AT[:, j, :], rhs=wdb[s][:, j, 512:1024], start=(j == 0), stop=(j == 3)),
                       reads=[R_w[s], R_actTb[t2]], writes=[rbb])
                mk("dve", lambda e: e.tensor_copy(out=yo[yi][:, 0:512], in_=ba[:, 0:512]),
                   reads=[rba], writes=[R_yo[yi]])
                mk("dve", lambda e: e.tensor_copy(out=yo[yi][:, 512:1024], in_=bb[:, 0:512]),
                   reads=[rbb], writes=[R_yo[yi]])
                mk("pool", lambda e: e.dma_start(out=ys_d[row0:row0 + 128, :], in_=yo[yi][:, :]),
                   reads=[R_yo[yi]], accum=[R_ys], dma=yosem[yi])

            def pair(e_, bp):
                i0 = blk_ctr[0]
                blk_ctr[0] += 2
                stage1(e_, 2 * bp, i0)
                stage1(e_, 2 * bp + 1, i0 + 1)
                stage1b(e_, 2 * bp, i0)
                stage1b(e_, 2 * bp + 1, i0 + 1)
                stage2(e_, 2 * bp, i0)
                stage2(e_, 2 * bp + 1, i0 + 1)
                stage2b(e_, 2 * bp, i0)
                stage2b(e_, 2 * bp + 1, i0 + 1)

            load_w(0)
            NP = NB // 2
            NPM = min(NP, 4)
            for e_ in range(NE):
                if e_ + 1 < NE:
                    load_w(e_ + 1)
                for bp in range(NPM):
                    P.cond_begin(cnti[0:1, e_:e_ + 1], bp * 256)
                    pair(e_, bp)
                for bp in range(NPM):
                    P.cond_end()
            if NP > NPM:
                for e_ in range(NE):
                    P.cond_begin(cnti[0:1, e_:e_ + 1], NPM * 256)
                    load_w(e_)
                    pair(e_, NPM)
                    for bp in range(NPM + 1, NP):
                        P.cond_begin(cnti[0:1, e_:e_ + 1], bp * 256)
                        pair(e_, bp)
                    for bp in range(NPM + 1, NP):
                        P.cond_end()
                    P.cond_end()

            finC = [R_w[0], R_w[1]] + R_xg + R_xTb + R_actTb + R_actm + R_sil + R_yo + bank_res
            for eng in ("sp", "pe", "act", "dve", "pool"):
                P.wait_all(eng, finC)

        with contextlib.ExitStack() as st4:
            def sb4(name, shape, dtype=F32):
                return st4.enter_context(nc.sbuf_tensor("s_" + name, list(shape), dtype))

            GD = 4
            NSL = 2 * GD
            gfbc = sb4("gfbc", [128, D])
            R_gf = Res("gfbc")
            mk("sp", lambda e: e.dma_start(out=gfbc[:, :], in_=gf_d.partition_broadcast(128)), writes=[R_gf], dma=P.dsem("gf"))
            xa = [sb4("xa%d" % i, [128, D]) for i in range(NSL)]
            ya = [sb4("ya%d" % i, [128, D]) for i in range(NSL)]
            yb = [sb4("yb%d" % i, [128, D]) for i in range(NSL)]
            ob = [sb4("ob%d" % i, [128, D]) for i in range(2)]
            R_xa = [Res("xa%d" % i) for i in range(NSL)]; R_ya = [Res("ya%d" % i) for i in range(NSL)]; R_yb = [Res("yb%d" % i) for i in range(NSL)]
            R_ob = [Res("ob0"), Res("ob1")]
            xasem = [P.dsem() for _ in range(NSL)]; yasem = [P.dsem() for _ in range(NSL)]; ybsem = [P.dsem() for _ in range(NSL)]; obsem = [P.dsem(), P.dsem()]
            fs = sb4("fs", [128, 4, NCH]); R_fs = Res("fs")
            jk = sb4("jk", [128, D], BF16); R_jk = Res("jk")

            def d_load(c):
                s = c % NSL
                mk("sp", lambda e: e.dma_start(out=xa[s][:, :], in_=x1_d[c * 128:(c + 1) * 128, :]), reads=[R_x1], writes=[R_xa[s]], dma=xasem[s])
                mk("pool", lambda e: e.indirect_dma_start(out=ya[s][:, :], out_offset=None, in_=ys_d,
                                                          in_offset=bass.IndirectOffsetOnAxis(ap=didx[:, 2 * c:2 * c + 1], axis=0)),
                   reads=[R_ys, R_didx], writes=[R_ya[s]], dma=yasem[s])
                mk("pool", lambda e: e.indirect_dma_start(out=yb[s][:, :], out_offset=None, in_=ys_d,
                                                          in_offset=bass.IndirectOffsetOnAxis(ap=didx[:, 2 * c + 1:2 * c + 2], axis=0)),
                   reads=[R_ys, R_didx], writes=[R_yb[s]], dma=ybsem[s])

            def d_combine(c):
                s = c % NSL
                mk("dve", lambda e: e.scalar_tensor_tensor(out=xa[s][:, :], in0=ya[s][:, :], scalar=gwt[:, 2 * c:2 * c + 1], in1=xa[s][:, :], op0=ALU.mult, op1=ALU.add),
                   reads=[R_ya[s], R_xa[s], R_gwt], writes=[R_xa[s]])
                mk("dve", lambda e: e.scalar_tensor_tensor(out=xa[s][:, :], in0=yb[s][:, :], scalar=gwt[:, 2 * c + 1:2 * c + 2], in1=xa[s][:, :], op0=ALU.mult, op1=ALU.add),
                   reads=[R_yb[s], R_xa[s], R_gwt], writes=[R_xa[s]])
                mk("act", lambda e: e.activation(out=jk[:, :], in_=xa[s][:, :], func=AF.Square, accum_out=fs[:, 0, c:c + 1]),
                   reads=[R_xa[s]], writes=[R_jk, R_fs])

            def d_finish(c):
                s = c % NSL
                o = c % 2
                mk("act", lambda e: e.activation(out=xa[s][:, :], in_=xa[s][:, :], func=AF.Identity, scale=fs[:, 2, c:c + 1]),
                   reads=[R_xa[s], R_fs], writes=[R_xa[s]])
                mk("pool", lambda e: e.tensor_tensor(out=ob[o][:, :], in0=xa[s][:, :], in1=gfbc[:, :], op=ALU.mult),
                   reads=[R_xa[s], R_gf], writes=[R_ob[o]])
                mk("sp", lambda e: e.dma_start(out=out_d[c * 128:(c + 1) * 128, :], in_=ob[o][:, :]),
                   reads=[R_ob[o]], accum=[R_out], dma=obsem[o])

            NGD = NCH // GD
            for c in range(min(GD, NCH)):
                d_load(c)
            for g in range(NGD):
                if g + 1 < NGD:
                    for c in range((g + 1) * GD, (g + 2) * GD):
                        d_load(c)
                cs_ = slice(g * GD, (g + 1) * GD)
                for c in range(g * GD, (g + 1) * GD):
                    d_combine(c)
                mk("dve", lambda e, cs_=cs_: e.tensor_scalar(out=fs[:, 1, cs_], in0=fs[:, 0, cs_], scalar1=1.0 / D, scalar2=EPS, op0=ALU.mult, op1=ALU.add),
                   reads=[R_fs], writes=[R_fs])
                emit_rsqrt(fs[:, 2, cs_], fs[:, 1, cs_], fs[:, 3, cs_], [R_fs])
                for c in range(g * GD, (g + 1) * GD):
                    d_finish(c)
            if debug:
                mk("sp", lambda e: e.dma_start(out=dbg["idx"], in_=didx[:, :]), reads=[R_didx], accum=[R_dbg], dma=obsem[0])
                mk("sp", lambda e: e.dma_start(out=dbg["gw"], in_=gwt[:, :]), reads=[R_gwt], accum=[R_dbg], dma=obsem[1])
            fin = [R_out, R_dbg, R_x1, R_xs, R_ys, R_wscr, R_didx, R_gwt, R_fs, R_jk, R_gf] + R_xa + R_ya + R_yb + R_ob + bank_res
            for eng in ("sp", "pe", "act", "dve", "pool"):
                P.wait_all(eng, fin)

        P.emit()
    return nc


def make_shared(inp, CAP):
    f = np.float32

    def a(v):
        return np.ascontiguousarray(v, dtype=f)

    def pk(vec, n):
        return a(np.asarray(vec).reshape(n, 128).T)

    b_in = np.asarray(inp["b_in"][0])
    sh = {
        "w_in": a(inp["w_in"][0]),
        "w_out": a(inp["w_out"][0]),
        "g1": pk(inp["norm_mix_g"][0], 8),
        "bfm": pk(b_in[0:2048], 16),
        "btm": a(b_in[2048:3080].reshape(1, 1032)),
        "cw": a(np.asarray(inp["conv_w"][0]).reshape(31, 4, 128).transpose(2, 1, 0).reshape(128, 124)),
        "cb": pk(inp["conv_b"][0], 4),
        "lng": pk(inp["conv_ln_g"][0], 4),
        "lnb": pk(inp["conv_ln_b"][0], 4),
        "qkw": a(np.asarray(inp["qk_conv_w"][0]).reshape(4, 8, 128).transpose(2, 1, 0).reshape(128, 32)),
        "qkb": pk(inp["qk_conv_b"][0], 8),
        "mg": pk(inp["mlstm_norm_g"][0], 4),
        "g2": a(np.asarray(inp["norm_ffn_g"][0]).reshape(1, D)),
        "gf": a(np.asarray(inp["final_norm_g"]).reshape(1, D)),
        "rw": a(np.concatenate([inp["router_group_w"][0], inp["router_expert_w"][0]], axis=1)),
        "rb": a(np.concatenate([inp["router_group_b"][0], inp["router_expert_b"][0]]).reshape(1, 36)),
        "ecap": a((np.arange(32) * CAP).reshape(1, 32)),
        "wg": a(inp["expert_w_gate"][0]),
        "wu": a(inp["expert_w_up"][0]),
        "wd": a(inp["expert_w_down"][0]),
        "ident": a(np.eye(128)),
        "tri_incl": a(np.triu(np.ones((128, 128)))),
        "tri_strict": a(np.triu(np.ones((128, 128)), 1)),
    }
    return sh


_CACHE = {}


def kernel(**inputs):
    x = np.asarray(inputs["x"], dtype=np.float32)
    B, S, _ = x.shape
    CAP = 1536
    key = (S, CAP)
    if key not in _CACHE:
        _CACHE[key] = build_program(S=S, CAP=CAP, NQ=2)
    nc = _CACHE[key]
    sh = make_shared(inputs, CAP)
    in_maps = []
    for b in range(B):
        m = dict(sh)
        m["x"] = np.ascontiguousarray(x[b])
        in_maps.append(m)
    res = run_bass_kernel_spmd(nc, in_maps, core_ids=list(range(B)))
    return np.stack([np.asarray(r["out"]) for r in res.results], axis=0).astype(np.float32)
```
